# Optimizing a Trainium2 kernel written in Bass

```python
import math
import jax
import jax.numpy as jnp
from jax import lax
import numpy as np

D_MODEL = 1024
BATCH = 4
SEQ = 8192
DEPTH = 1

GRID_W = 64
CTX_LEN = 256
S5_WIDTH = 512
S5_GROUP_CH = 16
S5_GROUPS = S5_WIDTH // S5_GROUP_CH
S5_STATE = 64
DT_MIN = 1e-3
DT_MAX = 1e-1
CONV_WIDTH = 512
CONV_K = 3
N_EXPERT_GROUPS = 4
EXPERTS_PER_GROUP = 8
N_EXPERTS = N_EXPERT_GROUPS * EXPERTS_PER_GROUP
EXPERT_FF = 512
TOP_K_IN_GROUP = 2
EXPERT_BLOCK = 128
IN_PROJ_WIDTH = S5_WIDTH + 3 * CONV_WIDTH + 2 * D_MODEL
ALPHA = (2.0 * DEPTH) ** 0.25
BETA = (8.0 * DEPTH) ** -0.25
LN_EPS = 1e-6
POS_BASE = 10000.0

kernel_name = 'hybrid_s5_shortconv_hmoe_prefix_block'


def _layer_norm(x, gain=None, bias=None):
    xf = x.astype(jnp.float32)
    mu = xf.mean(-1, keepdims=True)
    var = jnp.square(xf - mu).mean(-1, keepdims=True)
    y = (xf - mu) * lax.rsqrt(var + LN_EPS)
    if gain is not None:
        y = y * gain.astype(jnp.float32) + bias.astype(jnp.float32)
    return y.astype(x.dtype)


def _modulate(xn, shift, scale):
    return xn * (1.0 + scale) + shift


def _sincos_2d(rows, cols, dim):
    q = dim // 4
    omega = 1.0 / (POS_BASE ** (jnp.arange(q, dtype=jnp.float32) / q))
    r = jnp.arange(rows, dtype=jnp.float32)[:, None] * omega
    cl = jnp.arange(cols, dtype=jnp.float32)[:, None] * omega
    r_emb = jnp.concatenate([jnp.sin(r), jnp.cos(r)], -1)
    c_emb = jnp.concatenate([jnp.sin(cl), jnp.cos(cl)], -1)
    emb = jnp.concatenate([jnp.broadcast_to(r_emb[:, None, :], (rows, cols, 2 * q)),
                           jnp.broadcast_to(c_emb[None, :, :], (rows, cols, 2 * q))], -1)
    return emb.reshape(rows * cols, dim)


def _ssm_combine(e1, e2):
    a1r, a1i, b1r, b1i = e1
    a2r, a2i, b2r, b2i = e2
    return (a1r * a2r - a1i * a2i, a1r * a2i + a1i * a2r,
            a2r * b1r - a2i * b1i + b2r, a2r * b1i + a2i * b1r + b2i)


def _s5_groups(u):
    b, l, _ = u.shape
    return u.reshape(b, l, S5_GROUPS, S5_GROUP_CH).astype(jnp.float32)


def _s5_dir(lp, d):
    return (lp['s5_log_dt_' + d], lp['s5_a_re_' + d], lp['s5_a_im_' + d],
            lp['s5_b_re_' + d], lp['s5_b_im_' + d])


def _s5_states(ug, log_dt, a_re, a_im, b_re, b_im, s0, reverse):
    f32 = jnp.float32
    dt = jnp.exp(log_dt.astype(f32))[:, None]
    a_re = a_re.astype(f32)
    a_im = a_im.astype(f32)
    mag = jnp.exp(dt * a_re)
    ab_re = mag * jnp.cos(dt * a_im)
    ab_im = mag * jnp.sin(dt * a_im)
    den = a_re * a_re + a_im * a_im
    x_re = ab_re - 1.0
    f_re = (x_re * a_re + ab_im * a_im) / den
    f_im = (ab_im * a_re - x_re * a_im) / den
    b_re = b_re.astype(f32)
    b_im = b_im.astype(f32)
    bb_re = f_re[..., None] * b_re - f_im[..., None] * b_im
    bb_im = f_re[..., None] * b_im + f_im[..., None] * b_re
    bu_re = jnp.einsum('blgc,gnc->blgn', ug, bb_re)
    bu_im = jnp.einsum('blgc,gnc->blgn', ug, bb_im)
    if s0 is not None:
        s0_re, s0_im = s0
        edge = -1 if reverse else 0
        bu_re = bu_re.at[:, edge].add(ab_re * s0_re - ab_im * s0_im)
        bu_im = bu_im.at[:, edge].add(ab_re * s0_im + ab_im * s0_re)
    l = ug.shape[1]
    a_seq_re = jnp.broadcast_to(ab_re, (1, l) + ab_re.shape)
    a_seq_im = jnp.broadcast_to(ab_im, (1, l) + ab_im.shape)
    _, _, s_re, s_im = lax.associative_scan(_ssm_combine, (a_seq_re, a_seq_im, bu_re, bu_im),
                                            reverse=reverse, axis=1)
    return s_re, s_im


def _edge_state(s_re, s_im, reverse):
    e = 0 if reverse else -1
    return (s_re[:, e], s_im[:, e])


def _s5_readout(s_re, s_im, c_re, c_im):
    return (jnp.einsum('blgn,gcn->blgc', s_re, c_re.astype(jnp.float32))
            - jnp.einsum('blgn,gcn->blgc', s_im, c_im.astype(jnp.float32)))


def _short_conv(v, w, row_w):
    b, l, ch = v.shape
    seg = l if row_w is None else row_w
    vr = v.reshape(b * (l // seg), seg, ch)
    vp = jnp.pad(vr, ((0, 0), (1, 1), (0, 0)))
    out = w[0] * vp[:, :-2] + w[1] * vp[:, 1:-1] + w[2] * vp[:, 2:]
    return out.reshape(b, l, ch)


def _token_mixer(h, lp, row_w, s0_f, s0_b):
    b, l, _ = h.shape
    proj = h @ lp['w_in']
    o1 = S5_WIDTH
    o2 = o1 + CONV_WIDTH
    o3 = o2 + CONV_WIDTH
    o4 = o3 + CONV_WIDTH
    o5 = o4 + D_MODEL
    u_a, z_b, gate_b, gate_c = proj[..., :o1], proj[..., o1:o2], proj[..., o2:o3], proj[..., o3:o4]
    merge_a, merge_b = proj[..., o4:o5], proj[..., o5:]
    ug = _s5_groups(u_a)
    sf_re, sf_im = _s5_states(ug, *_s5_dir(lp, 'f'), s0_f, False)
    y_a = _s5_readout(sf_re, sf_im, lp['s5_c_re_f'], lp['s5_c_im_f'])
    fin_f = _edge_state(sf_re, sf_im, False)
    sb_re, sb_im = _s5_states(ug, *_s5_dir(lp, 'b'), s0_b, True)
    y_a = y_a + _s5_readout(sb_re, sb_im, lp['s5_c_re_b'], lp['s5_c_im_b'])
    fin_b = _edge_state(sb_re, sb_im, True)
    y_a = (y_a + lp['s5_d'].astype(jnp.float32) * ug).reshape(b, l, S5_WIDTH).astype(h.dtype)
    ya = jax.nn.gelu(y_a)
    out_a = (ya @ lp['s5_w_glu_val']) * jax.nn.sigmoid(ya @ lp['s5_w_glu_gate'])
    v = _short_conv(gate_c * z_b, lp['conv_w'], row_w)
    out_b = (gate_b * v) @ lp['conv_w_out']
    merged = jax.nn.sigmoid(merge_a) * out_a + jax.nn.sigmoid(merge_b) * out_b
    return merged @ lp['w_o'], fin_f, fin_b


def _routed_experts(hf, e_idx, w_gate, w_up, w_down):
    t, k = e_idx.shape
    d = hf.shape[-1]
    n_exp = w_gate.shape[0]
    n_assign = t * k
    flat_e = e_idx.reshape(n_assign)
    order = jnp.argsort(flat_e)
    sorted_e = flat_e[order]
    counts = jnp.zeros((n_exp,), jnp.int32).at[flat_e].add(1)
    padded = (counts + EXPERT_BLOCK - 1) // EXPERT_BLOCK * EXPERT_BLOCK
    pad_end = jnp.cumsum(padded)
    pad_start = pad_end - padded
    start = jnp.cumsum(counts) - counts
    dest = pad_start[sorted_e] + jnp.arange(n_assign, dtype=jnp.int32) - start[sorted_e]
    n_blocks = (n_assign + EXPERT_BLOCK - 1) // EXPERT_BLOCK + n_exp
    n_rows = n_blocks * EXPERT_BLOCK
    src_tok = jnp.full((n_rows,), t, jnp.int32).at[dest].set(order // k)
    h_pad = jnp.concatenate([hf, jnp.zeros((1, d), hf.dtype)], 0)
    xb = h_pad[src_tok].reshape(n_blocks, EXPERT_BLOCK, d)
    block_e = jnp.minimum(jnp.searchsorted(pad_end, jnp.arange(n_blocks, dtype=jnp.int32) * EXPERT_BLOCK,
                                           side='right'), n_exp - 1)

    def _expert_block(args):
        xblk, e = args
        hid = jax.nn.silu(xblk @ w_gate[e]) * (xblk @ w_up[e])
        return hid @ w_down[e]

    yb = lax.map(_expert_block, (xb, block_e)).reshape(n_rows, d)
    dest_of_assign = jnp.zeros((n_assign,), jnp.int32).at[order].set(dest)
    return yb[dest_of_assign].reshape(t, k, d)


def _hier_moe(h, lp):
    b, l, d = h.shape
    f32 = jnp.float32
    hf = h.reshape(b * l, d)
    g_logits = (hf @ lp['router_w_group']).astype(f32) + lp['router_b_group'].astype(f32)
    g_prob = jax.nn.softmax(g_logits, axis=-1)
    g_idx = jnp.argmax(g_logits, axis=-1).astype(jnp.int32)
    p_group = jnp.take_along_axis(g_prob, g_idx[:, None], axis=-1)
    e_logits = ((hf @ lp['router_w_expert']).astype(f32) + lp['router_b_expert'].astype(f32)
                ).reshape(-1, N_EXPERT_GROUPS, EXPERTS_PER_GROUP)
    e_sel = jnp.take_along_axis(e_logits, g_idx[:, None, None], axis=1)[:, 0]
    e_prob = jax.nn.softmax(e_sel, axis=-1)
    top_p, top_i = lax.top_k(e_prob, TOP_K_IN_GROUP)
    weights = p_group * top_p / top_p.sum(-1, keepdims=True)
    e_idx = g_idx[:, None] * EXPERTS_PER_GROUP + top_i.astype(jnp.int32)
    y = _routed_experts(hf, e_idx, lp['exp_w_gate'], lp['exp_w_up'], lp['exp_w_down'])
    out = jnp.einsum('tkd,tk->td', y.astype(f32), weights)
    return out.astype(h.dtype).reshape(b, l, d)


def setup_inputs(seed: int = 0) -> dict:
    key = jax.random.key(seed)
    ks = iter(jax.random.split(key, 48))
    f32 = jnp.float32

    def nrm(shape, scale):
        return jax.random.normal(next(ks), shape, f32) * scale

    def s5_dir():
        log_dt = jax.random.uniform(next(ks), (DEPTH, S5_GROUPS), f32, math.log(DT_MIN), math.log(DT_MAX))
        a_re = -0.5 + nrm((DEPTH, S5_GROUPS, S5_STATE), 0.01)
        a_im = math.pi * jnp.arange(S5_STATE, dtype=f32) + nrm((DEPTH, S5_GROUPS, S5_STATE), 0.01)
        b_re = nrm((DEPTH, S5_GROUPS, S5_STATE, S5_GROUP_CH), (2 * S5_GROUP_CH) ** -0.5)
        b_im = nrm((DEPTH, S5_GROUPS, S5_STATE, S5_GROUP_CH), (2 * S5_GROUP_CH) ** -0.5)
        c_re = nrm((DEPTH, S5_GROUPS, S5_GROUP_CH, S5_STATE), 0.5)
        c_im = nrm((DEPTH, S5_GROUPS, S5_GROUP_CH, S5_STATE), 0.5)
        return log_dt, a_re, a_im, b_re, b_im, c_re, c_im

    x = nrm((BATCH, SEQ, D_MODEL), 1.0)
    c = nrm((BATCH, D_MODEL), 1.0)
    ctx = nrm((BATCH, CTX_LEN, D_MODEL), 1.0)
    c_ctx = nrm((D_MODEL,), 1.0)
    w_ada = nrm((DEPTH, D_MODEL, 6 * D_MODEL), 0.5 * D_MODEL ** -0.5)
    b_ada = nrm((DEPTH, 6 * D_MODEL), 0.02)
    w_in = nrm((DEPTH, D_MODEL, IN_PROJ_WIDTH), D_MODEL ** -0.5)
    f_par = s5_dir()
    b_par = s5_dir()
    s5_d = nrm((DEPTH, S5_GROUPS, S5_GROUP_CH), 1.0)
    s5_w_glu_val = nrm((DEPTH, S5_WIDTH, D_MODEL), BETA * S5_WIDTH ** -0.5)
    s5_w_glu_gate = nrm((DEPTH, S5_WIDTH, D_MODEL), S5_WIDTH ** -0.5)
    conv_w = nrm((DEPTH, CONV_K, CONV_WIDTH), CONV_K ** -0.5)
    conv_w_out = nrm((DEPTH, CONV_WIDTH, D_MODEL), BETA * CONV_WIDTH ** -0.5)
    w_o = nrm((DEPTH, D_MODEL, D_MODEL), BETA * D_MODEL ** -0.5)
    ln1_g = 1.0 + nrm((DEPTH, D_MODEL), 0.02)
    ln1_b = nrm((DEPTH, D_MODEL), 0.02)
    router_w_group = nrm((DEPTH, D_MODEL, N_EXPERT_GROUPS), D_MODEL ** -0.5)
    router_b_group = nrm((DEPTH, N_EXPERT_GROUPS), 0.01)
    router_w_expert = nrm((DEPTH, D_MODEL, N_EXPERTS), D_MODEL ** -0.5)
    router_b_expert = nrm((DEPTH, N_EXPERTS), 0.01)
    exp_w_gate = nrm((DEPTH, N_EXPERTS, D_MODEL, EXPERT_FF), D_MODEL ** -0.5)
    exp_w_up = nrm((DEPTH, N_EXPERTS, D_MODEL, EXPERT_FF), D_MODEL ** -0.5)
    exp_w_down = nrm((DEPTH, N_EXPERTS, EXPERT_FF, D_MODEL), BETA * EXPERT_FF ** -0.5)
    ln2_g = 1.0 + nrm((DEPTH, D_MODEL), 0.02)
    ln2_b = nrm((DEPTH, D_MODEL), 0.02)
    return {
        'x': x, 'c': c, 'ctx': ctx, 'c_ctx': c_ctx,
        'w_ada': w_ada, 'b_ada': b_ada, 'w_in': w_in,
        's5_log_dt_f': f_par[0], 's5_a_re_f': f_par[1], 's5_a_im_f': f_par[2],
        's5_b_re_f': f_par[3], 's5_b_im_f': f_par[4], 's5_c_re_f': f_par[5], 's5_c_im_f': f_par[6],
        's5_log_dt_b': b_par[0], 's5_a_re_b': b_par[1], 's5_a_im_b': b_par[2],
        's5_b_re_b': b_par[3], 's5_b_im_b': b_par[4], 's5_c_re_b': b_par[5], 's5_c_im_b': b_par[6],
        's5_d': s5_d, 's5_w_glu_val': s5_w_glu_val, 's5_w_glu_gate': s5_w_glu_gate,
        'conv_w': conv_w, 'conv_w_out': conv_w_out, 'w_o': w_o,
        'ln1_g': ln1_g, 'ln1_b': ln1_b,
        'router_w_group': router_w_group, 'router_b_group': router_b_group,
        'router_w_expert': router_w_expert, 'router_b_expert': router_b_expert,
        'exp_w_gate': exp_w_gate, 'exp_w_up': exp_w_up, 'exp_w_down': exp_w_down,
        'ln2_g': ln2_g, 'ln2_b': ln2_b,
    }


def reference(x, c, ctx, c_ctx, w_ada, b_ada, w_in,
              s5_log_dt_f, s5_a_re_f, s5_a_im_f, s5_b_re_f, s5_b_im_f, s5_c_re_f, s5_c_im_f,
              s5_log_dt_b, s5_a_re_b, s5_a_im_b, s5_b_re_b, s5_b_im_b, s5_c_re_b, s5_c_im_b,
              s5_d, s5_w_glu_val, s5_w_glu_gate, conv_w, conv_w_out, w_o, ln1_g, ln1_b,
              router_w_group, router_b_group, router_w_expert, router_b_expert,
              exp_w_gate, exp_w_up, exp_w_down, ln2_g, ln2_b):
    stacked = dict(
        w_ada=w_ada, b_ada=b_ada, w_in=w_in,
        s5_log_dt_f=s5_log_dt_f, s5_a_re_f=s5_a_re_f, s5_a_im_f=s5_a_im_f, s5_b_re_f=s5_b_re_f,
        s5_b_im_f=s5_b_im_f, s5_c_re_f=s5_c_re_f, s5_c_im_f=s5_c_im_f,
        s5_log_dt_b=s5_log_dt_b, s5_a_re_b=s5_a_re_b, s5_a_im_b=s5_a_im_b, s5_b_re_b=s5_b_re_b,
        s5_b_im_b=s5_b_im_b, s5_c_re_b=s5_c_re_b, s5_c_im_b=s5_c_im_b,
        s5_d=s5_d, s5_w_glu_val=s5_w_glu_val, s5_w_glu_gate=s5_w_glu_gate,
        conv_w=conv_w, conv_w_out=conv_w_out, w_o=w_o, ln1_g=ln1_g, ln1_b=ln1_b,
        router_w_group=router_w_group, router_b_group=router_b_group,
        router_w_expert=router_w_expert, router_b_expert=router_b_expert,
        exp_w_gate=exp_w_gate, exp_w_up=exp_w_up, exp_w_down=exp_w_down,
        ln2_g=ln2_g, ln2_b=ln2_b)
    rows = x.shape[1] // GRID_W
    x = x + _sincos_2d(rows, GRID_W, x.shape[-1]).astype(x.dtype)
    for layer in range(DEPTH):
        lp = {name: arr[layer] for name, arr in stacked.items()}
        mod_lat = jax.nn.silu(c) @ lp['w_ada'] + lp['b_ada']
        mod_ctx = jax.nn.silu(c_ctx) @ lp['w_ada'] + lp['b_ada']
        sh1, sc1, g1, sh2, sc2, g2 = jnp.split(mod_lat[:, None, :], 6, axis=-1)
        csh1, csc1, cg1, csh2, csc2, cg2 = jnp.split(mod_ctx, 6, axis=-1)
        h_ctx = _modulate(_layer_norm(ctx), csh1, csc1)
        if layer == DEPTH - 1:
            ug_ctx = _s5_groups(h_ctx @ lp['w_in'][:, :S5_WIDTH])
            s0_f = _edge_state(*_s5_states(ug_ctx, *_s5_dir(lp, 'f'), None, False), False)
            s0_b = _edge_state(*_s5_states(ug_ctx, *_s5_dir(lp, 'b'), None, True), True)
        else:
            mix_ctx, s0_f, s0_b = _token_mixer(h_ctx, lp, None, None, None)
            ctx = _layer_norm(ALPHA * ctx + cg1 * mix_ctx, lp['ln1_g'], lp['ln1_b'])
            h2_ctx = _modulate(_layer_norm(ctx), csh2, csc2)
            ctx = _layer_norm(ALPHA * ctx + cg2 * _hier_moe(h2_ctx, lp), lp['ln2_g'], lp['ln2_b'])
        h = _modulate(_layer_norm(x), sh1, sc1)
        mix, _, _ = _token_mixer(h, lp, GRID_W, s0_f, s0_b)
        x = _layer_norm(ALPHA * x + g1 * mix, lp['ln1_g'], lp['ln1_b'])
        h2 = _modulate(_layer_norm(x), sh2, sc2)
        x = _layer_norm(ALPHA * x + g2 * _hier_moe(h2, lp), lp['ln2_g'], lp['ln2_b'])
    return x
```

```python
import math
import numpy as np
import concourse.bass as bass
import concourse.mybir as mybir
from concourse.bass_utils import run_bass_kernel_spmd

F32 = mybir.dt.float32
BF16 = mybir.dt.bfloat16
I32 = mybir.dt.int32
AF = mybir.ActivationFunctionType
ALU = mybir.AluOpType
AX = mybir.AxisListType

ALPHA = 2.0 ** 0.25
LN_EPS = 1e-6
NT = 4096
D = 1024
NG = 32
GELU_C = 2.0 * math.sqrt(2.0 / math.pi)


class Buf:
    __slots__ = ("name", "writer", "readers", "sem", "semcount")

    def __init__(self, name):
        self.name = name
        self.writer = None
        self.readers = []
        self.sem = None
        self.semcount = 0


class Sched:
    SEM_CAP = 30000

    def __init__(self, nc):
        self.nc = nc
        self.eng = {n: dict(prog=[], sem=None, count=0, waited={}, nsem=0) for n in ("pe", "act", "dve", "pool", "sp")}
        self.nbufsem = 0

    def _engsem(self, E, name):
        if E["sem"] is None or E["count"] >= self.SEM_CAP:
            E["sem"] = self.nc.alloc_semaphore(f"s_{name}_{E['nsem']}")
            E["nsem"] += 1
            E["count"] = 0
        return E["sem"]

    def _waits(self, E, reads, writes):
        need = {}

        def add(tok):
            if tok is None:
                return
            s, v = tok
            k = id(s)
            if k not in need or need[k][1] < v:
                need[k] = (s, v)
        for b in reads:
            add(b.writer)
        for b in writes:
            add(b.writer)
            for r in b.readers:
                add(r)
        out = []
        for k, (s, v) in need.items():
            if E["waited"].get(k, 0) < v:
                E["waited"][k] = v
                out.append((s, v))
        return out

    def _commit(self, tok, reads, writes):
        for b in writes:
            b.writer = tok
            b.readers = []
        for b in reads:
            if b not in writes:
                b.readers.append(tok)
                if len(b.readers) > 48:
                    d = {}
                    for s, v in b.readers:
                        if id(s) not in d or d[id(s)][1] < v:
                            d[id(s)] = (s, v)
                    b.readers = list(d.values())

    def op(self, name, fn, reads=(), writes=()):
        E = self.eng[name]
        waits = self._waits(E, reads, writes)
        sem = self._engsem(E, name)
        E["count"] += 1
        val = E["count"]

        def run(h, waits=waits, fn=fn, sem=sem):
            for s, v in waits:
                h.wait_ge(s, v)
            fn(h).then_inc(sem, 1)
        E["prog"].append(run)
        self._commit((sem, val), reads, writes)

    def dma(self, qname, fn, owner, reads=(), writes=()):
        E = self.eng[qname]
        waits = self._waits(E, reads, writes)
        if owner.sem is None:
            owner.sem = self.nc.alloc_semaphore(f"d_{self.nbufsem}")
            self.nbufsem += 1
        owner.semcount += 16
        sem, val = owner.sem, owner.semcount

        def run(h, waits=waits, fn=fn, sem=sem):
            for s, v in waits:
                h.wait_ge(s, v)
            fn(h).then_inc(sem, 16)
        E["prog"].append(run)
        self._commit((sem, val), reads, writes)

    def final_wait(self, qname, bufs):
        E = self.eng[qname]
        waits = self._waits(E, bufs, ())

        def run(h, waits=waits):
            for s, v in waits:
                h.wait_ge(s, v)
        E["prog"].append(run)

    def emit(self):
        with self.nc.Block() as block:
            @block.tensor
            def _(h):
                for f in self.eng["pe"]["prog"]:
                    f(h)

            @block.scalar
            def _(h):
                for f in self.eng["act"]["prog"]:
                    f(h)

            @block.vector
            def _(h):
                for f in self.eng["dve"]["prog"]:
                    f(h)

            @block.gpsimd
            def _(h):
                for f in self.eng["pool"]["prog"]:
                    f(h)

            @block.sync
            def _(h):
                for f in self.eng["sp"]["prog"]:
                    f(h)


def mkap(base, off_elems, dims):
    return bass.AP(base.tensor, base.offset + off_elems, [list(base.ap[0])] + [list(d) for d in dims])


def build(debug=None):
    nc = bass.Bass("TRN2", target_bir_lowering=False)
    S = Sched(nc)

    def din(name, shape, dt=F32):
        return nc.dram_tensor(name, list(shape), dt, kind="ExternalInput").ap()

    x_own = din("x_own", [NT, D]); x_oth = din("x_oth", [NT, D]); ctx_in = din("ctx", [256, D])
    pos_own = din("pos_own", [NT, D]); pos_oth = din("pos_oth", [NT, D])
    cT_in = din("cT", [128, 16])
    w_ada = din("w_ada", [D, 6 * D]); b_adaT = din("b_adaT", [128, 48]); b_ada = din("b_ada", [1, 6 * D])
    w_in = din("w_in", [D, 4096])
    s5_small = din("s5_small", [128, 3, NG])
    s5_b = din("s5_b", [128, 2, NG, 16]); s5_c = din("s5_c", [128, 2, NG, 16]); s5_drep = din("s5_drep", [128, NG])
    masks_in = din("masks", [128, 2, 512]); ident_in = din("ident", [128, 128]); sel_in = din("sel", [32, 32, 128])
    w_val = din("w_val", [512, D]); w_gate = din("w_gate", [512, D])
    convw_in = din("convwT", [128, 4, 3]); conv_out = din("conv_out", [512, D]); w_o = din("w_o", [D, D])
    lnp_in = din("lnp", [1, 4 * D])
    rw_in = din("rw", [D, 36]); rb_in = din("rb", [1, 36])
    wg_in = din("wg", [32, D, 512]); wu_in = din("wu", [32, D, 512]); wd_in = din("wd", [32, 512, D])
    out_d = nc.dram_tensor("out", [NT, D], F32, kind="ExternalOutput").ap()
    x1s = nc.dram_tensor("x1s", [NT, D], F32, kind=("ExternalOutput" if debug else "Internal")).ap()
    h2s = nc.dram_tensor("h2s", [128, 8, NT], BF16, kind=("ExternalOutput" if debug else "Internal")).ap()
    wrs = nc.dram_tensor("wrs", [128, 32, 32], F32, kind="ExternalOutput").ap() if debug else None
    dbg_d = None
    if debug == "ya":
        dbg_d = nc.dram_tensor("dbg", [128, 4 * NT], BF16, kind="ExternalOutput").ap()

    def sb(name, shape, dt=F32):
        return nc.alloc_sbuf_tensor("sb_" + name, list(shape), dt)

    ident = sb("ident", [128, 128]); b_ident = Buf("ident")
    identb = sb("identb", [128, 128], BF16); b_identb = Buf("identb")
    masks = sb("masks", [128, 2, 512]); b_masks = Buf("masks")
    modfm = sb("modfm", [128, 48, 2]); b_modfm = Buf("modfm")
    sc1p = sb("sc1p", [128, 8, 2]); sc2p = sb("sc2p", [128, 8])
    convw = sb("convw", [128, 4, 3]); b_convw = Buf("convw")
    rb_bc = sb("rb_bc", [128, 36]); b_rb = Buf("rb")
    epst = sb("epst", [128, 1]); b_eps = Buf("eps")

    S.dma("sp", lambda h: h.dma_start(out=ident[:], in_=ident_in), b_ident, writes=[b_ident])
    S.dma("sp", lambda h: h.dma_start(out=masks[:], in_=masks_in), b_masks, writes=[b_masks])
    S.dma("sp", lambda h: h.dma_start(out=convw[:], in_=convw_in), b_convw, writes=[b_convw])
    S.dma("sp", lambda h: h.dma_start(out=rb_bc[:], in_=rb_in.partition_broadcast(128)), b_rb, writes=[b_rb])
    S.op("dve", lambda h: h.tensor_copy(out=identb[:], in_=ident[:]), reads=[b_ident], writes=[b_identb])
    S.op("dve", lambda h: h.memset(epst[:], LN_EPS), writes=[b_eps])

    psg = [nc.alloc_psum_tensor(f"psg{i}", [128, 512], F32) for i in range(5)]
    b_psg = [Buf(f"psg{i}") for i in range(5)]
    psT = nc.alloc_psum_tensor("psT", [128, 1024], F32); b_psT = Buf("psT")
    psB = nc.alloc_psum_tensor("psB", [128, 1024], BF16); b_psB = Buf("psB")
    pctr = [0]

    def ps_next():
        i = pctr[0] % 5
        pctr[0] += 1
        return psg[i], b_psg[i]

    cT = sb("cT", [128, 16]); b_cT = Buf("cT")
    csil = sb("csil", [128, 16]); b_csil = Buf("csil")
    csil2 = sb("csil2", [128, 8, 2]); b_csil2 = Buf("csil2")
    blockA = sb("blockA", [128, 16384], BF16)
    b_hTb = Buf("hTb")
    badT = sb("badT", [128, 48]); b_badT = Buf("badT")
    S.dma("sp", lambda h: h.dma_start(out=cT[:], in_=cT_in), b_cT, writes=[b_cT])
    S.dma("sp", lambda h: h.dma_start(out=badT[:], in_=b_adaT), b_badT, writes=[b_badT])
    S.op("act", lambda h: h.activation(out=csil[:], in_=cT[:], func=AF.Silu), reads=[b_cT], writes=[b_csil])
    S.op("dve", lambda h: h.tensor_copy(out=csil2[:, :, 0], in_=csil[:, 0:8]), reads=[b_csil], writes=[b_csil2])
    S.op("dve", lambda h: h.tensor_copy(out=csil2[:, :, 1], in_=csil[:, 8:16]), reads=[b_csil], writes=[b_csil2])

    arena = sb("arena", [128, 16384])
    b_arena = Buf("arena")
    wsec = arena[:, 0:8192].rearrange("p (k n) -> p k n", k=8)
    for sec in (0, 1, 3, 4):
        S.dma("sp", lambda h, sec=sec: h.dma_start(out=wsec, in_=w_ada[:, sec * D:(sec + 1) * D].rearrange("(k p) n -> p k n", p=128)),
              b_arena, writes=[b_arena])
        ps, bps = ps_next()
        for jj in range(8):
            for kc in range(8):
                S.op("pe", lambda h, ps=ps, kc=kc, jj=jj: h.matmul(ps[:, jj * 2:jj * 2 + 2], lhsT=wsec[:, kc, jj * 128:(jj + 1) * 128],
                                                                  rhs=csil2[:, kc, :], start=(kc == 0), stop=(kc == 7)),
                     reads=[b_csil2, b_arena], writes=[bps])
        S.op("dve", lambda h, ps=ps, sec=sec: h.tensor_tensor(
            out=modfm[:, sec * 8:(sec + 1) * 8, :], in0=ps[:, 0:16].rearrange("p (j t) -> p j t", t=2),
            in1=mkap(badT[:, sec * 8:(sec + 1) * 8], 0, [[1, 8], [0, 2]]), op=ALU.add),
            reads=[bps, b_badT], writes=[b_modfm])
    S.op("dve", lambda h: h.tensor_scalar(out=sc1p[:], in0=modfm[:, 8:16, :], scalar1=1.0, scalar2=None, op0=ALU.add), reads=[b_modfm], writes=[b_modfm])
    S.op("dve", lambda h: h.tensor_scalar(out=sc2p[:], in0=modfm[:, 32:40, 0], scalar1=1.0, scalar2=None, op0=ALU.add), reads=[b_modfm], writes=[b_modfm])

    xt_t = [sb(f"xt{i}", [128, D]) for i in range(2)]; b_xt = [Buf(f"xt{i}") for i in range(2)]
    xn_t = sb("xn", [128, D]); b_xn = Buf("xn")
    stt = sb("stt", [128, 16]); b_stt = Buf("stt")
    xctr = [0]

    def ln_stats(src, bsrc, dst, bdst):
        S.op("dve", lambda h: h.bn_stats(out=stt[:, 0:6], in_=src[:, 0:512]), reads=[bsrc], writes=[b_stt])
        S.op("dve", lambda h: h.bn_stats(out=stt[:, 6:12], in_=src[:, 512:1024]), reads=[bsrc], writes=[b_stt])
        S.op("dve", lambda h: h.bn_aggr(out=stt[:, 12:14], in_=stt[:, 0:12]), reads=[b_stt], writes=[b_stt])
        S.op("act", lambda h: h.activation(out=stt[:, 14:15], in_=stt[:, 13:14], func=AF.Sqrt, bias=epst[:, 0:1], scale=1.0),
             reads=[b_stt, b_eps], writes=[b_stt])
        S.op("dve", lambda h: h.reciprocal(out=stt[:, 15:16], in_=stt[:, 14:15]), reads=[b_stt], writes=[b_stt])
        S.op("dve", lambda h: h.tensor_scalar(out=dst[:, :], in0=src[:, :], scalar1=stt[:, 12:13], scalar2=stt[:, 15:16],
                                              op0=ALU.subtract, op1=ALU.mult), reads=[bsrc, b_stt], writes=[bdst])

    def transpose_mod(src, bsrc, dst_fn, bdst, scale_fn, shift_fn, bmods):
        for kc in range(8):
            S.op("pe", lambda h, kc=kc: h.transpose(out=psT[:, kc * 128:(kc + 1) * 128], in_=src[:, kc * 128:(kc + 1) * 128], identity=ident[:]),
                 reads=[bsrc, b_ident], writes=[b_psT])
        for kc in range(8):
            S.op("act", lambda h, kc=kc: h.activation(out=dst_fn(kc), in_=psT[:, kc * 128:(kc + 1) * 128], func=AF.Identity,
                                                     bias=shift_fn(kc), scale=scale_fn(kc)),
                 reads=[b_psT] + bmods, writes=[bdst])

    def load_tile(xsrc, possrc, r0):
        i = xctr[0] % 2
        xctr[0] += 1
        xt, bx = xt_t[i], b_xt[i]
        S.dma("sp", lambda h: h.dma_start(out=xt[:], in_=xsrc[r0:r0 + 128, :]), bx, writes=[bx])
        if possrc is not None:
            S.dma("pool", lambda h: h.dma_start(out=xt[:], in_=possrc[r0:r0 + 128, :], accum_op=ALU.add), bx, writes=[bx])
        return xt, bx

    blockB = sb("blockB", [128, 10240])
    sm = sb("s5sm", [128, 3, NG]); b_sm = Buf("s5sm")
    sbv = blockB[:, 0:1024].rearrange("p (r g c) -> p r g c", r=2, g=NG); scv = blockB[:, 1024:2048].rearrange("p (r g c) -> p r g c", r=2, g=NG); b_bc = Buf("s5bc")
    drep = sb("drep", [128, NG]); b_drep = Buf("drep")
    S.dma("sp", lambda h: h.dma_start(out=sm[:], in_=s5_small), b_sm, writes=[b_sm])
    S.dma("sp", lambda h: h.dma_start(out=sbv[:], in_=s5_b), b_bc, writes=[b_bc])
    S.dma("sp", lambda h: h.dma_start(out=scv[:], in_=s5_c), b_bc, writes=[b_bc])
    S.dma("sp", lambda h: h.dma_start(out=drep[:], in_=s5_drep), b_drep, writes=[b_drep])

    tb_ = blockB[:, 2048:2048 + 11 * NG * 9].rearrange("p (t g k) -> p t g k", t=11, g=NG); b_tab = Buf("s5tab")
    T_ANG, T_Q, T_SIN, T_COS, T_MAG, T_MAGN, T_WRE, T_WIM, T_VRE, T_VIM, T_TMP = range(11)
    qi = sb("s5qi", [128, NG * 9], I32)
    vec = sb("s5vec", [128, 12, NG]); b_vec = Buf("s5vec")
    V_DT, V_TH, V_LR, V_DEN, V_XRE, V_FRE, V_FIM, V_T1, V_T2, V_RDEN = range(10)

    def vop(fn, r=(b_vec,), w=(b_vec,)):
        S.op("dve", fn, reads=list(r), writes=list(w))

    S.op("act", lambda h: h.activation(out=vec[:, V_DT, :], in_=sm[:, 0, :], func=AF.Exp), reads=[b_sm], writes=[b_vec])
    vop(lambda h: h.tensor_tensor(out=vec[:, V_TH, :], in0=vec[:, V_DT, :], in1=sm[:, 2, :], op=ALU.mult), r=(b_vec, b_sm))
    vop(lambda h: h.tensor_tensor(out=vec[:, V_LR, :], in0=vec[:, V_DT, :], in1=sm[:, 1, :], op=ALU.mult), r=(b_vec, b_sm))
    T = lambda t: tb_[:, t, :, :]
    Tf = lambda t: tb_[:, t, :, :].rearrange("p g k -> p (g k)")
    for k in range(9):
        S.op("dve", lambda h, k=k: h.tensor_scalar(out=tb_[:, T_ANG, :, k], in0=vec[:, V_TH, :], scalar1=float(k), scalar2=None, op0=ALU.mult),
             reads=[b_vec], writes=[b_tab])
        S.op("act", lambda h, k=k: h.activation(out=tb_[:, T_MAG, :, k], in_=vec[:, V_LR, :], func=AF.Exp, scale=float(k)), reads=[b_vec], writes=[b_tab])
        S.op("act", lambda h, k=k: h.activation(out=tb_[:, T_MAGN, :, k], in_=vec[:, V_LR, :], func=AF.Exp, scale=-float(k)), reads=[b_vec], writes=[b_tab])

    def sin_of(dst_t, shift):
        top = lambda fn: S.op("dve", fn, reads=[b_tab], writes=[b_tab])
        top(lambda h: h.tensor_scalar(out=Tf(T_TMP), in0=Tf(T_ANG), scalar1=shift, scalar2=None, op0=ALU.add))
        top(lambda h: h.tensor_scalar(out=qi[:], in0=Tf(T_TMP), scalar1=1.0 / (2 * math.pi), scalar2=None, op0=ALU.mult))
        top(lambda h: h.tensor_copy(out=Tf(T_Q), in_=qi[:]))
        top(lambda h: h.scalar_tensor_tensor(out=Tf(T_TMP), in0=Tf(T_Q), scalar=-2 * math.pi, in1=Tf(T_TMP), op0=ALU.mult, op1=ALU.add))
        top(lambda h: h.tensor_scalar(out=Tf(T_Q), in0=Tf(T_TMP), scalar1=math.pi, scalar2=2 * math.pi, op0=ALU.is_gt, op1=ALU.mult))
        top(lambda h: h.tensor_tensor(out=Tf(T_TMP), in0=Tf(T_TMP), in1=Tf(T_Q), op=ALU.subtract))
        top(lambda h: h.tensor_scalar(out=Tf(T_Q), in0=Tf(T_TMP), scalar1=-math.pi, scalar2=2 * math.pi, op0=ALU.is_lt, op1=ALU.mult))
        top(lambda h: h.tensor_tensor(out=Tf(T_TMP), in0=Tf(T_TMP), in1=Tf(T_Q), op=ALU.add))
        S.op("act", lambda h: h.activation(out=Tf(dst_t), in_=Tf(T_TMP), func=AF.Sin), reads=[b_tab], writes=[b_tab])

    sin_of(T_SIN, 0.0)
    sin_of(T_COS, math.pi / 2)
    tt = lambda o, a, b, op: S.op("dve", lambda h: h.tensor_tensor(out=Tf(o), in0=Tf(a), in1=Tf(b), op=op), reads=[b_tab], writes=[b_tab])
    tt(T_WRE, T_MAG, T_COS, ALU.mult)
    tt(T_WIM, T_MAG, T_SIN, ALU.mult)
    tt(T_VRE, T_MAGN, T_COS, ALU.mult)
    tt(T_VIM, T_MAGN, T_SIN, ALU.mult)
    S.op("dve", lambda h: h.tensor_scalar(out=Tf(T_VIM), in0=Tf(T_VIM), scalar1=-1.0, scalar2=None, op0=ALU.mult), reads=[b_tab], writes=[b_tab])
    are = sm[:, 1, :]; aim = sm[:, 2, :]
    abre = tb_[:, T_WRE, :, 1]; abim = tb_[:, T_WIM, :, 1]
    vr = (b_vec, b_sm, b_tab)
    vop(lambda h: h.tensor_tensor(out=vec[:, V_DEN, :], in0=are, in1=are, op=ALU.mult), r=vr)
    vop(lambda h: h.tensor_tensor(out=vec[:, V_T1, :], in0=aim, in1=aim, op=ALU.mult), r=vr)
    vop(lambda h: h.tensor_tensor(out=vec[:, V_DEN, :], in0=vec[:, V_DEN, :], in1=vec[:, V_T1, :], op=ALU.add), r=vr)
    vop(lambda h: h.reciprocal(out=vec[:, V_RDEN, :], in_=vec[:, V_DEN, :]), r=vr)
    vop(lambda h: h.tensor_scalar(out=vec[:, V_XRE, :], in0=abre, scalar1=-1.0, scalar2=None, op0=ALU.add), r=vr)
    vop(lambda h: h.tensor_tensor(out=vec[:, V_T1, :], in0=vec[:, V_XRE, :], in1=are, op=ALU.mult), r=vr)
    vop(lambda h: h.tensor_tensor(out=vec[:, V_T2, :], in0=abim, in1=aim, op=ALU.mult), r=vr)
    vop(lambda h: h.tensor_tensor(out=vec[:, V_T1, :], in0=vec[:, V_T1, :], in1=vec[:, V_T2, :], op=ALU.add), r=vr)
    vop(lambda h: h.tensor_tensor(out=vec[:, V_FRE, :], in0=vec[:, V_T1, :], in1=vec[:, V_RDEN, :], op=ALU.mult), r=vr)
    vop(lambda h: h.tensor_tensor(out=vec[:, V_T1, :], in0=abim, in1=are, op=ALU.mult), r=vr)
    vop(lambda h: h.tensor_tensor(out=vec[:, V_T2, :], in0=vec[:, V_XRE, :], in1=aim, op=ALU.mult), r=vr)
    vop(lambda h: h.tensor_tensor(out=vec[:, V_T1, :], in0=vec[:, V_T1, :], in1=vec[:, V_T2, :], op=ALU.subtract), r=vr)
    vop(lambda h: h.tensor_tensor(out=vec[:, V_FIM, :], in0=vec[:, V_T1, :], in1=vec[:, V_RDEN, :], op=ALU.mult), r=vr)
    bbar = blockB[:, 5632:6656].rearrange("p (r g c) -> p r g c", r=2, g=NG); b_bbar = Buf("bbar")
    tmpb = blockB[:, 6656:7168].rearrange("p (g c) -> p g c", g=NG); b_tmpb = Buf("tmpb")
    fre_b = mkap(vec[:, V_FRE, :], 0, [[1, NG], [0, 16]]); fim_b = mkap(vec[:, V_FIM, :], 0, [[1, NG], [0, 16]])
    bo = lambda fn, r, w: S.op("dve", fn, reads=r, writes=w)
    bo(lambda h: h.tensor_tensor(out=bbar[:, 0], in0=sbv[:, 0], in1=fre_b, op=ALU.mult), [b_bc, b_vec], [b_bbar])
    bo(lambda h: h.tensor_tensor(out=tmpb[:], in0=sbv[:, 1], in1=fim_b, op=ALU.mult), [b_bc, b_vec], [b_tmpb])
    bo(lambda h: h.tensor_tensor(out=bbar[:, 0], in0=bbar[:, 0], in1=tmpb[:], op=ALU.subtract), [b_bbar, b_tmpb], [b_bbar])
    bo(lambda h: h.tensor_tensor(out=bbar[:, 1], in0=sbv[:, 1], in1=fre_b, op=ALU.mult), [b_bc, b_vec], [b_bbar])
    bo(lambda h: h.tensor_tensor(out=tmpb[:], in0=sbv[:, 0], in1=fim_b, op=ALU.mult), [b_bc, b_vec], [b_tmpb])
    bo(lambda h: h.tensor_tensor(out=bbar[:, 1], in0=bbar[:, 1], in1=tmpb[:], op=ALU.add), [b_bbar, b_tmpb], [b_bbar])

    WB, WBp, WC = [blockB[:, 7168 + i * 512:7168 + (i + 1) * 512].rearrange("p (r g k) -> p r g k", r=2, g=NG) for i in range(3)]; b_W = Buf("W")

    def fwd_slice(t, lanes, k0):
        return tb_[lanes, t, :, k0:k0 + 8]

    def rev_slice(t, lanes, k_hi):
        base = tb_[lanes, t, :, k_hi:k_hi + 1]
        return mkap(base, 0, [[9, NG], [-1, 8]])
    LF = slice(0, 64); LB = slice(64, 128)
    for ri, (tw, tv) in enumerate(((T_WRE, T_VRE), (T_WIM, T_VIM))):
        cp = lambda o, i_: S.op("dve", lambda h: h.tensor_copy(out=o, in_=i_), reads=[b_tab], writes=[b_W])
        cp(WB[LF, ri], rev_slice(tw, LF, 7))
        cp(WB[LB, ri], fwd_slice(tw, LB, 0))
        cp(WBp[LF, ri], fwd_slice(tv, LF, 1))
        cp(WBp[LB, ri], rev_slice(tv, LB, 8))
        cp(WC[LF, ri], fwd_slice(tw, LF, 1))
        cp(WC[LB, ri], rev_slice(tw, LB, 8))

    D0m = sb("D0m", [128, NG, 128], BF16); b_D0 = Buf("D0")
    PTre = sb("PTre", [128, NG, 128], BF16); PTim = sb("PTim", [128, NG, 128], BF16); b_PT = Buf("PT")
    Qre = sb("Qre", [128, NG, 128], BF16); Qimn = sb("Qimn", [128, NG, 128], BF16); b_Q = Buf("Q")
    A1 = sb("A1", [128, NG, 2]); A2 = sb("A2", [128, NG, 2]); b_A = Buf("A12")
    S.op("dve", lambda h: h.tensor_copy(out=A1[:], in_=mkap(tb_[:, T_WRE, :, 8:9], 0, [[9, NG], [0, 2]])), reads=[b_tab], writes=[b_A])
    S.op("dve", lambda h: h.tensor_copy(out=A2[:, :, 1], in_=tb_[:, T_WIM, :, 8]), reads=[b_tab], writes=[b_A])
    S.op("dve", lambda h: h.tensor_scalar(out=A2[:, :, 0], in0=tb_[:, T_WIM, :, 8], scalar1=-1.0, scalar2=None, op0=ALU.mult), reads=[b_tab], writes=[b_A])

    GC = 8
    gen = arena[:, 0:8192]

    def gslot(i):
        return gen[:, i * 1024:(i + 1) * 1024].rearrange("p (g i c) -> p g i c", g=GC, i=8)

    def cprod(Wt, X, g0, o_re, o_im, breads):
        def wv(ri):
            return mkap(Wt[:, ri, g0:g0 + GC, :], 0, [[8, GC], [1, 8], [0, 16]])

        def xv(ri):
            return mkap(X[:, ri, g0:g0 + GC, :], 0, [[16, GC], [0, 8], [1, 16]])
        t1 = gslot(6); t2 = gslot(7)
        o = lambda fn: S.op("dve", fn, reads=[b_W, b_arena] + breads, writes=[b_arena])
        o(lambda h: h.tensor_tensor(out=t1, in0=wv(0), in1=xv(0), op=ALU.mult))
        o(lambda h: h.tensor_tensor(out=t2, in0=wv(1), in1=xv(1), op=ALU.mult))
        o(lambda h: h.tensor_tensor(out=o_re, in0=t1, in1=t2, op=ALU.subtract))
        o(lambda h: h.tensor_tensor(out=t1, in0=wv(0), in1=xv(1), op=ALU.mult))
        o(lambda h: h.tensor_tensor(out=t2, in0=wv(1), in1=xv(0), op=ALU.mult))
        o(lambda h: h.tensor_tensor(out=o_im, in0=t1, in1=t2, op=ALU.add))

    for gch in range(NG // GC):
        g0 = gch * GC
        Bt_re, Bt_im, Bp_re, Bp_im, Ct_re, Ct_im = [gslot(i) for i in range(6)]
        cprod(WB, bbar, g0, Bt_re, Bt_im, [b_bbar])
        cprod(WBp, bbar, g0, Bp_re, Bp_im, [b_bbar])
        cprod(WC, scv, g0, Ct_re, Ct_im, [b_bc])
        fl = lambda a: a.rearrange("p g i c -> p g (i c)")
        S.op("act", lambda h, g0=g0, Ct_re=Ct_re: h.activation(out=Qre[:, g0:g0 + GC, :], in_=Ct_re.rearrange("p g i c -> p g (i c)"), func=AF.Identity), reads=[b_arena], writes=[b_Q])
        S.op("act", lambda h, g0=g0, Ct_im=Ct_im: h.activation(out=Qimn[:, g0:g0 + GC, :], in_=Ct_im.rearrange("p g i c -> p g (i c)"), func=AF.Identity, scale=-1.0), reads=[b_arena], writes=[b_Q])
        S.op("dve", lambda h, Ct_im=Ct_im: h.tensor_scalar(out=Ct_im.rearrange("p g i c -> p g (i c)"), in0=Ct_im.rearrange("p g i c -> p g (i c)"), scalar1=-1.0, scalar2=None, op0=ALU.mult), reads=[b_arena, b_Q], writes=[b_arena])
        for ri, (Bt, PT) in enumerate(((Bt_re, PTre), (Bt_im, PTim))):
            for gg in range(GC):
                S.op("pe", lambda h, gg=gg, Bt=Bt: h.transpose(out=psT[:, gg * 128:(gg + 1) * 128], in_=Bt.rearrange("p g i c -> p g (i c)")[:, gg, :], identity=ident[:]),
                     reads=[b_arena, b_ident], writes=[b_psT])
            S.op("act", lambda h, PT=PT, g0=g0: h.activation(out=PT[:, g0:g0 + GC, :], in_=psT[:, :].rearrange("p (g m) -> p g m", g=GC), func=AF.Identity),
                 reads=[b_psT], writes=[b_PT])
        for g4 in range(GC // 4):
            pF, bpF = ps_next(); pB, bpB = ps_next()
            for gg in range(4):
                g = g4 * 4 + gg
                for lanes, pp, bpp in ((LF, pF, bpF), (LB, pB, bpB)):
                    S.op("pe", lambda h, g=g, gg=gg, lanes=lanes, pp=pp, Bp_re=Bp_re, Ct_re=Ct_re: h.matmul(pp[:, gg * 128:(gg + 1) * 128], lhsT=Bp_re.rearrange("p g i c -> p g (i c)")[lanes, g, :],
                                                                                  rhs=Ct_re.rearrange("p g i c -> p g (i c)")[lanes, g, :], start=True, stop=False),
                         reads=[b_arena], writes=[bpp])
                    S.op("pe", lambda h, g=g, gg=gg, lanes=lanes, pp=pp, Bp_im=Bp_im, Ct_im=Ct_im: h.matmul(pp[:, gg * 128:(gg + 1) * 128], lhsT=Bp_im.rearrange("p g i c -> p g (i c)")[lanes, g, :],
                                                                                  rhs=Ct_im.rearrange("p g i c -> p g (i c)")[lanes, g, :], start=False, stop=True),
                         reads=[b_arena], writes=[bpp])
            t1 = gslot(6).rearrange("p g i c -> p (g i c)")[:, 0:512]
            t2 = gslot(7).rearrange("p g i c -> p (g i c)")[:, 0:512]
            S.op("dve", lambda h, pF=pF, t1=t1: h.tensor_tensor(out=t1, in0=pF[:, :], in1=masks[:, 0, :], op=ALU.mult), reads=[bpF, b_masks, b_arena], writes=[b_arena])
            S.op("dve", lambda h, pB=pB, t2=t2: h.tensor_tensor(out=t2, in0=pB[:, :], in1=masks[:, 1, :], op=ALU.mult), reads=[bpB, b_masks, b_arena], writes=[b_arena])
            S.op("dve", lambda h, t1=t1, t2=t2: h.tensor_tensor(out=t1, in0=t1, in1=t2, op=ALU.add), reads=[b_arena], writes=[b_arena])
            for gg in range(4):
                g = g0 + g4 * 4 + gg
                S.op("dve", lambda h, g=g, gg=gg, t1=t1: h.scalar_tensor_tensor(out=D0m[:, g, :], in0=ident[:], scalar=drep[:, g:g + 1],
                                                                               in1=t1[:, gg * 128:(gg + 1) * 128], op0=ALU.mult, op1=ALU.add),
                     reads=[b_arena, b_ident, b_drep], writes=[b_D0])

    winu = sb("winu", [128, 8, 512], BF16); b_winu = Buf("winu")
    S.dma("pool", lambda h: h.dma_start(out=winu[:], in_=w_in[:, 0:512].rearrange("(k p) n -> p k n", p=128)), b_winu, writes=[b_winu])
    hTb = blockA[:, 0:4096].rearrange("p (k t) -> p k t", k=8)
    U_sb = blockA[:, 4096:8192]; b_Usb = Buf("U_sb")
    UT = blockB[:, 0:8192].bitcast(BF16).rearrange("p (g j) -> p g j", g=NG); b_UT = Buf("UT")
    UTo = blockA[:, 8192:10240].rearrange("p (g j) -> p g j", g=NG); b_UTo = Buf("UTo")
    Zoth = blockA[:, 10240:14336].rearrange("p (j e) -> p j e", e=64); b_Zoth = Buf("Zoth")
    Zctx = blockA[:, 14336:16384].rearrange("p (j e) -> p j e", e=64); b_Zctx = Buf("Zctx")
    Zown = arena[:, :].bitcast(BF16).rearrange("p (j e) -> p j e", e=64)
    b_Zown = b_arena
    cur = [sb(f"cur{i}", [128, NG, 2]) for i in range(2)]; b_cur = [Buf(f"cur{i}") for i in range(2)]
    st1 = sb("st1", [128, NG, 2]); st2 = sb("st2", [128, NG, 2]); b_st = Buf("st12")
    S.op("dve", lambda h: h.memset(cur[0][:], 0.0), writes=[b_cur[0]])
    scan_k = [0]

    def scan_step(zap, bz, lanes, store):
        k = scan_k[0]
        scan_k[0] += 1
        c0, bc0 = cur[k % 2], b_cur[k % 2]
        c1, bc1 = cur[(k + 1) % 2], b_cur[(k + 1) % 2]
        L = lanes
        sw = mkap(c0[L, :, 1:2], 0, [[2, NG], [-1, 2]])
        S.op("dve", lambda h: h.tensor_tensor(out=st1[L], in0=c0[L], in1=A1[L], op=ALU.mult), reads=[bc0, b_A], writes=[b_st])
        S.op("dve", lambda h: h.tensor_tensor(out=st2[L], in0=sw, in1=A2[L], op=ALU.mult), reads=[bc0, b_A, b_st], writes=[b_st])
        S.op("dve", lambda h: h.tensor_tensor(out=st1[L], in0=st1[L], in1=st2[L], op=ALU.add), reads=[b_st], writes=[b_st])
        S.op("dve", lambda h: h.tensor_tensor(out=c1[L], in0=st1[L], in1=zap, op=ALU.add), reads=[b_st, bz], writes=[bc1])
        if store:
            S.op("dve", lambda h: h.tensor_copy(out=zap, in_=c0[L]), reads=[bc0, bz], writes=[bz])
        if L != slice(0, 128):
            other = slice(64, 128) if L == slice(0, 64) else slice(0, 64)
            S.op("dve", lambda h: h.tensor_copy(out=c1[other], in_=c0[other]), reads=[bc0], writes=[bc1])

    def phaseA_block(xsrc, possrc, t0, ntok, sh_fn, sc_fn, UTdst, bUT, J0):
        ntile = ntok // 128
        nJ = ntok // 8
        for tl in range(ntile):
            xt, bx = load_tile(xsrc, possrc, t0 + tl * 128)
            ln_stats(xt, bx, xn_t, b_xn)
            transpose_mod(xn_t, b_xn, lambda kc, tl=tl: hTb[:, kc, tl * 128:(tl + 1) * 128], b_hTb, sc_fn, sh_fn, [b_modfm])
        for i in range(8):
            ps, bps = ps_next()
            for kc in range(8):
                S.op("pe", lambda h, ps=ps, kc=kc, i=i: h.matmul(ps[0:nJ, :], lhsT=mkap(hTb[:, kc, i:i + 1], 0, [[8, nJ]]), rhs=winu[:, kc, :],
                                                                start=(kc == 0), stop=(kc == 7)),
                     reads=[b_hTb, b_winu], writes=[bps])
            S.op("act", lambda h, ps=ps, i=i: h.activation(out=mkap(U_sb[0:nJ, i * 16:i * 16 + 1], 0, [[128, NG], [1, 16]]), in_=ps[0:nJ, :].rearrange("p (g c) -> p g c", g=NG), func=AF.Identity), reads=[bps], writes=[b_Usb])
        for g8 in range(4):
            for gg in range(8):
                g = g8 * 8 + gg
                S.op("pe", lambda h, g=g, gg=gg: h.transpose(out=psB[:, gg * 128:gg * 128 + nJ], in_=U_sb[0:nJ, g * 128:(g + 1) * 128],
                                                             identity=identb[0:nJ, 0:nJ]),
                     reads=[b_Usb, b_identb], writes=[b_psB])
            S.op("dve", lambda h, g8=g8: h.tensor_copy(out=UTdst[:, g8 * 8:(g8 + 1) * 8, J0:J0 + nJ],
                                                       in_=psB[:, :].rearrange("p (g j) -> p g j", g=8)[:, :, 0:nJ]),
                 reads=[b_psB], writes=[bUT])

    def z_block(UTsrc, bUT, J0, nJ, Zdst_fn, bZ, lanes_list):
        for g in range(NG):
            ps, bps = ps_next()
            S.op("pe", lambda h, ps=ps, g=g: h.matmul(ps[:, 0:nJ], lhsT=PTre[:, g, :], rhs=UTsrc[:, g, J0:J0 + nJ], start=True, stop=True),
                 reads=[b_PT, bUT], writes=[bps])
            S.op("pe", lambda h, ps=ps, g=g: h.matmul(ps[:, 256:256 + nJ], lhsT=PTim[:, g, :], rhs=UTsrc[:, g, J0:J0 + nJ], start=True, stop=True),
                 reads=[b_PT, bUT, bps], writes=[bps])
            for lanes in lanes_list:
                S.op("act", lambda h, ps=ps, g=g, lanes=lanes: h.activation(
                    out=Zdst_fn(lanes, g), in_=ps[lanes, :].rearrange("p (r j) -> p r j", r=2)[:, :, 0:nJ], func=AF.Identity),
                    reads=[bps], writes=[bZ])

    def zdst(Zt, nJtot, Jbase, nJ):
        def fn(lanes, g):
            if lanes == LF:
                base = Zt[LF, Jbase:Jbase + 1, 2 * g:2 * g + 1]
                return mkap(base, 0, [[1, 2], [64, nJ]])
            base = Zt[LB, nJtot - 1 - Jbase:nJtot - Jbase, 2 * g:2 * g + 1]
            return mkap(base, 0, [[1, 2], [-64, nJ]])
        return fn

    ALL = slice(0, 128)
    sh1_fn = lambda kc: modfm[:, 0 + kc, 0:1]; sc1_fn = lambda kc: sc1p[:, kc, 0:1]
    csh1_fn = lambda kc: modfm[:, 0 + kc, 1:2]; csc1_fn = lambda kc: sc1p[:, kc, 1:2]
    phaseA_block(ctx_in, None, 0, 256, csh1_fn, csc1_fn, UTo, b_UTo, 0)
    z_block(UTo, b_UTo, 0, 32, zdst(Zctx, 32, 0, 32), b_Zctx, [LF, LB])
    for k in range(32):
        scan_step(Zctx[:, k, :].rearrange("p (g r) -> p g r", r=2), b_Zctx, ALL, False)
    for blk in range(8):
        phaseA_block(x_oth, pos_oth, blk * 512, 512, sh1_fn, sc1_fn, UTo, b_UTo, 0)
        z_block(UTo, b_UTo, 0, 64, zdst(Zoth, 64, 0, 64), b_Zoth, [LF])
        for k in range(64):
            scan_step(Zoth[LF, k, :].rearrange("p (g r) -> p g r", r=2), b_Zoth, LF, False)
    for blk in range(8):
        phaseA_block(x_own, pos_own, blk * 512, 512, sh1_fn, sc1_fn, UT, b_UT, blk * 64)
    for jb in range(4):
        z_block(UT, b_UT, jb * 128, 128, zdst(Zown, 512, jb * 128, 128), b_Zown, [LF, LB])
    for k in range(512):
        scan_step(Zown[:, k, :].rearrange("p (g r) -> p g r", r=2), b_Zown, ALL, True)


    Ysb = blockA[:, :].rearrange("p (g j) -> p g j", g=NG); b_Ysb = Buf("Ysb")
    TM = arena[:, 8192:10240].bitcast(BF16).rearrange("p (j c) -> p j c", j=8); b_TM = b_arena
    ya = arena[:, 0:8192].bitcast(BF16).rearrange("p (a t) -> p a t", a=4); b_ya = b_arena
    for g in range(NG):
        ps, bps = ps_next()
        S.op("pe", lambda h, ps=ps, g=g: h.matmul(ps[:, :], lhsT=D0m[:, g, :], rhs=UT[:, g, :], start=True, stop=False), reads=[b_D0, b_UT], writes=[bps])
        for lanes in (LF, LB):
            for ri, Qm in enumerate((Qre, Qimn)):
                if lanes == LF:
                    rhs = mkap(Zown[LF, 0:1, 2 * g + ri:2 * g + ri + 1], 0, [[64, 512]])
                else:
                    rhs = mkap(Zown[LB, 511:512, 2 * g + ri:2 * g + ri + 1], 0, [[-64, 512]])
                last = (lanes == LB and ri == 1)
                S.op("pe", lambda h, ps=ps, g=g, lanes=lanes, Qm=Qm, rhs=rhs, last=last: h.matmul(ps[:, :], lhsT=Qm[lanes, g, :], rhs=rhs, start=False, stop=last),
                     reads=[b_Q, b_Zown, bps], writes=[bps])
        S.op("act", lambda h, ps=ps, g=g: h.activation(out=Ysb[:, g, :], in_=ps[:, :], func=AF.Identity), reads=[bps], writes=[b_Ysb, b_hTb, b_Usb, b_UTo, b_Zoth, b_Zctx])
    gt = [arena[:, 10240 + i * 1024:10240 + (i + 1) * 1024] for i in range(2)]; b_gt = b_arena
    for jb in range(4):
        for g8 in range(4):
            for gg in range(8):
                g = g8 * 8 + gg
                S.op("pe", lambda h, g=g, gg=gg, jb=jb: h.transpose(out=psB[:, gg * 128:(gg + 1) * 128], in_=Ysb[:, g, jb * 128:(jb + 1) * 128], identity=identb[:]),
                     reads=[b_Ysb, b_identb], writes=[b_psB])
            S.op("dve", lambda h, g8=g8: h.tensor_copy(
                out=mkap(TM[:, 0:1, g8 * 128:g8 * 128 + 1], 0, [[16, 8], [512, 8], [1, 16]]),
                in_=psB[:, :].rearrange("p (g j c) -> p g j c", g=8, j=8)), reads=[b_psB], writes=[b_TM])
        for ct in range(4):
            for j in range(8):
                S.op("pe", lambda h, ct=ct, j=j: h.transpose(out=psB[:, j * 128:(j + 1) * 128], in_=TM[:, j, ct * 128:(ct + 1) * 128], identity=identb[:]),
                     reads=[b_TM, b_identb], writes=[b_psB])
            xin = psB[:, :]
            g0t, g1t = gt[0], gt[1]
            S.op("act", lambda h: h.activation(out=g0t, in_=xin, func=AF.Square), reads=[b_psB], writes=[b_gt])
            S.op("dve", lambda h: h.tensor_scalar(out=g0t, in0=g0t, scalar1=0.044715, scalar2=1.0, op0=ALU.mult, op1=ALU.add), reads=[b_gt], writes=[b_gt])
            S.op("dve", lambda h: h.tensor_tensor(out=g0t, in0=g0t, in1=xin, op=ALU.mult), reads=[b_gt, b_psB], writes=[b_gt])
            S.op("act", lambda h: h.activation(out=g1t, in_=g0t, func=AF.Sigmoid, scale=GELU_C), reads=[b_gt], writes=[b_gt])
            S.op("dve", lambda h, ct=ct, jb=jb: h.tensor_tensor(
                out=mkap(ya[:, ct, jb * 1024:jb * 1024 + 1], 0, [[1, 8], [8, 128]]),
                in0=g1t.rearrange("p (j J) -> p j J", j=8), in1=psB[:, :].rearrange("p (j J) -> p j J", j=8), op=ALU.mult),
                reads=[b_gt, b_psB], writes=[b_ya])

    if debug == "ya":
        b_dd = b_arena
        S.dma("sp", lambda h: h.dma_start(out=dbg_d, in_=ya.rearrange("p a b -> p (a b)")), b_dd, reads=[b_dd])
        S.final_wait("sp", [b_dd])
        S.emit()
        return nc

    def alias(new, olds):
        for o in olds:
            if o.writer is not None:
                new.readers.append(o.writer)
            new.readers.extend(o.readers)
        return new

    dead_A = [b_Ysb, b_hTb, b_Usb, b_UTo, b_Zoth, b_Zctx]
    dead_B = [b_UT, b_bc, b_tab, b_bbar, b_tmpb, b_W]
    b_blkA = alias(Buf("blkA"), dead_A)
    b_up0 = alias(Buf("up0"), [b_arena])
    gsec = blockA[:, :].bitcast(F32).rearrange("p (k n) -> p k n", k=8)
    crep = arena[:, 8192:9216].rearrange("p (k m) -> p k m", k=8)
    badrow = arena[:, 9216:10240]
    g1bc = blockB[:, 6144:7168]; g2bc = blockB[:, 7168:8192]; b_gbc = alias(Buf("gbc"), dead_B)
    lnp = blockB[:, 8192:10240]; b_lnp = alias(Buf("lnp"), dead_B)
    S.op("dve", lambda h: h.tensor_copy(out=crep, in_=mkap(csil[:], 0, [[1, 8], [0, 128]])), reads=[b_csil], writes=[b_up0])
    for sec, gdst in ((2, g1bc), (5, g2bc)):
        S.dma("sp", lambda h, sec=sec: h.dma_start(out=gsec, in_=w_ada[:, sec * D:(sec + 1) * D].rearrange("(k p) n -> p k n", p=128)),
              b_blkA, writes=[b_blkA])
        S.dma("sp", lambda h, sec=sec: h.dma_start(out=badrow, in_=b_ada[:, sec * D:(sec + 1) * D].partition_broadcast(128)),
              b_up0, writes=[b_up0])
        for nb in range(2):
            ps, bps = ps_next()
            for kc in range(8):
                S.op("pe", lambda h, ps=ps, kc=kc, nb=nb: h.matmul(ps[:, :], lhsT=crep[:, kc, :], rhs=gsec[:, kc, nb * 512:(nb + 1) * 512],
                                                                  start=(kc == 0), stop=(kc == 7)),
                     reads=[b_up0, b_blkA], writes=[bps])
            S.op("dve", lambda h, ps=ps, nb=nb, gdst=gdst: h.tensor_tensor(out=gdst[:, nb * 512:(nb + 1) * 512], in0=ps[:, :],
                                                                          in1=badrow[:, nb * 512:(nb + 1) * 512], op=ALU.add),
                 reads=[bps, b_up0], writes=[b_gbc])
    S.dma("sp", lambda h: h.dma_start(out=lnp, in_=lnp_in[:, 0:2048].partition_broadcast(128)), b_lnp, writes=[b_lnp])

    winA = blockA[:, :].rearrange("p (k n) -> p k n", k=8)
    winB = blockB[:, 0:6144].bitcast(BF16).rearrange("p (k n) -> p k n", k=8)
    b_winB = alias(Buf("winB"), dead_B)
    for c0, c1, dst, bd in ((512, 1536, winA[:, :, 0:1024], b_blkA), (1536, 2560, winA[:, :, 1024:2048], b_blkA),
                            (2560, 3584, winB[:, :, 0:1024], b_winB), (3584, 4096, winB[:, :, 1024:1536], b_winB)):
        S.dma("pool", lambda h, c0=c0, c1=c1, dst=dst: h.dma_start(out=dst, in_=w_in[:, c0:c1].rearrange("(k p) n -> p k n", p=128)),
              bd, writes=[bd])
    flat = lambda t: t[:].rearrange("p g m -> p (g m)")
    wval = flat(PTre).rearrange("p (k n) -> p k n", k=4); wgate = flat(PTim).rearrange("p (k n) -> p k n", k=4)
    convo = flat(Qre).rearrange("p (k n) -> p k n", k=4)
    wo_lo = flat(Qimn).rearrange("p (k n) -> p k n", k=4); wo_hi = flat(D0m).rearrange("p (k n) -> p k n", k=4)
    b_wglu = alias(Buf("wglu"), [b_PT]); b_convo = alias(Buf("convo"), [b_Q]); b_wo = alias(Buf("wo"), [b_Q, b_D0])
    S.dma("pool", lambda h: h.dma_start(out=wval, in_=w_val.rearrange("(k p) n -> p k n", p=128)), b_wglu, writes=[b_wglu])
    S.dma("pool", lambda h: h.dma_start(out=wgate, in_=w_gate.rearrange("(k p) n -> p k n", p=128)), b_wglu, writes=[b_wglu])
    S.dma("pool", lambda h: h.dma_start(out=convo, in_=conv_out.rearrange("(k p) n -> p k n", p=128)), b_convo, writes=[b_convo])
    S.dma("pool", lambda h: h.dma_start(out=wo_lo, in_=w_o[0:512, :].rearrange("(k p) n -> p k n", p=128)), b_wo, writes=[b_wo])
    S.dma("pool", lambda h: h.dma_start(out=wo_hi, in_=w_o[512:1024, :].rearrange("(k p) n -> p k n", p=128)), b_wo, writes=[b_wo])
    rw32 = masks[:, 1, 0:288].rearrange("p (k n) -> p k n", k=8); b_rw = alias(Buf("rw32"), [b_masks])
    S.dma("sp", lambda h: h.dma_start(out=rw32, in_=rw_in.rearrange("(k p) n -> p k n", p=128)), b_rw, writes=[b_rw])
    rt = masks[:, 0, 0:160]; b_rt = alias(Buf("rt"), [b_masks])

    hT = arena[:, 8192:10240].bitcast(BF16).rearrange("p (k t) -> p k t", k=8); b_hT = b_up0
    merged = arena[:, 10240:12288].bitcast(BF16).rearrange("p (k t) -> p k t", k=8); b_merged = alias(Buf("merged"), [b_arena])
    gv = arena[:, 12288:13312].bitcast(BF16).rearrange("p (k t) -> p k t", k=4); b_gv = alias(Buf("gv"), [b_arena])
    tmp = [arena[:, 13312 + i * 512:13312 + (i + 1) * 512] for i in range(5)]
    b_tmp = [alias(Buf(f"tmp{i}"), [b_arena]) for i in range(5)]
    h2b = arena[:, 15872:16384].bitcast(BF16).rearrange("p (k t) -> p k t", k=8); b_h2b = alias(Buf("h2b"), [b_arena])
    wr_f = winu[:].rearrange("p k n -> p (k n)").bitcast(F32)
    Wr = wr_f[:, 0:1024].rearrange("p (t e) -> p t e", e=32); b_Wr = alias(Buf("Wr"), [b_winu])
    h2f = wr_f[:, 1024:2048].rearrange("p (k t) -> p k t", k=8); b_h2f = alias(Buf("h2f"), [b_winu])
    b_x1s = [Buf(f"x1s{i}") for i in range(32)]
    b_h2s = [Buf(f"h2s{i}") for i in range(32)]
    BIG = 1.0e30

    def proj(ft, rhs_ap, brhs):
        piece, off, bp = (winA, (ft - 4) * 128, b_blkA) if ft < 20 else (winB, (ft - 20) * 128, b_winB)
        ps, bps = ps_next()
        for kc in range(8):
            S.op("pe", lambda h, ps=ps, kc=kc, piece=piece, off=off: h.matmul(ps[:, :], lhsT=piece[:, kc, off:off + 128], rhs=rhs_ap(kc),
                                                                             start=(kc == 0), stop=(kc == 7)),
                 reads=[bp, brhs], writes=[bps])
        return ps, bps

    def mm4(wt, bw, dt, rhs_fn, brhs):
        ps, bps = ps_next()
        for ct in range(4):
            S.op("pe", lambda h, ps=ps, ct=ct: h.matmul(ps[:, :], lhsT=wt[:, ct, dt * 128:(dt + 1) * 128], rhs=rhs_fn(ct), start=(ct == 0), stop=(ct == 3)),
                 reads=[bw, brhs], writes=[bps])
        return ps, bps

    def dv(fn, r, w):
        S.op("dve", fn, reads=r, writes=w)

    for tb in range(8):
        t0 = tb * 512
        for tl in range(4):
            xt, bx = load_tile(x_own, pos_own, t0 + tl * 128)
            ln_stats(xt, bx, xn_t, b_xn)
            transpose_mod(xn_t, b_xn, lambda kc, tl=tl: hT[:, kc, tl * 128:(tl + 1) * 128], b_hT, sc1_fn, sh1_fn, [b_modfm])
        hrhs = lambda kc: hT[:, kc, :]
        for ct in range(4):
            pz, bpz = proj(4 + ct, hrhs, b_hT)
            pgc, bpgc = proj(12 + ct, hrhs, b_hT)
            S.op("act", lambda h, pgc=pgc: h.activation(out=tmp[0], in_=pgc[:, :], func=AF.Identity), reads=[bpgc], writes=[b_tmp[0]])
            dv(lambda h, pz=pz: h.tensor_tensor(out=tmp[1], in0=pz[:, :], in1=tmp[0], op=ALU.mult), [bpz, b_tmp[0]], [b_tmp[1]])
            dv(lambda h, ct=ct: h.tensor_scalar(out=tmp[2], in0=tmp[1], scalar1=convw[:, ct, 1:2], scalar2=None, op0=ALU.mult), [b_tmp[1], b_convw], [b_tmp[2]])
            zv = tmp[1].rearrange("p (r c) -> p r c", c=64); vv = tmp[2].rearrange("p (r c) -> p r c", c=64)
            dv(lambda h, ct=ct, zv=zv, vv=vv: h.scalar_tensor_tensor(out=vv[:, :, 1:64], in0=zv[:, :, 0:63], scalar=convw[:, ct, 0:1], in1=vv[:, :, 1:64],
                                                                     op0=ALU.mult, op1=ALU.add), [b_tmp[1], b_tmp[2], b_convw], [b_tmp[2]])
            dv(lambda h, ct=ct, zv=zv, vv=vv: h.scalar_tensor_tensor(out=vv[:, :, 0:63], in0=zv[:, :, 1:64], scalar=convw[:, ct, 2:3], in1=vv[:, :, 0:63],
                                                                     op0=ALU.mult, op1=ALU.add), [b_tmp[1], b_tmp[2], b_convw], [b_tmp[2]])
            pgb, bpgb = proj(8 + ct, hrhs, b_hT)
            dv(lambda h, ct=ct, pgb=pgb: h.tensor_tensor(out=gv[:, ct, :], in0=pgb[:, :], in1=tmp[2], op=ALU.mult), [bpgb, b_tmp[2]], [b_gv])
        for dt in range(8):
            pob, bpob = mm4(convo, b_convo, dt, lambda ct: gv[:, ct, :], b_gv)
            pval, bpval = mm4(wval, b_wglu, dt, lambda ct, t0=t0: ya[:, ct, t0:t0 + 512], b_arena)
            pgt, bpgt = mm4(wgate, b_wglu, dt, lambda ct, t0=t0: ya[:, ct, t0:t0 + 512], b_arena)
            pma, bpma = proj(16 + dt, hrhs, b_hT)
            pmb, bpmb = proj(24 + dt, hrhs, b_hT)
            S.op("act", lambda h, pgt=pgt: h.activation(out=tmp[0], in_=pgt[:, :], func=AF.Sigmoid), reads=[bpgt], writes=[b_tmp[0]])
            S.op("act", lambda h, pma=pma: h.activation(out=tmp[1], in_=pma[:, :], func=AF.Sigmoid), reads=[bpma], writes=[b_tmp[1]])
            S.op("act", lambda h, pmb=pmb: h.activation(out=tmp[2], in_=pmb[:, :], func=AF.Sigmoid), reads=[bpmb], writes=[b_tmp[2]])
            dv(lambda h, pval=pval: h.tensor_tensor(out=tmp[3], in0=pval[:, :], in1=tmp[0], op=ALU.mult), [bpval, b_tmp[0]], [b_tmp[3]])
            dv(lambda h: h.tensor_tensor(out=tmp[3], in0=tmp[3], in1=tmp[1], op=ALU.mult), [b_tmp[3], b_tmp[1]], [b_tmp[3]])
            dv(lambda h, pob=pob: h.tensor_tensor(out=tmp[4], in0=pob[:, :], in1=tmp[2], op=ALU.mult), [bpob, b_tmp[2]], [b_tmp[4]])
            dv(lambda h, dt=dt: h.tensor_tensor(out=merged[:, dt, :], in0=tmp[3], in1=tmp[4], op=ALU.add), [b_tmp[3], b_tmp[4]], [b_merged])
        for tl in range(4):
            gti = tb * 4 + tl
            r0 = gti * 128
            xt, bx = load_tile(x_own, pos_own, r0)
            for nb in range(2):
                ps, bps = ps_next()
                for dt in range(8):
                    wsl = wo_lo[:, dt, nb * 512:(nb + 1) * 512] if dt < 4 else wo_hi[:, dt - 4, nb * 512:(nb + 1) * 512]
                    S.op("pe", lambda h, ps=ps, dt=dt, tl=tl, wsl=wsl: h.matmul(ps[:, :], lhsT=merged[:, dt, tl * 128:(tl + 1) * 128], rhs=wsl,
                                                                               start=(dt == 0), stop=(dt == 7)),
                         reads=[b_merged, b_wo], writes=[bps])
                dv(lambda h, ps=ps, nb=nb: h.tensor_tensor(out=xn_t[:, nb * 512:(nb + 1) * 512], in0=ps[:, :], in1=g1bc[:, nb * 512:(nb + 1) * 512], op=ALU.mult),
                   [bps, b_gbc], [b_xn])
            dv(lambda h, xt=xt: h.scalar_tensor_tensor(out=xt[:, :], in0=xt[:, :], scalar=ALPHA, in1=xn_t[:, :], op0=ALU.mult, op1=ALU.add), [bx, b_xn], [bx])
            ln_stats(xt, bx, xn_t, b_xn)
            dv(lambda h: h.tensor_tensor(out=xn_t[:, :], in0=xn_t[:, :], in1=lnp[:, 0:1024], op=ALU.mult), [b_xn, b_lnp], [b_xn])
            dv(lambda h, xt=xt: h.tensor_tensor(out=xt[:, :], in0=xn_t[:, :], in1=lnp[:, 1024:2048], op=ALU.add), [b_xn, b_lnp], [bx])
            S.dma("sp", lambda h, xt=xt, r0=r0: h.dma_start(out=x1s[r0:r0 + 128, :], in_=xt[:, :]), bx, reads=[bx], writes=[b_x1s[gti]])
            ln_stats(xt, bx, xn_t, b_xn)
            for kc in range(8):
                S.op("pe", lambda h, kc=kc: h.transpose(out=psT[:, kc * 128:(kc + 1) * 128], in_=xn_t[:, kc * 128:(kc + 1) * 128], identity=ident[:]),
                     reads=[b_xn, b_ident], writes=[b_psT])
            for kc in range(8):
                S.op("act", lambda h, kc=kc: h.activation(out=h2b[:, kc, :], in_=psT[:, kc * 128:(kc + 1) * 128], func=AF.Identity,
                                                         bias=modfm[:, 24 + kc, 0:1], scale=sc2p[:, kc:kc + 1]),
                     reads=[b_psT, b_modfm], writes=[b_h2b])
                S.op("act", lambda h, kc=kc: h.activation(out=h2f[:, kc, :], in_=psT[:, kc * 128:(kc + 1) * 128], func=AF.Identity,
                                                         bias=modfm[:, 24 + kc, 0:1], scale=sc2p[:, kc:kc + 1]),
                     reads=[b_psT, b_modfm], writes=[b_h2f])
            S.dma("sp", lambda h, r0=r0: h.dma_start(out=h2s[:, :, r0:r0 + 128], in_=h2b), b_h2b, reads=[b_h2b], writes=[b_h2s[gti]])
            ps, bps = ps_next()
            for kc in range(8):
                S.op("pe", lambda h, ps=ps, kc=kc: h.matmul(ps[:, 0:36], lhsT=h2f[:, kc, :], rhs=rw32[:, kc, :], start=(kc == 0), stop=(kc == 7)),
                     reads=[b_h2f, b_rw], writes=[bps])
            R = lambda a, b_: rt[:, a:b_]
            rr = [b_rt]
            dv(lambda h, ps=ps: h.tensor_tensor(out=R(0, 36), in0=ps[:, 0:36], in1=rb_bc[:, :], op=ALU.add), [bps, b_rb, b_rt], rr)
            dv(lambda h: h.tensor_reduce(out=R(36, 37), in_=R(0, 4), axis=AX.X, op=ALU.max), rr, rr)
            dv(lambda h: h.tensor_scalar(out=R(38, 42), in0=R(0, 4), scalar1=R(36, 37), scalar2=None, op0=ALU.is_equal), rr, rr)
            dv(lambda h: h.tensor_scalar(out=R(37, 38), in0=R(36, 37), scalar1=-1.0, scalar2=None, op0=ALU.mult), rr, rr)
            S.op("act", lambda h: h.activation(out=R(42, 46), in_=R(0, 4), func=AF.Exp, bias=R(37, 38), scale=1.0), reads=rr, writes=rr)
            dv(lambda h: h.tensor_reduce(out=R(46, 47), in_=R(42, 46), axis=AX.X, op=ALU.add), rr, rr)
            dv(lambda h: h.reciprocal(out=R(47, 48), in_=R(46, 47)), rr, rr)
            dv(lambda h: h.tensor_scalar(out=R(48, 52), in0=R(38, 42), scalar1=BIG, scalar2=-BIG, op0=ALU.mult, op1=ALU.add), rr, rr)
            dv(lambda h: h.tensor_tensor(out=R(52, 84).rearrange("p (g e) -> p g e", e=8), in0=R(4, 36).rearrange("p (g e) -> p g e", e=8),
                                         in1=mkap(R(48, 52), 0, [[1, 4], [0, 8]]), op=ALU.add), rr, rr)
            dv(lambda h: h.tensor_reduce(out=R(84, 85), in_=R(52, 84), axis=AX.X, op=ALU.max), rr, rr)
            dv(lambda h: h.tensor_scalar(out=R(85, 117), in0=R(52, 84), scalar1=R(84, 85), scalar2=None, op0=ALU.is_equal), rr, rr)
            dv(lambda h: h.scalar_tensor_tensor(out=R(117, 149), in0=R(85, 117), scalar=-BIG, in1=R(52, 84), op0=ALU.mult, op1=ALU.add), rr, rr)
            dv(lambda h: h.tensor_reduce(out=R(149, 150), in_=R(117, 149), axis=AX.X, op=ALU.max), rr, rr)
            dv(lambda h: h.tensor_scalar(out=R(52, 84), in0=R(117, 149), scalar1=R(149, 150), scalar2=None, op0=ALU.is_equal), rr, rr)
            dv(lambda h: h.tensor_tensor(out=R(150, 151), in0=R(149, 150), in1=R(84, 85), op=ALU.subtract), rr, rr)
            S.op("act", lambda h: h.activation(out=R(151, 152), in_=R(150, 151), func=AF.Exp), reads=rr, writes=rr)
            dv(lambda h: h.tensor_scalar(out=R(152, 153), in0=R(151, 152), scalar1=1.0, scalar2=None, op0=ALU.add), rr, rr)
            dv(lambda h: h.reciprocal(out=R(153, 154), in_=R(152, 153)), rr, rr)
            dv(lambda h: h.tensor_tensor(out=R(154, 155), in0=R(151, 152), in1=R(153, 154), op=ALU.mult), rr, rr)
            dv(lambda h: h.tensor_tensor(out=R(155, 156), in0=R(153, 154), in1=R(47, 48), op=ALU.mult), rr, rr)
            dv(lambda h: h.tensor_tensor(out=R(156, 157), in0=R(154, 155), in1=R(47, 48), op=ALU.mult), rr, rr)
            dv(lambda h: h.tensor_scalar(out=R(85, 117), in0=R(85, 117), scalar1=R(155, 156), scalar2=None, op0=ALU.mult), rr, rr)
            dv(lambda h, gti=gti: h.scalar_tensor_tensor(out=Wr[:, gti, :], in0=R(52, 84), scalar=R(156, 157), in1=R(85, 117), op0=ALU.mult, op1=ALU.add),
               rr + [b_Wr], [b_Wr])

    if debug == "d":
        S.dma("sp", lambda h: h.dma_start(out=wrs, in_=Wr), b_Wr, reads=[b_Wr])
        S.final_wait("sp", b_x1s + b_h2s + [b_Wr])
        S.emit()
        return nc

    acc = arena[:, :].rearrange("p (t d) -> p t d", d=1024)
    b_acc = alias(Buf("acc"), [b_arena, b_up0, b_merged, b_gv, b_h2b] + b_tmp)
    h2T = blockA[:, :].rearrange("p (k t) -> p k t", k=8)
    b_h2T = b_blkA
    wbuf = [
        (flat(PTre).rearrange("p (k n) -> p k n", k=8), flat(PTim).rearrange("p (k n) -> p k n", k=8), flat(Qre).rearrange("p (k n) -> p k n", k=4)),
        (flat(Qimn).rearrange("p (k n) -> p k n", k=8), flat(D0m).rearrange("p (k n) -> p k n", k=8),
         blockB[:, 0:2048].bitcast(BF16).rearrange("p (k n) -> p k n", k=4)),
    ]
    b_wb = [alias(Buf("wb0"), [b_wglu, b_convo]), alias(Buf("wb1"), [b_wo, b_winB])]
    hid = [blockB[:, 2048 + i * 1024:2048 + (i + 1) * 1024].bitcast(BF16).rearrange("p (k t) -> p k t", k=4) for i in range(2)]
    b_hid = [alias(Buf(f"hid{i}"), [b_winB]) for i in range(2)]
    stmp = [blockB[:, 4096 + i * 512:4096 + (i + 1) * 512] for i in range(2)]
    b_stmp = [alias(Buf(f"stmp{i}"), [b_winB]) for i in range(2)]
    b_out = [Buf(f"out{i}") for i in range(32)]
    S.dma("sp", lambda h: h.dma_start(out=lnp, in_=lnp_in[:, 2048:4096].partition_broadcast(128)), b_lnp, writes=[b_lnp])
    hctr = 0
    for sbk in range(2):
        S.dma("sp", lambda h, sbk=sbk: h.dma_start(out=h2T, in_=h2s[:, :, sbk * 2048:(sbk + 1) * 2048]), b_h2T,
              reads=b_h2s[sbk * 16:(sbk + 1) * 16], writes=[b_h2T])
        for e in range(32):
            wi = (sbk * 32 + e) % 2
            wg_t, wu_t, wd_t = wbuf[wi]
            bw = b_wb[wi]
            S.dma("pool", lambda h, e=e, wg_t=wg_t: h.dma_start(out=wg_t, in_=wg_in[e].rearrange("(k p) n -> p k n", p=128)), bw, writes=[bw])
            S.dma("pool", lambda h, e=e, wu_t=wu_t: h.dma_start(out=wu_t, in_=wu_in[e].rearrange("(k p) n -> p k n", p=128)), bw, writes=[bw])
            S.dma("pool", lambda h, e=e, wd_t=wd_t: h.dma_start(out=wd_t, in_=wd_in[e].rearrange("(k p) n -> p k n", p=128)), bw, writes=[bw])
            for tb in range(4):
                hb, bhb = hid[hctr % 2], b_hid[hctr % 2]
                hctr += 1
                for fc in range(4):
                    pg, bpg = ps_next()
                    for kc in range(8):
                        S.op("pe", lambda h, pg=pg, kc=kc, fc=fc, tb=tb, wg_t=wg_t: h.matmul(pg[:, :], lhsT=wg_t[:, kc, fc * 128:(fc + 1) * 128],
                                                                                            rhs=h2T[:, kc, tb * 512:(tb + 1) * 512], start=(kc == 0), stop=(kc == 7)),
                             reads=[bw, b_h2T], writes=[bpg])
                    pu, bpu = ps_next()
                    for kc in range(8):
                        S.op("pe", lambda h, pu=pu, kc=kc, fc=fc, tb=tb, wu_t=wu_t: h.matmul(pu[:, :], lhsT=wu_t[:, kc, fc * 128:(fc + 1) * 128],
                                                                                            rhs=h2T[:, kc, tb * 512:(tb + 1) * 512], start=(kc == 0), stop=(kc == 7)),
                             reads=[bw, b_h2T], writes=[bpu])
                    st_, bst_ = stmp[fc % 2], b_stmp[fc % 2]
                    S.op("act", lambda h, pg=pg, st_=st_: h.activation(out=st_, in_=pg[:, :], func=AF.Silu), reads=[bpg], writes=[bst_])
                    dv(lambda h, pu=pu, st_=st_, hb=hb, fc=fc: h.tensor_tensor(out=hb[:, fc, :], in0=pu[:, :], in1=st_, op=ALU.mult), [bpu, bst_], [bhb])
                for tl in range(4):
                    ti = tb * 4 + tl
                    gti = sbk * 16 + ti
                    for nb in range(2):
                        pd, bpd = ps_next()
                        for fc in range(4):
                            S.op("pe", lambda h, pd=pd, fc=fc, tl=tl, nb=nb, hb=hb, wd_t=wd_t: h.matmul(pd[:, :], lhsT=hb[:, fc, tl * 128:(tl + 1) * 128],
                                                                                                     rhs=wd_t[:, fc, nb * 512:(nb + 1) * 512], start=(fc == 0), stop=(fc == 3)),
                                 reads=[bhb, bw], writes=[bpd])
                        if e == 0:
                            dv(lambda h, pd=pd, ti=ti, nb=nb, gti=gti, e=e: h.tensor_scalar(out=acc[:, ti, nb * 512:(nb + 1) * 512], in0=pd[:, :], scalar1=Wr[:, gti, e:e + 1],
                                                                                          scalar2=None, op0=ALU.mult), [bpd, b_Wr], [b_acc])
                        else:
                            dv(lambda h, pd=pd, ti=ti, nb=nb, gti=gti, e=e: h.scalar_tensor_tensor(out=acc[:, ti, nb * 512:(nb + 1) * 512], in0=pd[:, :], scalar=Wr[:, gti, e:e + 1],
                                                                                                 in1=acc[:, ti, nb * 512:(nb + 1) * 512], op0=ALU.mult, op1=ALU.add),
                               [bpd, b_Wr, b_acc], [b_acc])
        for ti in range(16):
            gti = sbk * 16 + ti
            r0 = gti * 128
            i = xctr[0] % 2
            xctr[0] += 1
            xt, bx = xt_t[i], b_xt[i]
            S.dma("sp", lambda h, xt=xt, r0=r0: h.dma_start(out=xt[:], in_=x1s[r0:r0 + 128, :]), bx, reads=[b_x1s[gti]], writes=[bx])
            dv(lambda h, ti=ti: h.tensor_tensor(out=acc[:, ti, :], in0=acc[:, ti, :], in1=g2bc, op=ALU.mult), [b_acc, b_gbc], [b_acc])
            dv(lambda h, xt=xt, ti=ti: h.scalar_tensor_tensor(out=xt[:, :], in0=xt[:, :], scalar=ALPHA, in1=acc[:, ti, :], op0=ALU.mult, op1=ALU.add), [bx, b_acc], [bx])
            ln_stats(xt, bx, xn_t, b_xn)
            dv(lambda h: h.tensor_tensor(out=xn_t[:, :], in0=xn_t[:, :], in1=lnp[:, 0:1024], op=ALU.mult), [b_xn, b_lnp], [b_xn])
            dv(lambda h, xt=xt: h.tensor_tensor(out=xt[:, :], in0=xn_t[:, :], in1=lnp[:, 1024:2048], op=ALU.add), [b_xn, b_lnp], [bx])
            S.dma("sp", lambda h, xt=xt, r0=r0: h.dma_start(out=out_d[r0:r0 + 128, :], in_=xt[:, :]), bx, reads=[bx], writes=[b_out[gti]])
    S.final_wait("sp", b_out)
    S.emit()
    return nc


def host_inputs(inputs):
    f32 = np.float32
    g = {k: np.asarray(v) for k, v in inputs.items()}
    D_ = 1024
    q = D_ // 4
    omega = (1.0 / (10000.0 ** (np.arange(q, dtype=f32) / f32(q)))).astype(f32)
    r = (np.arange(128, dtype=f32)[:, None] * omega).astype(f32)
    cl = (np.arange(64, dtype=f32)[:, None] * omega).astype(f32)
    r_emb = np.concatenate([np.sin(r), np.cos(r)], -1).astype(f32)
    c_emb = np.concatenate([np.sin(cl), np.cos(cl)], -1).astype(f32)
    pos = np.concatenate([np.broadcast_to(r_emb[:, None, :], (128, 64, 2 * q)),
                          np.broadcast_to(c_emb[None, :, :], (128, 64, 2 * q))], -1).reshape(8192, D_).astype(f32)
    ident = np.eye(128, dtype=f32)
    ii = np.arange(128) // 16
    mF = (ii[None, :] >= ii[:, None]).astype(f32)
    mB = (ii[:, None] >= ii[None, :]).astype(f32)
    masks = np.stack([np.tile(mF, (1, 4)), np.tile(mB, (1, 4))], 1).astype(f32)
    sel = np.zeros((32, 32, 128), f32)
    for e in range(32):
        sel[e, e, :] = 1.0

    def tr(a):
        return np.ascontiguousarray(a.T)
    maps = []
    for core in range(8):
        b, hf = core // 2, core % 2
        xb = g["x"][b]
        if hf == 1:
            x_oth, x_own = xb[0:4096], xb[4096:8192]
            p_oth, p_own = pos[0:4096], pos[4096:8192]
            ctxl = g["ctx"][b]
            F, B = "f", "b"
            convw = g["conv_w"][0]
        else:
            x_oth, x_own = xb[4096:8192][::-1], xb[0:4096][::-1]
            p_oth, p_own = pos[4096:8192][::-1], pos[0:4096][::-1]
            ctxl = g["ctx"][b][::-1]
            F, B = "b", "f"
            convw = g["conv_w"][0][::-1]
        cT = np.concatenate([g["c"][b].reshape(8, 128).T, g["c_ctx"].reshape(8, 128).T], 1)
        small = np.zeros((128, 3, 32), f32)
        sbb = np.zeros((128, 2, 32, 16), f32)
        scc = np.zeros((128, 2, 32, 16), f32)
        for li, dname in ((0, F), (1, B)):
            L = slice(li * 64, li * 64 + 64)
            small[L, 0, :] = np.broadcast_to(g["s5_log_dt_" + dname][0][None, :], (64, 32))
            small[L, 1, :] = tr(g["s5_a_re_" + dname][0])
            small[L, 2, :] = tr(g["s5_a_im_" + dname][0])
            sbb[L, 0] = g["s5_b_re_" + dname][0].transpose(1, 0, 2)
            sbb[L, 1] = g["s5_b_im_" + dname][0].transpose(1, 0, 2)
            scc[L, 0] = g["s5_c_re_" + dname][0].transpose(2, 0, 1)
            scc[L, 1] = g["s5_c_im_" + dname][0].transpose(2, 0, 1)
        drep = np.tile(g["s5_d"][0].T, (8, 1))
        m = {
            "x_own": x_own, "x_oth": x_oth, "ctx": ctxl, "pos_own": p_own, "pos_oth": p_oth,
            "cT": cT, "w_ada": g["w_ada"][0], "b_adaT": g["b_ada"][0].reshape(48, 128).T, "b_ada": g["b_ada"][0][None, :],
            "w_in": g["w_in"][0], "s5_small": small, "s5_b": sbb, "s5_c": scc, "s5_drep": drep,
            "masks": masks, "ident": ident, "sel": sel,
            "w_val": g["s5_w_glu_val"][0], "w_gate": g["s5_w_glu_gate"][0],
            "convwT": convw.reshape(3, 4, 128).transpose(2, 1, 0), "conv_out": g["conv_w_out"][0], "w_o": g["w_o"][0],
            "lnp": np.concatenate([g["ln1_g"][0], g["ln1_b"][0], g["ln2_g"][0], g["ln2_b"][0]])[None, :],
            "rw": np.concatenate([g["router_w_group"][0], g["router_w_expert"][0]], 1),
            "rb": np.concatenate([g["router_b_group"][0], g["router_b_expert"][0]])[None, :],
            "wg": g["exp_w_gate"][0], "wu": g["exp_w_up"][0], "wd": g["exp_w_down"][0],
        }
        maps.append({k: np.ascontiguousarray(v, dtype=f32) for k, v in m.items()})
    return maps


def kernel(**inputs):
    maps = host_inputs(inputs)
    nc = build()
    res = run_bass_kernel_spmd(nc, maps, core_ids=list(range(8)))
    out = np.zeros((4, 8192, 1024), np.float32)
    for core in range(8):
        b, hf = core // 2, core % 2
        o = res.results[core]["out"]
        if hf == 1:
            out[b, 4096:8192] = o
        else:
            out[b, 0:4096] = o[::-1]
    return out
```

```python
import math
import numpy as np
import concourse.bass as bass
import concourse.mybir as mybir
from concourse.bass_utils import run_bass_kernel_spmd

F32 = mybir.dt.float32
BF16 = mybir.dt.bfloat16
I32 = mybir.dt.int32
AF = mybir.ActivationFunctionType
ALU = mybir.AluOpType
AX = mybir.AxisListType

ALPHA = 2.0 ** 0.25
LN_EPS = 1e-6
NT = 4096
D = 1024
NG = 32
GELU_C = 2.0 * math.sqrt(2.0 / math.pi)


class Buf:
    __slots__ = ("name", "writer", "readers", "sem", "semcount")

    def __init__(self, name):
        self.name = name
        self.writer = None
        self.readers = []
        self.sem = None
        self.semcount = 0


class Sched:
    SEM_CAP = 30000

    def __init__(self, nc):
        self.nc = nc
        self.eng = {n: dict(prog=[], sem=None, count=0, waited={}, nsem=0) for n in ("pe", "act", "dve", "pool", "sp")}
        self.nbufsem = 0

    def _engsem(self, E, name):
        if E["sem"] is None or E["count"] >= self.SEM_CAP:
            E["sem"] = self.nc.alloc_semaphore(f"s_{name}_{E['nsem']}")
            E["nsem"] += 1
            E["count"] = 0
        return E["sem"]

    def _waits(self, E, reads, writes):
        need = {}

        def add(tok):
            if tok is None:
                return
            s, v = tok
            k = id(s)
            if k not in need or need[k][1] < v:
                need[k] = (s, v)
        for b in reads:
            add(b.writer)
        for b in writes:
            add(b.writer)
            for r in b.readers:
                add(r)
        out = []
        for k, (s, v) in need.items():
            if E["waited"].get(k, 0) < v:
                E["waited"][k] = v
                out.append((s, v))
        return out

    def _commit(self, tok, reads, writes):
        for b in writes:
            b.writer = tok
            b.readers = []
        for b in reads:
            if b not in writes:
                b.readers.append(tok)
                if len(b.readers) > 48:
                    d = {}
                    for s, v in b.readers:
                        if id(s) not in d or d[id(s)][1] < v:
                            d[id(s)] = (s, v)
                    b.readers = list(d.values())

    def op(self, name, fn, reads=(), writes=()):
        E = self.eng[name]
        waits = self._waits(E, reads, writes)
        sem = self._engsem(E, name)
        E["count"] += 1
        val = E["count"]

        def run(h, waits=waits, fn=fn, sem=sem):
            for s, v in waits:
                h.wait_ge(s, v)
            fn(h).then_inc(sem, 1)
        E["prog"].append(run)
        self._commit((sem, val), reads, writes)

    def dma(self, qname, fn, owner, reads=(), writes=()):
        E = self.eng[qname]
        waits = self._waits(E, reads, writes)
        if owner.sem is None:
            owner.sem = self.nc.alloc_semaphore(f"d_{self.nbufsem}")
            self.nbufsem += 1
        owner.semcount += 16
        sem, val = owner.sem, owner.semcount

        def run(h, waits=waits, fn=fn, sem=sem):
            for s, v in waits:
                h.wait_ge(s, v)
            fn(h).then_inc(sem, 16)
        E["prog"].append(run)
        self._commit((sem, val), reads, writes)

    def final_wait(self, qname, bufs):
        E = self.eng[qname]
        waits = self._waits(E, bufs, ())

        def run(h, waits=waits):
            for s, v in waits:
                h.wait_ge(s, v)
        E["prog"].append(run)

    def emit(self):
        with self.nc.Block() as block:
            @block.tensor
            def _(h):
                for f in self.eng["pe"]["prog"]:
                    f(h)

            @block.scalar
            def _(h):
                for f in self.eng["act"]["prog"]:
                    f(h)

            @block.vector
            def _(h):
                for f in self.eng["dve"]["prog"]:
                    f(h)

            @block.gpsimd
            def _(h):
                for f in self.eng["pool"]["prog"]:
                    f(h)

            @block.sync
            def _(h):
                for f in self.eng["sp"]["prog"]:
                    f(h)


def mkap(base, off_elems, dims):
    return bass.AP(base.tensor, base.offset + off_elems, [list(base.ap[0])] + [list(d) for d in dims])


def build(debug=None):
    nc = bass.Bass("TRN2", target_bir_lowering=False)
    S = Sched(nc)

    def din(name, shape, dt=F32):
        return nc.dram_tensor(name, list(shape), dt, kind="ExternalInput").ap()

    x_own = din("x_own", [NT, D]); x_oth = din("x_oth", [NT, D]); ctx_in = din("ctx", [256, D])
    pos_own = din("pos_own", [NT, D]); pos_oth = din("pos_oth", [NT, D])
    cT_in = din("cT", [128, 16])
    w_ada = din("w_ada", [D, 6 * D]); b_adaT = din("b_adaT", [128, 48]); b_ada = din("b_ada", [1, 6 * D])
    w_in = din("w_in", [D, 4096])
    s5_small = din("s5_small", [128, 3, NG])
    s5_b = din("s5_b", [128, 2, NG, 16]); s5_c = din("s5_c", [128, 2, NG, 16]); s5_drep = din("s5_drep", [128, NG])
    masks_in = din("masks", [128, 2, 512]); ident_in = din("ident", [128, 128]); cst_in = din("cst", [128, 192]); tri_in = din("tri", [128, 256])
    w_val = din("w_val", [512, D]); w_gate = din("w_gate", [512, D])
    convw_in = din("convwT", [128, 4, 3]); conv_out = din("conv_out", [512, D]); w_o = din("w_o", [D, D])
    lnp_in = din("lnp", [1, 4 * D])
    rw_in = din("rw", [D, 36]); rb_in = din("rb", [1, 36])
    wg_in = din("wg", [32, D, 512]); wu_in = din("wu", [32, D, 512]); wd_in = din("wd", [32, 512, D])
    out_d = nc.dram_tensor("out", [NT, D], F32, kind="ExternalOutput").ap()
    x1s = nc.dram_tensor("x1s", [NT, D], F32, kind=("ExternalOutput" if debug else "Internal")).ap()
    h2s = nc.dram_tensor("h2s", [128, 8, NT], BF16, kind=("ExternalOutput" if debug else "Internal")).ap()
    wrs = nc.dram_tensor("wrs", [128, 32, 32], F32, kind="ExternalOutput").ap() if debug else None
    dbg_d = None
    if debug == "ya":
        dbg_d = nc.dram_tensor("dbg", [128, 4 * NT], BF16, kind="ExternalOutput").ap()

    def sb(name, shape, dt=F32):
        return nc.alloc_sbuf_tensor("sb_" + name, list(shape), dt)

    ident = sb("ident", [128, 128]); b_ident = Buf("ident")
    identb = sb("identb", [128, 128], BF16); b_identb = Buf("identb")
    masks = sb("masks", [128, 2, 512]); b_masks = Buf("masks")
    modfm = sb("modfm", [128, 48, 2]); b_modfm = Buf("modfm")
    sc1p = sb("sc1p", [128, 8, 2]); sc2p = sb("sc2p", [128, 8])
    convw = sb("convw", [128, 4, 3]); b_convw = Buf("convw")
    rb_bc = sb("rb_bc", [128, 36]); b_rb = Buf("rb")
    epst = sb("epst", [128, 1]); b_eps = Buf("eps")

    S.dma("sp", lambda h: h.dma_start(out=ident[:], in_=ident_in), b_ident, writes=[b_ident])
    S.dma("sp", lambda h: h.dma_start(out=masks[:], in_=masks_in), b_masks, writes=[b_masks])
    S.dma("sp", lambda h: h.dma_start(out=convw[:], in_=convw_in), b_convw, writes=[b_convw])
    S.dma("sp", lambda h: h.dma_start(out=rb_bc[:], in_=rb_in.partition_broadcast(128)), b_rb, writes=[b_rb])
    S.op("dve", lambda h: h.tensor_copy(out=identb[:], in_=ident[:]), reads=[b_ident], writes=[b_identb])
    S.op("dve", lambda h: h.memset(epst[:], LN_EPS), writes=[b_eps])

    psg = [nc.alloc_psum_tensor(f"psg{i}", [128, 512], F32) for i in range(5)]
    b_psg = [Buf(f"psg{i}") for i in range(5)]
    psT = nc.alloc_psum_tensor("psT", [128, 1024], F32); b_psT = Buf("psT")
    psB = nc.alloc_psum_tensor("psB", [128, 1024], BF16); b_psB = Buf("psB")
    pctr = [0]

    def ps_next():
        i = pctr[0] % 5
        pctr[0] += 1
        return psg[i], b_psg[i]

    cT = sb("cT", [128, 16]); b_cT = Buf("cT")
    csil = sb("csil", [128, 16]); b_csil = Buf("csil")
    csil2 = sb("csil2", [128, 8, 2]); b_csil2 = Buf("csil2")
    blockA = sb("blockA", [128, 16384], BF16)
    b_hTb = Buf("hTb")
    badT = sb("badT", [128, 48]); b_badT = Buf("badT")
    S.dma("sp", lambda h: h.dma_start(out=cT[:], in_=cT_in), b_cT, writes=[b_cT])
    S.dma("sp", lambda h: h.dma_start(out=badT[:], in_=b_adaT), b_badT, writes=[b_badT])
    S.op("act", lambda h: h.activation(out=csil[:], in_=cT[:], func=AF.Silu), reads=[b_cT], writes=[b_csil])
    S.op("dve", lambda h: h.tensor_copy(out=csil2[:, :, 0], in_=csil[:, 0:8]), reads=[b_csil], writes=[b_csil2])
    S.op("dve", lambda h: h.tensor_copy(out=csil2[:, :, 1], in_=csil[:, 8:16]), reads=[b_csil], writes=[b_csil2])

    arena = sb("arena", [128, 16384])
    b_arena = Buf("arena")
    wsec = arena[:, 0:8192].rearrange("p (k n) -> p k n", k=8)
    for sec in (0, 1, 3, 4):
        S.dma("sp", lambda h, sec=sec: h.dma_start(out=wsec, in_=w_ada[:, sec * D:(sec + 1) * D].rearrange("(k p) n -> p k n", p=128)),
              b_arena, writes=[b_arena])
        ps, bps = ps_next()
        for jj in range(8):
            for kc in range(8):
                S.op("pe", lambda h, ps=ps, kc=kc, jj=jj: h.matmul(ps[:, jj * 2:jj * 2 + 2], lhsT=wsec[:, kc, jj * 128:(jj + 1) * 128],
                                                                  rhs=csil2[:, kc, :], start=(kc == 0), stop=(kc == 7)),
                     reads=[b_csil2, b_arena], writes=[bps])
        S.op("dve", lambda h, ps=ps, sec=sec: h.tensor_tensor(
            out=modfm[:, sec * 8:(sec + 1) * 8, :], in0=ps[:, 0:16].rearrange("p (j t) -> p j t", t=2),
            in1=mkap(badT[:, sec * 8:(sec + 1) * 8], 0, [[1, 8], [0, 2]]), op=ALU.add),
            reads=[bps, b_badT], writes=[b_modfm])
    S.op("dve", lambda h: h.tensor_scalar(out=sc1p[:], in0=modfm[:, 8:16, :], scalar1=1.0, scalar2=None, op0=ALU.add), reads=[b_modfm], writes=[b_modfm])
    S.op("dve", lambda h: h.tensor_scalar(out=sc2p[:], in0=modfm[:, 32:40, 0], scalar1=1.0, scalar2=None, op0=ALU.add), reads=[b_modfm], writes=[b_modfm])

    xt_t = [sb(f"xt{i}", [128, D]) for i in range(2)]; b_xt = [Buf(f"xt{i}") for i in range(2)]
    xn_t = sb("xn", [128, D]); b_xn = Buf("xn")
    stt = sb("stt", [128, 16]); b_stt = Buf("stt")
    xctr = [0]

    def ln_stats(src, bsrc, dst, bdst):
        S.op("dve", lambda h: h.bn_stats(out=stt[:, 0:6], in_=src[:, 0:512]), reads=[bsrc], writes=[b_stt])
        S.op("dve", lambda h: h.bn_stats(out=stt[:, 6:12], in_=src[:, 512:1024]), reads=[bsrc], writes=[b_stt])
        S.op("dve", lambda h: h.bn_aggr(out=stt[:, 12:14], in_=stt[:, 0:12]), reads=[b_stt], writes=[b_stt])
        S.op("act", lambda h: h.activation(out=stt[:, 14:15], in_=stt[:, 13:14], func=AF.Sqrt, bias=epst[:, 0:1], scale=1.0),
             reads=[b_stt, b_eps], writes=[b_stt])
        S.op("dve", lambda h: h.reciprocal(out=stt[:, 15:16], in_=stt[:, 14:15]), reads=[b_stt], writes=[b_stt])
        S.op("dve", lambda h: h.tensor_scalar(out=dst[:, :], in0=src[:, :], scalar1=stt[:, 12:13], scalar2=stt[:, 15:16],
                                              op0=ALU.subtract, op1=ALU.mult), reads=[bsrc, b_stt], writes=[bdst])

    def transpose_mod(src, bsrc, dst_fn, bdst, scale_fn, shift_fn, bmods):
        for kc in range(8):
            S.op("pe", lambda h, kc=kc: h.transpose(out=psT[:, kc * 128:(kc + 1) * 128], in_=src[:, kc * 128:(kc + 1) * 128], identity=ident[:]),
                 reads=[bsrc, b_ident], writes=[b_psT])
        for kc in range(8):
            S.op("act", lambda h, kc=kc: h.activation(out=dst_fn(kc), in_=psT[:, kc * 128:(kc + 1) * 128], func=AF.Identity,
                                                     bias=shift_fn(kc), scale=scale_fn(kc)),
                 reads=[b_psT] + bmods, writes=[bdst])

    def load_tile(xsrc, possrc, r0):
        i = xctr[0] % 2
        xctr[0] += 1
        xt, bx = xt_t[i], b_xt[i]
        S.dma("sp", lambda h: h.dma_start(out=xt[:], in_=xsrc[r0:r0 + 128, :]), bx, writes=[bx])
        if possrc is not None:
            S.dma("pool", lambda h: h.dma_start(out=xt[:], in_=possrc[r0:r0 + 128, :], accum_op=ALU.add), bx, writes=[bx])
        return xt, bx

    blockB = sb("blockB", [128, 10240])
    sm = sb("s5sm", [128, 3, NG]); b_sm = Buf("s5sm")
    sbv = blockB[:, 0:1024].rearrange("p (r g c) -> p r g c", r=2, g=NG); scv = blockB[:, 1024:2048].rearrange("p (r g c) -> p r g c", r=2, g=NG); b_bc = Buf("s5bc")
    drep = sb("drep", [128, NG]); b_drep = Buf("drep")
    S.dma("sp", lambda h: h.dma_start(out=sm[:], in_=s5_small), b_sm, writes=[b_sm])
    S.dma("sp", lambda h: h.dma_start(out=sbv[:], in_=s5_b), b_bc, writes=[b_bc])
    S.dma("sp", lambda h: h.dma_start(out=scv[:], in_=s5_c), b_bc, writes=[b_bc])
    S.dma("sp", lambda h: h.dma_start(out=drep[:], in_=s5_drep), b_drep, writes=[b_drep])

    tb_ = blockB[:, 2048:2048 + 11 * NG * 9].rearrange("p (t g k) -> p t g k", t=11, g=NG); b_tab = Buf("s5tab")
    T_ANG, T_Q, T_SIN, T_COS, T_MAG, T_MAGN, T_WRE, T_WIM, T_VRE, T_VIM, T_TMP = range(11)
    qi = sb("s5qi", [128, NG * 9], I32)
    vec = sb("s5vec", [128, 12, NG]); b_vec = Buf("s5vec")
    V_DT, V_TH, V_LR, V_DEN, V_XRE, V_FRE, V_FIM, V_T1, V_T2, V_RDEN = range(10)

    def vop(fn, r=(b_vec,), w=(b_vec,)):
        S.op("dve", fn, reads=list(r), writes=list(w))

    S.op("act", lambda h: h.activation(out=vec[:, V_DT, :], in_=sm[:, 0, :], func=AF.Exp), reads=[b_sm], writes=[b_vec])
    vop(lambda h: h.tensor_tensor(out=vec[:, V_TH, :], in0=vec[:, V_DT, :], in1=sm[:, 2, :], op=ALU.mult), r=(b_vec, b_sm))
    vop(lambda h: h.tensor_tensor(out=vec[:, V_LR, :], in0=vec[:, V_DT, :], in1=sm[:, 1, :], op=ALU.mult), r=(b_vec, b_sm))
    T = lambda t: tb_[:, t, :, :]
    Tf = lambda t: tb_[:, t, :, :].rearrange("p g k -> p (g k)")
    for k in range(9):
        S.op("dve", lambda h, k=k: h.tensor_scalar(out=tb_[:, T_ANG, :, k], in0=vec[:, V_TH, :], scalar1=float(k), scalar2=None, op0=ALU.mult),
             reads=[b_vec], writes=[b_tab])
        S.op("act", lambda h, k=k: h.activation(out=tb_[:, T_MAG, :, k], in_=vec[:, V_LR, :], func=AF.Exp, scale=float(k)), reads=[b_vec], writes=[b_tab])
        S.op("act", lambda h, k=k: h.activation(out=tb_[:, T_MAGN, :, k], in_=vec[:, V_LR, :], func=AF.Exp, scale=-float(k)), reads=[b_vec], writes=[b_tab])

    def sin_of(dst_t, shift):
        top = lambda fn: S.op("dve", fn, reads=[b_tab], writes=[b_tab])
        top(lambda h: h.tensor_scalar(out=Tf(T_TMP), in0=Tf(T_ANG), scalar1=shift, scalar2=None, op0=ALU.add))
        top(lambda h: h.tensor_scalar(out=qi[:], in0=Tf(T_TMP), scalar1=1.0 / (2 * math.pi), scalar2=None, op0=ALU.mult))
        top(lambda h: h.tensor_copy(out=Tf(T_Q), in_=qi[:]))
        top(lambda h: h.scalar_tensor_tensor(out=Tf(T_TMP), in0=Tf(T_Q), scalar=-2 * math.pi, in1=Tf(T_TMP), op0=ALU.mult, op1=ALU.add))
        top(lambda h: h.tensor_scalar(out=Tf(T_Q), in0=Tf(T_TMP), scalar1=math.pi, scalar2=2 * math.pi, op0=ALU.is_gt, op1=ALU.mult))
        top(lambda h: h.tensor_tensor(out=Tf(T_TMP), in0=Tf(T_TMP), in1=Tf(T_Q), op=ALU.subtract))
        top(lambda h: h.tensor_scalar(out=Tf(T_Q), in0=Tf(T_TMP), scalar1=-math.pi, scalar2=2 * math.pi, op0=ALU.is_lt, op1=ALU.mult))
        top(lambda h: h.tensor_tensor(out=Tf(T_TMP), in0=Tf(T_TMP), in1=Tf(T_Q), op=ALU.add))
        S.op("act", lambda h: h.activation(out=Tf(dst_t), in_=Tf(T_TMP), func=AF.Sin), reads=[b_tab], writes=[b_tab])

    sin_of(T_SIN, 0.0)
    sin_of(T_COS, math.pi / 2)
    tt = lambda o, a, b, op: S.op("dve", lambda h: h.tensor_tensor(out=Tf(o), in0=Tf(a), in1=Tf(b), op=op), reads=[b_tab], writes=[b_tab])
    tt(T_WRE, T_MAG, T_COS, ALU.mult)
    tt(T_WIM, T_MAG, T_SIN, ALU.mult)
    tt(T_VRE, T_MAGN, T_COS, ALU.mult)
    tt(T_VIM, T_MAGN, T_SIN, ALU.mult)
    S.op("dve", lambda h: h.tensor_scalar(out=Tf(T_VIM), in0=Tf(T_VIM), scalar1=-1.0, scalar2=None, op0=ALU.mult), reads=[b_tab], writes=[b_tab])
    are = sm[:, 1, :]; aim = sm[:, 2, :]
    abre = tb_[:, T_WRE, :, 1]; abim = tb_[:, T_WIM, :, 1]
    vr = (b_vec, b_sm, b_tab)
    vop(lambda h: h.tensor_tensor(out=vec[:, V_DEN, :], in0=are, in1=are, op=ALU.mult), r=vr)
    vop(lambda h: h.tensor_tensor(out=vec[:, V_T1, :], in0=aim, in1=aim, op=ALU.mult), r=vr)
    vop(lambda h: h.tensor_tensor(out=vec[:, V_DEN, :], in0=vec[:, V_DEN, :], in1=vec[:, V_T1, :], op=ALU.add), r=vr)
    vop(lambda h: h.reciprocal(out=vec[:, V_RDEN, :], in_=vec[:, V_DEN, :]), r=vr)
    vop(lambda h: h.tensor_scalar(out=vec[:, V_XRE, :], in0=abre, scalar1=-1.0, scalar2=None, op0=ALU.add), r=vr)
    vop(lambda h: h.tensor_tensor(out=vec[:, V_T1, :], in0=vec[:, V_XRE, :], in1=are, op=ALU.mult), r=vr)
    vop(lambda h: h.tensor_tensor(out=vec[:, V_T2, :], in0=abim, in1=aim, op=ALU.mult), r=vr)
    vop(lambda h: h.tensor_tensor(out=vec[:, V_T1, :], in0=vec[:, V_T1, :], in1=vec[:, V_T2, :], op=ALU.add), r=vr)
    vop(lambda h: h.tensor_tensor(out=vec[:, V_FRE, :], in0=vec[:, V_T1, :], in1=vec[:, V_RDEN, :], op=ALU.mult), r=vr)
    vop(lambda h: h.tensor_tensor(out=vec[:, V_T1, :], in0=abim, in1=are, op=ALU.mult), r=vr)
    vop(lambda h: h.tensor_tensor(out=vec[:, V_T2, :], in0=vec[:, V_XRE, :], in1=aim, op=ALU.mult), r=vr)
    vop(lambda h: h.tensor_tensor(out=vec[:, V_T1, :], in0=vec[:, V_T1, :], in1=vec[:, V_T2, :], op=ALU.subtract), r=vr)
    vop(lambda h: h.tensor_tensor(out=vec[:, V_FIM, :], in0=vec[:, V_T1, :], in1=vec[:, V_RDEN, :], op=ALU.mult), r=vr)
    bbar = blockB[:, 5632:6656].rearrange("p (r g c) -> p r g c", r=2, g=NG); b_bbar = Buf("bbar")
    tmpb = blockB[:, 6656:7168].rearrange("p (g c) -> p g c", g=NG); b_tmpb = Buf("tmpb")
    fre_b = mkap(vec[:, V_FRE, :], 0, [[1, NG], [0, 16]]); fim_b = mkap(vec[:, V_FIM, :], 0, [[1, NG], [0, 16]])
    bo = lambda fn, r, w: S.op("dve", fn, reads=r, writes=w)
    bo(lambda h: h.tensor_tensor(out=bbar[:, 0], in0=sbv[:, 0], in1=fre_b, op=ALU.mult), [b_bc, b_vec], [b_bbar])
    bo(lambda h: h.tensor_tensor(out=tmpb[:], in0=sbv[:, 1], in1=fim_b, op=ALU.mult), [b_bc, b_vec], [b_tmpb])
    bo(lambda h: h.tensor_tensor(out=bbar[:, 0], in0=bbar[:, 0], in1=tmpb[:], op=ALU.subtract), [b_bbar, b_tmpb], [b_bbar])
    bo(lambda h: h.tensor_tensor(out=bbar[:, 1], in0=sbv[:, 1], in1=fre_b, op=ALU.mult), [b_bc, b_vec], [b_bbar])
    bo(lambda h: h.tensor_tensor(out=tmpb[:], in0=sbv[:, 0], in1=fim_b, op=ALU.mult), [b_bc, b_vec], [b_tmpb])
    bo(lambda h: h.tensor_tensor(out=bbar[:, 1], in0=bbar[:, 1], in1=tmpb[:], op=ALU.add), [b_bbar, b_tmpb], [b_bbar])

    WB, WBp, WC = [blockB[:, 7168 + i * 512:7168 + (i + 1) * 512].rearrange("p (r g k) -> p r g k", r=2, g=NG) for i in range(3)]; b_W = Buf("W")

    def fwd_slice(t, lanes, k0):
        return tb_[lanes, t, :, k0:k0 + 8]

    def rev_slice(t, lanes, k_hi):
        base = tb_[lanes, t, :, k_hi:k_hi + 1]
        return mkap(base, 0, [[9, NG], [-1, 8]])
    LF = slice(0, 64); LB = slice(64, 128)
    for ri, (tw, tv) in enumerate(((T_WRE, T_VRE), (T_WIM, T_VIM))):
        cp = lambda o, i_: S.op("dve", lambda h: h.tensor_copy(out=o, in_=i_), reads=[b_tab], writes=[b_W])
        cp(WB[LF, ri], rev_slice(tw, LF, 7))
        cp(WB[LB, ri], fwd_slice(tw, LB, 0))
        cp(WBp[LF, ri], fwd_slice(tv, LF, 1))
        cp(WBp[LB, ri], rev_slice(tv, LB, 8))
        cp(WC[LF, ri], fwd_slice(tw, LF, 1))
        cp(WC[LB, ri], rev_slice(tw, LB, 8))

    D0m = sb("D0m", [128, NG, 128], BF16); b_D0 = Buf("D0")
    PTre = sb("PTre", [128, NG, 128], BF16); PTim = sb("PTim", [128, NG, 128], BF16); b_PT = Buf("PT")
    Qre = sb("Qre", [128, NG, 128], BF16); Qimn = sb("Qimn", [128, NG, 128], BF16); b_Q = Buf("Q")
    A1 = sb("A1", [128, NG, 2]); A2 = sb("A2", [128, NG, 2]); b_A = Buf("A12")
    S.op("dve", lambda h: h.tensor_copy(out=A1[:], in_=mkap(tb_[:, T_WRE, :, 8:9], 0, [[9, NG], [0, 2]])), reads=[b_tab], writes=[b_A])
    S.op("dve", lambda h: h.tensor_copy(out=A2[:, :, 1], in_=tb_[:, T_WIM, :, 8]), reads=[b_tab], writes=[b_A])
    S.op("dve", lambda h: h.tensor_scalar(out=A2[:, :, 0], in0=tb_[:, T_WIM, :, 8], scalar1=-1.0, scalar2=None, op0=ALU.mult), reads=[b_tab], writes=[b_A])

    GC = 8
    gen = arena[:, 0:8192]

    def gslot(i):
        return gen[:, i * 1024:(i + 1) * 1024].rearrange("p (g i c) -> p g i c", g=GC, i=8)

    def cprod(Wt, X, g0, o_re, o_im, breads):
        def wv(ri):
            return mkap(Wt[:, ri, g0:g0 + GC, :], 0, [[8, GC], [1, 8], [0, 16]])

        def xv(ri):
            return mkap(X[:, ri, g0:g0 + GC, :], 0, [[16, GC], [0, 8], [1, 16]])
        t1 = gslot(6); t2 = gslot(7)
        o = lambda fn: S.op("dve", fn, reads=[b_W, b_arena] + breads, writes=[b_arena])
        o(lambda h: h.tensor_tensor(out=t1, in0=wv(0), in1=xv(0), op=ALU.mult))
        o(lambda h: h.tensor_tensor(out=t2, in0=wv(1), in1=xv(1), op=ALU.mult))
        o(lambda h: h.tensor_tensor(out=o_re, in0=t1, in1=t2, op=ALU.subtract))
        o(lambda h: h.tensor_tensor(out=t1, in0=wv(0), in1=xv(1), op=ALU.mult))
        o(lambda h: h.tensor_tensor(out=t2, in0=wv(1), in1=xv(0), op=ALU.mult))
        o(lambda h: h.tensor_tensor(out=o_im, in0=t1, in1=t2, op=ALU.add))

    for gch in range(NG // GC):
        g0 = gch * GC
        Bt_re, Bt_im, Bp_re, Bp_im, Ct_re, Ct_im = [gslot(i) for i in range(6)]
        cprod(WB, bbar, g0, Bt_re, Bt_im, [b_bbar])
        cprod(WBp, bbar, g0, Bp_re, Bp_im, [b_bbar])
        cprod(WC, scv, g0, Ct_re, Ct_im, [b_bc])
        fl = lambda a: a.rearrange("p g i c -> p g (i c)")
        S.op("act", lambda h, g0=g0, Ct_re=Ct_re: h.activation(out=Qre[:, g0:g0 + GC, :], in_=Ct_re.rearrange("p g i c -> p g (i c)"), func=AF.Identity), reads=[b_arena], writes=[b_Q])
        S.op("act", lambda h, g0=g0, Ct_im=Ct_im: h.activation(out=Qimn[:, g0:g0 + GC, :], in_=Ct_im.rearrange("p g i c -> p g (i c)"), func=AF.Identity, scale=-1.0), reads=[b_arena], writes=[b_Q])
        S.op("dve", lambda h, Ct_im=Ct_im: h.tensor_scalar(out=Ct_im.rearrange("p g i c -> p g (i c)"), in0=Ct_im.rearrange("p g i c -> p g (i c)"), scalar1=-1.0, scalar2=None, op0=ALU.mult), reads=[b_arena, b_Q], writes=[b_arena])
        for ri, (Bt, PT) in enumerate(((Bt_re, PTre), (Bt_im, PTim))):
            for gg in range(GC):
                S.op("pe", lambda h, gg=gg, Bt=Bt: h.transpose(out=psT[:, gg * 128:(gg + 1) * 128], in_=Bt.rearrange("p g i c -> p g (i c)")[:, gg, :], identity=ident[:]),
                     reads=[b_arena, b_ident], writes=[b_psT])
            S.op("act", lambda h, PT=PT, g0=g0: h.activation(out=PT[:, g0:g0 + GC, :], in_=psT[:, :].rearrange("p (g m) -> p g m", g=GC), func=AF.Identity),
                 reads=[b_psT], writes=[b_PT])
        for g4 in range(GC // 4):
            pF, bpF = ps_next(); pB, bpB = ps_next()
            for gg in range(4):
                g = g4 * 4 + gg
                for lanes, pp, bpp in ((LF, pF, bpF), (LB, pB, bpB)):
                    S.op("pe", lambda h, g=g, gg=gg, lanes=lanes, pp=pp, Bp_re=Bp_re, Ct_re=Ct_re: h.matmul(pp[:, gg * 128:(gg + 1) * 128], lhsT=Bp_re.rearrange("p g i c -> p g (i c)")[lanes, g, :],
                                                                                  rhs=Ct_re.rearrange("p g i c -> p g (i c)")[lanes, g, :], start=True, stop=False),
                         reads=[b_arena], writes=[bpp])
                    S.op("pe", lambda h, g=g, gg=gg, lanes=lanes, pp=pp, Bp_im=Bp_im, Ct_im=Ct_im: h.matmul(pp[:, gg * 128:(gg + 1) * 128], lhsT=Bp_im.rearrange("p g i c -> p g (i c)")[lanes, g, :],
                                                                                  rhs=Ct_im.rearrange("p g i c -> p g (i c)")[lanes, g, :], start=False, stop=True),
                         reads=[b_arena], writes=[bpp])
            t1 = gslot(6).rearrange("p g i c -> p (g i c)")[:, 0:512]
            t2 = gslot(7).rearrange("p g i c -> p (g i c)")[:, 0:512]
            S.op("dve", lambda h, pF=pF, t1=t1: h.tensor_tensor(out=t1, in0=pF[:, :], in1=masks[:, 0, :], op=ALU.mult), reads=[bpF, b_masks, b_arena], writes=[b_arena])
            S.op("dve", lambda h, pB=pB, t2=t2: h.tensor_tensor(out=t2, in0=pB[:, :], in1=masks[:, 1, :], op=ALU.mult), reads=[bpB, b_masks, b_arena], writes=[b_arena])
            S.op("dve", lambda h, t1=t1, t2=t2: h.tensor_tensor(out=t1, in0=t1, in1=t2, op=ALU.add), reads=[b_arena], writes=[b_arena])
            for gg in range(4):
                g = g0 + g4 * 4 + gg
                S.op("dve", lambda h, g=g, gg=gg, t1=t1: h.scalar_tensor_tensor(out=D0m[:, g, :], in0=ident[:], scalar=drep[:, g:g + 1],
                                                                               in1=t1[:, gg * 128:(gg + 1) * 128], op0=ALU.mult, op1=ALU.add),
                     reads=[b_arena, b_ident, b_drep], writes=[b_D0])

    winu = sb("winu", [128, 8, 512], BF16); b_winu = Buf("winu")
    S.dma("pool", lambda h: h.dma_start(out=winu[:], in_=w_in[:, 0:512].rearrange("(k p) n -> p k n", p=128)), b_winu, writes=[b_winu])
    hTb = blockA[:, 0:4096].rearrange("p (k t) -> p k t", k=8)
    U_sb = blockA[:, 4096:8192]; b_Usb = Buf("U_sb")
    UT = blockB[:, 0:8192].bitcast(BF16).rearrange("p (g j) -> p g j", g=NG); b_UT = Buf("UT")
    UTo = blockA[:, 8192:10240].rearrange("p (g j) -> p g j", g=NG); b_UTo = Buf("UTo")
    Zoth = blockA[:, 10240:14336].rearrange("p (j e) -> p j e", e=64); b_Zoth = Buf("Zoth")
    Zctx = blockA[:, 14336:16384].rearrange("p (j e) -> p j e", e=64); b_Zctx = Buf("Zctx")
    Zown = arena[:, :].bitcast(BF16).rearrange("p (j e) -> p j e", e=64)
    b_Zown = b_arena
    cur = [sb(f"cur{i}", [128, NG, 2]) for i in range(2)]; b_cur = [Buf(f"cur{i}") for i in range(2)]
    st1 = sb("st1", [128, NG, 2]); st2 = sb("st2", [128, NG, 2]); b_st = Buf("st12")
    S.op("dve", lambda h: h.memset(cur[0][:], 0.0), writes=[b_cur[0]])
    scan_k = [0]

    def scan_step(zap, bz, lanes, store):
        k = scan_k[0]
        scan_k[0] += 1
        c0, bc0 = cur[k % 2], b_cur[k % 2]
        c1, bc1 = cur[(k + 1) % 2], b_cur[(k + 1) % 2]
        L = lanes
        sw = mkap(c0[L, :, 1:2], 0, [[2, NG], [-1, 2]])
        S.op("dve", lambda h: h.tensor_tensor(out=st1[L], in0=c0[L], in1=A1[L], op=ALU.mult), reads=[bc0, b_A], writes=[b_st])
        S.op("dve", lambda h: h.tensor_tensor(out=st2[L], in0=sw, in1=A2[L], op=ALU.mult), reads=[bc0, b_A, b_st], writes=[b_st])
        S.op("dve", lambda h: h.tensor_tensor(out=st1[L], in0=st1[L], in1=st2[L], op=ALU.add), reads=[b_st], writes=[b_st])
        S.op("dve", lambda h: h.tensor_tensor(out=c1[L], in0=st1[L], in1=zap, op=ALU.add), reads=[b_st, bz], writes=[bc1])
        if store:
            S.op("dve", lambda h: h.tensor_copy(out=zap, in_=c0[L]), reads=[bc0, bz], writes=[bz])
        if L != slice(0, 128):
            other = slice(64, 128) if L == slice(0, 64) else slice(0, 64)
            S.op("dve", lambda h: h.tensor_copy(out=c1[other], in_=c0[other]), reads=[bc0], writes=[bc1])

    def phaseA_block(xsrc, possrc, t0, ntok, sh_fn, sc_fn, UTdst, bUT, J0):
        ntile = ntok // 128
        nJ = ntok // 8
        for tl in range(ntile):
            xt, bx = load_tile(xsrc, possrc, t0 + tl * 128)
            ln_stats(xt, bx, xn_t, b_xn)
            transpose_mod(xn_t, b_xn, lambda kc, tl=tl: hTb[:, kc, tl * 128:(tl + 1) * 128], b_hTb, sc_fn, sh_fn, [b_modfm])
        for i in range(8):
            ps, bps = ps_next()
            for kc in range(8):
                S.op("pe", lambda h, ps=ps, kc=kc, i=i: h.matmul(ps[0:nJ, :], lhsT=mkap(hTb[:, kc, i:i + 1], 0, [[8, nJ]]), rhs=winu[:, kc, :],
                                                                start=(kc == 0), stop=(kc == 7)),
                     reads=[b_hTb, b_winu], writes=[bps])
            S.op("act", lambda h, ps=ps, i=i: h.activation(out=mkap(U_sb[0:nJ, i * 16:i * 16 + 1], 0, [[128, NG], [1, 16]]), in_=ps[0:nJ, :].rearrange("p (g c) -> p g c", g=NG), func=AF.Identity), reads=[bps], writes=[b_Usb])
        for g8 in range(4):
            for gg in range(8):
                g = g8 * 8 + gg
                S.op("pe", lambda h, g=g, gg=gg: h.transpose(out=psB[:, gg * 128:gg * 128 + nJ], in_=U_sb[0:nJ, g * 128:(g + 1) * 128],
                                                             identity=identb[0:nJ, 0:nJ]),
                     reads=[b_Usb, b_identb], writes=[b_psB])
            S.op("dve", lambda h, g8=g8: h.tensor_copy(out=UTdst[:, g8 * 8:(g8 + 1) * 8, J0:J0 + nJ],
                                                       in_=psB[:, :].rearrange("p (g j) -> p g j", g=8)[:, :, 0:nJ]),
                 reads=[b_psB], writes=[bUT])

    def z_block(UTsrc, bUT, J0, nJ, Zdst_fn, bZ, lanes_list):
        for g in range(NG):
            ps, bps = ps_next()
            S.op("pe", lambda h, ps=ps, g=g: h.matmul(ps[:, 0:nJ], lhsT=PTre[:, g, :], rhs=UTsrc[:, g, J0:J0 + nJ], start=True, stop=True),
                 reads=[b_PT, bUT], writes=[bps])
            S.op("pe", lambda h, ps=ps, g=g: h.matmul(ps[:, 256:256 + nJ], lhsT=PTim[:, g, :], rhs=UTsrc[:, g, J0:J0 + nJ], start=True, stop=True),
                 reads=[b_PT, bUT, bps], writes=[bps])
            for lanes in lanes_list:
                S.op("act", lambda h, ps=ps, g=g, lanes=lanes: h.activation(
                    out=Zdst_fn(lanes, g), in_=ps[lanes, :].rearrange("p (r j) -> p r j", r=2)[:, :, 0:nJ], func=AF.Identity),
                    reads=[bps], writes=[bZ])

    def zdst(Zt, nJtot, Jbase, nJ):
        def fn(lanes, g):
            if lanes == LF:
                base = Zt[LF, Jbase:Jbase + 1, 2 * g:2 * g + 1]
                return mkap(base, 0, [[1, 2], [64, nJ]])
            base = Zt[LB, nJtot - 1 - Jbase:nJtot - Jbase, 2 * g:2 * g + 1]
            return mkap(base, 0, [[1, 2], [-64, nJ]])
        return fn

    ALL = slice(0, 128)
    sh1_fn = lambda kc: modfm[:, 0 + kc, 0:1]; sc1_fn = lambda kc: sc1p[:, kc, 0:1]
    csh1_fn = lambda kc: modfm[:, 0 + kc, 1:2]; csc1_fn = lambda kc: sc1p[:, kc, 1:2]
    phaseA_block(ctx_in, None, 0, 256, csh1_fn, csc1_fn, UTo, b_UTo, 0)
    z_block(UTo, b_UTo, 0, 32, zdst(Zctx, 32, 0, 32), b_Zctx, [LF, LB])
    for k in range(32):
        scan_step(Zctx[:, k, :].rearrange("p (g r) -> p g r", r=2), b_Zctx, ALL, False)
    for blk in range(8):
        phaseA_block(x_oth, pos_oth, blk * 512, 512, sh1_fn, sc1_fn, UTo, b_UTo, 0)
        z_block(UTo, b_UTo, 0, 64, zdst(Zoth, 64, 0, 64), b_Zoth, [LF])
        for k in range(64):
            scan_step(Zoth[LF, k, :].rearrange("p (g r) -> p g r", r=2), b_Zoth, LF, False)
    for blk in range(8):
        phaseA_block(x_own, pos_own, blk * 512, 512, sh1_fn, sc1_fn, UT, b_UT, blk * 64)
    for jb in range(4):
        z_block(UT, b_UT, jb * 128, 128, zdst(Zown, 512, jb * 128, 128), b_Zown, [LF, LB])
    for k in range(512):
        scan_step(Zown[:, k, :].rearrange("p (g r) -> p g r", r=2), b_Zown, ALL, True)


    Ysb = blockA[:, :].rearrange("p (g j) -> p g j", g=NG); b_Ysb = Buf("Ysb")
    TM = arena[:, 8192:10240].bitcast(BF16).rearrange("p (j c) -> p j c", j=8); b_TM = b_arena
    ya = arena[:, 0:8192].bitcast(BF16).rearrange("p (a t) -> p a t", a=4); b_ya = b_arena
    for g in range(NG):
        ps, bps = ps_next()
        S.op("pe", lambda h, ps=ps, g=g: h.matmul(ps[:, :], lhsT=D0m[:, g, :], rhs=UT[:, g, :], start=True, stop=False), reads=[b_D0, b_UT], writes=[bps])
        for lanes in (LF, LB):
            for ri, Qm in enumerate((Qre, Qimn)):
                if lanes == LF:
                    rhs = mkap(Zown[LF, 0:1, 2 * g + ri:2 * g + ri + 1], 0, [[64, 512]])
                else:
                    rhs = mkap(Zown[LB, 511:512, 2 * g + ri:2 * g + ri + 1], 0, [[-64, 512]])
                last = (lanes == LB and ri == 1)
                S.op("pe", lambda h, ps=ps, g=g, lanes=lanes, Qm=Qm, rhs=rhs, last=last: h.matmul(ps[:, :], lhsT=Qm[lanes, g, :], rhs=rhs, start=False, stop=last),
                     reads=[b_Q, b_Zown, bps], writes=[bps])
        S.op("act", lambda h, ps=ps, g=g: h.activation(out=Ysb[:, g, :], in_=ps[:, :], func=AF.Identity), reads=[bps], writes=[b_Ysb, b_hTb, b_Usb, b_UTo, b_Zoth, b_Zctx])
    gt = [arena[:, 10240 + i * 1024:10240 + (i + 1) * 1024] for i in range(2)]; b_gt = b_arena
    for jb in range(4):
        for g8 in range(4):
            for gg in range(8):
                g = g8 * 8 + gg
                S.op("pe", lambda h, g=g, gg=gg, jb=jb: h.transpose(out=psB[:, gg * 128:(gg + 1) * 128], in_=Ysb[:, g, jb * 128:(jb + 1) * 128], identity=identb[:]),
                     reads=[b_Ysb, b_identb], writes=[b_psB])
            S.op("dve", lambda h, g8=g8: h.tensor_copy(
                out=mkap(TM[:, 0:1, g8 * 128:g8 * 128 + 1], 0, [[16, 8], [512, 8], [1, 16]]),
                in_=psB[:, :].rearrange("p (g j c) -> p g j c", g=8, j=8)), reads=[b_psB], writes=[b_TM])
        for ct in range(4):
            for j in range(8):
                S.op("pe", lambda h, ct=ct, j=j: h.transpose(out=psB[:, j * 128:(j + 1) * 128], in_=TM[:, j, ct * 128:(ct + 1) * 128], identity=identb[:]),
                     reads=[b_TM, b_identb], writes=[b_psB])
            xin = psB[:, :]
            g0t, g1t = gt[0], gt[1]
            S.op("act", lambda h: h.activation(out=g0t, in_=xin, func=AF.Square), reads=[b_psB], writes=[b_gt])
            S.op("dve", lambda h: h.tensor_scalar(out=g0t, in0=g0t, scalar1=0.044715, scalar2=1.0, op0=ALU.mult, op1=ALU.add), reads=[b_gt], writes=[b_gt])
            S.op("dve", lambda h: h.tensor_tensor(out=g0t, in0=g0t, in1=xin, op=ALU.mult), reads=[b_gt, b_psB], writes=[b_gt])
            S.op("act", lambda h: h.activation(out=g1t, in_=g0t, func=AF.Sigmoid, scale=GELU_C), reads=[b_gt], writes=[b_gt])
            S.op("dve", lambda h, ct=ct, jb=jb: h.tensor_tensor(
                out=mkap(ya[:, ct, jb * 1024:jb * 1024 + 1], 0, [[1, 8], [8, 128]]),
                in0=g1t.rearrange("p (j J) -> p j J", j=8), in1=psB[:, :].rearrange("p (j J) -> p j J", j=8), op=ALU.mult),
                reads=[b_gt, b_psB], writes=[b_ya])

    if debug == "ya":
        b_dd = b_arena
        S.dma("sp", lambda h: h.dma_start(out=dbg_d, in_=ya.rearrange("p a b -> p (a b)")), b_dd, reads=[b_dd])
        S.final_wait("sp", [b_dd])
        S.emit()
        return nc

    def alias(new, olds):
        for o in olds:
            if o.writer is not None:
                new.readers.append(o.writer)
            new.readers.extend(o.readers)
        return new

    dead_A = [b_Ysb, b_hTb, b_Usb, b_UTo, b_Zoth, b_Zctx]
    dead_B = [b_UT, b_bc, b_tab, b_bbar, b_tmpb, b_W]
    b_blkA = alias(Buf("blkA"), dead_A)
    b_up0 = alias(Buf("up0"), [b_arena])
    gsec = blockA[:, :].bitcast(F32).rearrange("p (k n) -> p k n", k=8)
    crep = arena[:, 8192:9216].rearrange("p (k m) -> p k m", k=8)
    badrow = arena[:, 9216:10240]
    g1bc = blockB[:, 6144:7168]; g2bc = blockB[:, 7168:8192]; b_gbc = alias(Buf("gbc"), dead_B)
    lnp = blockB[:, 8192:10240]; b_lnp = alias(Buf("lnp"), dead_B)
    S.op("dve", lambda h: h.tensor_copy(out=crep, in_=mkap(csil[:], 0, [[1, 8], [0, 128]])), reads=[b_csil], writes=[b_up0])
    for sec, gdst in ((2, g1bc), (5, g2bc)):
        S.dma("sp", lambda h, sec=sec: h.dma_start(out=gsec, in_=w_ada[:, sec * D:(sec + 1) * D].rearrange("(k p) n -> p k n", p=128)),
              b_blkA, writes=[b_blkA])
        S.dma("sp", lambda h, sec=sec: h.dma_start(out=badrow, in_=b_ada[:, sec * D:(sec + 1) * D].partition_broadcast(128)),
              b_up0, writes=[b_up0])
        for nb in range(2):
            ps, bps = ps_next()
            for kc in range(8):
                S.op("pe", lambda h, ps=ps, kc=kc, nb=nb: h.matmul(ps[:, :], lhsT=crep[:, kc, :], rhs=gsec[:, kc, nb * 512:(nb + 1) * 512],
                                                                  start=(kc == 0), stop=(kc == 7)),
                     reads=[b_up0, b_blkA], writes=[bps])
            S.op("dve", lambda h, ps=ps, nb=nb, gdst=gdst: h.tensor_tensor(out=gdst[:, nb * 512:(nb + 1) * 512], in0=ps[:, :],
                                                                          in1=badrow[:, nb * 512:(nb + 1) * 512], op=ALU.add),
                 reads=[bps, b_up0], writes=[b_gbc])
    S.dma("sp", lambda h: h.dma_start(out=lnp, in_=lnp_in[:, 0:2048].partition_broadcast(128)), b_lnp, writes=[b_lnp])

    winA = blockA[:, :].rearrange("p (k n) -> p k n", k=8)
    winB = blockB[:, 0:6144].bitcast(BF16).rearrange("p (k n) -> p k n", k=8)
    b_winB = alias(Buf("winB"), dead_B)
    for c0, c1, dst, bd in ((512, 1536, winA[:, :, 0:1024], b_blkA), (1536, 2560, winA[:, :, 1024:2048], b_blkA),
                            (2560, 3584, winB[:, :, 0:1024], b_winB), (3584, 4096, winB[:, :, 1024:1536], b_winB)):
        S.dma("pool", lambda h, c0=c0, c1=c1, dst=dst: h.dma_start(out=dst, in_=w_in[:, c0:c1].rearrange("(k p) n -> p k n", p=128)),
              bd, writes=[bd])
    flat = lambda t: t[:].rearrange("p g m -> p (g m)")
    wval = flat(PTre).rearrange("p (k n) -> p k n", k=4); wgate = flat(PTim).rearrange("p (k n) -> p k n", k=4)
    convo = flat(Qre).rearrange("p (k n) -> p k n", k=4)
    wo_lo = flat(Qimn).rearrange("p (k n) -> p k n", k=4); wo_hi = flat(D0m).rearrange("p (k n) -> p k n", k=4)
    b_wglu = alias(Buf("wglu"), [b_PT]); b_convo = alias(Buf("convo"), [b_Q]); b_wo = alias(Buf("wo"), [b_Q, b_D0])
    S.dma("pool", lambda h: h.dma_start(out=wval, in_=w_val.rearrange("(k p) n -> p k n", p=128)), b_wglu, writes=[b_wglu])
    S.dma("pool", lambda h: h.dma_start(out=wgate, in_=w_gate.rearrange("(k p) n -> p k n", p=128)), b_wglu, writes=[b_wglu])
    S.dma("pool", lambda h: h.dma_start(out=convo, in_=conv_out.rearrange("(k p) n -> p k n", p=128)), b_convo, writes=[b_convo])
    S.dma("pool", lambda h: h.dma_start(out=wo_lo, in_=w_o[0:512, :].rearrange("(k p) n -> p k n", p=128)), b_wo, writes=[b_wo])
    S.dma("pool", lambda h: h.dma_start(out=wo_hi, in_=w_o[512:1024, :].rearrange("(k p) n -> p k n", p=128)), b_wo, writes=[b_wo])
    rw32 = masks[:, 1, 0:288].rearrange("p (k n) -> p k n", k=8); b_rw = alias(Buf("rw32"), [b_masks])
    S.dma("sp", lambda h: h.dma_start(out=rw32, in_=rw_in.rearrange("(k p) n -> p k n", p=128)), b_rw, writes=[b_rw])
    rt = masks[:, 0, 0:160]; b_rt = alias(Buf("rt"), [b_masks])

    cst = masks[:, 1, 288:480]; b_cst = alias(Buf("cst"), [b_masks])
    S.dma("sp", lambda h: h.dma_start(out=cst, in_=cst_in), b_cst, writes=[b_cst])
    RT = masks[:, 0, 256:384].rearrange("p (t f) -> p t f", f=4); b_RT = alias(Buf("RT"), [b_masks])
    DEST = masks[:, 0, 384:448].bitcast(I32).rearrange("p (t f) -> p t f", f=2); b_DEST = alias(Buf("DEST"), [b_masks])
    hT = arena[:, 8192:10240].bitcast(BF16).rearrange("p (k t) -> p k t", k=8); b_hT = b_up0
    merged = arena[:, 10240:12288].bitcast(BF16).rearrange("p (k t) -> p k t", k=8); b_merged = alias(Buf("merged"), [b_arena])
    gv = arena[:, 12288:13312].bitcast(BF16).rearrange("p (k t) -> p k t", k=4); b_gv = alias(Buf("gv"), [b_arena])
    tmp = [arena[:, 13312 + i * 512:13312 + (i + 1) * 512] for i in range(5)]
    b_tmp = [alias(Buf(f"tmp{i}"), [b_arena]) for i in range(5)]
    h2b = arena[:, 15872:16384].bitcast(BF16).rearrange("p (k t) -> p k t", k=8); b_h2b = alias(Buf("h2b"), [b_arena])
    wr_f = winu[:].rearrange("p k n -> p (k n)").bitcast(F32)
    A_all = wr_f[:, 0:1024].rearrange("p (t e) -> p t e", e=32); b_Aall = alias(Buf("A_all"), [b_winu])
    h2f = wr_f[:, 1024:2048].rearrange("p (k t) -> p k t", k=8); b_h2f = alias(Buf("h2f"), [b_winu])
    b_x1s = [Buf(f"x1s{i}") for i in range(32)]
    b_h2s = [Buf(f"h2s{i}") for i in range(32)]
    BIG = 1.0e30

    def proj(ft, rhs_ap, brhs):
        piece, off, bp = (winA, (ft - 4) * 128, b_blkA) if ft < 20 else (winB, (ft - 20) * 128, b_winB)
        ps, bps = ps_next()
        for kc in range(8):
            S.op("pe", lambda h, ps=ps, kc=kc, piece=piece, off=off: h.matmul(ps[:, :], lhsT=piece[:, kc, off:off + 128], rhs=rhs_ap(kc),
                                                                             start=(kc == 0), stop=(kc == 7)),
                 reads=[bp, brhs], writes=[bps])
        return ps, bps

    def mm4(wt, bw, dt, rhs_fn, brhs):
        ps, bps = ps_next()
        for ct in range(4):
            S.op("pe", lambda h, ps=ps, ct=ct: h.matmul(ps[:, :], lhsT=wt[:, ct, dt * 128:(dt + 1) * 128], rhs=rhs_fn(ct), start=(ct == 0), stop=(ct == 3)),
                 reads=[bw, brhs], writes=[bps])
        return ps, bps

    def dv(fn, r, w):
        S.op("dve", fn, reads=r, writes=w)

    for tb in range(8):
        t0 = tb * 512
        for tl in range(4):
            xt, bx = load_tile(x_own, pos_own, t0 + tl * 128)
            ln_stats(xt, bx, xn_t, b_xn)
            transpose_mod(xn_t, b_xn, lambda kc, tl=tl: hT[:, kc, tl * 128:(tl + 1) * 128], b_hT, sc1_fn, sh1_fn, [b_modfm])
        hrhs = lambda kc: hT[:, kc, :]
        for ct in range(4):
            pz, bpz = proj(4 + ct, hrhs, b_hT)
            pgc, bpgc = proj(12 + ct, hrhs, b_hT)
            S.op("act", lambda h, pgc=pgc: h.activation(out=tmp[0], in_=pgc[:, :], func=AF.Identity), reads=[bpgc], writes=[b_tmp[0]])
            dv(lambda h, pz=pz: h.tensor_tensor(out=tmp[1], in0=pz[:, :], in1=tmp[0], op=ALU.mult), [bpz, b_tmp[0]], [b_tmp[1]])
            dv(lambda h, ct=ct: h.tensor_scalar(out=tmp[2], in0=tmp[1], scalar1=convw[:, ct, 1:2], scalar2=None, op0=ALU.mult), [b_tmp[1], b_convw], [b_tmp[2]])
            zv = tmp[1].rearrange("p (r c) -> p r c", c=64); vv = tmp[2].rearrange("p (r c) -> p r c", c=64)
            dv(lambda h, ct=ct, zv=zv, vv=vv: h.scalar_tensor_tensor(out=vv[:, :, 1:64], in0=zv[:, :, 0:63], scalar=convw[:, ct, 0:1], in1=vv[:, :, 1:64],
                                                                     op0=ALU.mult, op1=ALU.add), [b_tmp[1], b_tmp[2], b_convw], [b_tmp[2]])
            dv(lambda h, ct=ct, zv=zv, vv=vv: h.scalar_tensor_tensor(out=vv[:, :, 0:63], in0=zv[:, :, 1:64], scalar=convw[:, ct, 2:3], in1=vv[:, :, 0:63],
                                                                     op0=ALU.mult, op1=ALU.add), [b_tmp[1], b_tmp[2], b_convw], [b_tmp[2]])
            pgb, bpgb = proj(8 + ct, hrhs, b_hT)
            dv(lambda h, ct=ct, pgb=pgb: h.tensor_tensor(out=gv[:, ct, :], in0=pgb[:, :], in1=tmp[2], op=ALU.mult), [bpgb, b_tmp[2]], [b_gv])
        for dt in range(8):
            pob, bpob = mm4(convo, b_convo, dt, lambda ct: gv[:, ct, :], b_gv)
            pval, bpval = mm4(wval, b_wglu, dt, lambda ct, t0=t0: ya[:, ct, t0:t0 + 512], b_arena)
            pgt, bpgt = mm4(wgate, b_wglu, dt, lambda ct, t0=t0: ya[:, ct, t0:t0 + 512], b_arena)
            pma, bpma = proj(16 + dt, hrhs, b_hT)
            pmb, bpmb = proj(24 + dt, hrhs, b_hT)
            S.op("act", lambda h, pgt=pgt: h.activation(out=tmp[0], in_=pgt[:, :], func=AF.Sigmoid), reads=[bpgt], writes=[b_tmp[0]])
            S.op("act", lambda h, pma=pma: h.activation(out=tmp[1], in_=pma[:, :], func=AF.Sigmoid), reads=[bpma], writes=[b_tmp[1]])
            S.op("act", lambda h, pmb=pmb: h.activation(out=tmp[2], in_=pmb[:, :], func=AF.Sigmoid), reads=[bpmb], writes=[b_tmp[2]])
            dv(lambda h, pval=pval: h.tensor_tensor(out=tmp[3], in0=pval[:, :], in1=tmp[0], op=ALU.mult), [bpval, b_tmp[0]], [b_tmp[3]])
            dv(lambda h: h.tensor_tensor(out=tmp[3], in0=tmp[3], in1=tmp[1], op=ALU.mult), [b_tmp[3], b_tmp[1]], [b_tmp[3]])
            dv(lambda h, pob=pob: h.tensor_tensor(out=tmp[4], in0=pob[:, :], in1=tmp[2], op=ALU.mult), [bpob, b_tmp[2]], [b_tmp[4]])
            dv(lambda h, dt=dt: h.tensor_tensor(out=merged[:, dt, :], in0=tmp[3], in1=tmp[4], op=ALU.add), [b_tmp[3], b_tmp[4]], [b_merged])
        for tl in range(4):
            gti = tb * 4 + tl
            r0 = gti * 128
            xt, bx = load_tile(x_own, pos_own, r0)
            for nb in range(2):
                ps, bps = ps_next()
                for dt in range(8):
                    wsl = wo_lo[:, dt, nb * 512:(nb + 1) * 512] if dt < 4 else wo_hi[:, dt - 4, nb * 512:(nb + 1) * 512]
                    S.op("pe", lambda h, ps=ps, dt=dt, tl=tl, wsl=wsl: h.matmul(ps[:, :], lhsT=merged[:, dt, tl * 128:(tl + 1) * 128], rhs=wsl,
                                                                               start=(dt == 0), stop=(dt == 7)),
                         reads=[b_merged, b_wo], writes=[bps])
                dv(lambda h, ps=ps, nb=nb: h.tensor_tensor(out=xn_t[:, nb * 512:(nb + 1) * 512], in0=ps[:, :], in1=g1bc[:, nb * 512:(nb + 1) * 512], op=ALU.mult),
                   [bps, b_gbc], [b_xn])
            dv(lambda h, xt=xt: h.scalar_tensor_tensor(out=xt[:, :], in0=xt[:, :], scalar=ALPHA, in1=xn_t[:, :], op0=ALU.mult, op1=ALU.add), [bx, b_xn], [bx])
            ln_stats(xt, bx, xn_t, b_xn)
            dv(lambda h: h.tensor_tensor(out=xn_t[:, :], in0=xn_t[:, :], in1=lnp[:, 0:1024], op=ALU.mult), [b_xn, b_lnp], [b_xn])
            dv(lambda h, xt=xt: h.tensor_tensor(out=xt[:, :], in0=xn_t[:, :], in1=lnp[:, 1024:2048], op=ALU.add), [b_xn, b_lnp], [bx])
            S.dma("sp", lambda h, xt=xt, r0=r0: h.dma_start(out=x1s[r0:r0 + 128, :], in_=xt[:, :]), bx, reads=[bx], writes=[b_x1s[gti]])
            ln_stats(xt, bx, xn_t, b_xn)
            for kc in range(8):
                S.op("pe", lambda h, kc=kc: h.transpose(out=psT[:, kc * 128:(kc + 1) * 128], in_=xn_t[:, kc * 128:(kc + 1) * 128], identity=ident[:]),
                     reads=[b_xn, b_ident], writes=[b_psT])
            for kc in range(8):
                S.op("act", lambda h, kc=kc: h.activation(out=h2f[:, kc, :], in_=psT[:, kc * 128:(kc + 1) * 128], func=AF.Identity,
                                                         bias=modfm[:, 24 + kc, 0:1], scale=sc2p[:, kc:kc + 1]),
                     reads=[b_psT, b_modfm], writes=[b_h2f])
            ps, bps = ps_next()
            for kc in range(8):
                S.op("pe", lambda h, ps=ps, kc=kc: h.matmul(ps[:, 0:36], lhsT=h2f[:, kc, :], rhs=rw32[:, kc, :], start=(kc == 0), stop=(kc == 7)),
                     reads=[b_h2f, b_rw], writes=[bps])
            R = lambda a, b_: rt[:, a:b_]
            rr = [b_rt]
            dv(lambda h, ps=ps: h.tensor_tensor(out=R(0, 36), in0=ps[:, 0:36], in1=rb_bc[:, :], op=ALU.add), [bps, b_rb, b_rt], rr)
            dv(lambda h: h.tensor_reduce(out=R(36, 37), in_=R(0, 4), axis=AX.X, op=ALU.max), rr, rr)
            dv(lambda h: h.tensor_scalar(out=R(38, 42), in0=R(0, 4), scalar1=R(36, 37), scalar2=None, op0=ALU.is_equal), rr, rr)
            dv(lambda h: h.tensor_scalar(out=R(37, 38), in0=R(36, 37), scalar1=-1.0, scalar2=None, op0=ALU.mult), rr, rr)
            S.op("act", lambda h: h.activation(out=R(42, 46), in_=R(0, 4), func=AF.Exp, bias=R(37, 38), scale=1.0), reads=rr, writes=rr)
            dv(lambda h: h.tensor_reduce(out=R(46, 47), in_=R(42, 46), axis=AX.X, op=ALU.add), rr, rr)
            dv(lambda h: h.reciprocal(out=R(47, 48), in_=R(46, 47)), rr, rr)
            dv(lambda h: h.tensor_scalar(out=R(48, 52), in0=R(38, 42), scalar1=BIG, scalar2=-BIG, op0=ALU.mult, op1=ALU.add), rr, rr)
            dv(lambda h: h.tensor_tensor(out=R(52, 84).rearrange("p (g e) -> p g e", e=8), in0=R(4, 36).rearrange("p (g e) -> p g e", e=8),
                                         in1=mkap(R(48, 52), 0, [[1, 4], [0, 8]]), op=ALU.add), rr, rr)
            dv(lambda h: h.tensor_reduce(out=R(84, 85), in_=R(52, 84), axis=AX.X, op=ALU.max), rr, rr)
            dv(lambda h: h.tensor_scalar(out=R(85, 117), in0=R(52, 84), scalar1=R(84, 85), scalar2=None, op0=ALU.is_equal), rr, rr)
            dv(lambda h: h.scalar_tensor_tensor(out=R(117, 149), in0=R(85, 117), scalar=-BIG, in1=R(52, 84), op0=ALU.mult, op1=ALU.add), rr, rr)
            dv(lambda h: h.tensor_reduce(out=R(149, 150), in_=R(117, 149), axis=AX.X, op=ALU.max), rr, rr)
            dv(lambda h: h.tensor_scalar(out=R(52, 84), in0=R(117, 149), scalar1=R(149, 150), scalar2=None, op0=ALU.is_equal), rr, rr)
            dv(lambda h: h.tensor_tensor(out=R(150, 151), in0=R(149, 150), in1=R(84, 85), op=ALU.subtract), rr, rr)
            S.op("act", lambda h: h.activation(out=R(151, 152), in_=R(150, 151), func=AF.Exp), reads=rr, writes=rr)
            dv(lambda h: h.tensor_scalar(out=R(152, 153), in0=R(151, 152), scalar1=1.0, scalar2=None, op0=ALU.add), rr, rr)
            dv(lambda h: h.reciprocal(out=R(153, 154), in_=R(152, 153)), rr, rr)
            dv(lambda h: h.tensor_tensor(out=R(154, 155), in0=R(151, 152), in1=R(153, 154), op=ALU.mult), rr, rr)
            dv(lambda h: h.tensor_tensor(out=R(155, 156), in0=R(153, 154), in1=R(47, 48), op=ALU.mult), rr, rr)
            dv(lambda h: h.tensor_tensor(out=R(156, 157), in0=R(154, 155), in1=R(47, 48), op=ALU.mult), rr, rr)
            dv(lambda h, gti=gti: h.tensor_tensor(out=A_all[:, gti, :], in0=R(85, 117), in1=R(52, 84), op=ALU.add), rr + [b_Aall], [b_Aall])
            dv(lambda h: h.tensor_tensor(out=R(117, 149), in0=R(85, 117), in1=cst[:, 0:32], op=ALU.mult), rr + [b_cst], rr)
            dv(lambda h, gti=gti: h.tensor_reduce(out=RT[:, gti, 0:1], in_=R(117, 149), axis=AX.X, op=ALU.add), rr + [b_RT], [b_RT])
            dv(lambda h: h.tensor_tensor(out=R(117, 149), in0=R(52, 84), in1=cst[:, 0:32], op=ALU.mult), rr + [b_cst, b_RT], rr)
            dv(lambda h, gti=gti: h.tensor_reduce(out=RT[:, gti, 1:2], in_=R(117, 149), axis=AX.X, op=ALU.add), rr + [b_RT], [b_RT])
            dv(lambda h, gti=gti: h.tensor_copy(out=RT[:, gti, 2:4], in_=R(155, 157)), rr + [b_RT], [b_RT])

    IOA = bass.IndirectOffsetOnAxis
    NBLK = 48
    rA = blockA[:, :].bitcast(F32)
    b_R = alias(Buf("phaseR"), [b_blkA])
    Dt = rA[:, 4096:5120].rearrange("p (t e) -> p t e", e=32)
    OH = rA[:, 5120:6144].rearrange("p (t e) -> p t e", e=32)
    CMP = rA[:, 6144:7680]
    tri = rA[:, 7680:7936]
    sm_ = rA[:, 7936:8192]
    run = sm_[:, 0:32]; nblk = sm_[:, 32:64]; pe_ = sm_[:, 64:96]; psr = sm_[:, 96:128]; ones32 = sm_[:, 128:160]
    blkE = sm_[:, 160:208]; tE = sm_[:, 208:256]
    S.dma("sp", lambda h: h.dma_start(out=tri, in_=tri_in), b_R, writes=[b_R])
    rdv = lambda fn, extra=(): S.op("dve", fn, reads=[b_R] + list(extra), writes=[b_R])
    pT1, pT2 = ps_next(), ps_next()
    for hf_ in range(2):
        asl = A_all[:, hf_ * 16:(hf_ + 1) * 16, :].rearrange("p t e -> p (t e)")
        S.op("pe", lambda h, hf_=hf_, asl=asl: h.matmul(psT[:, hf_ * 512:(hf_ + 1) * 512], lhsT=tri[:, 0:128], rhs=asl, start=True, stop=True),
             reads=[b_R, b_Aall], writes=[b_psT])
        pt, bpt = (pT1, pT2)[hf_]
        S.op("pe", lambda h, pt=pt, asl=asl: h.matmul(pt[:, :], lhsT=tri[:, 128:256], rhs=asl, start=True, stop=True),
             reads=[b_R, b_Aall], writes=[bpt])
    rdv(lambda h: h.memset(run, 0.0))
    rdv(lambda h: h.memset(ones32, 1.0))
    for t in range(32):
        pt, bpt = (pT1, pT2)[t // 16]
        tt_ = t % 16
        rdv(lambda h, t=t: h.tensor_tensor(out=Dt[:, t, :], in0=psT[:, t * 32:(t + 1) * 32], in1=run, op=ALU.add), [b_psT])
        rdv(lambda h, pt=pt, tt_=tt_: h.tensor_tensor(out=run, in0=pt[:, tt_ * 32:(tt_ + 1) * 32], in1=run, op=ALU.add), [bpt])
    cmp8 = CMP[:, 0:256].rearrange("p (e j) -> p e j", j=8)
    rdv(lambda h: h.tensor_tensor(out=cmp8, in0=mkap(run, 0, [[1, 32], [0, 8]]), in1=mkap(cst[:, 72:80], 0, [[0, 32], [1, 8]]), op=ALU.is_gt), [b_cst])
    rdv(lambda h: h.tensor_reduce(out=nblk, in_=cmp8, axis=AX.X, op=ALU.add))
    rdv(lambda h: h.tensor_tensor_scan(out=pe_, data0=ones32, data1=nblk, initial=0.0, op0=ALU.mult, op1=ALU.add))
    rdv(lambda h: h.tensor_tensor(out=psr, in0=pe_, in1=nblk, op=ALU.subtract))
    rdv(lambda h: h.tensor_scalar(out=psr, in0=psr, scalar1=512.0, scalar2=None, op0=ALU.mult))
    rdv(lambda h: h.tensor_tensor(out=Dt, in0=Dt, in1=mkap(psr, 0, [[0, 32], [1, 32]]), op=ALU.add))
    destF = CMP[:, 256:320].rearrange("p (t f) -> p t f", f=2)
    for k in range(2):
        rdv(lambda h, k=k: h.tensor_tensor(out=OH, in0=mkap(cst[:, 0:32], 0, [[0, 32], [1, 32]]), in1=mkap(RT[:, 0, k:k + 1], 0, [[4, 32], [0, 32]]), op=ALU.is_equal),
            [b_cst, b_RT])
        rdv(lambda h: h.tensor_tensor(out=OH, in0=OH, in1=Dt, op=ALU.mult))
        rdv(lambda h, k=k: h.tensor_reduce(out=destF[:, :, k], in_=OH, axis=AX.X, op=ALU.add))
    S.op("dve", lambda h: h.tensor_copy(out=DEST, in_=destF), reads=[b_R], writes=[b_DEST])
    cmpb = CMP[:, 0:1536].rearrange("p (b e) -> p b e", e=32)
    rdv(lambda h: h.tensor_tensor(out=cmpb, in0=mkap(pe_, 0, [[0, NBLK], [1, 32]]), in1=mkap(cst[:, 0:NBLK], 0, [[1, NBLK], [0, 32]]), op=ALU.is_le), [b_cst, b_DEST])
    rdv(lambda h: h.tensor_reduce(out=blkE, in_=cmpb, axis=AX.X, op=ALU.add))
    rdv(lambda h: h.tensor_scalar(out=blkE, in0=blkE, scalar1=31.0, scalar2=None, op0=ALU.min))
    gif = CMP[:, 0:576]
    gif_g = gif[:, 0:384].rearrange("p (b k) -> p b k", k=8); gif_d = gif[:, 384:576].rearrange("p (b k) -> p b k", k=4)
    rdv(lambda h: h.tensor_scalar(out=tE, in0=blkE, scalar1=1024.0, scalar2=None, op0=ALU.mult))
    rdv(lambda h: h.tensor_tensor(out=gif_g, in0=mkap(tE, 0, [[1, NBLK], [0, 8]]), in1=mkap(cst[:, 64:72], 0, [[0, NBLK], [1, 8]]), op=ALU.add), [b_cst])
    rdv(lambda h: h.tensor_scalar(out=tE, in0=blkE, scalar1=512.0, scalar2=None, op0=ALU.mult))
    rdv(lambda h: h.tensor_tensor(out=gif_d, in0=mkap(tE, 0, [[1, NBLK], [0, 4]]), in1=mkap(cst[:, 64:68], 0, [[0, NBLK], [1, 4]]), op=ALU.add), [b_cst])
    GI = wr_f[:, 1024:1600].bitcast(I32); b_GI = alias(Buf("GI"), [b_h2f])
    GI_g = GI[:, 0:384].rearrange("p (b k) -> p b k", k=8); GI_d = GI[:, 384:576].rearrange("p (b k) -> p b k", k=4)
    S.op("dve", lambda h: h.tensor_copy(out=GI, in_=gif), reads=[b_R], writes=[b_GI])

    if debug == "d":
        dbgr = nc.dram_tensor("dbgr", [128, 32 * 4 + 64 + 576], F32, kind="ExternalOutput").ap()
        dd = rA[:, 4096:4096 + 768]
        S.op("dve", lambda h: h.tensor_copy(out=dd[:, 0:128], in_=RT.rearrange("p t f -> p (t f)")), reads=[b_R, b_RT], writes=[b_R])
        S.op("dve", lambda h: h.tensor_copy(out=dd[:, 128:192], in_=DEST.rearrange("p t f -> p (t f)")), reads=[b_R, b_DEST], writes=[b_R])
        S.op("dve", lambda h: h.tensor_copy(out=dd[:, 192:768], in_=GI), reads=[b_R, b_GI], writes=[b_R])
        S.dma("sp", lambda h: h.dma_start(out=dbgr, in_=dd), b_R, reads=[b_R])
        S.final_wait("sp", b_x1s + [b_R])
        S.emit()
        return nc

    xs = nc.dram_tensor("xs", [NBLK * 512, D], F32, kind="Internal").ap()
    ybd = nc.dram_tensor("ybd", [NBLK * 512, D], F32, kind="Internal").ap()
    b_xs = Buf("xs")
    for t in range(32):
        r0 = t * 128
        i = xctr[0] % 2
        xctr[0] += 1
        xt, bx = xt_t[i], b_xt[i]
        S.dma("sp", lambda h, xt=xt, r0=r0: h.dma_start(out=xt[:], in_=x1s[r0:r0 + 128, :]), bx, reads=[b_x1s[t]], writes=[bx])
        ln_stats(xt, bx, xn_t, b_xn)
        for k in range(2):
            S.dma("pool", lambda h, t=t, k=k: h.indirect_dma_start(out=xs[:, :], out_offset=IOA(ap=DEST[:, t, k:k + 1], axis=0), in_=xn_t[:, :], in_offset=None),
                  b_xs, reads=[b_xn, b_DEST], writes=[b_xs])

    lnp_e = lnp
    S.dma("sp", lambda h: h.dma_start(out=lnp_e, in_=lnp_in[:, 2048:4096].partition_broadcast(128)), b_lnp, writes=[b_lnp])
    xinb = [arena[:, i * 4096:(i + 1) * 4096].rearrange("p (t d) -> p t d", d=1024) for i in range(2)]
    b_xin = [alias(Buf(f"xin{i}"), [b_arena, b_up0, b_merged, b_gv, b_h2b] + b_tmp) for i in range(2)]
    ybuf = [arena[:, 8192 + i * 4096:8192 + (i + 1) * 4096].rearrange("p (t d) -> p t d", d=1024) for i in range(2)]
    b_ybuf = [alias(Buf(f"ybuf{i}"), [b_arena, b_up0, b_merged, b_gv, b_h2b] + b_tmp) for i in range(2)]
    b_ybd = [Buf("ybd0"), Buf("ybd1")]
    hblk = [blockA[:, i * 4096:(i + 1) * 4096].rearrange("p (k t) -> p k t", k=8) for i in range(2)]
    b_hblk = [alias(Buf(f"hblk{i}"), [b_blkA]) for i in range(2)]
    wbuf = [
        (flat(PTre).rearrange("p (k n) -> p k n", k=8), flat(PTim).rearrange("p (k n) -> p k n", k=8), flat(Qre).rearrange("p (k n) -> p k n", k=4)),
        (flat(Qimn).rearrange("p (k n) -> p k n", k=8), flat(D0m).rearrange("p (k n) -> p k n", k=8),
         blockB[:, 0:2048].bitcast(BF16).rearrange("p (k n) -> p k n", k=4)),
    ]
    b_wb = [alias(Buf("wb0"), [b_wglu, b_convo]), alias(Buf("wb1"), [b_wo, b_winB])]
    hid = [blockB[:, 2048 + i * 1024:2048 + (i + 1) * 1024].bitcast(BF16).rearrange("p (k t) -> p k t", k=4) for i in range(2)]
    b_hid = [alias(Buf(f"hid{i}"), [b_winB]) for i in range(2)]
    stmp = [blockB[:, 4096 + i * 512:4096 + (i + 1) * 512] for i in range(2)]
    b_stmp = [alias(Buf(f"stmp{i}"), [b_winB]) for i in range(2)]
    wg_flat = wg_in.rearrange("e k n -> (e k) n"); wu_flat = wu_in.rearrange("e k n -> (e k) n"); wd_flat = wd_in.rearrange("e k n -> (e k) n")
    for b in range(NBLK):
        par = b % 2
        xi, bxi = xinb[par], b_xin[par]
        S.dma("sp", lambda h, b=b, xi=xi: h.dma_start(out=xi, in_=xs[b * 512:(b + 1) * 512, :].rearrange("(t p) d -> p t d", p=128)), bxi,
              reads=[b_xs], writes=[bxi])
        wg_t, wu_t, wd_t = wbuf[par]
        bw = b_wb[par]
        for kc in range(8):
            S.dma("pool", lambda h, b=b, kc=kc, wg_t=wg_t: h.indirect_dma_start(out=wg_t[:, kc, :], out_offset=None, in_=wg_flat[:, :],
                                                                             in_offset=IOA(ap=GI_g[:, b, kc:kc + 1], axis=0)), bw, reads=[b_GI], writes=[bw])
            S.dma("pool", lambda h, b=b, kc=kc, wu_t=wu_t: h.indirect_dma_start(out=wu_t[:, kc, :], out_offset=None, in_=wu_flat[:, :],
                                                                             in_offset=IOA(ap=GI_g[:, b, kc:kc + 1], axis=0)), bw, reads=[b_GI], writes=[bw])
        for fc in range(4):
            S.dma("pool", lambda h, b=b, fc=fc, wd_t=wd_t: h.indirect_dma_start(out=wd_t[:, fc, :], out_offset=None, in_=wd_flat[:, :],
                                                                             in_offset=IOA(ap=GI_d[:, b, fc:fc + 1], axis=0)), bw, reads=[b_GI], writes=[bw])
        hbk, bhbk = hblk[par], b_hblk[par]
        for tl in range(4):
            for kc in range(8):
                S.op("pe", lambda h, xi=xi, tl=tl, kc=kc: h.transpose(out=psT[:, kc * 128:(kc + 1) * 128], in_=xi[:, tl, kc * 128:(kc + 1) * 128], identity=ident[:]),
                     reads=[bxi, b_ident], writes=[b_psT])
            for kc in range(8):
                S.op("act", lambda h, hbk=hbk, tl=tl, kc=kc: h.activation(out=hbk[:, kc, tl * 128:(tl + 1) * 128], in_=psT[:, kc * 128:(kc + 1) * 128], func=AF.Identity,
                                                                         bias=modfm[:, 24 + kc, 0:1], scale=sc2p[:, kc:kc + 1]),
                     reads=[b_psT, b_modfm], writes=[bhbk])
        hb, bhb = hid[par], b_hid[par]
        for fc in range(4):
            pg, bpg = ps_next()
            for kc in range(8):
                S.op("pe", lambda h, pg=pg, kc=kc, fc=fc, wg_t=wg_t, hbk=hbk: h.matmul(pg[:, :], lhsT=wg_t[:, kc, fc * 128:(fc + 1) * 128], rhs=hbk[:, kc, :],
                                                                                      start=(kc == 0), stop=(kc == 7)), reads=[bw, bhbk], writes=[bpg])
            pu, bpu = ps_next()
            for kc in range(8):
                S.op("pe", lambda h, pu=pu, kc=kc, fc=fc, wu_t=wu_t, hbk=hbk: h.matmul(pu[:, :], lhsT=wu_t[:, kc, fc * 128:(fc + 1) * 128], rhs=hbk[:, kc, :],
                                                                                      start=(kc == 0), stop=(kc == 7)), reads=[bw, bhbk], writes=[bpu])
            st_, bst_ = stmp[fc % 2], b_stmp[fc % 2]
            S.op("act", lambda h, pg=pg, st_=st_: h.activation(out=st_, in_=pg[:, :], func=AF.Silu), reads=[bpg], writes=[bst_])
            dv(lambda h, pu=pu, st_=st_, hb=hb, fc=fc: h.tensor_tensor(out=hb[:, fc, :], in0=pu[:, :], in1=st_, op=ALU.mult), [bpu, bst_], [bhb])
        yb_, byb_ = ybuf[par], b_ybuf[par]
        for tl in range(4):
            for nb in range(2):
                pd, bpd = ps_next()
                for fc in range(4):
                    S.op("pe", lambda h, pd=pd, fc=fc, tl=tl, nb=nb, hb=hb, wd_t=wd_t: h.matmul(pd[:, :], lhsT=hb[:, fc, tl * 128:(tl + 1) * 128],
                                                                                             rhs=wd_t[:, fc, nb * 512:(nb + 1) * 512], start=(fc == 0), stop=(fc == 3)),
                         reads=[bhb, bw], writes=[bpd])
                eng = "act" if (tl * 2 + nb) % 2 == 0 else "dve"
                if eng == "act":
                    S.op("act", lambda h, pd=pd, yb_=yb_, tl=tl, nb=nb: h.activation(out=yb_[:, tl, nb * 512:(nb + 1) * 512], in_=pd[:, :], func=AF.Identity),
                         reads=[bpd], writes=[byb_])
                else:
                    dv(lambda h, pd=pd, yb_=yb_, tl=tl, nb=nb: h.tensor_copy(out=yb_[:, tl, nb * 512:(nb + 1) * 512], in_=pd[:, :]), [bpd], [byb_])
        S.dma("sp", lambda h, b=b, yb_=yb_: h.dma_start(out=ybd[b * 512:(b + 1) * 512, :].rearrange("(t p) d -> p t d", p=128), in_=yb_), byb_,
              reads=[byb_], writes=[b_ybd[par]])

    y12 = [rA[:, 4096 + i * 1024:4096 + (i + 1) * 1024] for i in range(2)]
    b_y12 = [alias(Buf(f"y12_{i}"), [b_R]) for i in range(2)]
    b_out = [Buf(f"out{i}") for i in range(32)]
    for t in range(32):
        r0 = t * 128
        i = xctr[0] % 2
        xctr[0] += 1
        xt, bx = xt_t[i], b_xt[i]
        S.dma("sp", lambda h, xt=xt, r0=r0: h.dma_start(out=xt[:], in_=x1s[r0:r0 + 128, :]), bx, reads=[b_x1s[t]], writes=[bx])
        for k in range(2):
            S.dma("pool", lambda h, t=t, k=k: h.indirect_dma_start(out=y12[k], out_offset=None, in_=ybd[:, :], in_offset=IOA(ap=DEST[:, t, k:k + 1], axis=0)),
                  b_y12[k], reads=[b_ybd[0], b_ybd[1], b_DEST], writes=[b_y12[k]])
        dv(lambda h, t=t: h.tensor_scalar(out=y12[0], in0=y12[0], scalar1=RT[:, t, 2:3], scalar2=None, op0=ALU.mult), [b_y12[0], b_RT], [b_y12[0]])
        dv(lambda h, t=t: h.scalar_tensor_tensor(out=y12[0], in0=y12[1], scalar=RT[:, t, 3:4], in1=y12[0], op0=ALU.mult, op1=ALU.add), [b_y12[0], b_y12[1], b_RT], [b_y12[0]])
        dv(lambda h: h.tensor_tensor(out=y12[0], in0=y12[0], in1=g2bc, op=ALU.mult), [b_y12[0], b_gbc], [b_y12[0]])
        dv(lambda h, xt=xt: h.scalar_tensor_tensor(out=xt[:, :], in0=xt[:, :], scalar=ALPHA, in1=y12[0], op0=ALU.mult, op1=ALU.add), [bx, b_y12[0]], [bx])
        ln_stats(xt, bx, xn_t, b_xn)
        dv(lambda h: h.tensor_tensor(out=xn_t[:, :], in0=xn_t[:, :], in1=lnp_e[:, 0:1024], op=ALU.mult), [b_xn, b_lnp], [b_xn])
        dv(lambda h, xt=xt: h.tensor_tensor(out=xt[:, :], in0=xn_t[:, :], in1=lnp_e[:, 1024:2048], op=ALU.add), [b_xn, b_lnp], [bx])
        S.dma("sp", lambda h, xt=xt, r0=r0: h.dma_start(out=out_d[r0:r0 + 128, :], in_=xt[:, :]), bx, reads=[bx], writes=[b_out[t]])
    S.final_wait("sp", b_out)
    S.emit()
    return nc


def host_inputs(inputs):
    f32 = np.float32
    g = {k: np.asarray(v) for k, v in inputs.items()}
    D_ = 1024
    q = D_ // 4
    omega = (1.0 / (10000.0 ** (np.arange(q, dtype=f32) / f32(q)))).astype(f32)
    r = (np.arange(128, dtype=f32)[:, None] * omega).astype(f32)
    cl = (np.arange(64, dtype=f32)[:, None] * omega).astype(f32)
    r_emb = np.concatenate([np.sin(r), np.cos(r)], -1).astype(f32)
    c_emb = np.concatenate([np.sin(cl), np.cos(cl)], -1).astype(f32)
    pos = np.concatenate([np.broadcast_to(r_emb[:, None, :], (128, 64, 2 * q)),
                          np.broadcast_to(c_emb[None, :, :], (128, 64, 2 * q))], -1).reshape(8192, D_).astype(f32)
    ident = np.eye(128, dtype=f32)
    ii = np.arange(128) // 16
    mF = (ii[None, :] >= ii[:, None]).astype(f32)
    mB = (ii[:, None] >= ii[None, :]).astype(f32)
    masks = np.stack([np.tile(mF, (1, 4)), np.tile(mB, (1, 4))], 1).astype(f32)
    cst = np.zeros((128, 192), f32)
    cst[:, 0:64] = np.arange(64, dtype=f32)[None, :]
    cst[:, 64:72] = np.arange(8, dtype=f32)[None, :] * 128.0 + np.arange(128, dtype=f32)[:, None]
    cst[:, 72:80] = np.arange(8, dtype=f32)[None, :] * 512.0
    tri = np.zeros((128, 256), f32)
    tri[:, 0:128] = (np.arange(128)[:, None] < np.arange(128)[None, :]).astype(f32)
    tri[:, 128:256] = 1.0

    def tr(a):
        return np.ascontiguousarray(a.T)
    maps = []
    for core in range(8):
        b, hf = core // 2, core % 2
        xb = g["x"][b]
        if hf == 1:
            x_oth, x_own = xb[0:4096], xb[4096:8192]
            p_oth, p_own = pos[0:4096], pos[4096:8192]
            ctxl = g["ctx"][b]
            F, B = "f", "b"
            convw = g["conv_w"][0]
        else:
            x_oth, x_own = xb[4096:8192][::-1], xb[0:4096][::-1]
            p_oth, p_own = pos[4096:8192][::-1], pos[0:4096][::-1]
            ctxl = g["ctx"][b][::-1]
            F, B = "b", "f"
            convw = g["conv_w"][0][::-1]
        cT = np.concatenate([g["c"][b].reshape(8, 128).T, g["c_ctx"].reshape(8, 128).T], 1)
        small = np.zeros((128, 3, 32), f32)
        sbb = np.zeros((128, 2, 32, 16), f32)
        scc = np.zeros((128, 2, 32, 16), f32)
        for li, dname in ((0, F), (1, B)):
            L = slice(li * 64, li * 64 + 64)
            small[L, 0, :] = np.broadcast_to(g["s5_log_dt_" + dname][0][None, :], (64, 32))
            small[L, 1, :] = tr(g["s5_a_re_" + dname][0])
            small[L, 2, :] = tr(g["s5_a_im_" + dname][0])
            sbb[L, 0] = g["s5_b_re_" + dname][0].transpose(1, 0, 2)
            sbb[L, 1] = g["s5_b_im_" + dname][0].transpose(1, 0, 2)
            scc[L, 0] = g["s5_c_re_" + dname][0].transpose(2, 0, 1)
            scc[L, 1] = g["s5_c_im_" + dname][0].transpose(2, 0, 1)
        drep = np.tile(g["s5_d"][0].T, (8, 1))
        m = {
            "x_own": x_own, "x_oth": x_oth, "ctx": ctxl, "pos_own": p_own, "pos_oth": p_oth,
            "cT": cT, "w_ada": g["w_ada"][0], "b_adaT": g["b_ada"][0].reshape(48, 128).T, "b_ada": g["b_ada"][0][None, :],
            "w_in": g["w_in"][0], "s5_small": small, "s5_b": sbb, "s5_c": scc, "s5_drep": drep,
            "masks": masks, "ident": ident, "cst": cst, "tri": tri,
            "w_val": g["s5_w_glu_val"][0], "w_gate": g["s5_w_glu_gate"][0],
            "convwT": convw.reshape(3, 4, 128).transpose(2, 1, 0), "conv_out": g["conv_w_out"][0], "w_o": g["w_o"][0],
            "lnp": np.concatenate([g["ln1_g"][0], g["ln1_b"][0], g["ln2_g"][0], g["ln2_b"][0]])[None, :],
            "rw": np.concatenate([g["router_w_group"][0], g["router_w_expert"][0]], 1),
            "rb": np.concatenate([g["router_b_group"][0], g["router_b_expert"][0]])[None, :],
            "wg": g["exp_w_gate"][0], "wu": g["exp_w_up"][0], "wd": g["exp_w_down"][0],
        }
        maps.append({k: np.ascontiguousarray(v, dtype=f32) for k, v in m.items()})
    return maps


def kernel(**inputs):
    maps = host_inputs(inputs)
    nc = build()
    res = run_bass_kernel_spmd(nc, maps, core_ids=list(range(8)))
    out = np.zeros((4, 8192, 1024), np.float32)
    for core in range(8):
        b, hf = core // 2, core % 2
        o = res.results[core]["out"]
        if hf == 1:
            out[b, 4096:8192] = o
        else:
            out[b, 0:4096] = o[::-1]
    return out
```

```python
import math
import numpy as np
import concourse.bass as bass
import concourse.mybir as mybir
from concourse.bass_utils import run_bass_kernel_spmd

F32 = mybir.dt.float32
BF16 = mybir.dt.bfloat16
I32 = mybir.dt.int32
AF = mybir.ActivationFunctionType
ALU = mybir.AluOpType
AX = mybir.AxisListType

ALPHA = 2.0 ** 0.25
LN_EPS = 1e-6
NT = 4096
D = 1024
NG = 32
GELU_C = 2.0 * math.sqrt(2.0 / math.pi)


class Buf:
    __slots__ = ("name", "writer", "readers", "sem", "semcount")

    def __init__(self, name):
        self.name = name
        self.writer = None
        self.readers = []
        self.sem = None
        self.semcount = 0


class Sched:
    SEM_CAP = 30000

    def __init__(self, nc):
        self.nc = nc
        self.eng = {n: dict(prog=[], sem=None, count=0, waited={}, nsem=0) for n in ("pe", "act", "dve", "pool", "sp")}
        self.nbufsem = 0

    def _engsem(self, E, name):
        if E["sem"] is None or E["count"] >= self.SEM_CAP:
            E["sem"] = self.nc.alloc_semaphore(f"s_{name}_{E['nsem']}")
            E["nsem"] += 1
            E["count"] = 0
        return E["sem"]

    def _waits(self, E, reads, writes):
        need = {}

        def add(tok):
            if tok is None:
                return
            s, v = tok
            k = id(s)
            if k not in need or need[k][1] < v:
                need[k] = (s, v)
        for b in reads:
            add(b.writer)
        for b in writes:
            add(b.writer)
            for r in b.readers:
                add(r)
        out = []
        for k, (s, v) in need.items():
            if E["waited"].get(k, 0) < v:
                E["waited"][k] = v
                out.append((s, v))
        return out

    def _commit(self, tok, reads, writes):
        for b in writes:
            b.writer = tok
            b.readers = []
        for b in reads:
            if b not in writes:
                b.readers.append(tok)
                if len(b.readers) > 48:
                    d = {}
                    for s, v in b.readers:
                        if id(s) not in d or d[id(s)][1] < v:
                            d[id(s)] = (s, v)
                    b.readers = list(d.values())

    def op(self, name, fn, reads=(), writes=()):
        E = self.eng[name]
        waits = self._waits(E, reads, writes)
        sem = self._engsem(E, name)
        E["count"] += 1
        val = E["count"]

        def run(h, waits=waits, fn=fn, sem=sem):
            for s, v in waits:
                h.wait_ge(s, v)
            fn(h).then_inc(sem, 1)
        E["prog"].append(run)
        self._commit((sem, val), reads, writes)

    def dma(self, qname, fn, owner, reads=(), writes=()):
        E = self.eng[qname]
        waits = self._waits(E, reads, writes)
        if owner.sem is None:
            owner.sem = self.nc.alloc_semaphore(f"d_{self.nbufsem}")
            self.nbufsem += 1
        owner.semcount += 16
        sem, val = owner.sem, owner.semcount

        def run(h, waits=waits, fn=fn, sem=sem):
            for s, v in waits:
                h.wait_ge(s, v)
            fn(h).then_inc(sem, 16)
        E["prog"].append(run)
        self._commit((sem, val), reads, writes)

    def final_wait(self, qname, bufs):
        E = self.eng[qname]
        waits = self._waits(E, bufs, ())

        def run(h, waits=waits):
            for s, v in waits:
                h.wait_ge(s, v)
        E["prog"].append(run)

    def emit(self):
        with self.nc.Block() as block:
            @block.tensor
            def _(h):
                for f in self.eng["pe"]["prog"]:
                    f(h)

            @block.scalar
            def _(h):
                for f in self.eng["act"]["prog"]:
                    f(h)

            @block.vector
            def _(h):
                for f in self.eng["dve"]["prog"]:
                    f(h)

            @block.gpsimd
            def _(h):
                for f in self.eng["pool"]["prog"]:
                    f(h)

            @block.sync
            def _(h):
                for f in self.eng["sp"]["prog"]:
                    f(h)


def mkap(base, off_elems, dims):
    return bass.AP(base.tensor, base.offset + off_elems, [list(base.ap[0])] + [list(d) for d in dims])


def build(debug=None):
    nc = bass.Bass("TRN2", target_bir_lowering=False)
    S = Sched(nc)

    def din(name, shape, dt=F32):
        return nc.dram_tensor(name, list(shape), dt, kind="ExternalInput").ap()

    x_own = din("x_own", [NT, D]); x_oth = din("x_oth", [NT, D]); ctx_in = din("ctx", [256, D])
    pos_own = din("pos_own", [NT, D]); pos_oth = din("pos_oth", [NT, D])
    cT_in = din("cT", [128, 16])
    w_ada = din("w_ada", [D, 6 * D]); b_adaT = din("b_adaT", [128, 48]); b_ada = din("b_ada", [1, 6 * D])
    w_in = din("w_in", [D, 4096])
    s5_small = din("s5_small", [128, 3, NG])
    s5_b = din("s5_b", [128, 2, NG, 16]); s5_c = din("s5_c", [128, 2, NG, 16]); s5_drep = din("s5_drep", [128, NG])
    masks_in = din("masks", [128, 2, 512]); ident_in = din("ident", [128, 128]); cst_in = din("cst", [128, 192]); tri_in = din("tri", [128, 256])
    w_val = din("w_val", [512, D]); w_gate = din("w_gate", [512, D])
    convw_in = din("convwT", [128, 4, 3]); conv_out = din("conv_out", [512, D]); w_o = din("w_o", [D, D])
    lnp_in = din("lnp", [1, 4 * D])
    rw_in = din("rw", [D, 36]); rb_in = din("rb", [1, 36])
    wg_in = din("wg", [32, D, 512]); wu_in = din("wu", [32, D, 512]); wd_in = din("wd", [32, 512, D])
    out_d = nc.dram_tensor("out", [NT, D], F32, kind="ExternalOutput").ap()
    x1s = nc.dram_tensor("x1s", [NT, D], F32, kind=("ExternalOutput" if debug else "Internal")).ap()
    h2s = nc.dram_tensor("h2s", [128, 8, NT], BF16, kind=("ExternalOutput" if debug else "Internal")).ap()
    wrs = nc.dram_tensor("wrs", [128, 32, 32], F32, kind="ExternalOutput").ap() if debug else None
    dbg_d = None
    if debug == "ya":
        dbg_d = nc.dram_tensor("dbg", [128, 4 * NT], BF16, kind="ExternalOutput").ap()

    def sb(name, shape, dt=F32):
        return nc.alloc_sbuf_tensor("sb_" + name, list(shape), dt)

    ident = sb("ident", [128, 128]); b_ident = Buf("ident")
    identb = sb("identb", [128, 128], BF16); b_identb = Buf("identb")
    masks = sb("masks", [128, 2, 512]); b_masks = Buf("masks")
    modfm = sb("modfm", [128, 48, 2]); b_modfm = Buf("modfm")
    sc1p = sb("sc1p", [128, 8, 2]); sc2p = sb("sc2p", [128, 8])
    convw = sb("convw", [128, 4, 3]); b_convw = Buf("convw")
    rb_bc = sb("rb_bc", [128, 36]); b_rb = Buf("rb")
    epst = sb("epst", [128, 1]); b_eps = Buf("eps")

    S.dma("sp", lambda h: h.dma_start(out=ident[:], in_=ident_in), b_ident, writes=[b_ident])
    S.dma("sp", lambda h: h.dma_start(out=masks[:], in_=masks_in), b_masks, writes=[b_masks])
    S.dma("sp", lambda h: h.dma_start(out=convw[:], in_=convw_in), b_convw, writes=[b_convw])
    S.dma("sp", lambda h: h.dma_start(out=rb_bc[:], in_=rb_in.partition_broadcast(128)), b_rb, writes=[b_rb])
    S.op("dve", lambda h: h.tensor_copy(out=identb[:], in_=ident[:]), reads=[b_ident], writes=[b_identb])
    S.op("dve", lambda h: h.memset(epst[:], LN_EPS), writes=[b_eps])

    psg = [nc.alloc_psum_tensor(f"psg{i}", [128, 512], F32) for i in range(5)]
    b_psg = [Buf(f"psg{i}") for i in range(5)]
    psT = nc.alloc_psum_tensor("psT", [128, 1024], F32); b_psT = Buf("psT")
    psB = nc.alloc_psum_tensor("psB", [128, 1024], BF16); b_psB = Buf("psB")
    pctr = [0]

    def ps_next():
        i = pctr[0] % 5
        pctr[0] += 1
        return psg[i], b_psg[i]

    cT = sb("cT", [128, 16]); b_cT = Buf("cT")
    csil = sb("csil", [128, 16]); b_csil = Buf("csil")
    csil2 = sb("csil2", [128, 8, 2]); b_csil2 = Buf("csil2")
    blockA = sb("blockA", [128, 16384], BF16)
    b_hTb = Buf("hTb")
    badT = sb("badT", [128, 48]); b_badT = Buf("badT")
    S.dma("sp", lambda h: h.dma_start(out=cT[:], in_=cT_in), b_cT, writes=[b_cT])
    S.dma("sp", lambda h: h.dma_start(out=badT[:], in_=b_adaT), b_badT, writes=[b_badT])
    S.op("act", lambda h: h.activation(out=csil[:], in_=cT[:], func=AF.Silu), reads=[b_cT], writes=[b_csil])
    S.op("dve", lambda h: h.tensor_copy(out=csil2[:, :, 0], in_=csil[:, 0:8]), reads=[b_csil], writes=[b_csil2])
    S.op("dve", lambda h: h.tensor_copy(out=csil2[:, :, 1], in_=csil[:, 8:16]), reads=[b_csil], writes=[b_csil2])

    arena = sb("arena", [128, 16384])
    b_arena = Buf("arena")
    wsec = arena[:, 0:8192].rearrange("p (k n) -> p k n", k=8)
    for sec in (0, 1, 3, 4):
        S.dma("sp", lambda h, sec=sec: h.dma_start(out=wsec, in_=w_ada[:, sec * D:(sec + 1) * D].rearrange("(k p) n -> p k n", p=128)),
              b_arena, writes=[b_arena])
        ps, bps = ps_next()
        for jj in range(8):
            for kc in range(8):
                S.op("pe", lambda h, ps=ps, kc=kc, jj=jj: h.matmul(ps[:, jj * 2:jj * 2 + 2], lhsT=wsec[:, kc, jj * 128:(jj + 1) * 128],
                                                                  rhs=csil2[:, kc, :], start=(kc == 0), stop=(kc == 7)),
                     reads=[b_csil2, b_arena], writes=[bps])
        S.op("dve", lambda h, ps=ps, sec=sec: h.tensor_tensor(
            out=modfm[:, sec * 8:(sec + 1) * 8, :], in0=ps[:, 0:16].rearrange("p (j t) -> p j t", t=2),
            in1=mkap(badT[:, sec * 8:(sec + 1) * 8], 0, [[1, 8], [0, 2]]), op=ALU.add),
            reads=[bps, b_badT], writes=[b_modfm])
    S.op("dve", lambda h: h.tensor_scalar(out=sc1p[:], in0=modfm[:, 8:16, :], scalar1=1.0, scalar2=None, op0=ALU.add), reads=[b_modfm], writes=[b_modfm])
    S.op("dve", lambda h: h.tensor_scalar(out=sc2p[:], in0=modfm[:, 32:40, 0], scalar1=1.0, scalar2=None, op0=ALU.add), reads=[b_modfm], writes=[b_modfm])

    xt_t = [sb(f"xt{i}", [128, D]) for i in range(2)]; b_xt = [Buf(f"xt{i}") for i in range(2)]
    xn_t = sb("xn", [128, D]); b_xn = Buf("xn")
    stt = sb("stt", [128, 16]); b_stt = Buf("stt")
    xctr = [0]

    def ln_stats(src, bsrc, dst, bdst):
        S.op("dve", lambda h: h.bn_stats(out=stt[:, 0:6], in_=src[:, 0:512]), reads=[bsrc], writes=[b_stt])
        S.op("dve", lambda h: h.bn_stats(out=stt[:, 6:12], in_=src[:, 512:1024]), reads=[bsrc], writes=[b_stt])
        S.op("dve", lambda h: h.bn_aggr(out=stt[:, 12:14], in_=stt[:, 0:12]), reads=[b_stt], writes=[b_stt])
        S.op("act", lambda h: h.activation(out=stt[:, 14:15], in_=stt[:, 13:14], func=AF.Sqrt, bias=epst[:, 0:1], scale=1.0),
             reads=[b_stt, b_eps], writes=[b_stt])
        S.op("dve", lambda h: h.reciprocal(out=stt[:, 15:16], in_=stt[:, 14:15]), reads=[b_stt], writes=[b_stt])
        S.op("dve", lambda h: h.tensor_scalar(out=dst[:, :], in0=src[:, :], scalar1=stt[:, 12:13], scalar2=stt[:, 15:16],
                                              op0=ALU.subtract, op1=ALU.mult), reads=[bsrc, b_stt], writes=[bdst])

    def transpose_mod(src, bsrc, dst_fn, bdst, scale_fn, shift_fn, bmods):
        for kc in range(8):
            S.op("pe", lambda h, kc=kc: h.transpose(out=psT[:, kc * 128:(kc + 1) * 128], in_=src[:, kc * 128:(kc + 1) * 128], identity=ident[:]),
                 reads=[bsrc, b_ident], writes=[b_psT])
        for kc in range(8):
            S.op("act", lambda h, kc=kc: h.activation(out=dst_fn(kc), in_=psT[:, kc * 128:(kc + 1) * 128], func=AF.Identity,
                                                     bias=shift_fn(kc), scale=scale_fn(kc)),
                 reads=[b_psT] + bmods, writes=[bdst])

    def load_tile(xsrc, possrc, r0):
        i = xctr[0] % 2
        xctr[0] += 1
        xt, bx = xt_t[i], b_xt[i]
        S.dma("sp", lambda h: h.dma_start(out=xt[:], in_=xsrc[r0:r0 + 128, :]), bx, writes=[bx])
        if possrc is not None:
            S.dma("pool", lambda h: h.dma_start(out=xt[:], in_=possrc[r0:r0 + 128, :], accum_op=ALU.add), bx, writes=[bx])
        return xt, bx

    blockB = sb("blockB", [128, 10240])
    sm = sb("s5sm", [128, 3, NG]); b_sm = Buf("s5sm")
    sbv = blockB[:, 0:1024].rearrange("p (r g c) -> p r g c", r=2, g=NG); scv = blockB[:, 1024:2048].rearrange("p (r g c) -> p r g c", r=2, g=NG); b_bc = Buf("s5bc")
    drep = sb("drep", [128, NG]); b_drep = Buf("drep")
    S.dma("sp", lambda h: h.dma_start(out=sm[:], in_=s5_small), b_sm, writes=[b_sm])
    S.dma("sp", lambda h: h.dma_start(out=sbv[:], in_=s5_b), b_bc, writes=[b_bc])
    S.dma("sp", lambda h: h.dma_start(out=scv[:], in_=s5_c), b_bc, writes=[b_bc])
    S.dma("sp", lambda h: h.dma_start(out=drep[:], in_=s5_drep), b_drep, writes=[b_drep])

    tb_ = blockB[:, 2048:2048 + 11 * NG * 9].rearrange("p (t g k) -> p t g k", t=11, g=NG); b_tab = Buf("s5tab")
    T_ANG, T_Q, T_SIN, T_COS, T_MAG, T_MAGN, T_WRE, T_WIM, T_VRE, T_VIM, T_TMP = range(11)
    qi = sb("s5qi", [128, NG * 9], I32)
    vec = sb("s5vec", [128, 12, NG]); b_vec = Buf("s5vec")
    V_DT, V_TH, V_LR, V_DEN, V_XRE, V_FRE, V_FIM, V_T1, V_T2, V_RDEN = range(10)

    def vop(fn, r=(b_vec,), w=(b_vec,)):
        S.op("dve", fn, reads=list(r), writes=list(w))

    S.op("act", lambda h: h.activation(out=vec[:, V_DT, :], in_=sm[:, 0, :], func=AF.Exp), reads=[b_sm], writes=[b_vec])
    vop(lambda h: h.tensor_tensor(out=vec[:, V_TH, :], in0=vec[:, V_DT, :], in1=sm[:, 2, :], op=ALU.mult), r=(b_vec, b_sm))
    vop(lambda h: h.tensor_tensor(out=vec[:, V_LR, :], in0=vec[:, V_DT, :], in1=sm[:, 1, :], op=ALU.mult), r=(b_vec, b_sm))
    T = lambda t: tb_[:, t, :, :]
    Tf = lambda t: tb_[:, t, :, :].rearrange("p g k -> p (g k)")
    for k in range(9):
        S.op("dve", lambda h, k=k: h.tensor_scalar(out=tb_[:, T_ANG, :, k], in0=vec[:, V_TH, :], scalar1=float(k), scalar2=None, op0=ALU.mult),
             reads=[b_vec], writes=[b_tab])
        S.op("act", lambda h, k=k: h.activation(out=tb_[:, T_MAG, :, k], in_=vec[:, V_LR, :], func=AF.Exp, scale=float(k)), reads=[b_vec], writes=[b_tab])
        S.op("act", lambda h, k=k: h.activation(out=tb_[:, T_MAGN, :, k], in_=vec[:, V_LR, :], func=AF.Exp, scale=-float(k)), reads=[b_vec], writes=[b_tab])

    def sin_of(dst_t, shift):
        top = lambda fn: S.op("dve", fn, reads=[b_tab], writes=[b_tab])
        top(lambda h: h.tensor_scalar(out=Tf(T_TMP), in0=Tf(T_ANG), scalar1=shift, scalar2=None, op0=ALU.add))
        top(lambda h: h.tensor_scalar(out=qi[:], in0=Tf(T_TMP), scalar1=1.0 / (2 * math.pi), scalar2=None, op0=ALU.mult))
        top(lambda h: h.tensor_copy(out=Tf(T_Q), in_=qi[:]))
        top(lambda h: h.scalar_tensor_tensor(out=Tf(T_TMP), in0=Tf(T_Q), scalar=-2 * math.pi, in1=Tf(T_TMP), op0=ALU.mult, op1=ALU.add))
        top(lambda h: h.tensor_scalar(out=Tf(T_Q), in0=Tf(T_TMP), scalar1=math.pi, scalar2=2 * math.pi, op0=ALU.is_gt, op1=ALU.mult))
        top(lambda h: h.tensor_tensor(out=Tf(T_TMP), in0=Tf(T_TMP), in1=Tf(T_Q), op=ALU.subtract))
        top(lambda h: h.tensor_scalar(out=Tf(T_Q), in0=Tf(T_TMP), scalar1=-math.pi, scalar2=2 * math.pi, op0=ALU.is_lt, op1=ALU.mult))
        top(lambda h: h.tensor_tensor(out=Tf(T_TMP), in0=Tf(T_TMP), in1=Tf(T_Q), op=ALU.add))
        S.op("act", lambda h: h.activation(out=Tf(dst_t), in_=Tf(T_TMP), func=AF.Sin), reads=[b_tab], writes=[b_tab])

    sin_of(T_SIN, 0.0)
    sin_of(T_COS, math.pi / 2)
    tt = lambda o, a, b, op: S.op("dve", lambda h: h.tensor_tensor(out=Tf(o), in0=Tf(a), in1=Tf(b), op=op), reads=[b_tab], writes=[b_tab])
    tt(T_WRE, T_MAG, T_COS, ALU.mult)
    tt(T_WIM, T_MAG, T_SIN, ALU.mult)
    tt(T_VRE, T_MAGN, T_COS, ALU.mult)
    tt(T_VIM, T_MAGN, T_SIN, ALU.mult)
    S.op("dve", lambda h: h.tensor_scalar(out=Tf(T_VIM), in0=Tf(T_VIM), scalar1=-1.0, scalar2=None, op0=ALU.mult), reads=[b_tab], writes=[b_tab])
    are = sm[:, 1, :]; aim = sm[:, 2, :]
    abre = tb_[:, T_WRE, :, 1]; abim = tb_[:, T_WIM, :, 1]
    vr = (b_vec, b_sm, b_tab)
    vop(lambda h: h.tensor_tensor(out=vec[:, V_DEN, :], in0=are, in1=are, op=ALU.mult), r=vr)
    vop(lambda h: h.tensor_tensor(out=vec[:, V_T1, :], in0=aim, in1=aim, op=ALU.mult), r=vr)
    vop(lambda h: h.tensor_tensor(out=vec[:, V_DEN, :], in0=vec[:, V_DEN, :], in1=vec[:, V_T1, :], op=ALU.add), r=vr)
    vop(lambda h: h.reciprocal(out=vec[:, V_RDEN, :], in_=vec[:, V_DEN, :]), r=vr)
    vop(lambda h: h.tensor_scalar(out=vec[:, V_XRE, :], in0=abre, scalar1=-1.0, scalar2=None, op0=ALU.add), r=vr)
    vop(lambda h: h.tensor_tensor(out=vec[:, V_T1, :], in0=vec[:, V_XRE, :], in1=are, op=ALU.mult), r=vr)
    vop(lambda h: h.tensor_tensor(out=vec[:, V_T2, :], in0=abim, in1=aim, op=ALU.mult), r=vr)
    vop(lambda h: h.tensor_tensor(out=vec[:, V_T1, :], in0=vec[:, V_T1, :], in1=vec[:, V_T2, :], op=ALU.add), r=vr)
    vop(lambda h: h.tensor_tensor(out=vec[:, V_FRE, :], in0=vec[:, V_T1, :], in1=vec[:, V_RDEN, :], op=ALU.mult), r=vr)
    vop(lambda h: h.tensor_tensor(out=vec[:, V_T1, :], in0=abim, in1=are, op=ALU.mult), r=vr)
    vop(lambda h: h.tensor_tensor(out=vec[:, V_T2, :], in0=vec[:, V_XRE, :], in1=aim, op=ALU.mult), r=vr)
    vop(lambda h: h.tensor_tensor(out=vec[:, V_T1, :], in0=vec[:, V_T1, :], in1=vec[:, V_T2, :], op=ALU.subtract), r=vr)
    vop(lambda h: h.tensor_tensor(out=vec[:, V_FIM, :], in0=vec[:, V_T1, :], in1=vec[:, V_RDEN, :], op=ALU.mult), r=vr)
    bbar = blockB[:, 5632:6656].rearrange("p (r g c) -> p r g c", r=2, g=NG); b_bbar = Buf("bbar")
    tmpb = blockB[:, 6656:7168].rearrange("p (g c) -> p g c", g=NG); b_tmpb = Buf("tmpb")
    fre_b = mkap(vec[:, V_FRE, :], 0, [[1, NG], [0, 16]]); fim_b = mkap(vec[:, V_FIM, :], 0, [[1, NG], [0, 16]])
    bo = lambda fn, r, w: S.op("dve", fn, reads=r, writes=w)
    bo(lambda h: h.tensor_tensor(out=bbar[:, 0], in0=sbv[:, 0], in1=fre_b, op=ALU.mult), [b_bc, b_vec], [b_bbar])
    bo(lambda h: h.tensor_tensor(out=tmpb[:], in0=sbv[:, 1], in1=fim_b, op=ALU.mult), [b_bc, b_vec], [b_tmpb])
    bo(lambda h: h.tensor_tensor(out=bbar[:, 0], in0=bbar[:, 0], in1=tmpb[:], op=ALU.subtract), [b_bbar, b_tmpb], [b_bbar])
    bo(lambda h: h.tensor_tensor(out=bbar[:, 1], in0=sbv[:, 1], in1=fre_b, op=ALU.mult), [b_bc, b_vec], [b_bbar])
    bo(lambda h: h.tensor_tensor(out=tmpb[:], in0=sbv[:, 0], in1=fim_b, op=ALU.mult), [b_bc, b_vec], [b_tmpb])
    bo(lambda h: h.tensor_tensor(out=bbar[:, 1], in0=bbar[:, 1], in1=tmpb[:], op=ALU.add), [b_bbar, b_tmpb], [b_bbar])

    WB, WBp, WC = [blockB[:, 7168 + i * 512:7168 + (i + 1) * 512].rearrange("p (r g k) -> p r g k", r=2, g=NG) for i in range(3)]; b_W = Buf("W")

    def fwd_slice(t, lanes, k0):
        return tb_[lanes, t, :, k0:k0 + 8]

    def rev_slice(t, lanes, k_hi):
        base = tb_[lanes, t, :, k_hi:k_hi + 1]
        return mkap(base, 0, [[9, NG], [-1, 8]])
    LF = slice(0, 64); LB = slice(64, 128)
    for ri, (tw, tv) in enumerate(((T_WRE, T_VRE), (T_WIM, T_VIM))):
        cp = lambda o, i_: S.op("dve", lambda h: h.tensor_copy(out=o, in_=i_), reads=[b_tab], writes=[b_W])
        cp(WB[LF, ri], rev_slice(tw, LF, 7))
        cp(WB[LB, ri], fwd_slice(tw, LB, 0))
        cp(WBp[LF, ri], fwd_slice(tv, LF, 1))
        cp(WBp[LB, ri], rev_slice(tv, LB, 8))
        cp(WC[LF, ri], fwd_slice(tw, LF, 1))
        cp(WC[LB, ri], rev_slice(tw, LB, 8))

    D0m = sb("D0m", [128, NG, 128], BF16); b_D0 = Buf("D0")
    PTre = sb("PTre", [128, NG, 128], BF16); PTim = sb("PTim", [128, NG, 128], BF16); b_PT = Buf("PT")
    Qre = sb("Qre", [128, NG, 128], BF16); Qimn = sb("Qimn", [128, NG, 128], BF16); b_Q = Buf("Q")
    A1 = sb("A1", [128, NG, 2]); A2 = sb("A2", [128, NG, 2]); b_A = Buf("A12")
    S.op("dve", lambda h: h.tensor_copy(out=A1[:], in_=mkap(tb_[:, T_WRE, :, 8:9], 0, [[9, NG], [0, 2]])), reads=[b_tab], writes=[b_A])
    S.op("dve", lambda h: h.tensor_copy(out=A2[:, :, 1], in_=tb_[:, T_WIM, :, 8]), reads=[b_tab], writes=[b_A])
    S.op("dve", lambda h: h.tensor_scalar(out=A2[:, :, 0], in0=tb_[:, T_WIM, :, 8], scalar1=-1.0, scalar2=None, op0=ALU.mult), reads=[b_tab], writes=[b_A])

    GC = 8
    gen = arena[:, 0:8192]

    def gslot(i):
        return gen[:, i * 1024:(i + 1) * 1024].rearrange("p (g i c) -> p g i c", g=GC, i=8)

    def cprod(Wt, X, g0, o_re, o_im, breads):
        def wv(ri):
            return mkap(Wt[:, ri, g0:g0 + GC, :], 0, [[8, GC], [1, 8], [0, 16]])

        def xv(ri):
            return mkap(X[:, ri, g0:g0 + GC, :], 0, [[16, GC], [0, 8], [1, 16]])
        t1 = gslot(6); t2 = gslot(7)
        o = lambda fn: S.op("dve", fn, reads=[b_W, b_arena] + breads, writes=[b_arena])
        o(lambda h: h.tensor_tensor(out=t1, in0=wv(0), in1=xv(0), op=ALU.mult))
        o(lambda h: h.tensor_tensor(out=t2, in0=wv(1), in1=xv(1), op=ALU.mult))
        o(lambda h: h.tensor_tensor(out=o_re, in0=t1, in1=t2, op=ALU.subtract))
        o(lambda h: h.tensor_tensor(out=t1, in0=wv(0), in1=xv(1), op=ALU.mult))
        o(lambda h: h.tensor_tensor(out=t2, in0=wv(1), in1=xv(0), op=ALU.mult))
        o(lambda h: h.tensor_tensor(out=o_im, in0=t1, in1=t2, op=ALU.add))

    for gch in range(NG // GC):
        g0 = gch * GC
        Bt_re, Bt_im, Bp_re, Bp_im, Ct_re, Ct_im = [gslot(i) for i in range(6)]
        cprod(WB, bbar, g0, Bt_re, Bt_im, [b_bbar])
        cprod(WBp, bbar, g0, Bp_re, Bp_im, [b_bbar])
        cprod(WC, scv, g0, Ct_re, Ct_im, [b_bc])
        fl = lambda a: a.rearrange("p g i c -> p g (i c)")
        S.op("act", lambda h, g0=g0, Ct_re=Ct_re: h.activation(out=Qre[:, g0:g0 + GC, :], in_=Ct_re.rearrange("p g i c -> p g (i c)"), func=AF.Identity), reads=[b_arena], writes=[b_Q])
        S.op("act", lambda h, g0=g0, Ct_im=Ct_im: h.activation(out=Qimn[:, g0:g0 + GC, :], in_=Ct_im.rearrange("p g i c -> p g (i c)"), func=AF.Identity, scale=-1.0), reads=[b_arena], writes=[b_Q])
        S.op("dve", lambda h, Ct_im=Ct_im: h.tensor_scalar(out=Ct_im.rearrange("p g i c -> p g (i c)"), in0=Ct_im.rearrange("p g i c -> p g (i c)"), scalar1=-1.0, scalar2=None, op0=ALU.mult), reads=[b_arena, b_Q], writes=[b_arena])
        for ri, (Bt, PT) in enumerate(((Bt_re, PTre), (Bt_im, PTim))):
            for gg in range(GC):
                S.op("pe", lambda h, gg=gg, Bt=Bt: h.transpose(out=psT[:, gg * 128:(gg + 1) * 128], in_=Bt.rearrange("p g i c -> p g (i c)")[:, gg, :], identity=ident[:]),
                     reads=[b_arena, b_ident], writes=[b_psT])
            S.op("act", lambda h, PT=PT, g0=g0: h.activation(out=PT[:, g0:g0 + GC, :], in_=psT[:, :].rearrange("p (g m) -> p g m", g=GC), func=AF.Identity),
                 reads=[b_psT], writes=[b_PT])
        for g4 in range(GC // 4):
            pF, bpF = ps_next(); pB, bpB = ps_next()
            for gg in range(4):
                g = g4 * 4 + gg
                for lanes, pp, bpp in ((LF, pF, bpF), (LB, pB, bpB)):
                    S.op("pe", lambda h, g=g, gg=gg, lanes=lanes, pp=pp, Bp_re=Bp_re, Ct_re=Ct_re: h.matmul(pp[:, gg * 128:(gg + 1) * 128], lhsT=Bp_re.rearrange("p g i c -> p g (i c)")[lanes, g, :],
                                                                                  rhs=Ct_re.rearrange("p g i c -> p g (i c)")[lanes, g, :], start=True, stop=False),
                         reads=[b_arena], writes=[bpp])
                    S.op("pe", lambda h, g=g, gg=gg, lanes=lanes, pp=pp, Bp_im=Bp_im, Ct_im=Ct_im: h.matmul(pp[:, gg * 128:(gg + 1) * 128], lhsT=Bp_im.rearrange("p g i c -> p g (i c)")[lanes, g, :],
                                                                                  rhs=Ct_im.rearrange("p g i c -> p g (i c)")[lanes, g, :], start=False, stop=True),
                         reads=[b_arena], writes=[bpp])
            t1 = gslot(6).rearrange("p g i c -> p (g i c)")[:, 0:512]
            t2 = gslot(7).rearrange("p g i c -> p (g i c)")[:, 0:512]
            S.op("dve", lambda h, pF=pF, t1=t1: h.tensor_tensor(out=t1, in0=pF[:, :], in1=masks[:, 0, :], op=ALU.mult), reads=[bpF, b_masks, b_arena], writes=[b_arena])
            S.op("dve", lambda h, pB=pB, t2=t2: h.tensor_tensor(out=t2, in0=pB[:, :], in1=masks[:, 1, :], op=ALU.mult), reads=[bpB, b_masks, b_arena], writes=[b_arena])
            S.op("dve", lambda h, t1=t1, t2=t2: h.tensor_tensor(out=t1, in0=t1, in1=t2, op=ALU.add), reads=[b_arena], writes=[b_arena])
            for gg in range(4):
                g = g0 + g4 * 4 + gg
                S.op("dve", lambda h, g=g, gg=gg, t1=t1: h.scalar_tensor_tensor(out=D0m[:, g, :], in0=ident[:], scalar=drep[:, g:g + 1],
                                                                               in1=t1[:, gg * 128:(gg + 1) * 128], op0=ALU.mult, op1=ALU.add),
                     reads=[b_arena, b_ident, b_drep], writes=[b_D0])

    winu = sb("winu", [128, 8, 512], BF16); b_winu = Buf("winu")
    S.dma("pool", lambda h: h.dma_start(out=winu[:], in_=w_in[:, 0:512].rearrange("(k p) n -> p k n", p=128)), b_winu, writes=[b_winu])
    hTb = blockA[:, 0:4096].rearrange("p (k t) -> p k t", k=8)
    U_sb = blockA[:, 4096:8192]; b_Usb = Buf("U_sb")
    UT = blockB[:, 0:8192].bitcast(BF16).rearrange("p (g j) -> p g j", g=NG); b_UT = Buf("UT")
    UTo = blockA[:, 8192:10240].rearrange("p (g j) -> p g j", g=NG); b_UTo = Buf("UTo")
    Zoth = blockA[:, 10240:14336].rearrange("p (j e) -> p j e", e=64); b_Zoth = Buf("Zoth")
    Zctx = blockA[:, 14336:16384].rearrange("p (j e) -> p j e", e=64); b_Zctx = Buf("Zctx")
    Zown = arena[:, :].bitcast(BF16).rearrange("p (j e) -> p j e", e=64)
    b_Zown = b_arena
    cur = [sb(f"cur{i}", [128, NG, 2]) for i in range(2)]; b_cur = [Buf(f"cur{i}") for i in range(2)]
    st1 = sb("st1", [128, NG, 2]); st2 = sb("st2", [128, NG, 2]); b_st = Buf("st12")
    S.op("dve", lambda h: h.memset(cur[0][:], 0.0), writes=[b_cur[0]])
    scan_k = [0]

    def scan_step(zap, bz, lanes, store):
        k = scan_k[0]
        scan_k[0] += 1
        c0, bc0 = cur[k % 2], b_cur[k % 2]
        c1, bc1 = cur[(k + 1) % 2], b_cur[(k + 1) % 2]
        L = lanes
        sw = mkap(c0[L, :, 1:2], 0, [[2, NG], [-1, 2]])
        S.op("dve", lambda h: h.tensor_tensor(out=st1[L], in0=c0[L], in1=A1[L], op=ALU.mult), reads=[bc0, b_A], writes=[b_st])
        S.op("dve", lambda h: h.tensor_tensor(out=st2[L], in0=sw, in1=A2[L], op=ALU.mult), reads=[bc0, b_A, b_st], writes=[b_st])
        S.op("dve", lambda h: h.tensor_tensor(out=st1[L], in0=st1[L], in1=st2[L], op=ALU.add), reads=[b_st], writes=[b_st])
        S.op("dve", lambda h: h.tensor_tensor(out=c1[L], in0=st1[L], in1=zap, op=ALU.add), reads=[b_st, bz], writes=[bc1])
        if store:
            S.op("dve", lambda h: h.tensor_copy(out=zap, in_=c0[L]), reads=[bc0, bz], writes=[bz])
        if L != slice(0, 128):
            other = slice(64, 128) if L == slice(0, 64) else slice(0, 64)
            S.op("dve", lambda h: h.tensor_copy(out=c1[other], in_=c0[other]), reads=[bc0], writes=[bc1])

    def phaseA_block(xsrc, possrc, t0, ntok, sh_fn, sc_fn, UTdst, bUT, J0):
        ntile = ntok // 128
        nJ = ntok // 8
        for tl in range(ntile):
            xt, bx = load_tile(xsrc, possrc, t0 + tl * 128)
            ln_stats(xt, bx, xn_t, b_xn)
            transpose_mod(xn_t, b_xn, lambda kc, tl=tl: hTb[:, kc, tl * 128:(tl + 1) * 128], b_hTb, sc_fn, sh_fn, [b_modfm])
        for i in range(8):
            ps, bps = ps_next()
            for kc in range(8):
                S.op("pe", lambda h, ps=ps, kc=kc, i=i: h.matmul(ps[0:nJ, :], lhsT=mkap(hTb[:, kc, i:i + 1], 0, [[8, nJ]]), rhs=winu[:, kc, :],
                                                                start=(kc == 0), stop=(kc == 7)),
                     reads=[b_hTb, b_winu], writes=[bps])
            S.op("act", lambda h, ps=ps, i=i: h.activation(out=mkap(U_sb[0:nJ, i * 16:i * 16 + 1], 0, [[128, NG], [1, 16]]), in_=ps[0:nJ, :].rearrange("p (g c) -> p g c", g=NG), func=AF.Identity), reads=[bps], writes=[b_Usb])
        for g8 in range(4):
            for gg in range(8):
                g = g8 * 8 + gg
                S.op("pe", lambda h, g=g, gg=gg: h.transpose(out=psB[:, gg * 128:gg * 128 + nJ], in_=U_sb[0:nJ, g * 128:(g + 1) * 128],
                                                             identity=identb[0:nJ, 0:nJ]),
                     reads=[b_Usb, b_identb], writes=[b_psB])
            S.op("dve", lambda h, g8=g8: h.tensor_copy(out=UTdst[:, g8 * 8:(g8 + 1) * 8, J0:J0 + nJ],
                                                       in_=psB[:, :].rearrange("p (g j) -> p g j", g=8)[:, :, 0:nJ]),
                 reads=[b_psB], writes=[bUT])

    def z_block(UTsrc, bUT, J0, nJ, Zdst_fn, bZ, lanes_list):
        for g in range(NG):
            ps, bps = ps_next()
            S.op("pe", lambda h, ps=ps, g=g: h.matmul(ps[:, 0:nJ], lhsT=PTre[:, g, :], rhs=UTsrc[:, g, J0:J0 + nJ], start=True, stop=True),
                 reads=[b_PT, bUT], writes=[bps])
            S.op("pe", lambda h, ps=ps, g=g: h.matmul(ps[:, 256:256 + nJ], lhsT=PTim[:, g, :], rhs=UTsrc[:, g, J0:J0 + nJ], start=True, stop=True),
                 reads=[b_PT, bUT, bps], writes=[bps])
            for lanes in lanes_list:
                if lanes == LF:
                    S.op("act", lambda h, ps=ps, g=g, lanes=lanes: h.activation(
                        out=Zdst_fn(lanes, g), in_=ps[lanes, :].rearrange("p (r j) -> p r j", r=2)[:, :, 0:nJ], func=AF.Identity),
                        reads=[bps], writes=[bZ])
                else:
                    S.op("dve", lambda h, ps=ps, g=g, lanes=lanes: h.tensor_copy(
                        out=Zdst_fn(lanes, g), in_=ps[lanes, :].rearrange("p (r j) -> p r j", r=2)[:, :, 0:nJ]),
                        reads=[bps], writes=[bZ])

    def zdst(Zt, nJtot, Jbase, nJ):
        def fn(lanes, g):
            if lanes == LF:
                base = Zt[LF, Jbase:Jbase + 1, 2 * g:2 * g + 1]
                return mkap(base, 0, [[1, 2], [64, nJ]])
            base = Zt[LB, nJtot - 1 - Jbase:nJtot - Jbase, 2 * g:2 * g + 1]
            return mkap(base, 0, [[1, 2], [-64, nJ]])
        return fn

    ALL = slice(0, 128)
    sh1_fn = lambda kc: modfm[:, 0 + kc, 0:1]; sc1_fn = lambda kc: sc1p[:, kc, 0:1]
    csh1_fn = lambda kc: modfm[:, 0 + kc, 1:2]; csc1_fn = lambda kc: sc1p[:, kc, 1:2]
    phaseA_block(ctx_in, None, 0, 256, csh1_fn, csc1_fn, UTo, b_UTo, 0)
    z_block(UTo, b_UTo, 0, 32, zdst(Zctx, 32, 0, 32), b_Zctx, [LF, LB])
    for k in range(32):
        scan_step(Zctx[:, k, :].rearrange("p (g r) -> p g r", r=2), b_Zctx, ALL, False)
    for blk in range(8):
        phaseA_block(x_oth, pos_oth, blk * 512, 512, sh1_fn, sc1_fn, UTo, b_UTo, 0)
        z_block(UTo, b_UTo, 0, 64, zdst(Zoth, 64, 0, 64), b_Zoth, [LF])
        for k in range(64):
            scan_step(Zoth[LF, k, :].rearrange("p (g r) -> p g r", r=2), b_Zoth, LF, False)
    for blk in range(8):
        phaseA_block(x_own, pos_own, blk * 512, 512, sh1_fn, sc1_fn, UT, b_UT, blk * 64)
    for jb in range(4):
        z_block(UT, b_UT, jb * 128, 128, zdst(Zown, 512, jb * 128, 128), b_Zown, [LF, LB])
    for k in range(512):
        scan_step(Zown[:, k, :].rearrange("p (g r) -> p g r", r=2), b_Zown, ALL, True)


    Ysb = blockA[:, :].rearrange("p (g j) -> p g j", g=NG); b_Ysb = Buf("Ysb")
    TM = arena[:, 8192:10240].bitcast(BF16).rearrange("p (j c) -> p j c", j=8); b_TM = b_arena
    ya = arena[:, 0:8192].bitcast(BF16).rearrange("p (a t) -> p a t", a=4); b_ya = b_arena
    for g in range(NG):
        ps, bps = ps_next()
        S.op("pe", lambda h, ps=ps, g=g: h.matmul(ps[:, :], lhsT=D0m[:, g, :], rhs=UT[:, g, :], start=True, stop=False), reads=[b_D0, b_UT], writes=[bps])
        for lanes in (LF, LB):
            for ri, Qm in enumerate((Qre, Qimn)):
                if lanes == LF:
                    rhs = mkap(Zown[LF, 0:1, 2 * g + ri:2 * g + ri + 1], 0, [[64, 512]])
                else:
                    rhs = mkap(Zown[LB, 511:512, 2 * g + ri:2 * g + ri + 1], 0, [[-64, 512]])
                last = (lanes == LB and ri == 1)
                S.op("pe", lambda h, ps=ps, g=g, lanes=lanes, Qm=Qm, rhs=rhs, last=last: h.matmul(ps[:, :], lhsT=Qm[lanes, g, :], rhs=rhs, start=False, stop=last),
                     reads=[b_Q, b_Zown, bps], writes=[bps])
        S.op("act", lambda h, ps=ps, g=g: h.activation(out=Ysb[:, g, :], in_=ps[:, :], func=AF.Identity), reads=[bps], writes=[b_Ysb, b_hTb, b_Usb, b_UTo, b_Zoth, b_Zctx])
    gt = [arena[:, 10240 + i * 1024:10240 + (i + 1) * 1024] for i in range(2)]; b_gt = b_arena
    for jb in range(4):
        for g8 in range(4):
            for gg in range(8):
                g = g8 * 8 + gg
                S.op("pe", lambda h, g=g, gg=gg, jb=jb: h.transpose(out=psB[:, gg * 128:(gg + 1) * 128], in_=Ysb[:, g, jb * 128:(jb + 1) * 128], identity=identb[:]),
                     reads=[b_Ysb, b_identb], writes=[b_psB])
            S.op("dve", lambda h, g8=g8: h.tensor_copy(
                out=mkap(TM[:, 0:1, g8 * 128:g8 * 128 + 1], 0, [[16, 8], [512, 8], [1, 16]]),
                in_=psB[:, :].rearrange("p (g j c) -> p g j c", g=8, j=8)), reads=[b_psB], writes=[b_TM])
        for ct in range(4):
            for j in range(8):
                S.op("pe", lambda h, ct=ct, j=j: h.transpose(out=psB[:, j * 128:(j + 1) * 128], in_=TM[:, j, ct * 128:(ct + 1) * 128], identity=identb[:]),
                     reads=[b_TM, b_identb], writes=[b_psB])
            xin = psB[:, :]
            g0t, g1t = gt[0], gt[1]
            S.op("act", lambda h: h.activation(out=g0t, in_=xin, func=AF.Square), reads=[b_psB], writes=[b_gt])
            S.op("dve", lambda h: h.tensor_scalar(out=g0t, in0=g0t, scalar1=0.044715, scalar2=1.0, op0=ALU.mult, op1=ALU.add), reads=[b_gt], writes=[b_gt])
            S.op("dve", lambda h: h.tensor_tensor(out=g0t, in0=g0t, in1=xin, op=ALU.mult), reads=[b_gt, b_psB], writes=[b_gt])
            S.op("act", lambda h: h.activation(out=g1t, in_=g0t, func=AF.Sigmoid, scale=GELU_C), reads=[b_gt], writes=[b_gt])
            S.op("dve", lambda h, ct=ct, jb=jb: h.tensor_tensor(
                out=mkap(ya[:, ct, jb * 1024:jb * 1024 + 1], 0, [[1, 8], [8, 128]]),
                in0=g1t.rearrange("p (j J) -> p j J", j=8), in1=psB[:, :].rearrange("p (j J) -> p j J", j=8), op=ALU.mult),
                reads=[b_gt, b_psB], writes=[b_ya])

    if debug == "ya":
        b_dd = b_arena
        S.dma("sp", lambda h: h.dma_start(out=dbg_d, in_=ya.rearrange("p a b -> p (a b)")), b_dd, reads=[b_dd])
        S.final_wait("sp", [b_dd])
        S.emit()
        return nc

    def alias(new, olds):
        for o in olds:
            if o.writer is not None:
                new.readers.append(o.writer)
            new.readers.extend(o.readers)
        return new

    dead_A = [b_Ysb, b_hTb, b_Usb, b_UTo, b_Zoth, b_Zctx]
    dead_B = [b_UT, b_bc, b_tab, b_bbar, b_tmpb, b_W]
    b_blkA = alias(Buf("blkA"), dead_A)
    b_up0 = alias(Buf("up0"), [b_arena])
    gsec = blockA[:, :].bitcast(F32).rearrange("p (k n) -> p k n", k=8)
    crep = arena[:, 8192:9216].rearrange("p (k m) -> p k m", k=8)
    badrow = arena[:, 9216:10240]
    g1bc = blockB[:, 6144:7168]; g2bc = blockB[:, 7168:8192]; b_gbc = alias(Buf("gbc"), dead_B)
    lnp = blockB[:, 8192:10240]; b_lnp = alias(Buf("lnp"), dead_B)
    S.op("dve", lambda h: h.tensor_copy(out=crep, in_=mkap(csil[:], 0, [[1, 8], [0, 128]])), reads=[b_csil], writes=[b_up0])
    for sec, gdst in ((2, g1bc), (5, g2bc)):
        S.dma("sp", lambda h, sec=sec: h.dma_start(out=gsec, in_=w_ada[:, sec * D:(sec + 1) * D].rearrange("(k p) n -> p k n", p=128)),
              b_blkA, writes=[b_blkA])
        S.dma("sp", lambda h, sec=sec: h.dma_start(out=badrow, in_=b_ada[:, sec * D:(sec + 1) * D].partition_broadcast(128)),
              b_up0, writes=[b_up0])
        for nb in range(2):
            ps, bps = ps_next()
            for kc in range(8):
                S.op("pe", lambda h, ps=ps, kc=kc, nb=nb: h.matmul(ps[:, :], lhsT=crep[:, kc, :], rhs=gsec[:, kc, nb * 512:(nb + 1) * 512],
                                                                  start=(kc == 0), stop=(kc == 7)),
                     reads=[b_up0, b_blkA], writes=[bps])
            S.op("dve", lambda h, ps=ps, nb=nb, gdst=gdst: h.tensor_tensor(out=gdst[:, nb * 512:(nb + 1) * 512], in0=ps[:, :],
                                                                          in1=badrow[:, nb * 512:(nb + 1) * 512], op=ALU.add),
                 reads=[bps, b_up0], writes=[b_gbc])
    S.dma("sp", lambda h: h.dma_start(out=lnp, in_=lnp_in[:, 0:2048].partition_broadcast(128)), b_lnp, writes=[b_lnp])

    winA = blockA[:, :].rearrange("p (k n) -> p k n", k=8)
    winB = blockB[:, 0:6144].bitcast(BF16).rearrange("p (k n) -> p k n", k=8)
    b_winB = alias(Buf("winB"), dead_B)
    for c0, c1, dst, bd in ((512, 1536, winA[:, :, 0:1024], b_blkA), (1536, 2560, winA[:, :, 1024:2048], b_blkA),
                            (2560, 3584, winB[:, :, 0:1024], b_winB), (3584, 4096, winB[:, :, 1024:1536], b_winB)):
        S.dma("pool", lambda h, c0=c0, c1=c1, dst=dst: h.dma_start(out=dst, in_=w_in[:, c0:c1].rearrange("(k p) n -> p k n", p=128)),
              bd, writes=[bd])
    flat = lambda t: t[:].rearrange("p g m -> p (g m)")
    wval = flat(PTre).rearrange("p (k n) -> p k n", k=4); wgate = flat(PTim).rearrange("p (k n) -> p k n", k=4)
    convo = flat(Qre).rearrange("p (k n) -> p k n", k=4)
    wo_lo = flat(Qimn).rearrange("p (k n) -> p k n", k=4); wo_hi = flat(D0m).rearrange("p (k n) -> p k n", k=4)
    b_wglu = alias(Buf("wglu"), [b_PT]); b_convo = alias(Buf("convo"), [b_Q]); b_wo = alias(Buf("wo"), [b_Q, b_D0])
    S.dma("pool", lambda h: h.dma_start(out=wval, in_=w_val.rearrange("(k p) n -> p k n", p=128)), b_wglu, writes=[b_wglu])
    S.dma("pool", lambda h: h.dma_start(out=wgate, in_=w_gate.rearrange("(k p) n -> p k n", p=128)), b_wglu, writes=[b_wglu])
    S.dma("pool", lambda h: h.dma_start(out=convo, in_=conv_out.rearrange("(k p) n -> p k n", p=128)), b_convo, writes=[b_convo])
    S.dma("pool", lambda h: h.dma_start(out=wo_lo, in_=w_o[0:512, :].rearrange("(k p) n -> p k n", p=128)), b_wo, writes=[b_wo])
    S.dma("pool", lambda h: h.dma_start(out=wo_hi, in_=w_o[512:1024, :].rearrange("(k p) n -> p k n", p=128)), b_wo, writes=[b_wo])
    rw32 = masks[:, 1, 0:288].rearrange("p (k n) -> p k n", k=8); b_rw = alias(Buf("rw32"), [b_masks])
    S.dma("sp", lambda h: h.dma_start(out=rw32, in_=rw_in.rearrange("(k p) n -> p k n", p=128)), b_rw, writes=[b_rw])
    rt = masks[:, 0, 0:160]; b_rt = alias(Buf("rt"), [b_masks])

    cst = masks[:, 1, 288:480]; b_cst = alias(Buf("cst"), [b_masks])
    S.dma("sp", lambda h: h.dma_start(out=cst, in_=cst_in), b_cst, writes=[b_cst])
    RT = masks[:, 0, 256:384].rearrange("p (t f) -> p t f", f=4); b_RT = alias(Buf("RT"), [b_masks])
    DEST = masks[:, 0, 384:448].bitcast(I32).rearrange("p (t f) -> p t f", f=2); b_DEST = alias(Buf("DEST"), [b_masks])
    hT = arena[:, 8192:10240].bitcast(BF16).rearrange("p (k t) -> p k t", k=8); b_hT = b_up0
    merged = arena[:, 10240:12288].bitcast(BF16).rearrange("p (k t) -> p k t", k=8); b_merged = alias(Buf("merged"), [b_arena])
    gv = arena[:, 12288:13312].bitcast(BF16).rearrange("p (k t) -> p k t", k=4); b_gv = alias(Buf("gv"), [b_arena])
    tmp = [arena[:, 13312 + i * 512:13312 + (i + 1) * 512] for i in range(5)]
    b_tmp = [alias(Buf(f"tmp{i}"), [b_arena]) for i in range(5)]
    h2b = arena[:, 15872:16384].bitcast(BF16).rearrange("p (k t) -> p k t", k=8); b_h2b = alias(Buf("h2b"), [b_arena])
    wr_f = winu[:].rearrange("p k n -> p (k n)").bitcast(F32)
    A_all = wr_f[:, 0:1024].rearrange("p (t e) -> p t e", e=32); b_Aall = alias(Buf("A_all"), [b_winu])
    h2f = wr_f[:, 1024:2048].rearrange("p (k t) -> p k t", k=8); b_h2f = alias(Buf("h2f"), [b_winu])
    b_x1s = [Buf(f"x1s{i}") for i in range(32)]
    b_h2s = [Buf(f"h2s{i}") for i in range(32)]
    BIG = 1.0e30

    def proj(ft, rhs_ap, brhs):
        piece, off, bp = (winA, (ft - 4) * 128, b_blkA) if ft < 20 else (winB, (ft - 20) * 128, b_winB)
        ps, bps = ps_next()
        for kc in range(8):
            S.op("pe", lambda h, ps=ps, kc=kc, piece=piece, off=off: h.matmul(ps[:, :], lhsT=piece[:, kc, off:off + 128], rhs=rhs_ap(kc),
                                                                             start=(kc == 0), stop=(kc == 7)),
                 reads=[bp, brhs], writes=[bps])
        return ps, bps

    def mm4(wt, bw, dt, rhs_fn, brhs):
        ps, bps = ps_next()
        for ct in range(4):
            S.op("pe", lambda h, ps=ps, ct=ct: h.matmul(ps[:, :], lhsT=wt[:, ct, dt * 128:(dt + 1) * 128], rhs=rhs_fn(ct), start=(ct == 0), stop=(ct == 3)),
                 reads=[bw, brhs], writes=[bps])
        return ps, bps

    def dv(fn, r, w):
        S.op("dve", fn, reads=r, writes=w)

    for tb in range(8):
        t0 = tb * 512
        for tl in range(4):
            xt, bx = load_tile(x_own, pos_own, t0 + tl * 128)
            ln_stats(xt, bx, xn_t, b_xn)
            transpose_mod(xn_t, b_xn, lambda kc, tl=tl: hT[:, kc, tl * 128:(tl + 1) * 128], b_hT, sc1_fn, sh1_fn, [b_modfm])
        hrhs = lambda kc: hT[:, kc, :]
        for ct in range(4):
            pz, bpz = proj(4 + ct, hrhs, b_hT)
            pgc, bpgc = proj(12 + ct, hrhs, b_hT)
            S.op("act", lambda h, pgc=pgc: h.activation(out=tmp[0], in_=pgc[:, :], func=AF.Identity), reads=[bpgc], writes=[b_tmp[0]])
            dv(lambda h, pz=pz: h.tensor_tensor(out=tmp[1], in0=pz[:, :], in1=tmp[0], op=ALU.mult), [bpz, b_tmp[0]], [b_tmp[1]])
            dv(lambda h, ct=ct: h.tensor_scalar(out=tmp[2], in0=tmp[1], scalar1=convw[:, ct, 1:2], scalar2=None, op0=ALU.mult), [b_tmp[1], b_convw], [b_tmp[2]])
            zv = tmp[1].rearrange("p (r c) -> p r c", c=64); vv = tmp[2].rearrange("p (r c) -> p r c", c=64)
            dv(lambda h, ct=ct, zv=zv, vv=vv: h.scalar_tensor_tensor(out=vv[:, :, 1:64], in0=zv[:, :, 0:63], scalar=convw[:, ct, 0:1], in1=vv[:, :, 1:64],
                                                                     op0=ALU.mult, op1=ALU.add), [b_tmp[1], b_tmp[2], b_convw], [b_tmp[2]])
            dv(lambda h, ct=ct, zv=zv, vv=vv: h.scalar_tensor_tensor(out=vv[:, :, 0:63], in0=zv[:, :, 1:64], scalar=convw[:, ct, 2:3], in1=vv[:, :, 0:63],
                                                                     op0=ALU.mult, op1=ALU.add), [b_tmp[1], b_tmp[2], b_convw], [b_tmp[2]])
            pgb, bpgb = proj(8 + ct, hrhs, b_hT)
            dv(lambda h, ct=ct, pgb=pgb: h.tensor_tensor(out=gv[:, ct, :], in0=pgb[:, :], in1=tmp[2], op=ALU.mult), [bpgb, b_tmp[2]], [b_gv])
        for dt in range(8):
            pob, bpob = mm4(convo, b_convo, dt, lambda ct: gv[:, ct, :], b_gv)
            pval, bpval = mm4(wval, b_wglu, dt, lambda ct, t0=t0: ya[:, ct, t0:t0 + 512], b_arena)
            pgt, bpgt = mm4(wgate, b_wglu, dt, lambda ct, t0=t0: ya[:, ct, t0:t0 + 512], b_arena)
            pma, bpma = proj(16 + dt, hrhs, b_hT)
            pmb, bpmb = proj(24 + dt, hrhs, b_hT)
            S.op("act", lambda h, pgt=pgt: h.activation(out=tmp[0], in_=pgt[:, :], func=AF.Sigmoid), reads=[bpgt], writes=[b_tmp[0]])
            S.op("act", lambda h, pma=pma: h.activation(out=tmp[1], in_=pma[:, :], func=AF.Sigmoid), reads=[bpma], writes=[b_tmp[1]])
            S.op("act", lambda h, pmb=pmb: h.activation(out=tmp[2], in_=pmb[:, :], func=AF.Sigmoid), reads=[bpmb], writes=[b_tmp[2]])
            dv(lambda h, pval=pval: h.tensor_tensor(out=tmp[3], in0=pval[:, :], in1=tmp[0], op=ALU.mult), [bpval, b_tmp[0]], [b_tmp[3]])
            dv(lambda h: h.tensor_tensor(out=tmp[3], in0=tmp[3], in1=tmp[1], op=ALU.mult), [b_tmp[3], b_tmp[1]], [b_tmp[3]])
            dv(lambda h, pob=pob: h.tensor_tensor(out=tmp[4], in0=pob[:, :], in1=tmp[2], op=ALU.mult), [bpob, b_tmp[2]], [b_tmp[4]])
            dv(lambda h, dt=dt: h.tensor_tensor(out=merged[:, dt, :], in0=tmp[3], in1=tmp[4], op=ALU.add), [b_tmp[3], b_tmp[4]], [b_merged])
        def w_o_pe(tl):
            outs = []
            for nb in range(2):
                ps, bps = ps_next()
                for dt in range(8):
                    wsl = wo_lo[:, dt, nb * 512:(nb + 1) * 512] if dt < 4 else wo_hi[:, dt - 4, nb * 512:(nb + 1) * 512]
                    S.op("pe", lambda h, ps=ps, dt=dt, tl=tl, wsl=wsl: h.matmul(ps[:, :], lhsT=merged[:, dt, tl * 128:(tl + 1) * 128], rhs=wsl,
                                                                               start=(dt == 0), stop=(dt == 7)),
                         reads=[b_merged, b_wo], writes=[bps])
                outs.append((ps, bps))
            return outs
        tiles_ = [load_tile(x_own, pos_own, (tb * 4) * 128)]
        pend_ = w_o_pe(0)
        for tl in range(4):
            gti = tb * 4 + tl
            r0 = gti * 128
            xt, bx = tiles_[tl]
            cur_ = pend_
            if tl < 3:
                tiles_.append(load_tile(x_own, pos_own, r0 + 128))
                pend_ = w_o_pe(tl + 1)
            for nb in range(2):
                ps, bps = cur_[nb]
                dv(lambda h, ps=ps, nb=nb: h.tensor_tensor(out=xn_t[:, nb * 512:(nb + 1) * 512], in0=ps[:, :], in1=g1bc[:, nb * 512:(nb + 1) * 512], op=ALU.mult),
                   [bps, b_gbc], [b_xn])
            dv(lambda h, xt=xt: h.scalar_tensor_tensor(out=xt[:, :], in0=xt[:, :], scalar=ALPHA, in1=xn_t[:, :], op0=ALU.mult, op1=ALU.add), [bx, b_xn], [bx])
            ln_stats(xt, bx, xn_t, b_xn)
            dv(lambda h: h.tensor_tensor(out=xn_t[:, :], in0=xn_t[:, :], in1=lnp[:, 0:1024], op=ALU.mult), [b_xn, b_lnp], [b_xn])
            dv(lambda h, xt=xt: h.tensor_tensor(out=xt[:, :], in0=xn_t[:, :], in1=lnp[:, 1024:2048], op=ALU.add), [b_xn, b_lnp], [bx])
            S.dma("sp", lambda h, xt=xt, r0=r0: h.dma_start(out=x1s[r0:r0 + 128, :], in_=xt[:, :]), bx, reads=[bx], writes=[b_x1s[gti]])
            ln_stats(xt, bx, xn_t, b_xn)
            for kc in range(8):
                S.op("pe", lambda h, kc=kc: h.transpose(out=psT[:, kc * 128:(kc + 1) * 128], in_=xn_t[:, kc * 128:(kc + 1) * 128], identity=ident[:]),
                     reads=[b_xn, b_ident], writes=[b_psT])
            for kc in range(8):
                S.op("act", lambda h, kc=kc: h.activation(out=h2f[:, kc, :], in_=psT[:, kc * 128:(kc + 1) * 128], func=AF.Identity,
                                                         bias=modfm[:, 24 + kc, 0:1], scale=sc2p[:, kc:kc + 1]),
                     reads=[b_psT, b_modfm], writes=[b_h2f])
            ps, bps = ps_next()
            for kc in range(8):
                S.op("pe", lambda h, ps=ps, kc=kc: h.matmul(ps[:, 0:36], lhsT=h2f[:, kc, :], rhs=rw32[:, kc, :], start=(kc == 0), stop=(kc == 7)),
                     reads=[b_h2f, b_rw], writes=[bps])
            R = lambda a, b_: rt[:, a:b_]
            rr = [b_rt]
            dv(lambda h, ps=ps: h.tensor_tensor(out=R(0, 36), in0=ps[:, 0:36], in1=rb_bc[:, :], op=ALU.add), [bps, b_rb, b_rt], rr)
            dv(lambda h: h.tensor_reduce(out=R(36, 37), in_=R(0, 4), axis=AX.X, op=ALU.max), rr, rr)
            dv(lambda h: h.tensor_scalar(out=R(38, 42), in0=R(0, 4), scalar1=R(36, 37), scalar2=None, op0=ALU.is_equal), rr, rr)
            dv(lambda h: h.tensor_scalar(out=R(37, 38), in0=R(36, 37), scalar1=-1.0, scalar2=None, op0=ALU.mult), rr, rr)
            S.op("act", lambda h: h.activation(out=R(42, 46), in_=R(0, 4), func=AF.Exp, bias=R(37, 38), scale=1.0), reads=rr, writes=rr)
            dv(lambda h: h.tensor_reduce(out=R(46, 47), in_=R(42, 46), axis=AX.X, op=ALU.add), rr, rr)
            dv(lambda h: h.reciprocal(out=R(47, 48), in_=R(46, 47)), rr, rr)
            dv(lambda h: h.tensor_scalar(out=R(48, 52), in0=R(38, 42), scalar1=BIG, scalar2=-BIG, op0=ALU.mult, op1=ALU.add), rr, rr)
            dv(lambda h: h.tensor_tensor(out=R(52, 84).rearrange("p (g e) -> p g e", e=8), in0=R(4, 36).rearrange("p (g e) -> p g e", e=8),
                                         in1=mkap(R(48, 52), 0, [[1, 4], [0, 8]]), op=ALU.add), rr, rr)
            dv(lambda h: h.tensor_reduce(out=R(84, 85), in_=R(52, 84), axis=AX.X, op=ALU.max), rr, rr)
            dv(lambda h: h.tensor_scalar(out=R(85, 117), in0=R(52, 84), scalar1=R(84, 85), scalar2=None, op0=ALU.is_equal), rr, rr)
            dv(lambda h: h.scalar_tensor_tensor(out=R(117, 149), in0=R(85, 117), scalar=-BIG, in1=R(52, 84), op0=ALU.mult, op1=ALU.add), rr, rr)
            dv(lambda h: h.tensor_reduce(out=R(149, 150), in_=R(117, 149), axis=AX.X, op=ALU.max), rr, rr)
            dv(lambda h: h.tensor_scalar(out=R(52, 84), in0=R(117, 149), scalar1=R(149, 150), scalar2=None, op0=ALU.is_equal), rr, rr)
            dv(lambda h: h.tensor_tensor(out=R(150, 151), in0=R(149, 150), in1=R(84, 85), op=ALU.subtract), rr, rr)
            S.op("act", lambda h: h.activation(out=R(151, 152), in_=R(150, 151), func=AF.Exp), reads=rr, writes=rr)
            dv(lambda h: h.tensor_scalar(out=R(152, 153), in0=R(151, 152), scalar1=1.0, scalar2=None, op0=ALU.add), rr, rr)
            dv(lambda h: h.reciprocal(out=R(153, 154), in_=R(152, 153)), rr, rr)
            dv(lambda h: h.tensor_tensor(out=R(154, 155), in0=R(151, 152), in1=R(153, 154), op=ALU.mult), rr, rr)
            dv(lambda h: h.tensor_tensor(out=R(155, 156), in0=R(153, 154), in1=R(47, 48), op=ALU.mult), rr, rr)
            dv(lambda h: h.tensor_tensor(out=R(156, 157), in0=R(154, 155), in1=R(47, 48), op=ALU.mult), rr, rr)
            dv(lambda h, gti=gti: h.tensor_tensor(out=A_all[:, gti, :], in0=R(85, 117), in1=R(52, 84), op=ALU.add), rr + [b_Aall], [b_Aall])
            dv(lambda h: h.tensor_tensor(out=R(117, 149), in0=R(85, 117), in1=cst[:, 0:32], op=ALU.mult), rr + [b_cst], rr)
            dv(lambda h, gti=gti: h.tensor_reduce(out=RT[:, gti, 0:1], in_=R(117, 149), axis=AX.X, op=ALU.add), rr + [b_RT], [b_RT])
            dv(lambda h: h.tensor_tensor(out=R(117, 149), in0=R(52, 84), in1=cst[:, 0:32], op=ALU.mult), rr + [b_cst, b_RT], rr)
            dv(lambda h, gti=gti: h.tensor_reduce(out=RT[:, gti, 1:2], in_=R(117, 149), axis=AX.X, op=ALU.add), rr + [b_RT], [b_RT])
            dv(lambda h, gti=gti: h.tensor_copy(out=RT[:, gti, 2:4], in_=R(155, 157)), rr + [b_RT], [b_RT])

    IOA = bass.IndirectOffsetOnAxis
    NBLK = 48
    rA = blockA[:, :].bitcast(F32)
    b_R = alias(Buf("phaseR"), [b_blkA])
    Dt = rA[:, 4096:5120].rearrange("p (t e) -> p t e", e=32)
    OH = rA[:, 5120:6144].rearrange("p (t e) -> p t e", e=32)
    CMP = rA[:, 6144:7680]
    tri = rA[:, 7680:7936]
    sm_ = rA[:, 7936:8192]
    run = sm_[:, 0:32]; nblk = sm_[:, 32:64]; pe_ = sm_[:, 64:96]; psr = sm_[:, 96:128]; ones32 = sm_[:, 128:160]
    blkE = sm_[:, 160:208]; tE = sm_[:, 208:256]
    S.dma("sp", lambda h: h.dma_start(out=tri, in_=tri_in), b_R, writes=[b_R])
    rdv = lambda fn, extra=(): S.op("dve", fn, reads=[b_R] + list(extra), writes=[b_R])
    pT1, pT2 = ps_next(), ps_next()
    for hf_ in range(2):
        asl = A_all[:, hf_ * 16:(hf_ + 1) * 16, :].rearrange("p t e -> p (t e)")
        S.op("pe", lambda h, hf_=hf_, asl=asl: h.matmul(psT[:, hf_ * 512:(hf_ + 1) * 512], lhsT=tri[:, 0:128], rhs=asl, start=True, stop=True),
             reads=[b_R, b_Aall], writes=[b_psT])
        pt, bpt = (pT1, pT2)[hf_]
        S.op("pe", lambda h, pt=pt, asl=asl: h.matmul(pt[:, :], lhsT=tri[:, 128:256], rhs=asl, start=True, stop=True),
             reads=[b_R, b_Aall], writes=[bpt])
    rdv(lambda h: h.memset(run, 0.0))
    rdv(lambda h: h.memset(ones32, 1.0))
    for t in range(32):
        pt, bpt = (pT1, pT2)[t // 16]
        tt_ = t % 16
        rdv(lambda h, t=t: h.tensor_tensor(out=Dt[:, t, :], in0=psT[:, t * 32:(t + 1) * 32], in1=run, op=ALU.add), [b_psT])
        rdv(lambda h, pt=pt, tt_=tt_: h.tensor_tensor(out=run, in0=pt[:, tt_ * 32:(tt_ + 1) * 32], in1=run, op=ALU.add), [bpt])
    cmp8 = CMP[:, 0:256].rearrange("p (e j) -> p e j", j=8)
    rdv(lambda h: h.tensor_tensor(out=cmp8, in0=mkap(run, 0, [[1, 32], [0, 8]]), in1=mkap(cst[:, 72:80], 0, [[0, 32], [1, 8]]), op=ALU.is_gt), [b_cst])
    rdv(lambda h: h.tensor_reduce(out=nblk, in_=cmp8, axis=AX.X, op=ALU.add))
    rdv(lambda h: h.tensor_tensor_scan(out=pe_, data0=ones32, data1=nblk, initial=0.0, op0=ALU.mult, op1=ALU.add))
    rdv(lambda h: h.tensor_tensor(out=psr, in0=pe_, in1=nblk, op=ALU.subtract))
    rdv(lambda h: h.tensor_scalar(out=psr, in0=psr, scalar1=512.0, scalar2=None, op0=ALU.mult))
    rdv(lambda h: h.tensor_tensor(out=Dt, in0=Dt, in1=mkap(psr, 0, [[0, 32], [1, 32]]), op=ALU.add))
    destF = CMP[:, 256:320].rearrange("p (t f) -> p t f", f=2)
    for k in range(2):
        rdv(lambda h, k=k: h.tensor_tensor(out=OH, in0=mkap(cst[:, 0:32], 0, [[0, 32], [1, 32]]), in1=mkap(RT[:, 0, k:k + 1], 0, [[4, 32], [0, 32]]), op=ALU.is_equal),
            [b_cst, b_RT])
        rdv(lambda h: h.tensor_tensor(out=OH, in0=OH, in1=Dt, op=ALU.mult))
        rdv(lambda h, k=k: h.tensor_reduce(out=destF[:, :, k], in_=OH, axis=AX.X, op=ALU.add))
    S.op("dve", lambda h: h.tensor_copy(out=DEST, in_=destF), reads=[b_R], writes=[b_DEST])
    cmpb = CMP[:, 0:1536].rearrange("p (b e) -> p b e", e=32)
    rdv(lambda h: h.tensor_tensor(out=cmpb, in0=mkap(pe_, 0, [[0, NBLK], [1, 32]]), in1=mkap(cst[:, 0:NBLK], 0, [[1, NBLK], [0, 32]]), op=ALU.is_le), [b_cst, b_DEST])
    rdv(lambda h: h.tensor_reduce(out=blkE, in_=cmpb, axis=AX.X, op=ALU.add))
    rdv(lambda h: h.tensor_scalar(out=blkE, in0=blkE, scalar1=31.0, scalar2=None, op0=ALU.min))
    gif = CMP[:, 0:576]
    gif_g = gif[:, 0:384].rearrange("p (b k) -> p b k", k=8); gif_d = gif[:, 384:576].rearrange("p (b k) -> p b k", k=4)
    rdv(lambda h: h.tensor_scalar(out=tE, in0=blkE, scalar1=1024.0, scalar2=None, op0=ALU.mult))
    rdv(lambda h: h.tensor_tensor(out=gif_g, in0=mkap(tE, 0, [[1, NBLK], [0, 8]]), in1=mkap(cst[:, 64:72], 0, [[0, NBLK], [1, 8]]), op=ALU.add), [b_cst])
    rdv(lambda h: h.tensor_scalar(out=tE, in0=blkE, scalar1=512.0, scalar2=None, op0=ALU.mult))
    rdv(lambda h: h.tensor_tensor(out=gif_d, in0=mkap(tE, 0, [[1, NBLK], [0, 4]]), in1=mkap(cst[:, 64:68], 0, [[0, NBLK], [1, 4]]), op=ALU.add), [b_cst])
    GI = wr_f[:, 1024:1600].bitcast(I32); b_GI = alias(Buf("GI"), [b_h2f])
    GI_g = GI[:, 0:384].rearrange("p (b k) -> p b k", k=8); GI_d = GI[:, 384:576].rearrange("p (b k) -> p b k", k=4)
    S.op("dve", lambda h: h.tensor_copy(out=GI, in_=gif), reads=[b_R], writes=[b_GI])

    if debug == "d":
        dbgr = nc.dram_tensor("dbgr", [128, 32 * 4 + 64 + 576], F32, kind="ExternalOutput").ap()
        dd = rA[:, 4096:4096 + 768]
        S.op("dve", lambda h: h.tensor_copy(out=dd[:, 0:128], in_=RT.rearrange("p t f -> p (t f)")), reads=[b_R, b_RT], writes=[b_R])
        S.op("dve", lambda h: h.tensor_copy(out=dd[:, 128:192], in_=DEST.rearrange("p t f -> p (t f)")), reads=[b_R, b_DEST], writes=[b_R])
        S.op("dve", lambda h: h.tensor_copy(out=dd[:, 192:768], in_=GI), reads=[b_R, b_GI], writes=[b_R])
        S.dma("sp", lambda h: h.dma_start(out=dbgr, in_=dd), b_R, reads=[b_R])
        S.final_wait("sp", b_x1s + [b_R])
        S.emit()
        return nc

    xs = nc.dram_tensor("xs", [NBLK * 512, D], F32, kind="Internal").ap()
    ybd = nc.dram_tensor("ybd", [NBLK * 512, D], F32, kind="Internal").ap()
    b_xs = Buf("xs")
    for t in range(32):
        r0 = t * 128
        i = xctr[0] % 2
        xctr[0] += 1
        xt, bx = xt_t[i], b_xt[i]
        S.dma("sp", lambda h, xt=xt, r0=r0: h.dma_start(out=xt[:], in_=x1s[r0:r0 + 128, :]), bx, reads=[b_x1s[t]], writes=[bx])
        ln_stats(xt, bx, xn_t, b_xn)
        for k in range(2):
            S.dma("pool", lambda h, t=t, k=k: h.indirect_dma_start(out=xs[:, :], out_offset=IOA(ap=DEST[:, t, k:k + 1], axis=0), in_=xn_t[:, :], in_offset=None),
                  b_xs, reads=[b_xn, b_DEST], writes=[b_xs])

    lnp_e = lnp
    S.dma("sp", lambda h: h.dma_start(out=lnp_e, in_=lnp_in[:, 2048:4096].partition_broadcast(128)), b_lnp, writes=[b_lnp])
    xinb = [arena[:, i * 4096:(i + 1) * 4096].rearrange("p (t d) -> p t d", d=1024) for i in range(2)]
    b_xin = [alias(Buf(f"xin{i}"), [b_arena, b_up0, b_merged, b_gv, b_h2b] + b_tmp) for i in range(2)]
    ybuf = [arena[:, 8192 + i * 4096:8192 + (i + 1) * 4096].rearrange("p (t d) -> p t d", d=1024) for i in range(2)]
    b_ybuf = [alias(Buf(f"ybuf{i}"), [b_arena, b_up0, b_merged, b_gv, b_h2b] + b_tmp) for i in range(2)]
    b_ybd = [Buf("ybd0"), Buf("ybd1")]
    hblk = [blockA[:, i * 4096:(i + 1) * 4096].rearrange("p (k t) -> p k t", k=8) for i in range(2)]
    b_hblk = [alias(Buf(f"hblk{i}"), [b_blkA]) for i in range(2)]
    wbuf = [
        (flat(PTre).rearrange("p (k n) -> p k n", k=8), flat(PTim).rearrange("p (k n) -> p k n", k=8), flat(Qre).rearrange("p (k n) -> p k n", k=4)),
        (flat(Qimn).rearrange("p (k n) -> p k n", k=8), flat(D0m).rearrange("p (k n) -> p k n", k=8),
         blockB[:, 0:2048].bitcast(BF16).rearrange("p (k n) -> p k n", k=4)),
    ]
    b_wb = [alias(Buf("wb0"), [b_wglu, b_convo]), alias(Buf("wb1"), [b_wo, b_winB])]
    hid = [blockB[:, 2048 + i * 1024:2048 + (i + 1) * 1024].bitcast(BF16).rearrange("p (k t) -> p k t", k=4) for i in range(2)]
    b_hid = [alias(Buf(f"hid{i}"), [b_winB]) for i in range(2)]
    stmp = [blockB[:, 4096 + i * 512:4096 + (i + 1) * 512] for i in range(2)]
    b_stmp = [alias(Buf(f"stmp{i}"), [b_winB]) for i in range(2)]
    bregs = {}

    def breg(h, v):
        if v not in bregs:
            bregs[v] = h.to_reg(v)
        return bregs[v]
    wg_flat = wg_in.rearrange("e k n -> (e k) n"); wu_flat = wu_in.rearrange("e k n -> (e k) n"); wd_flat = wd_in.rearrange("e k n -> (e k) n")
    for b in range(NBLK):
        par = b % 2
        xi, bxi = xinb[par], b_xin[par]
        S.dma("sp", lambda h, b=b, xi=xi: h.dma_start(out=xi, in_=xs[b * 512:(b + 1) * 512, :].rearrange("(t p) d -> p t d", p=128)), bxi,
              reads=[b_xs], writes=[bxi])
        wg_t, wu_t, wd_t = wbuf[par]
        bw = b_wb[par]
        for kc in range(8):
            S.dma("pool", lambda h, b=b, kc=kc, wg_t=wg_t: h.indirect_dma_start(out=wg_t[:, kc, :], out_offset=None, in_=wg_flat[:, :],
                                                                             in_offset=IOA(ap=GI_g[:, b, kc:kc + 1], axis=0)), bw, reads=[b_GI], writes=[bw])
            S.dma("pool", lambda h, b=b, kc=kc, wu_t=wu_t: h.indirect_dma_start(out=wu_t[:, kc, :], out_offset=None, in_=wu_flat[:, :],
                                                                             in_offset=IOA(ap=GI_g[:, b, kc:kc + 1], axis=0)), bw, reads=[b_GI], writes=[bw])
        for fc in range(4):
            S.dma("pool", lambda h, b=b, fc=fc, wd_t=wd_t: h.indirect_dma_start(out=wd_t[:, fc, :], out_offset=None, in_=wd_flat[:, :],
                                                                             in_offset=IOA(ap=GI_d[:, b, fc:fc + 1], axis=0)), bw, reads=[b_GI], writes=[bw])
        hbk, bhbk = hblk[par], b_hblk[par]
        for tl in range(4):
            for kc in range(8):
                S.op("pe", lambda h, xi=xi, tl=tl, kc=kc: h.transpose(out=psT[:, kc * 128:(kc + 1) * 128], in_=xi[:, tl, kc * 128:(kc + 1) * 128], identity=ident[:]),
                     reads=[bxi, b_ident], writes=[b_psT])
            for kc in range(8):
                S.op("act", lambda h, hbk=hbk, tl=tl, kc=kc: h.activation(out=hbk[:, kc, tl * 128:(tl + 1) * 128], in_=psT[:, kc * 128:(kc + 1) * 128], func=AF.Identity,
                                                                         bias=modfm[:, 24 + kc, 0:1], scale=sc2p[:, kc:kc + 1]),
                     reads=[b_psT, b_modfm], writes=[bhbk])
        hb, bhb = hid[par], b_hid[par]
        for fc in range(4):
            pg, bpg = ps_next()
            for kc in range(8):
                S.op("pe", lambda h, pg=pg, kc=kc, fc=fc, wg_t=wg_t, hbk=hbk: h.matmul(pg[:, :], lhsT=wg_t[:, kc, fc * 128:(fc + 1) * 128], rhs=hbk[:, kc, :],
                                                                                      start=(kc == 0), stop=(kc == 7)), reads=[bw, bhbk], writes=[bpg])
            pu, bpu = ps_next()
            for kc in range(8):
                S.op("pe", lambda h, pu=pu, kc=kc, fc=fc, wu_t=wu_t, hbk=hbk: h.matmul(pu[:, :], lhsT=wu_t[:, kc, fc * 128:(fc + 1) * 128], rhs=hbk[:, kc, :],
                                                                                      start=(kc == 0), stop=(kc == 7)), reads=[bw, bhbk], writes=[bpu])
            st_, bst_ = stmp[fc % 2], b_stmp[fc % 2]
            S.op("act", lambda h, pg=pg, st_=st_: h.activation(out=st_, in_=pg[:, :], func=AF.Silu), reads=[bpg], writes=[bst_])
            dv(lambda h, pu=pu, st_=st_, hb=hb, fc=fc: h.tensor_tensor(out=hb[:, fc, :], in0=pu[:, :], in1=st_, op=ALU.mult), [bpu, bst_], [bhb])
        yb_, byb_ = ybuf[par], b_ybuf[par]
        for tl in range(4):
            for nb in range(2):
                pd, bpd = ps_next()
                for fc in range(4):
                    S.op("pe", lambda h, pd=pd, fc=fc, tl=tl, nb=nb, hb=hb, wd_t=wd_t: h.matmul(pd[:, :], lhsT=hb[:, fc, tl * 128:(tl + 1) * 128],
                                                                                             rhs=wd_t[:, fc, nb * 512:(nb + 1) * 512], start=(fc == 0), stop=(fc == 3)),
                         reads=[bhb, bw], writes=[bpd])
                eng = "act" if (tl * 2 + nb) % 2 == 0 else "dve"
                if eng == "act":
                    S.op("act", lambda h, pd=pd, yb_=yb_, tl=tl, nb=nb: h.activation(out=yb_[:, tl, nb * 512:(nb + 1) * 512], in_=pd[:, :], func=AF.Identity),
                         reads=[bpd], writes=[byb_])
                else:
                    dv(lambda h, pd=pd, yb_=yb_, tl=tl, nb=nb: h.tensor_copy(out=yb_[:, tl, nb * 512:(nb + 1) * 512], in_=pd[:, :]), [bpd], [byb_])
        S.dma("sp", lambda h, b=b, yb_=yb_: h.dma_start(out=ybd[b * 512:(b + 1) * 512, :].rearrange("(t p) d -> p t d", p=128), in_=yb_), byb_,
              reads=[byb_], writes=[b_ybd[par]])

    y12 = [rA[:, 4096 + i * 1024:4096 + (i + 1) * 1024] for i in range(2)]
    b_y12 = [alias(Buf(f"y12_{i}"), [b_R]) for i in range(2)]
    b_out = [Buf(f"out{i}") for i in range(32)]
    for t in range(32):
        r0 = t * 128
        i = xctr[0] % 2
        xctr[0] += 1
        xt, bx = xt_t[i], b_xt[i]
        S.dma("sp", lambda h, xt=xt, r0=r0: h.dma_start(out=xt[:], in_=x1s[r0:r0 + 128, :]), bx, reads=[b_x1s[t]], writes=[bx])
        for k in range(2):
            S.dma("pool", lambda h, t=t, k=k: h.indirect_dma_start(out=y12[k], out_offset=None, in_=ybd[:, :], in_offset=IOA(ap=DEST[:, t, k:k + 1], axis=0)),
                  b_y12[k], reads=[b_ybd[0], b_ybd[1], b_DEST], writes=[b_y12[k]])
        dv(lambda h, t=t: h.tensor_scalar(out=y12[0], in0=y12[0], scalar1=RT[:, t, 2:3], scalar2=None, op0=ALU.mult), [b_y12[0], b_RT], [b_y12[0]])
        dv(lambda h, t=t: h.scalar_tensor_tensor(out=y12[0], in0=y12[1], scalar=RT[:, t, 3:4], in1=y12[0], op0=ALU.mult, op1=ALU.add), [b_y12[0], b_y12[1], b_RT], [b_y12[0]])
        dv(lambda h: h.tensor_tensor(out=y12[0], in0=y12[0], in1=g2bc, op=ALU.mult), [b_y12[0], b_gbc], [b_y12[0]])
        dv(lambda h, xt=xt: h.scalar_tensor_tensor(out=xt[:, :], in0=xt[:, :], scalar=ALPHA, in1=y12[0], op0=ALU.mult, op1=ALU.add), [bx, b_y12[0]], [bx])
        ln_stats(xt, bx, xn_t, b_xn)
        dv(lambda h: h.tensor_tensor(out=xn_t[:, :], in0=xn_t[:, :], in1=lnp_e[:, 0:1024], op=ALU.mult), [b_xn, b_lnp], [b_xn])
        dv(lambda h, xt=xt: h.tensor_tensor(out=xt[:, :], in0=xn_t[:, :], in1=lnp_e[:, 1024:2048], op=ALU.add), [b_xn, b_lnp], [bx])
        S.dma("sp", lambda h, xt=xt, r0=r0: h.dma_start(out=out_d[r0:r0 + 128, :], in_=xt[:, :]), bx, reads=[bx], writes=[b_out[t]])
    S.final_wait("sp", b_out)
    S.emit()
    return nc


def host_inputs(inputs):
    f32 = np.float32
    g = {k: np.asarray(v) for k, v in inputs.items()}
    D_ = 1024
    q = D_ // 4
    omega = (1.0 / (10000.0 ** (np.arange(q, dtype=f32) / f32(q)))).astype(f32)
    r = (np.arange(128, dtype=f32)[:, None] * omega).astype(f32)
    cl = (np.arange(64, dtype=f32)[:, None] * omega).astype(f32)
    r_emb = np.concatenate([np.sin(r), np.cos(r)], -1).astype(f32)
    c_emb = np.concatenate([np.sin(cl), np.cos(cl)], -1).astype(f32)
    pos = np.concatenate([np.broadcast_to(r_emb[:, None, :], (128, 64, 2 * q)),
                          np.broadcast_to(c_emb[None, :, :], (128, 64, 2 * q))], -1).reshape(8192, D_).astype(f32)
    ident = np.eye(128, dtype=f32)
    ii = np.arange(128) // 16
    mF = (ii[None, :] >= ii[:, None]).astype(f32)
    mB = (ii[:, None] >= ii[None, :]).astype(f32)
    masks = np.stack([np.tile(mF, (1, 4)), np.tile(mB, (1, 4))], 1).astype(f32)
    cst = np.zeros((128, 192), f32)
    cst[:, 0:64] = np.arange(64, dtype=f32)[None, :]
    cst[:, 64:72] = np.arange(8, dtype=f32)[None, :] * 128.0 + np.arange(128, dtype=f32)[:, None]
    cst[:, 72:80] = np.arange(8, dtype=f32)[None, :] * 512.0
    tri = np.zeros((128, 256), f32)
    tri[:, 0:128] = (np.arange(128)[:, None] < np.arange(128)[None, :]).astype(f32)
    tri[:, 128:256] = 1.0

    def tr(a):
        return np.ascontiguousarray(a.T)
    maps = []
    for core in range(8):
        b, hf = core // 2, core % 2
        xb = g["x"][b]
        if hf == 1:
            x_oth, x_own = xb[0:4096], xb[4096:8192]
            p_oth, p_own = pos[0:4096], pos[4096:8192]
            ctxl = g["ctx"][b]
            F, B = "f", "b"
            convw = g["conv_w"][0]
        else:
            x_oth, x_own = xb[4096:8192][::-1], xb[0:4096][::-1]
            p_oth, p_own = pos[4096:8192][::-1], pos[0:4096][::-1]
            ctxl = g["ctx"][b][::-1]
            F, B = "b", "f"
            convw = g["conv_w"][0][::-1]
        cT = np.concatenate([g["c"][b].reshape(8, 128).T, g["c_ctx"].reshape(8, 128).T], 1)
        small = np.zeros((128, 3, 32), f32)
        sbb = np.zeros((128, 2, 32, 16), f32)
        scc = np.zeros((128, 2, 32, 16), f32)
        for li, dname in ((0, F), (1, B)):
            L = slice(li * 64, li * 64 + 64)
            small[L, 0, :] = np.broadcast_to(g["s5_log_dt_" + dname][0][None, :], (64, 32))
            small[L, 1, :] = tr(g["s5_a_re_" + dname][0])
            small[L, 2, :] = tr(g["s5_a_im_" + dname][0])
            sbb[L, 0] = g["s5_b_re_" + dname][0].transpose(1, 0, 2)
            sbb[L, 1] = g["s5_b_im_" + dname][0].transpose(1, 0, 2)
            scc[L, 0] = g["s5_c_re_" + dname][0].transpose(2, 0, 1)
            scc[L, 1] = g["s5_c_im_" + dname][0].transpose(2, 0, 1)
        drep = np.tile(g["s5_d"][0].T, (8, 1))
        m = {
            "x_own": x_own, "x_oth": x_oth, "ctx": ctxl, "pos_own": p_own, "pos_oth": p_oth,
            "cT": cT, "w_ada": g["w_ada"][0], "b_adaT": g["b_ada"][0].reshape(48, 128).T, "b_ada": g["b_ada"][0][None, :],
            "w_in": g["w_in"][0], "s5_small": small, "s5_b": sbb, "s5_c": scc, "s5_drep": drep,
            "masks": masks, "ident": ident, "cst": cst, "tri": tri,
            "w_val": g["s5_w_glu_val"][0], "w_gate": g["s5_w_glu_gate"][0],
            "convwT": convw.reshape(3, 4, 128).transpose(2, 1, 0), "conv_out": g["conv_w_out"][0], "w_o": g["w_o"][0],
            "lnp": np.concatenate([g["ln1_g"][0], g["ln1_b"][0], g["ln2_g"][0], g["ln2_b"][0]])[None, :],
            "rw": np.concatenate([g["router_w_group"][0], g["router_w_expert"][0]], 1),
            "rb": np.concatenate([g["router_b_group"][0], g["router_b_expert"][0]])[None, :],
            "wg": g["exp_w_gate"][0], "wu": g["exp_w_up"][0], "wd": g["exp_w_down"][0],
        }
        maps.append({k: np.ascontiguousarray(v, dtype=f32) for k, v in m.items()})
    return maps


def kernel(**inputs):
    maps = host_inputs(inputs)
    nc = build()
    res = run_bass_kernel_spmd(nc, maps, core_ids=list(range(8)))
    out = np.zeros((4, 8192, 1024), np.float32)
    for core in range(8):
        b, hf = core // 2, core % 2
        o = res.results[core]["out"]
        if hf == 1:
            out[b, 4096:8192] = o
        else:
            out[b, 0:4096] = o[::-1]
    return out
```

```python
import math
import numpy as np
import concourse.bass as bass
import concourse.mybir as mybir
from concourse.bass_utils import run_bass_kernel_spmd

F32 = mybir.dt.float32
BF16 = mybir.dt.bfloat16
I32 = mybir.dt.int32
AF = mybir.ActivationFunctionType
ALU = mybir.AluOpType
AX = mybir.AxisListType

ALPHA = 2.0 ** 0.25
LN_EPS = 1e-6
NT = 4096
D = 1024
NG = 32
GELU_C = 2.0 * math.sqrt(2.0 / math.pi)


class Buf:
    __slots__ = ("name", "writer", "readers", "sem", "semcount")

    def __init__(self, name):
        self.name = name
        self.writer = None
        self.readers = []
        self.sem = None
        self.semcount = 0


class Sched:
    SEM_CAP = 30000

    def __init__(self, nc):
        self.nc = nc
        self.eng = {n: dict(prog=[], sem=None, count=0, waited={}, nsem=0) for n in ("pe", "act", "dve", "pool", "sp")}
        self.nbufsem = 0

    def _engsem(self, E, name):
        if E["sem"] is None or E["count"] >= self.SEM_CAP:
            E["sem"] = self.nc.alloc_semaphore(f"s_{name}_{E['nsem']}")
            E.setdefault("own", set()).add(id(E["sem"]))
            E["nsem"] += 1
            E["count"] = 0
        return E["sem"]

    def _waits(self, E, reads, writes):
        need = {}

        def add(tok):
            if tok is None:
                return
            s, v = tok
            k = id(s)
            if k not in need or need[k][1] < v:
                need[k] = (s, v)
        for b in reads:
            add(b.writer)
        for b in writes:
            add(b.writer)
            for r in b.readers:
                add(r)
        out = []
        for k, (s, v) in need.items():
            if E["waited"].get(k, 0) < v:
                E["waited"][k] = v
                out.append((s, v))
        return out

    def _commit(self, tok, reads, writes):
        for b in writes:
            b.writer = tok
            b.readers = []
        for b in reads:
            if b not in writes:
                b.readers.append(tok)
                if len(b.readers) > 48:
                    d = {}
                    for s, v in b.readers:
                        if id(s) not in d or d[id(s)][1] < v:
                            d[id(s)] = (s, v)
                    b.readers = list(d.values())

    def op(self, name, fn, reads=(), writes=(), mode=None):
        E = self.eng[name]
        waits = self._waits(E, reads, writes)
        if name == "pe":
            if mode is not None and E.get("last_mode") == mode:
                own = E.get("own", set())
                waits = [(s_, v_) for (s_, v_) in waits if id(s_) not in own]
            E["last_mode"] = mode
        sem = self._engsem(E, name)
        E["count"] += 1
        val = E["count"]

        def run(h, waits=waits, fn=fn, sem=sem):
            for s, v in waits:
                h.wait_ge(s, v)
            fn(h).then_inc(sem, 1)
        E["prog"].append(run)
        self._commit((sem, val), reads, writes)

    def dma(self, qname, fn, owner, reads=(), writes=()):
        E = self.eng[qname]
        waits = self._waits(E, reads, writes)
        if owner.sem is None:
            owner.sem = self.nc.alloc_semaphore(f"d_{self.nbufsem}")
            self.nbufsem += 1
        owner.semcount += 16
        sem, val = owner.sem, owner.semcount

        def run(h, waits=waits, fn=fn, sem=sem):
            for s, v in waits:
                h.wait_ge(s, v)
            fn(h).then_inc(sem, 16)
        E["prog"].append(run)
        self._commit((sem, val), reads, writes)

    def final_wait(self, qname, bufs):
        E = self.eng[qname]
        waits = self._waits(E, bufs, ())

        def run(h, waits=waits):
            for s, v in waits:
                h.wait_ge(s, v)
        E["prog"].append(run)

    def emit(self):
        with self.nc.Block() as block:
            @block.tensor
            def _(h):
                for f in self.eng["pe"]["prog"]:
                    f(h)

            @block.scalar
            def _(h):
                for f in self.eng["act"]["prog"]:
                    f(h)

            @block.vector
            def _(h):
                for f in self.eng["dve"]["prog"]:
                    f(h)

            @block.gpsimd
            def _(h):
                for f in self.eng["pool"]["prog"]:
                    f(h)

            @block.sync
            def _(h):
                for f in self.eng["sp"]["prog"]:
                    f(h)


def mkap(base, off_elems, dims):
    return bass.AP(base.tensor, base.offset + off_elems, [list(base.ap[0])] + [list(d) for d in dims])


def build(debug=None):
    nc = bass.Bass("TRN2", target_bir_lowering=False)
    S = Sched(nc)

    def din(name, shape, dt=F32):
        return nc.dram_tensor(name, list(shape), dt, kind="ExternalInput").ap()

    x_own = din("x_own", [NT, D]); x_oth = din("x_oth", [NT, D]); ctx_in = din("ctx", [256, D])
    pos_own = din("pos_own", [NT, D]); pos_oth = din("pos_oth", [NT, D])
    cT_in = din("cT", [128, 16])
    w_ada = din("w_ada", [D, 6 * D]); b_adaT = din("b_adaT", [128, 48]); b_ada = din("b_ada", [1, 6 * D])
    w_in = din("w_in", [D, 4096])
    s5_small = din("s5_small", [128, 3, NG])
    s5_b = din("s5_b", [128, 2, NG, 16]); s5_c = din("s5_c", [128, 2, NG, 16]); s5_drep = din("s5_drep", [128, NG])
    masks_in = din("masks", [128, 2, 512]); ident_in = din("ident", [128, 128]); cst_in = din("cst", [128, 192]); tri_in = din("tri", [128, 256])
    w_val = din("w_val", [512, D]); w_gate = din("w_gate", [512, D])
    convw_in = din("convwT", [128, 4, 3]); conv_out = din("conv_out", [512, D]); w_o = din("w_o", [D, D])
    lnp_in = din("lnp", [1, 4 * D])
    rw_in = din("rw", [D, 36]); rb_in = din("rb", [1, 36])
    wg_in = din("wg", [32, D, 512]); wu_in = din("wu", [32, D, 512]); wd_in = din("wd", [32, 512, D])
    out_d = nc.dram_tensor("out", [NT, D], F32, kind="ExternalOutput").ap()
    x1s = nc.dram_tensor("x1s", [NT, D], F32, kind=("ExternalOutput" if debug else "Internal")).ap()
    h2s = nc.dram_tensor("h2s", [128, 8, NT], BF16, kind=("ExternalOutput" if debug else "Internal")).ap()
    wrs = nc.dram_tensor("wrs", [128, 32, 32], F32, kind="ExternalOutput").ap() if debug else None
    dbg_d = None
    if debug == "ya":
        dbg_d = nc.dram_tensor("dbg", [128, 4 * NT], BF16, kind="ExternalOutput").ap()

    def sb(name, shape, dt=F32):
        return nc.alloc_sbuf_tensor("sb_" + name, list(shape), dt)

    ident = sb("ident", [128, 128]); b_ident = Buf("ident")
    identb = sb("identb", [128, 128], BF16); b_identb = Buf("identb")
    masks = sb("masks", [128, 2, 512]); b_masks = Buf("masks")
    modfm = sb("modfm", [128, 48, 2]); b_modfm = Buf("modfm")
    sc1p = sb("sc1p", [128, 8, 2]); sc2p = sb("sc2p", [128, 8])
    convw = sb("convw", [128, 4, 3]); b_convw = Buf("convw")
    rb_bc = sb("rb_bc", [128, 36]); b_rb = Buf("rb")
    epst = sb("epst", [128, 1]); b_eps = Buf("eps")

    S.dma("sp", lambda h: h.dma_start(out=ident[:], in_=ident_in), b_ident, writes=[b_ident])
    S.dma("sp", lambda h: h.dma_start(out=masks[:], in_=masks_in), b_masks, writes=[b_masks])
    S.dma("sp", lambda h: h.dma_start(out=convw[:], in_=convw_in), b_convw, writes=[b_convw])
    S.dma("sp", lambda h: h.dma_start(out=rb_bc[:], in_=rb_in.partition_broadcast(128)), b_rb, writes=[b_rb])
    S.op("dve", lambda h: h.tensor_copy(out=identb[:], in_=ident[:]), reads=[b_ident], writes=[b_identb])
    S.op("dve", lambda h: h.memset(epst[:], LN_EPS), writes=[b_eps])

    psg = [nc.alloc_psum_tensor(f"psg{i}", [128, 512], F32) for i in range(5)]
    b_psg = [Buf(f"psg{i}") for i in range(5)]
    psT = nc.alloc_psum_tensor("psT", [128, 1024], F32); b_psT = Buf("psT")
    psB = nc.alloc_psum_tensor("psB", [128, 1024], BF16); b_psB = Buf("psB")
    pctr = [0]

    def ps_next():
        i = pctr[0] % 5
        pctr[0] += 1
        return psg[i], b_psg[i]

    cT = sb("cT", [128, 16]); b_cT = Buf("cT")
    csil = sb("csil", [128, 16]); b_csil = Buf("csil")
    csil2 = sb("csil2", [128, 8, 2]); b_csil2 = Buf("csil2")
    blockA = sb("blockA", [128, 16384], BF16)
    b_hTb = Buf("hTb")
    badT = sb("badT", [128, 48]); b_badT = Buf("badT")
    S.dma("sp", lambda h: h.dma_start(out=cT[:], in_=cT_in), b_cT, writes=[b_cT])
    S.dma("sp", lambda h: h.dma_start(out=badT[:], in_=b_adaT), b_badT, writes=[b_badT])
    S.op("act", lambda h: h.activation(out=csil[:], in_=cT[:], func=AF.Silu), reads=[b_cT], writes=[b_csil])
    S.op("dve", lambda h: h.tensor_copy(out=csil2[:, :, 0], in_=csil[:, 0:8]), reads=[b_csil], writes=[b_csil2])
    S.op("dve", lambda h: h.tensor_copy(out=csil2[:, :, 1], in_=csil[:, 8:16]), reads=[b_csil], writes=[b_csil2])

    arena = sb("arena", [128, 16384])
    b_arena = Buf("arena")
    wsec = arena[:, 0:8192].rearrange("p (k n) -> p k n", k=8)
    for sec in (0, 1, 3, 4):
        S.dma("sp", lambda h, sec=sec: h.dma_start(out=wsec, in_=w_ada[:, sec * D:(sec + 1) * D].rearrange("(k p) n -> p k n", p=128)),
              b_arena, writes=[b_arena])
        ps, bps = ps_next()
        for jj in range(8):
            for kc in range(8):
                S.op("pe", lambda h, ps=ps, kc=kc, jj=jj: h.matmul(ps[:, jj * 2:jj * 2 + 2], lhsT=wsec[:, kc, jj * 128:(jj + 1) * 128],
                                                                  rhs=csil2[:, kc, :], start=(kc == 0), stop=(kc == 7)),
                     reads=[b_csil2, b_arena], writes=[bps])
        S.op("dve", lambda h, ps=ps, sec=sec: h.tensor_tensor(
            out=modfm[:, sec * 8:(sec + 1) * 8, :], in0=ps[:, 0:16].rearrange("p (j t) -> p j t", t=2),
            in1=mkap(badT[:, sec * 8:(sec + 1) * 8], 0, [[1, 8], [0, 2]]), op=ALU.add),
            reads=[bps, b_badT], writes=[b_modfm])
    S.op("dve", lambda h: h.tensor_scalar(out=sc1p[:], in0=modfm[:, 8:16, :], scalar1=1.0, scalar2=None, op0=ALU.add), reads=[b_modfm], writes=[b_modfm])
    S.op("dve", lambda h: h.tensor_scalar(out=sc2p[:], in0=modfm[:, 32:40, 0], scalar1=1.0, scalar2=None, op0=ALU.add), reads=[b_modfm], writes=[b_modfm])

    xt_t = [sb(f"xt{i}", [128, D]) for i in range(2)]; b_xt = [Buf(f"xt{i}") for i in range(2)]
    xn_t = sb("xn", [128, D]); b_xn = Buf("xn")
    stt = sb("stt", [128, 16]); b_stt = Buf("stt")
    xctr = [0]

    def ln_stats(src, bsrc, dst, bdst):
        S.op("dve", lambda h: h.bn_stats(out=stt[:, 0:6], in_=src[:, 0:512]), reads=[bsrc], writes=[b_stt])
        S.op("dve", lambda h: h.bn_stats(out=stt[:, 6:12], in_=src[:, 512:1024]), reads=[bsrc], writes=[b_stt])
        S.op("dve", lambda h: h.bn_aggr(out=stt[:, 12:14], in_=stt[:, 0:12]), reads=[b_stt], writes=[b_stt])
        S.op("act", lambda h: h.activation(out=stt[:, 14:15], in_=stt[:, 13:14], func=AF.Sqrt, bias=epst[:, 0:1], scale=1.0),
             reads=[b_stt, b_eps], writes=[b_stt])
        S.op("dve", lambda h: h.reciprocal(out=stt[:, 15:16], in_=stt[:, 14:15]), reads=[b_stt], writes=[b_stt])
        S.op("dve", lambda h: h.tensor_scalar(out=dst[:, :], in0=src[:, :], scalar1=stt[:, 12:13], scalar2=stt[:, 15:16],
                                              op0=ALU.subtract, op1=ALU.mult), reads=[bsrc, b_stt], writes=[bdst])

    def transpose_mod(src, bsrc, dst_fn, bdst, scale_fn, shift_fn, bmods):
        for kc in range(8):
            S.op("pe", lambda h, kc=kc: h.transpose(out=psT[:, kc * 128:(kc + 1) * 128], in_=src[:, kc * 128:(kc + 1) * 128], identity=ident[:]),
                 reads=[bsrc, b_ident], writes=[b_psT])
        for kc in range(8):
            S.op("act", lambda h, kc=kc: h.activation(out=dst_fn(kc), in_=psT[:, kc * 128:(kc + 1) * 128], func=AF.Identity,
                                                     bias=shift_fn(kc), scale=scale_fn(kc)),
                 reads=[b_psT] + bmods, writes=[bdst])

    def load_tile(xsrc, possrc, r0):
        i = xctr[0] % 2
        xctr[0] += 1
        xt, bx = xt_t[i], b_xt[i]
        S.dma("sp", lambda h: h.dma_start(out=xt[:], in_=xsrc[r0:r0 + 128, :]), bx, writes=[bx])
        if possrc is not None:
            S.dma("pool", lambda h: h.dma_start(out=xt[:], in_=possrc[r0:r0 + 128, :], accum_op=ALU.add), bx, writes=[bx])
        return xt, bx

    blockB = sb("blockB", [128, 10240])
    sm = sb("s5sm", [128, 3, NG]); b_sm = Buf("s5sm")
    sbv = blockB[:, 0:1024].rearrange("p (r g c) -> p r g c", r=2, g=NG); scv = blockB[:, 1024:2048].rearrange("p (r g c) -> p r g c", r=2, g=NG); b_bc = Buf("s5bc")
    drep = sb("drep", [128, NG]); b_drep = Buf("drep")
    S.dma("sp", lambda h: h.dma_start(out=sm[:], in_=s5_small), b_sm, writes=[b_sm])
    S.dma("sp", lambda h: h.dma_start(out=sbv[:], in_=s5_b), b_bc, writes=[b_bc])
    S.dma("sp", lambda h: h.dma_start(out=scv[:], in_=s5_c), b_bc, writes=[b_bc])
    S.dma("sp", lambda h: h.dma_start(out=drep[:], in_=s5_drep), b_drep, writes=[b_drep])

    tb_ = blockB[:, 2048:2048 + 11 * NG * 9].rearrange("p (t g k) -> p t g k", t=11, g=NG); b_tab = Buf("s5tab")
    T_ANG, T_Q, T_SIN, T_COS, T_MAG, T_MAGN, T_WRE, T_WIM, T_VRE, T_VIM, T_TMP = range(11)
    qi = sb("s5qi", [128, NG * 9], I32)
    vec = sb("s5vec", [128, 12, NG]); b_vec = Buf("s5vec")
    V_DT, V_TH, V_LR, V_DEN, V_XRE, V_FRE, V_FIM, V_T1, V_T2, V_RDEN = range(10)

    def vop(fn, r=(b_vec,), w=(b_vec,)):
        S.op("dve", fn, reads=list(r), writes=list(w))

    S.op("act", lambda h: h.activation(out=vec[:, V_DT, :], in_=sm[:, 0, :], func=AF.Exp), reads=[b_sm], writes=[b_vec])
    vop(lambda h: h.tensor_tensor(out=vec[:, V_TH, :], in0=vec[:, V_DT, :], in1=sm[:, 2, :], op=ALU.mult), r=(b_vec, b_sm))
    vop(lambda h: h.tensor_tensor(out=vec[:, V_LR, :], in0=vec[:, V_DT, :], in1=sm[:, 1, :], op=ALU.mult), r=(b_vec, b_sm))
    T = lambda t: tb_[:, t, :, :]
    Tf = lambda t: tb_[:, t, :, :].rearrange("p g k -> p (g k)")
    for k in range(9):
        S.op("dve", lambda h, k=k: h.tensor_scalar(out=tb_[:, T_ANG, :, k], in0=vec[:, V_TH, :], scalar1=float(k), scalar2=None, op0=ALU.mult),
             reads=[b_vec], writes=[b_tab])
        S.op("act", lambda h, k=k: h.activation(out=tb_[:, T_MAG, :, k], in_=vec[:, V_LR, :], func=AF.Exp, scale=float(k)), reads=[b_vec], writes=[b_tab])
        S.op("act", lambda h, k=k: h.activation(out=tb_[:, T_MAGN, :, k], in_=vec[:, V_LR, :], func=AF.Exp, scale=-float(k)), reads=[b_vec], writes=[b_tab])

    def sin_of(dst_t, shift):
        top = lambda fn: S.op("dve", fn, reads=[b_tab], writes=[b_tab])
        top(lambda h: h.tensor_scalar(out=Tf(T_TMP), in0=Tf(T_ANG), scalar1=shift, scalar2=None, op0=ALU.add))
        top(lambda h: h.tensor_scalar(out=qi[:], in0=Tf(T_TMP), scalar1=1.0 / (2 * math.pi), scalar2=None, op0=ALU.mult))
        top(lambda h: h.tensor_copy(out=Tf(T_Q), in_=qi[:]))
        top(lambda h: h.scalar_tensor_tensor(out=Tf(T_TMP), in0=Tf(T_Q), scalar=-2 * math.pi, in1=Tf(T_TMP), op0=ALU.mult, op1=ALU.add))
        top(lambda h: h.tensor_scalar(out=Tf(T_Q), in0=Tf(T_TMP), scalar1=math.pi, scalar2=2 * math.pi, op0=ALU.is_gt, op1=ALU.mult))
        top(lambda h: h.tensor_tensor(out=Tf(T_TMP), in0=Tf(T_TMP), in1=Tf(T_Q), op=ALU.subtract))
        top(lambda h: h.tensor_scalar(out=Tf(T_Q), in0=Tf(T_TMP), scalar1=-math.pi, scalar2=2 * math.pi, op0=ALU.is_lt, op1=ALU.mult))
        top(lambda h: h.tensor_tensor(out=Tf(T_TMP), in0=Tf(T_TMP), in1=Tf(T_Q), op=ALU.add))
        S.op("act", lambda h: h.activation(out=Tf(dst_t), in_=Tf(T_TMP), func=AF.Sin), reads=[b_tab], writes=[b_tab])

    sin_of(T_SIN, 0.0)
    sin_of(T_COS, math.pi / 2)
    tt = lambda o, a, b, op: S.op("dve", lambda h: h.tensor_tensor(out=Tf(o), in0=Tf(a), in1=Tf(b), op=op), reads=[b_tab], writes=[b_tab])
    tt(T_WRE, T_MAG, T_COS, ALU.mult)
    tt(T_WIM, T_MAG, T_SIN, ALU.mult)
    tt(T_VRE, T_MAGN, T_COS, ALU.mult)
    tt(T_VIM, T_MAGN, T_SIN, ALU.mult)
    S.op("dve", lambda h: h.tensor_scalar(out=Tf(T_VIM), in0=Tf(T_VIM), scalar1=-1.0, scalar2=None, op0=ALU.mult), reads=[b_tab], writes=[b_tab])
    are = sm[:, 1, :]; aim = sm[:, 2, :]
    abre = tb_[:, T_WRE, :, 1]; abim = tb_[:, T_WIM, :, 1]
    vr = (b_vec, b_sm, b_tab)
    vop(lambda h: h.tensor_tensor(out=vec[:, V_DEN, :], in0=are, in1=are, op=ALU.mult), r=vr)
    vop(lambda h: h.tensor_tensor(out=vec[:, V_T1, :], in0=aim, in1=aim, op=ALU.mult), r=vr)
    vop(lambda h: h.tensor_tensor(out=vec[:, V_DEN, :], in0=vec[:, V_DEN, :], in1=vec[:, V_T1, :], op=ALU.add), r=vr)
    vop(lambda h: h.reciprocal(out=vec[:, V_RDEN, :], in_=vec[:, V_DEN, :]), r=vr)
    vop(lambda h: h.tensor_scalar(out=vec[:, V_XRE, :], in0=abre, scalar1=-1.0, scalar2=None, op0=ALU.add), r=vr)
    vop(lambda h: h.tensor_tensor(out=vec[:, V_T1, :], in0=vec[:, V_XRE, :], in1=are, op=ALU.mult), r=vr)
    vop(lambda h: h.tensor_tensor(out=vec[:, V_T2, :], in0=abim, in1=aim, op=ALU.mult), r=vr)
    vop(lambda h: h.tensor_tensor(out=vec[:, V_T1, :], in0=vec[:, V_T1, :], in1=vec[:, V_T2, :], op=ALU.add), r=vr)
    vop(lambda h: h.tensor_tensor(out=vec[:, V_FRE, :], in0=vec[:, V_T1, :], in1=vec[:, V_RDEN, :], op=ALU.mult), r=vr)
    vop(lambda h: h.tensor_tensor(out=vec[:, V_T1, :], in0=abim, in1=are, op=ALU.mult), r=vr)
    vop(lambda h: h.tensor_tensor(out=vec[:, V_T2, :], in0=vec[:, V_XRE, :], in1=aim, op=ALU.mult), r=vr)
    vop(lambda h: h.tensor_tensor(out=vec[:, V_T1, :], in0=vec[:, V_T1, :], in1=vec[:, V_T2, :], op=ALU.subtract), r=vr)
    vop(lambda h: h.tensor_tensor(out=vec[:, V_FIM, :], in0=vec[:, V_T1, :], in1=vec[:, V_RDEN, :], op=ALU.mult), r=vr)
    bbar = blockB[:, 5632:6656].rearrange("p (r g c) -> p r g c", r=2, g=NG); b_bbar = Buf("bbar")
    tmpb = blockB[:, 6656:7168].rearrange("p (g c) -> p g c", g=NG); b_tmpb = Buf("tmpb")
    fre_b = mkap(vec[:, V_FRE, :], 0, [[1, NG], [0, 16]]); fim_b = mkap(vec[:, V_FIM, :], 0, [[1, NG], [0, 16]])
    bo = lambda fn, r, w: S.op("dve", fn, reads=r, writes=w)
    bo(lambda h: h.tensor_tensor(out=bbar[:, 0], in0=sbv[:, 0], in1=fre_b, op=ALU.mult), [b_bc, b_vec], [b_bbar])
    bo(lambda h: h.tensor_tensor(out=tmpb[:], in0=sbv[:, 1], in1=fim_b, op=ALU.mult), [b_bc, b_vec], [b_tmpb])
    bo(lambda h: h.tensor_tensor(out=bbar[:, 0], in0=bbar[:, 0], in1=tmpb[:], op=ALU.subtract), [b_bbar, b_tmpb], [b_bbar])
    bo(lambda h: h.tensor_tensor(out=bbar[:, 1], in0=sbv[:, 1], in1=fre_b, op=ALU.mult), [b_bc, b_vec], [b_bbar])
    bo(lambda h: h.tensor_tensor(out=tmpb[:], in0=sbv[:, 0], in1=fim_b, op=ALU.mult), [b_bc, b_vec], [b_tmpb])
    bo(lambda h: h.tensor_tensor(out=bbar[:, 1], in0=bbar[:, 1], in1=tmpb[:], op=ALU.add), [b_bbar, b_tmpb], [b_bbar])

    WB, WBp, WC = [blockB[:, 7168 + i * 512:7168 + (i + 1) * 512].rearrange("p (r g k) -> p r g k", r=2, g=NG) for i in range(3)]; b_W = Buf("W")

    def fwd_slice(t, lanes, k0):
        return tb_[lanes, t, :, k0:k0 + 8]

    def rev_slice(t, lanes, k_hi):
        base = tb_[lanes, t, :, k_hi:k_hi + 1]
        return mkap(base, 0, [[9, NG], [-1, 8]])
    LF = slice(0, 64); LB = slice(64, 128)
    for ri, (tw, tv) in enumerate(((T_WRE, T_VRE), (T_WIM, T_VIM))):
        cp = lambda o, i_: S.op("dve", lambda h: h.tensor_copy(out=o, in_=i_), reads=[b_tab], writes=[b_W])
        cp(WB[LF, ri], rev_slice(tw, LF, 7))
        cp(WB[LB, ri], fwd_slice(tw, LB, 0))
        cp(WBp[LF, ri], fwd_slice(tv, LF, 1))
        cp(WBp[LB, ri], rev_slice(tv, LB, 8))
        cp(WC[LF, ri], fwd_slice(tw, LF, 1))
        cp(WC[LB, ri], rev_slice(tw, LB, 8))

    D0m = sb("D0m", [128, NG, 128], BF16); b_D0 = Buf("D0")
    PTre = sb("PTre", [128, NG, 128], BF16); PTim = sb("PTim", [128, NG, 128], BF16); b_PT = Buf("PT")
    Qre = sb("Qre", [128, NG, 128], BF16); Qimn = sb("Qimn", [128, NG, 128], BF16); b_Q = Buf("Q")
    A1 = sb("A1", [128, NG, 2]); A2 = sb("A2", [128, NG, 2]); b_A = Buf("A12")
    S.op("dve", lambda h: h.tensor_copy(out=A1[:], in_=mkap(tb_[:, T_WRE, :, 8:9], 0, [[9, NG], [0, 2]])), reads=[b_tab], writes=[b_A])
    S.op("dve", lambda h: h.tensor_copy(out=A2[:, :, 1], in_=tb_[:, T_WIM, :, 8]), reads=[b_tab], writes=[b_A])
    S.op("dve", lambda h: h.tensor_scalar(out=A2[:, :, 0], in0=tb_[:, T_WIM, :, 8], scalar1=-1.0, scalar2=None, op0=ALU.mult), reads=[b_tab], writes=[b_A])

    GC = 8
    gen = arena[:, 0:8192]

    def gslot(i):
        return gen[:, i * 1024:(i + 1) * 1024].rearrange("p (g i c) -> p g i c", g=GC, i=8)

    def cprod(Wt, X, g0, o_re, o_im, breads):
        def wv(ri):
            return mkap(Wt[:, ri, g0:g0 + GC, :], 0, [[8, GC], [1, 8], [0, 16]])

        def xv(ri):
            return mkap(X[:, ri, g0:g0 + GC, :], 0, [[16, GC], [0, 8], [1, 16]])
        t1 = gslot(6); t2 = gslot(7)
        o = lambda fn: S.op("dve", fn, reads=[b_W, b_arena] + breads, writes=[b_arena])
        o(lambda h: h.tensor_tensor(out=t1, in0=wv(0), in1=xv(0), op=ALU.mult))
        o(lambda h: h.tensor_tensor(out=t2, in0=wv(1), in1=xv(1), op=ALU.mult))
        o(lambda h: h.tensor_tensor(out=o_re, in0=t1, in1=t2, op=ALU.subtract))
        o(lambda h: h.tensor_tensor(out=t1, in0=wv(0), in1=xv(1), op=ALU.mult))
        o(lambda h: h.tensor_tensor(out=t2, in0=wv(1), in1=xv(0), op=ALU.mult))
        o(lambda h: h.tensor_tensor(out=o_im, in0=t1, in1=t2, op=ALU.add))

    for gch in range(NG // GC):
        g0 = gch * GC
        Bt_re, Bt_im, Bp_re, Bp_im, Ct_re, Ct_im = [gslot(i) for i in range(6)]
        cprod(WB, bbar, g0, Bt_re, Bt_im, [b_bbar])
        cprod(WBp, bbar, g0, Bp_re, Bp_im, [b_bbar])
        cprod(WC, scv, g0, Ct_re, Ct_im, [b_bc])
        fl = lambda a: a.rearrange("p g i c -> p g (i c)")
        S.op("act", lambda h, g0=g0, Ct_re=Ct_re: h.activation(out=Qre[:, g0:g0 + GC, :], in_=Ct_re.rearrange("p g i c -> p g (i c)"), func=AF.Identity), reads=[b_arena], writes=[b_Q])
        S.op("act", lambda h, g0=g0, Ct_im=Ct_im: h.activation(out=Qimn[:, g0:g0 + GC, :], in_=Ct_im.rearrange("p g i c -> p g (i c)"), func=AF.Identity, scale=-1.0), reads=[b_arena], writes=[b_Q])
        S.op("dve", lambda h, Ct_im=Ct_im: h.tensor_scalar(out=Ct_im.rearrange("p g i c -> p g (i c)"), in0=Ct_im.rearrange("p g i c -> p g (i c)"), scalar1=-1.0, scalar2=None, op0=ALU.mult), reads=[b_arena, b_Q], writes=[b_arena])
        for ri, (Bt, PT) in enumerate(((Bt_re, PTre), (Bt_im, PTim))):
            for gg in range(GC):
                S.op("pe", lambda h, gg=gg, Bt=Bt: h.transpose(out=psT[:, gg * 128:(gg + 1) * 128], in_=Bt.rearrange("p g i c -> p g (i c)")[:, gg, :], identity=ident[:]),
                     reads=[b_arena, b_ident], writes=[b_psT])
            S.op("act", lambda h, PT=PT, g0=g0: h.activation(out=PT[:, g0:g0 + GC, :], in_=psT[:, :].rearrange("p (g m) -> p g m", g=GC), func=AF.Identity),
                 reads=[b_psT], writes=[b_PT])
        for g4 in range(GC // 4):
            pF, bpF = ps_next(); pB, bpB = ps_next()
            for gg in range(4):
                g = g4 * 4 + gg
                for lanes, pp, bpp in ((LF, pF, bpF), (LB, pB, bpB)):
                    S.op("pe", lambda h, g=g, gg=gg, lanes=lanes, pp=pp, Bp_re=Bp_re, Ct_re=Ct_re: h.matmul(pp[:, gg * 128:(gg + 1) * 128], lhsT=Bp_re.rearrange("p g i c -> p g (i c)")[lanes, g, :],
                                                                                  rhs=Ct_re.rearrange("p g i c -> p g (i c)")[lanes, g, :], start=True, stop=False),
                         reads=[b_arena], writes=[bpp])
                    S.op("pe", lambda h, g=g, gg=gg, lanes=lanes, pp=pp, Bp_im=Bp_im, Ct_im=Ct_im: h.matmul(pp[:, gg * 128:(gg + 1) * 128], lhsT=Bp_im.rearrange("p g i c -> p g (i c)")[lanes, g, :],
                                                                                  rhs=Ct_im.rearrange("p g i c -> p g (i c)")[lanes, g, :], start=False, stop=True),
                         reads=[b_arena], writes=[bpp])
            t1 = gslot(6).rearrange("p g i c -> p (g i c)")[:, 0:512]
            t2 = gslot(7).rearrange("p g i c -> p (g i c)")[:, 0:512]
            S.op("dve", lambda h, pF=pF, t1=t1: h.tensor_tensor(out=t1, in0=pF[:, :], in1=masks[:, 0, :], op=ALU.mult), reads=[bpF, b_masks, b_arena], writes=[b_arena])
            S.op("dve", lambda h, pB=pB, t2=t2: h.tensor_tensor(out=t2, in0=pB[:, :], in1=masks[:, 1, :], op=ALU.mult), reads=[bpB, b_masks, b_arena], writes=[b_arena])
            S.op("dve", lambda h, t1=t1, t2=t2: h.tensor_tensor(out=t1, in0=t1, in1=t2, op=ALU.add), reads=[b_arena], writes=[b_arena])
            for gg in range(4):
                g = g0 + g4 * 4 + gg
                S.op("dve", lambda h, g=g, gg=gg, t1=t1: h.scalar_tensor_tensor(out=D0m[:, g, :], in0=ident[:], scalar=drep[:, g:g + 1],
                                                                               in1=t1[:, gg * 128:(gg + 1) * 128], op0=ALU.mult, op1=ALU.add),
                     reads=[b_arena, b_ident, b_drep], writes=[b_D0])

    winu = sb("winu", [128, 8, 512], BF16); b_winu = Buf("winu")
    S.dma("pool", lambda h: h.dma_start(out=winu[:], in_=w_in[:, 0:512].rearrange("(k p) n -> p k n", p=128)), b_winu, writes=[b_winu])
    hTb = blockA[:, 0:4096].rearrange("p (k t) -> p k t", k=8)
    U_sb = blockA[:, 4096:8192]; b_Usb = Buf("U_sb")
    UT = blockB[:, 0:8192].bitcast(BF16).rearrange("p (g j) -> p g j", g=NG); b_UT = Buf("UT")
    UTo = blockA[:, 8192:10240].rearrange("p (g j) -> p g j", g=NG); b_UTo = Buf("UTo")
    Zoth = blockA[:, 10240:14336].rearrange("p (j e) -> p j e", e=64); b_Zoth = Buf("Zoth")
    Zctx = blockA[:, 14336:16384].rearrange("p (j e) -> p j e", e=64); b_Zctx = Buf("Zctx")
    Zown = arena[:, :].bitcast(BF16).rearrange("p (j e) -> p j e", e=64)
    b_Zown = b_arena
    cur = [sb(f"cur{i}", [128, NG, 2]) for i in range(2)]; b_cur = [Buf(f"cur{i}") for i in range(2)]
    st1 = sb("st1", [128, NG, 2]); st2 = sb("st2", [128, NG, 2]); b_st = Buf("st12")
    S.op("dve", lambda h: h.memset(cur[0][:], 0.0), writes=[b_cur[0]])
    scan_k = [0]

    def scan_step(zap, bz, lanes, store):
        k = scan_k[0]
        scan_k[0] += 1
        c0, bc0 = cur[k % 2], b_cur[k % 2]
        c1, bc1 = cur[(k + 1) % 2], b_cur[(k + 1) % 2]
        L = lanes
        sw = mkap(c0[L, :, 1:2], 0, [[2, NG], [-1, 2]])
        S.op("dve", lambda h: h.tensor_tensor(out=st1[L], in0=c0[L], in1=A1[L], op=ALU.mult), reads=[bc0, b_A], writes=[b_st])
        S.op("dve", lambda h: h.tensor_tensor(out=st2[L], in0=sw, in1=A2[L], op=ALU.mult), reads=[bc0, b_A, b_st], writes=[b_st])
        S.op("dve", lambda h: h.tensor_tensor(out=st1[L], in0=st1[L], in1=st2[L], op=ALU.add), reads=[b_st], writes=[b_st])
        S.op("dve", lambda h: h.tensor_tensor(out=c1[L], in0=st1[L], in1=zap, op=ALU.add), reads=[b_st, bz], writes=[bc1])
        if store:
            S.op("dve", lambda h: h.tensor_copy(out=zap, in_=c0[L]), reads=[bc0, bz], writes=[bz])
        if L != slice(0, 128):
            other = slice(64, 128) if L == slice(0, 64) else slice(0, 64)
            S.op("dve", lambda h: h.tensor_copy(out=c1[other], in_=c0[other]), reads=[bc0], writes=[bc1])

    def phaseA_block(xsrc, possrc, t0, ntok, sh_fn, sc_fn, UTdst, bUT, J0):
        ntile = ntok // 128
        nJ = ntok // 8
        for tl in range(ntile):
            xt, bx = load_tile(xsrc, possrc, t0 + tl * 128)
            ln_stats(xt, bx, xn_t, b_xn)
            transpose_mod(xn_t, b_xn, lambda kc, tl=tl: hTb[:, kc, tl * 128:(tl + 1) * 128], b_hTb, sc_fn, sh_fn, [b_modfm])
        for i in range(8):
            ps, bps = ps_next()
            for kc in range(8):
                S.op("pe", lambda h, ps=ps, kc=kc, i=i: h.matmul(ps[0:nJ, :], lhsT=mkap(hTb[:, kc, i:i + 1], 0, [[8, nJ]]), rhs=winu[:, kc, :],
                                                                start=(kc == 0), stop=(kc == 7)),
                     reads=[b_hTb, b_winu], writes=[bps])
            S.op("act", lambda h, ps=ps, i=i: h.activation(out=mkap(U_sb[0:nJ, i * 16:i * 16 + 1], 0, [[128, NG], [1, 16]]), in_=ps[0:nJ, :].rearrange("p (g c) -> p g c", g=NG), func=AF.Identity), reads=[bps], writes=[b_Usb])
        for g8 in range(4):
            for gg in range(8):
                g = g8 * 8 + gg
                S.op("pe", lambda h, g=g, gg=gg: h.transpose(out=psB[:, gg * 128:gg * 128 + nJ], in_=U_sb[0:nJ, g * 128:(g + 1) * 128],
                                                             identity=identb[0:nJ, 0:nJ]),
                     reads=[b_Usb, b_identb], writes=[b_psB])
            S.op("dve", lambda h, g8=g8: h.tensor_copy(out=UTdst[:, g8 * 8:(g8 + 1) * 8, J0:J0 + nJ],
                                                       in_=psB[:, :].rearrange("p (g j) -> p g j", g=8)[:, :, 0:nJ]),
                 reads=[b_psB], writes=[bUT])

    def z_block(UTsrc, bUT, J0, nJ, Zdst_fn, bZ, lanes_list):
        for g in range(NG):
            ps, bps = ps_next()
            S.op("pe", lambda h, ps=ps, g=g: h.matmul(ps[:, 0:nJ], lhsT=PTre[:, g, :], rhs=UTsrc[:, g, J0:J0 + nJ], start=True, stop=True),
                 reads=[b_PT, bUT], writes=[bps])
            S.op("pe", lambda h, ps=ps, g=g: h.matmul(ps[:, 256:256 + nJ], lhsT=PTim[:, g, :], rhs=UTsrc[:, g, J0:J0 + nJ], start=True, stop=True),
                 reads=[b_PT, bUT, bps], writes=[bps])
            for lanes in lanes_list:
                if lanes == LF:
                    S.op("act", lambda h, ps=ps, g=g, lanes=lanes: h.activation(
                        out=Zdst_fn(lanes, g), in_=ps[lanes, :].rearrange("p (r j) -> p r j", r=2)[:, :, 0:nJ], func=AF.Identity),
                        reads=[bps], writes=[bZ])
                else:
                    S.op("dve", lambda h, ps=ps, g=g, lanes=lanes: h.tensor_copy(
                        out=Zdst_fn(lanes, g), in_=ps[lanes, :].rearrange("p (r j) -> p r j", r=2)[:, :, 0:nJ]),
                        reads=[bps], writes=[bZ])

    def zdst(Zt, nJtot, Jbase, nJ):
        def fn(lanes, g):
            if lanes == LF:
                base = Zt[LF, Jbase:Jbase + 1, 2 * g:2 * g + 1]
                return mkap(base, 0, [[1, 2], [64, nJ]])
            base = Zt[LB, nJtot - 1 - Jbase:nJtot - Jbase, 2 * g:2 * g + 1]
            return mkap(base, 0, [[1, 2], [-64, nJ]])
        return fn

    ALL = slice(0, 128)
    sh1_fn = lambda kc: modfm[:, 0 + kc, 0:1]; sc1_fn = lambda kc: sc1p[:, kc, 0:1]
    csh1_fn = lambda kc: modfm[:, 0 + kc, 1:2]; csc1_fn = lambda kc: sc1p[:, kc, 1:2]
    phaseA_block(ctx_in, None, 0, 256, csh1_fn, csc1_fn, UTo, b_UTo, 0)
    z_block(UTo, b_UTo, 0, 32, zdst(Zctx, 32, 0, 32), b_Zctx, [LF, LB])
    for k in range(32):
        scan_step(Zctx[:, k, :].rearrange("p (g r) -> p g r", r=2), b_Zctx, ALL, False)
    for blk in range(8):
        phaseA_block(x_oth, pos_oth, blk * 512, 512, sh1_fn, sc1_fn, UTo, b_UTo, 0)
        z_block(UTo, b_UTo, 0, 64, zdst(Zoth, 64, 0, 64), b_Zoth, [LF])
        for k in range(64):
            scan_step(Zoth[LF, k, :].rearrange("p (g r) -> p g r", r=2), b_Zoth, LF, False)
    for blk in range(8):
        phaseA_block(x_own, pos_own, blk * 512, 512, sh1_fn, sc1_fn, UT, b_UT, blk * 64)
    for jb in range(4):
        z_block(UT, b_UT, jb * 128, 128, zdst(Zown, 512, jb * 128, 128), b_Zown, [LF, LB])
    for k in range(512):
        scan_step(Zown[:, k, :].rearrange("p (g r) -> p g r", r=2), b_Zown, ALL, True)


    Ysb = blockA[:, :].rearrange("p (g j) -> p g j", g=NG); b_Ysb = Buf("Ysb")
    TM = arena[:, 8192:10240].bitcast(BF16).rearrange("p (j c) -> p j c", j=8); b_TM = b_arena
    ya = arena[:, 0:8192].bitcast(BF16).rearrange("p (a t) -> p a t", a=4); b_ya = b_arena
    for g in range(NG):
        ps, bps = ps_next()
        S.op("pe", lambda h, ps=ps, g=g: h.matmul(ps[:, :], lhsT=D0m[:, g, :], rhs=UT[:, g, :], start=True, stop=False), reads=[b_D0, b_UT], writes=[bps])
        for lanes in (LF, LB):
            for ri, Qm in enumerate((Qre, Qimn)):
                if lanes == LF:
                    rhs = mkap(Zown[LF, 0:1, 2 * g + ri:2 * g + ri + 1], 0, [[64, 512]])
                else:
                    rhs = mkap(Zown[LB, 511:512, 2 * g + ri:2 * g + ri + 1], 0, [[-64, 512]])
                last = (lanes == LB and ri == 1)
                S.op("pe", lambda h, ps=ps, g=g, lanes=lanes, Qm=Qm, rhs=rhs, last=last: h.matmul(ps[:, :], lhsT=Qm[lanes, g, :], rhs=rhs, start=False, stop=last),
                     reads=[b_Q, b_Zown, bps], writes=[bps])
        S.op("act", lambda h, ps=ps, g=g: h.activation(out=Ysb[:, g, :], in_=ps[:, :], func=AF.Identity), reads=[bps], writes=[b_Ysb, b_hTb, b_Usb, b_UTo, b_Zoth, b_Zctx])
    gt = [arena[:, 10240 + i * 1024:10240 + (i + 1) * 1024] for i in range(2)]; b_gt = b_arena
    for jb in range(4):
        for g8 in range(4):
            for gg in range(8):
                g = g8 * 8 + gg
                S.op("pe", lambda h, g=g, gg=gg, jb=jb: h.transpose(out=psB[:, gg * 128:(gg + 1) * 128], in_=Ysb[:, g, jb * 128:(jb + 1) * 128], identity=identb[:]),
                     reads=[b_Ysb, b_identb], writes=[b_psB])
            S.op("dve", lambda h, g8=g8: h.tensor_copy(
                out=mkap(TM[:, 0:1, g8 * 128:g8 * 128 + 1], 0, [[16, 8], [512, 8], [1, 16]]),
                in_=psB[:, :].rearrange("p (g j c) -> p g j c", g=8, j=8)), reads=[b_psB], writes=[b_TM])
        for ct in range(4):
            for j in range(8):
                S.op("pe", lambda h, ct=ct, j=j: h.transpose(out=psB[:, j * 128:(j + 1) * 128], in_=TM[:, j, ct * 128:(ct + 1) * 128], identity=identb[:]),
                     reads=[b_TM, b_identb], writes=[b_psB])
            xin = psB[:, :]
            g0t, g1t = gt[0], gt[1]
            S.op("act", lambda h: h.activation(out=g0t, in_=xin, func=AF.Square), reads=[b_psB], writes=[b_gt])
            S.op("dve", lambda h: h.tensor_scalar(out=g0t, in0=g0t, scalar1=0.044715, scalar2=1.0, op0=ALU.mult, op1=ALU.add), reads=[b_gt], writes=[b_gt])
            S.op("dve", lambda h: h.tensor_tensor(out=g0t, in0=g0t, in1=xin, op=ALU.mult), reads=[b_gt, b_psB], writes=[b_gt])
            S.op("act", lambda h: h.activation(out=g1t, in_=g0t, func=AF.Sigmoid, scale=GELU_C), reads=[b_gt], writes=[b_gt])
            S.op("dve", lambda h, ct=ct, jb=jb: h.tensor_tensor(
                out=mkap(ya[:, ct, jb * 1024:jb * 1024 + 1], 0, [[1, 8], [8, 128]]),
                in0=g1t.rearrange("p (j J) -> p j J", j=8), in1=psB[:, :].rearrange("p (j J) -> p j J", j=8), op=ALU.mult),
                reads=[b_gt, b_psB], writes=[b_ya])

    if debug == "ya":
        b_dd = b_arena
        S.dma("sp", lambda h: h.dma_start(out=dbg_d, in_=ya.rearrange("p a b -> p (a b)")), b_dd, reads=[b_dd])
        S.final_wait("sp", [b_dd])
        S.emit()
        return nc

    def alias(new, olds):
        for o in olds:
            if o.writer is not None:
                new.readers.append(o.writer)
            new.readers.extend(o.readers)
        return new

    dead_A = [b_Ysb, b_hTb, b_Usb, b_UTo, b_Zoth, b_Zctx]
    dead_B = [b_UT, b_bc, b_tab, b_bbar, b_tmpb, b_W]
    b_blkA = alias(Buf("blkA"), dead_A)
    b_up0 = alias(Buf("up0"), [b_arena])
    gsec = blockA[:, :].bitcast(F32).rearrange("p (k n) -> p k n", k=8)
    crep = arena[:, 8192:9216].rearrange("p (k m) -> p k m", k=8)
    badrow = arena[:, 9216:10240]
    g1bc = blockB[:, 6144:7168]; g2bc = blockB[:, 7168:8192]; b_gbc = alias(Buf("gbc"), dead_B)
    lnp = blockB[:, 8192:10240]; b_lnp = alias(Buf("lnp"), dead_B)
    S.op("dve", lambda h: h.tensor_copy(out=crep, in_=mkap(csil[:], 0, [[1, 8], [0, 128]])), reads=[b_csil], writes=[b_up0])
    for sec, gdst in ((2, g1bc), (5, g2bc)):
        S.dma("sp", lambda h, sec=sec: h.dma_start(out=gsec, in_=w_ada[:, sec * D:(sec + 1) * D].rearrange("(k p) n -> p k n", p=128)),
              b_blkA, writes=[b_blkA])
        S.dma("sp", lambda h, sec=sec: h.dma_start(out=badrow, in_=b_ada[:, sec * D:(sec + 1) * D].partition_broadcast(128)),
              b_up0, writes=[b_up0])
        for nb in range(2):
            ps, bps = ps_next()
            for kc in range(8):
                S.op("pe", lambda h, ps=ps, kc=kc, nb=nb: h.matmul(ps[:, :], lhsT=crep[:, kc, :], rhs=gsec[:, kc, nb * 512:(nb + 1) * 512],
                                                                  start=(kc == 0), stop=(kc == 7)),
                     reads=[b_up0, b_blkA], writes=[bps])
            S.op("dve", lambda h, ps=ps, nb=nb, gdst=gdst: h.tensor_tensor(out=gdst[:, nb * 512:(nb + 1) * 512], in0=ps[:, :],
                                                                          in1=badrow[:, nb * 512:(nb + 1) * 512], op=ALU.add),
                 reads=[bps, b_up0], writes=[b_gbc])
    S.dma("sp", lambda h: h.dma_start(out=lnp, in_=lnp_in[:, 0:2048].partition_broadcast(128)), b_lnp, writes=[b_lnp])

    winA = blockA[:, :].rearrange("p (k n) -> p k n", k=8)
    winB = blockB[:, 0:6144].bitcast(BF16).rearrange("p (k n) -> p k n", k=8)
    b_winB = alias(Buf("winB"), dead_B)
    for c0, c1, dst, bd in ((512, 1536, winA[:, :, 0:1024], b_blkA), (1536, 2560, winA[:, :, 1024:2048], b_blkA),
                            (2560, 3584, winB[:, :, 0:1024], b_winB), (3584, 4096, winB[:, :, 1024:1536], b_winB)):
        S.dma("pool", lambda h, c0=c0, c1=c1, dst=dst: h.dma_start(out=dst, in_=w_in[:, c0:c1].rearrange("(k p) n -> p k n", p=128)),
              bd, writes=[bd])
    flat = lambda t: t[:].rearrange("p g m -> p (g m)")
    wval = flat(PTre).rearrange("p (k n) -> p k n", k=4); wgate = flat(PTim).rearrange("p (k n) -> p k n", k=4)
    convo = flat(Qre).rearrange("p (k n) -> p k n", k=4)
    wo_lo = flat(Qimn).rearrange("p (k n) -> p k n", k=4); wo_hi = flat(D0m).rearrange("p (k n) -> p k n", k=4)
    b_wglu = alias(Buf("wglu"), [b_PT]); b_convo = alias(Buf("convo"), [b_Q]); b_wo = alias(Buf("wo"), [b_Q, b_D0])
    S.dma("pool", lambda h: h.dma_start(out=wval, in_=w_val.rearrange("(k p) n -> p k n", p=128)), b_wglu, writes=[b_wglu])
    S.dma("pool", lambda h: h.dma_start(out=wgate, in_=w_gate.rearrange("(k p) n -> p k n", p=128)), b_wglu, writes=[b_wglu])
    S.dma("pool", lambda h: h.dma_start(out=convo, in_=conv_out.rearrange("(k p) n -> p k n", p=128)), b_convo, writes=[b_convo])
    S.dma("pool", lambda h: h.dma_start(out=wo_lo, in_=w_o[0:512, :].rearrange("(k p) n -> p k n", p=128)), b_wo, writes=[b_wo])
    S.dma("pool", lambda h: h.dma_start(out=wo_hi, in_=w_o[512:1024, :].rearrange("(k p) n -> p k n", p=128)), b_wo, writes=[b_wo])
    rw32 = masks[:, 1, 0:288].rearrange("p (k n) -> p k n", k=8); b_rw = alias(Buf("rw32"), [b_masks])
    S.dma("sp", lambda h: h.dma_start(out=rw32, in_=rw_in.rearrange("(k p) n -> p k n", p=128)), b_rw, writes=[b_rw])
    rt = masks[:, 0, 0:160]; b_rt = alias(Buf("rt"), [b_masks])

    cst = masks[:, 1, 288:480]; b_cst = alias(Buf("cst"), [b_masks])
    S.dma("sp", lambda h: h.dma_start(out=cst, in_=cst_in), b_cst, writes=[b_cst])
    RT = masks[:, 0, 256:384].rearrange("p (t f) -> p t f", f=4); b_RT = alias(Buf("RT"), [b_masks])
    DEST = masks[:, 0, 384:448].bitcast(I32).rearrange("p (t f) -> p t f", f=2); b_DEST = alias(Buf("DEST"), [b_masks])
    hT = arena[:, 8192:10240].bitcast(BF16).rearrange("p (k t) -> p k t", k=8); b_hT = b_up0
    merged = arena[:, 10240:12288].bitcast(BF16).rearrange("p (k t) -> p k t", k=8); b_merged = alias(Buf("merged"), [b_arena])
    gv = arena[:, 12288:13312].bitcast(BF16).rearrange("p (k t) -> p k t", k=4); b_gv = alias(Buf("gv"), [b_arena])
    tmp = [arena[:, 13312 + i * 512:13312 + (i + 1) * 512] for i in range(5)]
    b_tmp = [alias(Buf(f"tmp{i}"), [b_arena]) for i in range(5)]
    h2b = arena[:, 15872:16384].bitcast(BF16).rearrange("p (k t) -> p k t", k=8); b_h2b = alias(Buf("h2b"), [b_arena])
    wr_f = winu[:].rearrange("p k n -> p (k n)").bitcast(F32)
    A_all = wr_f[:, 0:1024].rearrange("p (t e) -> p t e", e=32); b_Aall = alias(Buf("A_all"), [b_winu])
    h2f = wr_f[:, 1024:2048].rearrange("p (k t) -> p k t", k=8); b_h2f = alias(Buf("h2f"), [b_winu])
    b_x1s = [Buf(f"x1s{i}") for i in range(32)]
    b_h2s = [Buf(f"h2s{i}") for i in range(32)]
    BIG = 1.0e30

    def proj(ft, rhs_ap, brhs):
        piece, off, bp = (winA, (ft - 4) * 128, b_blkA) if ft < 20 else (winB, (ft - 20) * 128, b_winB)
        ps, bps = ps_next()
        for kc in range(8):
            S.op("pe", lambda h, ps=ps, kc=kc, piece=piece, off=off: h.matmul(ps[:, :], lhsT=piece[:, kc, off:off + 128], rhs=rhs_ap(kc),
                                                                             start=(kc == 0), stop=(kc == 7)),
                 reads=[bp, brhs], writes=[bps], mode="bulk")
        return ps, bps

    def mm4(wt, bw, dt, rhs_fn, brhs):
        ps, bps = ps_next()
        for ct in range(4):
            S.op("pe", lambda h, ps=ps, ct=ct: h.matmul(ps[:, :], lhsT=wt[:, ct, dt * 128:(dt + 1) * 128], rhs=rhs_fn(ct), start=(ct == 0), stop=(ct == 3)),
                 reads=[bw, brhs], writes=[bps], mode="bulk")
        return ps, bps

    def dv(fn, r, w):
        S.op("dve", fn, reads=r, writes=w)

    for tb in range(8):
        t0 = tb * 512
        for tl in range(4):
            xt, bx = load_tile(x_own, pos_own, t0 + tl * 128)
            ln_stats(xt, bx, xn_t, b_xn)
            transpose_mod(xn_t, b_xn, lambda kc, tl=tl: hT[:, kc, tl * 128:(tl + 1) * 128], b_hT, sc1_fn, sh1_fn, [b_modfm])
        hrhs = lambda kc: hT[:, kc, :]
        for ct in range(4):
            pz, bpz = proj(4 + ct, hrhs, b_hT)
            pgc, bpgc = proj(12 + ct, hrhs, b_hT)
            S.op("act", lambda h, pgc=pgc: h.activation(out=tmp[0], in_=pgc[:, :], func=AF.Identity), reads=[bpgc], writes=[b_tmp[0]])
            dv(lambda h, pz=pz: h.tensor_tensor(out=tmp[1], in0=pz[:, :], in1=tmp[0], op=ALU.mult), [bpz, b_tmp[0]], [b_tmp[1]])
            dv(lambda h, ct=ct: h.tensor_scalar(out=tmp[2], in0=tmp[1], scalar1=convw[:, ct, 1:2], scalar2=None, op0=ALU.mult), [b_tmp[1], b_convw], [b_tmp[2]])
            zv = tmp[1].rearrange("p (r c) -> p r c", c=64); vv = tmp[2].rearrange("p (r c) -> p r c", c=64)
            dv(lambda h, ct=ct, zv=zv, vv=vv: h.scalar_tensor_tensor(out=vv[:, :, 1:64], in0=zv[:, :, 0:63], scalar=convw[:, ct, 0:1], in1=vv[:, :, 1:64],
                                                                     op0=ALU.mult, op1=ALU.add), [b_tmp[1], b_tmp[2], b_convw], [b_tmp[2]])
            dv(lambda h, ct=ct, zv=zv, vv=vv: h.scalar_tensor_tensor(out=vv[:, :, 0:63], in0=zv[:, :, 1:64], scalar=convw[:, ct, 2:3], in1=vv[:, :, 0:63],
                                                                     op0=ALU.mult, op1=ALU.add), [b_tmp[1], b_tmp[2], b_convw], [b_tmp[2]])
            pgb, bpgb = proj(8 + ct, hrhs, b_hT)
            dv(lambda h, ct=ct, pgb=pgb: h.tensor_tensor(out=gv[:, ct, :], in0=pgb[:, :], in1=tmp[2], op=ALU.mult), [bpgb, b_tmp[2]], [b_gv])
        for dt in range(8):
            pob, bpob = mm4(convo, b_convo, dt, lambda ct: gv[:, ct, :], b_gv)
            pval, bpval = mm4(wval, b_wglu, dt, lambda ct, t0=t0: ya[:, ct, t0:t0 + 512], b_arena)
            pgt, bpgt = mm4(wgate, b_wglu, dt, lambda ct, t0=t0: ya[:, ct, t0:t0 + 512], b_arena)
            pma, bpma = proj(16 + dt, hrhs, b_hT)
            pmb, bpmb = proj(24 + dt, hrhs, b_hT)
            S.op("act", lambda h, pgt=pgt: h.activation(out=tmp[0], in_=pgt[:, :], func=AF.Sigmoid), reads=[bpgt], writes=[b_tmp[0]])
            S.op("act", lambda h, pma=pma: h.activation(out=tmp[1], in_=pma[:, :], func=AF.Sigmoid), reads=[bpma], writes=[b_tmp[1]])
            S.op("act", lambda h, pmb=pmb: h.activation(out=tmp[2], in_=pmb[:, :], func=AF.Sigmoid), reads=[bpmb], writes=[b_tmp[2]])
            dv(lambda h, pval=pval: h.tensor_tensor(out=tmp[3], in0=pval[:, :], in1=tmp[0], op=ALU.mult), [bpval, b_tmp[0]], [b_tmp[3]])
            dv(lambda h: h.tensor_tensor(out=tmp[3], in0=tmp[3], in1=tmp[1], op=ALU.mult), [b_tmp[3], b_tmp[1]], [b_tmp[3]])
            dv(lambda h, pob=pob: h.tensor_tensor(out=tmp[4], in0=pob[:, :], in1=tmp[2], op=ALU.mult), [bpob, b_tmp[2]], [b_tmp[4]])
            dv(lambda h, dt=dt: h.tensor_tensor(out=merged[:, dt, :], in0=tmp[3], in1=tmp[4], op=ALU.add), [b_tmp[3], b_tmp[4]], [b_merged])
        def w_o_pe(tl):
            outs = []
            for nb in range(2):
                ps, bps = ps_next()
                for dt in range(8):
                    wsl = wo_lo[:, dt, nb * 512:(nb + 1) * 512] if dt < 4 else wo_hi[:, dt - 4, nb * 512:(nb + 1) * 512]
                    S.op("pe", lambda h, ps=ps, dt=dt, tl=tl, wsl=wsl: h.matmul(ps[:, :], lhsT=merged[:, dt, tl * 128:(tl + 1) * 128], rhs=wsl,
                                                                               start=(dt == 0), stop=(dt == 7)),
                         reads=[b_merged, b_wo], writes=[bps], mode="bulk")
                outs.append((ps, bps))
            return outs
        tiles_ = [load_tile(x_own, pos_own, (tb * 4) * 128)]
        pend_ = w_o_pe(0)
        for tl in range(4):
            gti = tb * 4 + tl
            r0 = gti * 128
            xt, bx = tiles_[tl]
            cur_ = pend_
            if tl < 3:
                tiles_.append(load_tile(x_own, pos_own, r0 + 128))
                pend_ = w_o_pe(tl + 1)
            for nb in range(2):
                ps, bps = cur_[nb]
                dv(lambda h, ps=ps, nb=nb: h.tensor_tensor(out=xn_t[:, nb * 512:(nb + 1) * 512], in0=ps[:, :], in1=g1bc[:, nb * 512:(nb + 1) * 512], op=ALU.mult),
                   [bps, b_gbc], [b_xn])
            dv(lambda h, xt=xt: h.scalar_tensor_tensor(out=xt[:, :], in0=xt[:, :], scalar=ALPHA, in1=xn_t[:, :], op0=ALU.mult, op1=ALU.add), [bx, b_xn], [bx])
            ln_stats(xt, bx, xn_t, b_xn)
            dv(lambda h: h.tensor_tensor(out=xn_t[:, :], in0=xn_t[:, :], in1=lnp[:, 0:1024], op=ALU.mult), [b_xn, b_lnp], [b_xn])
            dv(lambda h, xt=xt: h.tensor_tensor(out=xt[:, :], in0=xn_t[:, :], in1=lnp[:, 1024:2048], op=ALU.add), [b_xn, b_lnp], [bx])
            S.dma("sp", lambda h, xt=xt, r0=r0: h.dma_start(out=x1s[r0:r0 + 128, :], in_=xt[:, :]), bx, reads=[bx], writes=[b_x1s[gti]])
            ln_stats(xt, bx, xn_t, b_xn)
            for kc in range(8):
                S.op("pe", lambda h, kc=kc: h.transpose(out=psT[:, kc * 128:(kc + 1) * 128], in_=xn_t[:, kc * 128:(kc + 1) * 128], identity=ident[:]),
                     reads=[b_xn, b_ident], writes=[b_psT])
            for kc in range(8):
                S.op("act", lambda h, kc=kc: h.activation(out=h2f[:, kc, :], in_=psT[:, kc * 128:(kc + 1) * 128], func=AF.Identity,
                                                         bias=modfm[:, 24 + kc, 0:1], scale=sc2p[:, kc:kc + 1]),
                     reads=[b_psT, b_modfm], writes=[b_h2f])
            ps, bps = ps_next()
            for kc in range(8):
                S.op("pe", lambda h, ps=ps, kc=kc: h.matmul(ps[:, 0:36], lhsT=h2f[:, kc, :], rhs=rw32[:, kc, :], start=(kc == 0), stop=(kc == 7)),
                     reads=[b_h2f, b_rw], writes=[bps])
            R = lambda a, b_: rt[:, a:b_]
            rr = [b_rt]
            dv(lambda h, ps=ps: h.tensor_tensor(out=R(0, 36), in0=ps[:, 0:36], in1=rb_bc[:, :], op=ALU.add), [bps, b_rb, b_rt], rr)
            dv(lambda h: h.tensor_reduce(out=R(36, 37), in_=R(0, 4), axis=AX.X, op=ALU.max), rr, rr)
            dv(lambda h: h.tensor_scalar(out=R(38, 42), in0=R(0, 4), scalar1=R(36, 37), scalar2=None, op0=ALU.is_equal), rr, rr)
            dv(lambda h: h.tensor_scalar(out=R(37, 38), in0=R(36, 37), scalar1=-1.0, scalar2=None, op0=ALU.mult), rr, rr)
            S.op("act", lambda h: h.activation(out=R(42, 46), in_=R(0, 4), func=AF.Exp, bias=R(37, 38), scale=1.0), reads=rr, writes=rr)
            dv(lambda h: h.tensor_reduce(out=R(46, 47), in_=R(42, 46), axis=AX.X, op=ALU.add), rr, rr)
            dv(lambda h: h.reciprocal(out=R(47, 48), in_=R(46, 47)), rr, rr)
            dv(lambda h: h.tensor_scalar(out=R(48, 52), in0=R(38, 42), scalar1=BIG, scalar2=-BIG, op0=ALU.mult, op1=ALU.add), rr, rr)
            dv(lambda h: h.tensor_tensor(out=R(52, 84).rearrange("p (g e) -> p g e", e=8), in0=R(4, 36).rearrange("p (g e) -> p g e", e=8),
                                         in1=mkap(R(48, 52), 0, [[1, 4], [0, 8]]), op=ALU.add), rr, rr)
            dv(lambda h: h.tensor_reduce(out=R(84, 85), in_=R(52, 84), axis=AX.X, op=ALU.max), rr, rr)
            dv(lambda h: h.tensor_scalar(out=R(85, 117), in0=R(52, 84), scalar1=R(84, 85), scalar2=None, op0=ALU.is_equal), rr, rr)
            dv(lambda h: h.scalar_tensor_tensor(out=R(117, 149), in0=R(85, 117), scalar=-BIG, in1=R(52, 84), op0=ALU.mult, op1=ALU.add), rr, rr)
            dv(lambda h: h.tensor_reduce(out=R(149, 150), in_=R(117, 149), axis=AX.X, op=ALU.max), rr, rr)
            dv(lambda h: h.tensor_scalar(out=R(52, 84), in0=R(117, 149), scalar1=R(149, 150), scalar2=None, op0=ALU.is_equal), rr, rr)
            dv(lambda h: h.tensor_tensor(out=R(150, 151), in0=R(149, 150), in1=R(84, 85), op=ALU.subtract), rr, rr)
            S.op("act", lambda h: h.activation(out=R(151, 152), in_=R(150, 151), func=AF.Exp), reads=rr, writes=rr)
            dv(lambda h: h.tensor_scalar(out=R(152, 153), in0=R(151, 152), scalar1=1.0, scalar2=None, op0=ALU.add), rr, rr)
            dv(lambda h: h.reciprocal(out=R(153, 154), in_=R(152, 153)), rr, rr)
            dv(lambda h: h.tensor_tensor(out=R(154, 155), in0=R(151, 152), in1=R(153, 154), op=ALU.mult), rr, rr)
            dv(lambda h: h.tensor_tensor(out=R(155, 156), in0=R(153, 154), in1=R(47, 48), op=ALU.mult), rr, rr)
            dv(lambda h: h.tensor_tensor(out=R(156, 157), in0=R(154, 155), in1=R(47, 48), op=ALU.mult), rr, rr)
            dv(lambda h, gti=gti: h.tensor_tensor(out=A_all[:, gti, :], in0=R(85, 117), in1=R(52, 84), op=ALU.add), rr + [b_Aall], [b_Aall])
            dv(lambda h: h.tensor_tensor(out=R(117, 149), in0=R(85, 117), in1=cst[:, 0:32], op=ALU.mult), rr + [b_cst], rr)
            dv(lambda h, gti=gti: h.tensor_reduce(out=RT[:, gti, 0:1], in_=R(117, 149), axis=AX.X, op=ALU.add), rr + [b_RT], [b_RT])
            dv(lambda h: h.tensor_tensor(out=R(117, 149), in0=R(52, 84), in1=cst[:, 0:32], op=ALU.mult), rr + [b_cst, b_RT], rr)
            dv(lambda h, gti=gti: h.tensor_reduce(out=RT[:, gti, 1:2], in_=R(117, 149), axis=AX.X, op=ALU.add), rr + [b_RT], [b_RT])
            dv(lambda h, gti=gti: h.tensor_copy(out=RT[:, gti, 2:4], in_=R(155, 157)), rr + [b_RT], [b_RT])

    IOA = bass.IndirectOffsetOnAxis
    NBLK = 48
    rA = blockA[:, :].bitcast(F32)
    b_R = alias(Buf("phaseR"), [b_blkA])
    Dt = rA[:, 4096:5120].rearrange("p (t e) -> p t e", e=32)
    OH = rA[:, 5120:6144].rearrange("p (t e) -> p t e", e=32)
    CMP = rA[:, 6144:7680]
    tri = rA[:, 7680:7936]
    sm_ = rA[:, 7936:8192]
    run = sm_[:, 0:32]; nblk = sm_[:, 32:64]; pe_ = sm_[:, 64:96]; psr = sm_[:, 96:128]; ones32 = sm_[:, 128:160]
    blkE = sm_[:, 160:208]; tE = sm_[:, 208:256]
    S.dma("sp", lambda h: h.dma_start(out=tri, in_=tri_in), b_R, writes=[b_R])
    rdv = lambda fn, extra=(): S.op("dve", fn, reads=[b_R] + list(extra), writes=[b_R])
    pT1, pT2 = ps_next(), ps_next()
    for hf_ in range(2):
        asl = A_all[:, hf_ * 16:(hf_ + 1) * 16, :].rearrange("p t e -> p (t e)")
        S.op("pe", lambda h, hf_=hf_, asl=asl: h.matmul(psT[:, hf_ * 512:(hf_ + 1) * 512], lhsT=tri[:, 0:128], rhs=asl, start=True, stop=True),
             reads=[b_R, b_Aall], writes=[b_psT])
        pt, bpt = (pT1, pT2)[hf_]
        S.op("pe", lambda h, pt=pt, asl=asl: h.matmul(pt[:, :], lhsT=tri[:, 128:256], rhs=asl, start=True, stop=True),
             reads=[b_R, b_Aall], writes=[bpt])
    rdv(lambda h: h.memset(run, 0.0))
    rdv(lambda h: h.memset(ones32, 1.0))
    for t in range(32):
        pt, bpt = (pT1, pT2)[t // 16]
        tt_ = t % 16
        rdv(lambda h, t=t: h.tensor_tensor(out=Dt[:, t, :], in0=psT[:, t * 32:(t + 1) * 32], in1=run, op=ALU.add), [b_psT])
        rdv(lambda h, pt=pt, tt_=tt_: h.tensor_tensor(out=run, in0=pt[:, tt_ * 32:(tt_ + 1) * 32], in1=run, op=ALU.add), [bpt])
    cmp8 = CMP[:, 0:256].rearrange("p (e j) -> p e j", j=8)
    rdv(lambda h: h.tensor_tensor(out=cmp8, in0=mkap(run, 0, [[1, 32], [0, 8]]), in1=mkap(cst[:, 72:80], 0, [[0, 32], [1, 8]]), op=ALU.is_gt), [b_cst])
    rdv(lambda h: h.tensor_reduce(out=nblk, in_=cmp8, axis=AX.X, op=ALU.add))
    rdv(lambda h: h.tensor_tensor_scan(out=pe_, data0=ones32, data1=nblk, initial=0.0, op0=ALU.mult, op1=ALU.add))
    rdv(lambda h: h.tensor_tensor(out=psr, in0=pe_, in1=nblk, op=ALU.subtract))
    rdv(lambda h: h.tensor_scalar(out=psr, in0=psr, scalar1=512.0, scalar2=None, op0=ALU.mult))
    rdv(lambda h: h.tensor_tensor(out=Dt, in0=Dt, in1=mkap(psr, 0, [[0, 32], [1, 32]]), op=ALU.add))
    destF = CMP[:, 256:320].rearrange("p (t f) -> p t f", f=2)
    for k in range(2):
        rdv(lambda h, k=k: h.tensor_tensor(out=OH, in0=mkap(cst[:, 0:32], 0, [[0, 32], [1, 32]]), in1=mkap(RT[:, 0, k:k + 1], 0, [[4, 32], [0, 32]]), op=ALU.is_equal),
            [b_cst, b_RT])
        rdv(lambda h: h.tensor_tensor(out=OH, in0=OH, in1=Dt, op=ALU.mult))
        rdv(lambda h, k=k: h.tensor_reduce(out=destF[:, :, k], in_=OH, axis=AX.X, op=ALU.add))
    S.op("dve", lambda h: h.tensor_copy(out=DEST, in_=destF), reads=[b_R], writes=[b_DEST])
    cmpb = CMP[:, 0:1536].rearrange("p (b e) -> p b e", e=32)
    rdv(lambda h: h.tensor_tensor(out=cmpb, in0=mkap(pe_, 0, [[0, NBLK], [1, 32]]), in1=mkap(cst[:, 0:NBLK], 0, [[1, NBLK], [0, 32]]), op=ALU.is_le), [b_cst, b_DEST])
    rdv(lambda h: h.tensor_reduce(out=blkE, in_=cmpb, axis=AX.X, op=ALU.add))
    rdv(lambda h: h.tensor_scalar(out=blkE, in0=blkE, scalar1=31.0, scalar2=None, op0=ALU.min))
    gif = CMP[:, 0:576]
    gif_g = gif[:, 0:384].rearrange("p (b k) -> p b k", k=8); gif_d = gif[:, 384:576].rearrange("p (b k) -> p b k", k=4)
    rdv(lambda h: h.tensor_scalar(out=tE, in0=blkE, scalar1=1024.0, scalar2=None, op0=ALU.mult))
    rdv(lambda h: h.tensor_tensor(out=gif_g, in0=mkap(tE, 0, [[1, NBLK], [0, 8]]), in1=mkap(cst[:, 64:72], 0, [[0, NBLK], [1, 8]]), op=ALU.add), [b_cst])
    rdv(lambda h: h.tensor_scalar(out=tE, in0=blkE, scalar1=512.0, scalar2=None, op0=ALU.mult))
    rdv(lambda h: h.tensor_tensor(out=gif_d, in0=mkap(tE, 0, [[1, NBLK], [0, 4]]), in1=mkap(cst[:, 64:68], 0, [[0, NBLK], [1, 4]]), op=ALU.add), [b_cst])
    GI = wr_f[:, 1024:1600].bitcast(I32); b_GI = alias(Buf("GI"), [b_h2f])
    GI_g = GI[:, 0:384].rearrange("p (b k) -> p b k", k=8); GI_d = GI[:, 384:576].rearrange("p (b k) -> p b k", k=4)
    S.op("dve", lambda h: h.tensor_copy(out=GI, in_=gif), reads=[b_R], writes=[b_GI])

    if debug == "d":
        dbgr = nc.dram_tensor("dbgr", [128, 32 * 4 + 64 + 576], F32, kind="ExternalOutput").ap()
        dd = rA[:, 4096:4096 + 768]
        S.op("dve", lambda h: h.tensor_copy(out=dd[:, 0:128], in_=RT.rearrange("p t f -> p (t f)")), reads=[b_R, b_RT], writes=[b_R])
        S.op("dve", lambda h: h.tensor_copy(out=dd[:, 128:192], in_=DEST.rearrange("p t f -> p (t f)")), reads=[b_R, b_DEST], writes=[b_R])
        S.op("dve", lambda h: h.tensor_copy(out=dd[:, 192:768], in_=GI), reads=[b_R, b_GI], writes=[b_R])
        S.dma("sp", lambda h: h.dma_start(out=dbgr, in_=dd), b_R, reads=[b_R])
        S.final_wait("sp", b_x1s + [b_R])
        S.emit()
        return nc

    xs = nc.dram_tensor("xs", [NBLK * 512, D], F32, kind="Internal").ap()
    ybd = nc.dram_tensor("ybd", [NBLK * 512, D], F32, kind="Internal").ap()
    b_xs = Buf("xs")
    for t in range(32):
        r0 = t * 128
        i = xctr[0] % 2
        xctr[0] += 1
        xt, bx = xt_t[i], b_xt[i]
        S.dma("sp", lambda h, xt=xt, r0=r0: h.dma_start(out=xt[:], in_=x1s[r0:r0 + 128, :]), bx, reads=[b_x1s[t]], writes=[bx])
        ln_stats(xt, bx, xn_t, b_xn)
        for k in range(2):
            S.dma("pool", lambda h, t=t, k=k: h.indirect_dma_start(out=xs[:, :], out_offset=IOA(ap=DEST[:, t, k:k + 1], axis=0), in_=xn_t[:, :], in_offset=None),
                  b_xs, reads=[b_xn, b_DEST], writes=[b_xs])

    lnp_e = lnp
    S.dma("sp", lambda h: h.dma_start(out=lnp_e, in_=lnp_in[:, 2048:4096].partition_broadcast(128)), b_lnp, writes=[b_lnp])
    xinb = [arena[:, i * 4096:(i + 1) * 4096].rearrange("p (t d) -> p t d", d=1024) for i in range(2)]
    b_xin = [alias(Buf(f"xin{i}"), [b_arena, b_up0, b_merged, b_gv, b_h2b] + b_tmp) for i in range(2)]
    ybuf = [arena[:, 8192 + i * 4096:8192 + (i + 1) * 4096].rearrange("p (t d) -> p t d", d=1024) for i in range(2)]
    b_ybuf = [alias(Buf(f"ybuf{i}"), [b_arena, b_up0, b_merged, b_gv, b_h2b] + b_tmp) for i in range(2)]
    b_ybd = [Buf("ybd0"), Buf("ybd1")]
    hblk = [blockA[:, i * 4096:(i + 1) * 4096].rearrange("p (k t) -> p k t", k=8) for i in range(2)]
    b_hblk = [alias(Buf(f"hblk{i}"), [b_blkA]) for i in range(2)]
    wbuf = [
        (flat(PTre).rearrange("p (k n) -> p k n", k=8), flat(PTim).rearrange("p (k n) -> p k n", k=8), flat(Qre).rearrange("p (k n) -> p k n", k=4)),
        (flat(Qimn).rearrange("p (k n) -> p k n", k=8), flat(D0m).rearrange("p (k n) -> p k n", k=8),
         blockB[:, 0:2048].bitcast(BF16).rearrange("p (k n) -> p k n", k=4)),
    ]
    b_wb = [alias(Buf("wb0"), [b_wglu, b_convo]), alias(Buf("wb1"), [b_wo, b_winB])]
    hid = [blockB[:, 2048 + i * 1024:2048 + (i + 1) * 1024].bitcast(BF16).rearrange("p (k t) -> p k t", k=4) for i in range(2)]
    b_hid = [alias(Buf(f"hid{i}"), [b_winB]) for i in range(2)]
    stmp = [blockB[:, 4096 + i * 512:4096 + (i + 1) * 512] for i in range(2)]
    b_stmp = [alias(Buf(f"stmp{i}"), [b_winB]) for i in range(2)]
    bregs = {}

    def breg(h, v):
        if v not in bregs:
            bregs[v] = h.to_reg(v)
        return bregs[v]
    wg_flat = wg_in.rearrange("e k n -> (e k) n"); wu_flat = wu_in.rearrange("e k n -> (e k) n"); wd_flat = wd_in.rearrange("e k n -> (e k) n")
    for b in range(NBLK):
        par = b % 2
        xi, bxi = xinb[par], b_xin[par]
        S.dma("sp", lambda h, b=b, xi=xi: h.dma_start(out=xi, in_=xs[b * 512:(b + 1) * 512, :].rearrange("(t p) d -> p t d", p=128)), bxi,
              reads=[b_xs], writes=[bxi])
        wg_t, wu_t, wd_t = wbuf[par]
        bw = b_wb[par]
        for kc in range(8):
            S.dma("pool", lambda h, b=b, kc=kc, wg_t=wg_t: h.indirect_dma_start(out=wg_t[:, kc, :], out_offset=None, in_=wg_flat[:, :],
                                                                             in_offset=IOA(ap=GI_g[:, b, kc:kc + 1], axis=0)), bw, reads=[b_GI], writes=[bw])
            S.dma("pool", lambda h, b=b, kc=kc, wu_t=wu_t: h.indirect_dma_start(out=wu_t[:, kc, :], out_offset=None, in_=wu_flat[:, :],
                                                                             in_offset=IOA(ap=GI_g[:, b, kc:kc + 1], axis=0)), bw, reads=[b_GI], writes=[bw])
        for fc in range(4):
            S.dma("pool", lambda h, b=b, fc=fc, wd_t=wd_t: h.indirect_dma_start(out=wd_t[:, fc, :], out_offset=None, in_=wd_flat[:, :],
                                                                             in_offset=IOA(ap=GI_d[:, b, fc:fc + 1], axis=0)), bw, reads=[b_GI], writes=[bw])
        hbk, bhbk = hblk[par], b_hblk[par]
        for tl in range(4):
            for kc in range(8):
                S.op("pe", lambda h, xi=xi, tl=tl, kc=kc: h.transpose(out=psT[:, kc * 128:(kc + 1) * 128], in_=xi[:, tl, kc * 128:(kc + 1) * 128], identity=ident[:]),
                     reads=[bxi, b_ident], writes=[b_psT])
            for kc in range(8):
                S.op("act", lambda h, hbk=hbk, tl=tl, kc=kc: h.activation(out=hbk[:, kc, tl * 128:(tl + 1) * 128], in_=psT[:, kc * 128:(kc + 1) * 128], func=AF.Identity,
                                                                         bias=modfm[:, 24 + kc, 0:1], scale=sc2p[:, kc:kc + 1]),
                     reads=[b_psT, b_modfm], writes=[bhbk])
        hb, bhb = hid[par], b_hid[par]
        for fc in range(4):
            pg, bpg = ps_next()
            for kc in range(8):
                S.op("pe", lambda h, pg=pg, kc=kc, fc=fc, wg_t=wg_t, hbk=hbk: h.matmul(pg[:, :], lhsT=wg_t[:, kc, fc * 128:(fc + 1) * 128], rhs=hbk[:, kc, :],
                                                                                      start=(kc == 0), stop=(kc == 7)), reads=[bw, bhbk], writes=[bpg], mode="bulk")
            pu, bpu = ps_next()
            for kc in range(8):
                S.op("pe", lambda h, pu=pu, kc=kc, fc=fc, wu_t=wu_t, hbk=hbk: h.matmul(pu[:, :], lhsT=wu_t[:, kc, fc * 128:(fc + 1) * 128], rhs=hbk[:, kc, :],
                                                                                      start=(kc == 0), stop=(kc == 7)), reads=[bw, bhbk], writes=[bpu], mode="bulk")
            st_, bst_ = stmp[fc % 2], b_stmp[fc % 2]
            S.op("act", lambda h, pg=pg, st_=st_: h.activation(out=st_, in_=pg[:, :], func=AF.Silu), reads=[bpg], writes=[bst_])
            dv(lambda h, pu=pu, st_=st_, hb=hb, fc=fc: h.tensor_tensor(out=hb[:, fc, :], in0=pu[:, :], in1=st_, op=ALU.mult), [bpu, bst_], [bhb])
        yb_, byb_ = ybuf[par], b_ybuf[par]
        for tl in range(4):
            for nb in range(2):
                pd, bpd = ps_next()
                for fc in range(4):
                    S.op("pe", lambda h, pd=pd, fc=fc, tl=tl, nb=nb, hb=hb, wd_t=wd_t: h.matmul(pd[:, :], lhsT=hb[:, fc, tl * 128:(tl + 1) * 128],
                                                                                             rhs=wd_t[:, fc, nb * 512:(nb + 1) * 512], start=(fc == 0), stop=(fc == 3)),
                         reads=[bhb, bw], writes=[bpd], mode="bulk")
                eng = "act" if (tl * 2 + nb) % 2 == 0 else "dve"
                if eng == "act":
                    S.op("act", lambda h, pd=pd, yb_=yb_, tl=tl, nb=nb: h.activation(out=yb_[:, tl, nb * 512:(nb + 1) * 512], in_=pd[:, :], func=AF.Identity),
                         reads=[bpd], writes=[byb_])
                else:
                    dv(lambda h, pd=pd, yb_=yb_, tl=tl, nb=nb: h.tensor_copy(out=yb_[:, tl, nb * 512:(nb + 1) * 512], in_=pd[:, :]), [bpd], [byb_])
        S.dma("sp", lambda h, b=b, yb_=yb_: h.dma_start(out=ybd[b * 512:(b + 1) * 512, :].rearrange("(t p) d -> p t d", p=128), in_=yb_), byb_,
              reads=[byb_], writes=[b_ybd[par]])

    y12 = [rA[:, 4096 + i * 1024:4096 + (i + 1) * 1024] for i in range(2)]
    b_y12 = [alias(Buf(f"y12_{i}"), [b_R]) for i in range(2)]
    b_out = [Buf(f"out{i}") for i in range(32)]
    for t in range(32):
        r0 = t * 128
        i = xctr[0] % 2
        xctr[0] += 1
        xt, bx = xt_t[i], b_xt[i]
        S.dma("sp", lambda h, xt=xt, r0=r0: h.dma_start(out=xt[:], in_=x1s[r0:r0 + 128, :]), bx, reads=[b_x1s[t]], writes=[bx])
        for k in range(2):
            S.dma("pool", lambda h, t=t, k=k: h.indirect_dma_start(out=y12[k], out_offset=None, in_=ybd[:, :], in_offset=IOA(ap=DEST[:, t, k:k + 1], axis=0)),
                  b_y12[k], reads=[b_ybd[0], b_ybd[1], b_DEST], writes=[b_y12[k]])
        dv(lambda h, t=t: h.tensor_scalar(out=y12[0], in0=y12[0], scalar1=RT[:, t, 2:3], scalar2=None, op0=ALU.mult), [b_y12[0], b_RT], [b_y12[0]])
        dv(lambda h, t=t: h.scalar_tensor_tensor(out=y12[0], in0=y12[1], scalar=RT[:, t, 3:4], in1=y12[0], op0=ALU.mult, op1=ALU.add), [b_y12[0], b_y12[1], b_RT], [b_y12[0]])
        dv(lambda h: h.tensor_tensor(out=y12[0], in0=y12[0], in1=g2bc, op=ALU.mult), [b_y12[0], b_gbc], [b_y12[0]])
        dv(lambda h, xt=xt: h.scalar_tensor_tensor(out=xt[:, :], in0=xt[:, :], scalar=ALPHA, in1=y12[0], op0=ALU.mult, op1=ALU.add), [bx, b_y12[0]], [bx])
        ln_stats(xt, bx, xn_t, b_xn)
        dv(lambda h: h.tensor_tensor(out=xn_t[:, :], in0=xn_t[:, :], in1=lnp_e[:, 0:1024], op=ALU.mult), [b_xn, b_lnp], [b_xn])
        dv(lambda h, xt=xt: h.tensor_tensor(out=xt[:, :], in0=xn_t[:, :], in1=lnp_e[:, 1024:2048], op=ALU.add), [b_xn, b_lnp], [bx])
        S.dma("sp", lambda h, xt=xt, r0=r0: h.dma_start(out=out_d[r0:r0 + 128, :], in_=xt[:, :]), bx, reads=[bx], writes=[b_out[t]])
    S.final_wait("sp", b_out)
    S.emit()
    return nc


def host_inputs(inputs):
    f32 = np.float32
    g = {k: np.asarray(v) for k, v in inputs.items()}
    D_ = 1024
    q = D_ // 4
    omega = (1.0 / (10000.0 ** (np.arange(q, dtype=f32) / f32(q)))).astype(f32)
    r = (np.arange(128, dtype=f32)[:, None] * omega).astype(f32)
    cl = (np.arange(64, dtype=f32)[:, None] * omega).astype(f32)
    r_emb = np.concatenate([np.sin(r), np.cos(r)], -1).astype(f32)
    c_emb = np.concatenate([np.sin(cl), np.cos(cl)], -1).astype(f32)
    pos = np.concatenate([np.broadcast_to(r_emb[:, None, :], (128, 64, 2 * q)),
                          np.broadcast_to(c_emb[None, :, :], (128, 64, 2 * q))], -1).reshape(8192, D_).astype(f32)
    ident = np.eye(128, dtype=f32)
    ii = np.arange(128) // 16
    mF = (ii[None, :] >= ii[:, None]).astype(f32)
    mB = (ii[:, None] >= ii[None, :]).astype(f32)
    masks = np.stack([np.tile(mF, (1, 4)), np.tile(mB, (1, 4))], 1).astype(f32)
    cst = np.zeros((128, 192), f32)
    cst[:, 0:64] = np.arange(64, dtype=f32)[None, :]
    cst[:, 64:72] = np.arange(8, dtype=f32)[None, :] * 128.0 + np.arange(128, dtype=f32)[:, None]
    cst[:, 72:80] = np.arange(8, dtype=f32)[None, :] * 512.0
    tri = np.zeros((128, 256), f32)
    tri[:, 0:128] = (np.arange(128)[:, None] < np.arange(128)[None, :]).astype(f32)
    tri[:, 128:256] = 1.0

    def tr(a):
        return np.ascontiguousarray(a.T)
    maps = []
    for core in range(8):
        b, hf = core // 2, core % 2
        xb = g["x"][b]
        if hf == 1:
            x_oth, x_own = xb[0:4096], xb[4096:8192]
            p_oth, p_own = pos[0:4096], pos[4096:8192]
            ctxl = g["ctx"][b]
            F, B = "f", "b"
            convw = g["conv_w"][0]
        else:
            x_oth, x_own = xb[4096:8192][::-1], xb[0:4096][::-1]
            p_oth, p_own = pos[4096:8192][::-1], pos[0:4096][::-1]
            ctxl = g["ctx"][b][::-1]
            F, B = "b", "f"
            convw = g["conv_w"][0][::-1]
        cT = np.concatenate([g["c"][b].reshape(8, 128).T, g["c_ctx"].reshape(8, 128).T], 1)
        small = np.zeros((128, 3, 32), f32)
        sbb = np.zeros((128, 2, 32, 16), f32)
        scc = np.zeros((128, 2, 32, 16), f32)
        for li, dname in ((0, F), (1, B)):
            L = slice(li * 64, li * 64 + 64)
            small[L, 0, :] = np.broadcast_to(g["s5_log_dt_" + dname][0][None, :], (64, 32))
            small[L, 1, :] = tr(g["s5_a_re_" + dname][0])
            small[L, 2, :] = tr(g["s5_a_im_" + dname][0])
            sbb[L, 0] = g["s5_b_re_" + dname][0].transpose(1, 0, 2)
            sbb[L, 1] = g["s5_b_im_" + dname][0].transpose(1, 0, 2)
            scc[L, 0] = g["s5_c_re_" + dname][0].transpose(2, 0, 1)
            scc[L, 1] = g["s5_c_im_" + dname][0].transpose(2, 0, 1)
        drep = np.tile(g["s5_d"][0].T, (8, 1))
        m = {
            "x_own": x_own, "x_oth": x_oth, "ctx": ctxl, "pos_own": p_own, "pos_oth": p_oth,
            "cT": cT, "w_ada": g["w_ada"][0], "b_adaT": g["b_ada"][0].reshape(48, 128).T, "b_ada": g["b_ada"][0][None, :],
            "w_in": g["w_in"][0], "s5_small": small, "s5_b": sbb, "s5_c": scc, "s5_drep": drep,
            "masks": masks, "ident": ident, "cst": cst, "tri": tri,
            "w_val": g["s5_w_glu_val"][0], "w_gate": g["s5_w_glu_gate"][0],
            "convwT": convw.reshape(3, 4, 128).transpose(2, 1, 0), "conv_out": g["conv_w_out"][0], "w_o": g["w_o"][0],
            "lnp": np.concatenate([g["ln1_g"][0], g["ln1_b"][0], g["ln2_g"][0], g["ln2_b"][0]])[None, :],
            "rw": np.concatenate([g["router_w_group"][0], g["router_w_expert"][0]], 1),
            "rb": np.concatenate([g["router_b_group"][0], g["router_b_expert"][0]])[None, :],
            "wg": g["exp_w_gate"][0], "wu": g["exp_w_up"][0], "wd": g["exp_w_down"][0],
        }
        maps.append({k: np.ascontiguousarray(v, dtype=f32) for k, v in m.items()})
    return maps


def kernel(**inputs):
    maps = host_inputs(inputs)
    nc = build()
    res = run_bass_kernel_spmd(nc, maps, core_ids=list(range(8)))
    out = np.zeros((4, 8192, 1024), np.float32)
    for core in range(8):
        b, hf = core // 2, core % 2
        o = res.results[core]["out"]
        if hf == 1:
            out[b, 4096:8192] = o
        else:
            out[b, 0:4096] = o[::-1]
    return out
```

```python
import math
import numpy as np
import concourse.bass as bass
import concourse.mybir as mybir
from concourse.bass_utils import run_bass_kernel_spmd

F32 = mybir.dt.float32
BF16 = mybir.dt.bfloat16
I32 = mybir.dt.int32
AF = mybir.ActivationFunctionType
ALU = mybir.AluOpType
AX = mybir.AxisListType

ALPHA = 2.0 ** 0.25
LN_EPS = 1e-6
NT = 4096
D = 1024
NG = 32
GELU_C = 2.0 * math.sqrt(2.0 / math.pi)


class Buf:
    __slots__ = ("name", "writer", "readers", "sem", "semcount")

    def __init__(self, name):
        self.name = name
        self.writer = None
        self.readers = []
        self.sem = None
        self.semcount = 0


class Sched:
    SEM_CAP = 30000

    def __init__(self, nc):
        self.nc = nc
        self.eng = {n: dict(prog=[], sem=None, count=0, waited={}, nsem=0) for n in ("pe", "act", "dve", "pool", "sp")}
        self.nbufsem = 0

    def _engsem(self, E, name):
        if E["sem"] is None or E["count"] >= self.SEM_CAP:
            E["sem"] = self.nc.alloc_semaphore(f"s_{name}_{E['nsem']}")
            E.setdefault("own", set()).add(id(E["sem"]))
            E["nsem"] += 1
            E["count"] = 0
        return E["sem"]

    def _waits(self, E, reads, writes):
        need = {}

        def add(tok):
            if tok is None:
                return
            s, v = tok
            k = id(s)
            if k not in need or need[k][1] < v:
                need[k] = (s, v)
        for b in reads:
            add(b.writer)
        for b in writes:
            add(b.writer)
            for r in b.readers:
                add(r)
        out = []
        for k, (s, v) in need.items():
            if E["waited"].get(k, 0) < v:
                E["waited"][k] = v
                out.append((s, v))
        return out

    def _commit(self, tok, reads, writes):
        for b in writes:
            b.writer = tok
            b.readers = []
        for b in reads:
            if b not in writes:
                b.readers.append(tok)
                if len(b.readers) > 48:
                    d = {}
                    for s, v in b.readers:
                        if id(s) not in d or d[id(s)][1] < v:
                            d[id(s)] = (s, v)
                    b.readers = list(d.values())

    def op(self, name, fn, reads=(), writes=(), mode=None):
        E = self.eng[name]
        waits = self._waits(E, reads, writes)
        if name == "pe":
            if mode is not None and E.get("last_mode") == mode:
                own = E.get("own", set())
                waits = [(s_, v_) for (s_, v_) in waits if id(s_) not in own]
            E["last_mode"] = mode
        sem = self._engsem(E, name)
        E["count"] += 1
        val = E["count"]

        def run(h, waits=waits, fn=fn, sem=sem):
            for s, v in waits:
                h.wait_ge(s, v)
            fn(h).then_inc(sem, 1)
        E["prog"].append(run)
        self._commit((sem, val), reads, writes)

    def dma(self, qname, fn, owner, reads=(), writes=()):
        E = self.eng[qname]
        waits = self._waits(E, reads, writes)
        if owner.sem is None:
            owner.sem = self.nc.alloc_semaphore(f"d_{self.nbufsem}")
            self.nbufsem += 1
        owner.semcount += 16
        sem, val = owner.sem, owner.semcount

        def run(h, waits=waits, fn=fn, sem=sem):
            for s, v in waits:
                h.wait_ge(s, v)
            fn(h).then_inc(sem, 16)
        E["prog"].append(run)
        self._commit((sem, val), reads, writes)

    def final_wait(self, qname, bufs):
        E = self.eng[qname]
        waits = self._waits(E, bufs, ())

        def run(h, waits=waits):
            for s, v in waits:
                h.wait_ge(s, v)
        E["prog"].append(run)

    def emit(self):
        with self.nc.Block() as block:
            @block.tensor
            def _(h):
                for f in self.eng["pe"]["prog"]:
                    f(h)

            @block.scalar
            def _(h):
                for f in self.eng["act"]["prog"]:
                    f(h)

            @block.vector
            def _(h):
                for f in self.eng["dve"]["prog"]:
                    f(h)

            @block.gpsimd
            def _(h):
                for f in self.eng["pool"]["prog"]:
                    f(h)

            @block.sync
            def _(h):
                for f in self.eng["sp"]["prog"]:
                    f(h)


def mkap(base, off_elems, dims):
    return bass.AP(base.tensor, base.offset + off_elems, [list(base.ap[0])] + [list(d) for d in dims])


def build(debug=None):
    nc = bass.Bass("TRN2", target_bir_lowering=False)
    S = Sched(nc)

    def din(name, shape, dt=F32):
        return nc.dram_tensor(name, list(shape), dt, kind="ExternalInput").ap()

    x_own = din("x_own", [NT, D]); x_oth = din("x_oth", [NT, D]); ctx_in = din("ctx", [256, D])
    pos_own = din("pos_own", [NT, D]); pos_oth = din("pos_oth", [NT, D])
    cT_in = din("cT", [128, 16])
    w_ada = din("w_ada", [D, 6 * D]); b_adaT = din("b_adaT", [128, 48]); b_ada = din("b_ada", [1, 6 * D])
    w_in = din("w_in", [D, 4096])
    s5_small = din("s5_small", [128, 3, NG])
    s5_b = din("s5_b", [128, 2, NG, 16]); s5_c = din("s5_c", [128, 2, NG, 16]); s5_drep = din("s5_drep", [128, NG])
    masks_in = din("masks", [128, 2, 512]); ident_in = din("ident", [128, 128]); cst_in = din("cst", [128, 192]); tri_in = din("tri", [128, 256])
    w_val = din("w_val", [512, D]); w_gate = din("w_gate", [512, D])
    convw_in = din("convwT", [128, 4, 3]); conv_out = din("conv_out", [512, D]); w_o = din("w_o", [D, D])
    lnp_in = din("lnp", [1, 4 * D])
    rw_in = din("rw", [D, 36]); rb_in = din("rb", [1, 36])
    wg_in = din("wg", [32, D, 512]); wu_in = din("wu", [32, D, 512]); wd_in = din("wd", [32, 512, D])
    out_d = nc.dram_tensor("out", [NT, D], F32, kind="ExternalOutput").ap()
    x1s = nc.dram_tensor("x1s", [NT, D], F32, kind=("ExternalOutput" if debug else "Internal")).ap()
    h2s = nc.dram_tensor("h2s", [128, 8, NT], BF16, kind=("ExternalOutput" if debug else "Internal")).ap()
    wrs = nc.dram_tensor("wrs", [128, 32, 32], F32, kind="ExternalOutput").ap() if debug else None
    dbg_d = None
    if debug == "ya":
        dbg_d = nc.dram_tensor("dbg", [128, 4 * NT], BF16, kind="ExternalOutput").ap()

    def sb(name, shape, dt=F32):
        return nc.alloc_sbuf_tensor("sb_" + name, list(shape), dt)

    ident = sb("ident", [128, 128]); b_ident = Buf("ident")
    identb = sb("identb", [128, 128], BF16); b_identb = Buf("identb")
    masks = sb("masks", [128, 2, 512]); b_masks = Buf("masks")
    modfm = sb("modfm", [128, 48, 2]); b_modfm = Buf("modfm")
    sc1p = sb("sc1p", [128, 8, 2]); sc2p = sb("sc2p", [128, 8])
    convw = sb("convw", [128, 4, 3]); b_convw = Buf("convw")
    rb_bc = sb("rb_bc", [128, 36]); b_rb = Buf("rb")
    epst = sb("epst", [128, 1]); b_eps = Buf("eps")

    S.dma("sp", lambda h: h.dma_start(out=ident[:], in_=ident_in), b_ident, writes=[b_ident])
    S.dma("sp", lambda h: h.dma_start(out=masks[:], in_=masks_in), b_masks, writes=[b_masks])
    S.dma("sp", lambda h: h.dma_start(out=convw[:], in_=convw_in), b_convw, writes=[b_convw])
    S.dma("sp", lambda h: h.dma_start(out=rb_bc[:], in_=rb_in.partition_broadcast(128)), b_rb, writes=[b_rb])
    S.op("dve", lambda h: h.tensor_copy(out=identb[:], in_=ident[:]), reads=[b_ident], writes=[b_identb])
    S.op("dve", lambda h: h.memset(epst[:], LN_EPS), writes=[b_eps])

    psg = [nc.alloc_psum_tensor(f"psg{i}", [128, 512], F32) for i in range(5)]
    b_psg = [Buf(f"psg{i}") for i in range(5)]
    psT = nc.alloc_psum_tensor("psT", [128, 1024], F32); b_psT = Buf("psT")
    psB = nc.alloc_psum_tensor("psB", [128, 1024], BF16); b_psB = Buf("psB")
    pctr = [0]

    def ps_next():
        i = pctr[0] % 5
        pctr[0] += 1
        return psg[i], b_psg[i]

    cT = sb("cT", [128, 16]); b_cT = Buf("cT")
    csil = sb("csil", [128, 16]); b_csil = Buf("csil")
    csil2 = sb("csil2", [128, 8, 2]); b_csil2 = Buf("csil2")
    blockA = sb("blockA", [128, 16384], BF16)
    b_hTb = Buf("hTb")
    badT = sb("badT", [128, 48]); b_badT = Buf("badT")
    S.dma("sp", lambda h: h.dma_start(out=cT[:], in_=cT_in), b_cT, writes=[b_cT])
    S.dma("sp", lambda h: h.dma_start(out=badT[:], in_=b_adaT), b_badT, writes=[b_badT])
    S.op("act", lambda h: h.activation(out=csil[:], in_=cT[:], func=AF.Silu), reads=[b_cT], writes=[b_csil])
    S.op("dve", lambda h: h.tensor_copy(out=csil2[:, :, 0], in_=csil[:, 0:8]), reads=[b_csil], writes=[b_csil2])
    S.op("dve", lambda h: h.tensor_copy(out=csil2[:, :, 1], in_=csil[:, 8:16]), reads=[b_csil], writes=[b_csil2])

    arena = sb("arena", [128, 16384])
    b_arena = Buf("arena")
    wsec = arena[:, 0:8192].rearrange("p (k n) -> p k n", k=8)
    for sec in (0, 1, 3, 4):
        S.dma("sp", lambda h, sec=sec: h.dma_start(out=wsec, in_=w_ada[:, sec * D:(sec + 1) * D].rearrange("(k p) n -> p k n", p=128)),
              b_arena, writes=[b_arena])
        ps, bps = ps_next()
        for jj in range(8):
            for kc in range(8):
                S.op("pe", lambda h, ps=ps, kc=kc, jj=jj: h.matmul(ps[:, jj * 2:jj * 2 + 2], lhsT=wsec[:, kc, jj * 128:(jj + 1) * 128],
                                                                  rhs=csil2[:, kc, :], start=(kc == 0), stop=(kc == 7)),
                     reads=[b_csil2, b_arena], writes=[bps])
        S.op("dve", lambda h, ps=ps, sec=sec: h.tensor_tensor(
            out=modfm[:, sec * 8:(sec + 1) * 8, :], in0=ps[:, 0:16].rearrange("p (j t) -> p j t", t=2),
            in1=mkap(badT[:, sec * 8:(sec + 1) * 8], 0, [[1, 8], [0, 2]]), op=ALU.add),
            reads=[bps, b_badT], writes=[b_modfm])
    S.op("dve", lambda h: h.tensor_scalar(out=sc1p[:], in0=modfm[:, 8:16, :], scalar1=1.0, scalar2=None, op0=ALU.add), reads=[b_modfm], writes=[b_modfm])
    S.op("dve", lambda h: h.tensor_scalar(out=sc2p[:], in0=modfm[:, 32:40, 0], scalar1=1.0, scalar2=None, op0=ALU.add), reads=[b_modfm], writes=[b_modfm])

    xt_t = [sb(f"xt{i}", [128, D]) for i in range(2)]; b_xt = [Buf(f"xt{i}") for i in range(2)]
    xn_t = sb("xn", [128, D]); b_xn = Buf("xn")
    stt = sb("stt", [128, 16]); b_stt = Buf("stt")
    xctr = [0]

    def ln_stats(src, bsrc, dst, bdst):
        S.op("dve", lambda h: h.bn_stats(out=stt[:, 0:6], in_=src[:, 0:512]), reads=[bsrc], writes=[b_stt])
        S.op("dve", lambda h: h.bn_stats(out=stt[:, 6:12], in_=src[:, 512:1024]), reads=[bsrc], writes=[b_stt])
        S.op("dve", lambda h: h.bn_aggr(out=stt[:, 12:14], in_=stt[:, 0:12]), reads=[b_stt], writes=[b_stt])
        S.op("act", lambda h: h.activation(out=stt[:, 14:15], in_=stt[:, 13:14], func=AF.Sqrt, bias=epst[:, 0:1], scale=1.0),
             reads=[b_stt, b_eps], writes=[b_stt])
        S.op("dve", lambda h: h.reciprocal(out=stt[:, 15:16], in_=stt[:, 14:15]), reads=[b_stt], writes=[b_stt])
        S.op("dve", lambda h: h.tensor_scalar(out=dst[:, :], in0=src[:, :], scalar1=stt[:, 12:13], scalar2=stt[:, 15:16],
                                              op0=ALU.subtract, op1=ALU.mult), reads=[bsrc, b_stt], writes=[bdst])

    def transpose_mod(src, bsrc, dst_fn, bdst, scale_fn, shift_fn, bmods):
        for kc in range(8):
            S.op("pe", lambda h, kc=kc: h.transpose(out=psT[:, kc * 128:(kc + 1) * 128], in_=src[:, kc * 128:(kc + 1) * 128], identity=ident[:]),
                 reads=[bsrc, b_ident], writes=[b_psT], mode="trf")
        for kc in range(8):
            S.op("act", lambda h, kc=kc: h.activation(out=dst_fn(kc), in_=psT[:, kc * 128:(kc + 1) * 128], func=AF.Identity,
                                                     bias=shift_fn(kc), scale=scale_fn(kc)),
                 reads=[b_psT] + bmods, writes=[bdst])

    def load_tile(xsrc, possrc, r0):
        i = xctr[0] % 2
        xctr[0] += 1
        xt, bx = xt_t[i], b_xt[i]
        S.dma("sp", lambda h: h.dma_start(out=xt[:], in_=xsrc[r0:r0 + 128, :]), bx, writes=[bx])
        if possrc is not None:
            S.dma("pool", lambda h: h.dma_start(out=xt[:], in_=possrc[r0:r0 + 128, :], accum_op=ALU.add), bx, writes=[bx])
        return xt, bx

    blockB = sb("blockB", [128, 10240])
    sm = sb("s5sm", [128, 3, NG]); b_sm = Buf("s5sm")
    sbv = blockB[:, 0:1024].rearrange("p (r g c) -> p r g c", r=2, g=NG); scv = blockB[:, 1024:2048].rearrange("p (r g c) -> p r g c", r=2, g=NG); b_bc = Buf("s5bc")
    drep = sb("drep", [128, NG]); b_drep = Buf("drep")
    S.dma("sp", lambda h: h.dma_start(out=sm[:], in_=s5_small), b_sm, writes=[b_sm])
    S.dma("sp", lambda h: h.dma_start(out=sbv[:], in_=s5_b), b_bc, writes=[b_bc])
    S.dma("sp", lambda h: h.dma_start(out=scv[:], in_=s5_c), b_bc, writes=[b_bc])
    S.dma("sp", lambda h: h.dma_start(out=drep[:], in_=s5_drep), b_drep, writes=[b_drep])

    tb_ = blockB[:, 2048:2048 + 11 * NG * 9].rearrange("p (t g k) -> p t g k", t=11, g=NG); b_tab = Buf("s5tab")
    T_ANG, T_Q, T_SIN, T_COS, T_MAG, T_MAGN, T_WRE, T_WIM, T_VRE, T_VIM, T_TMP = range(11)
    qi = sb("s5qi", [128, NG * 9], I32)
    vec = sb("s5vec", [128, 12, NG]); b_vec = Buf("s5vec")
    V_DT, V_TH, V_LR, V_DEN, V_XRE, V_FRE, V_FIM, V_T1, V_T2, V_RDEN = range(10)

    def vop(fn, r=(b_vec,), w=(b_vec,)):
        S.op("dve", fn, reads=list(r), writes=list(w))

    S.op("act", lambda h: h.activation(out=vec[:, V_DT, :], in_=sm[:, 0, :], func=AF.Exp), reads=[b_sm], writes=[b_vec])
    vop(lambda h: h.tensor_tensor(out=vec[:, V_TH, :], in0=vec[:, V_DT, :], in1=sm[:, 2, :], op=ALU.mult), r=(b_vec, b_sm))
    vop(lambda h: h.tensor_tensor(out=vec[:, V_LR, :], in0=vec[:, V_DT, :], in1=sm[:, 1, :], op=ALU.mult), r=(b_vec, b_sm))
    T = lambda t: tb_[:, t, :, :]
    Tf = lambda t: tb_[:, t, :, :].rearrange("p g k -> p (g k)")
    for k in range(9):
        S.op("dve", lambda h, k=k: h.tensor_scalar(out=tb_[:, T_ANG, :, k], in0=vec[:, V_TH, :], scalar1=float(k), scalar2=None, op0=ALU.mult),
             reads=[b_vec], writes=[b_tab])
        S.op("act", lambda h, k=k: h.activation(out=tb_[:, T_MAG, :, k], in_=vec[:, V_LR, :], func=AF.Exp, scale=float(k)), reads=[b_vec], writes=[b_tab])
        S.op("act", lambda h, k=k: h.activation(out=tb_[:, T_MAGN, :, k], in_=vec[:, V_LR, :], func=AF.Exp, scale=-float(k)), reads=[b_vec], writes=[b_tab])

    def sin_of(dst_t, shift):
        top = lambda fn: S.op("dve", fn, reads=[b_tab], writes=[b_tab])
        top(lambda h: h.tensor_scalar(out=Tf(T_TMP), in0=Tf(T_ANG), scalar1=shift, scalar2=None, op0=ALU.add))
        top(lambda h: h.tensor_scalar(out=qi[:], in0=Tf(T_TMP), scalar1=1.0 / (2 * math.pi), scalar2=None, op0=ALU.mult))
        top(lambda h: h.tensor_copy(out=Tf(T_Q), in_=qi[:]))
        top(lambda h: h.scalar_tensor_tensor(out=Tf(T_TMP), in0=Tf(T_Q), scalar=-2 * math.pi, in1=Tf(T_TMP), op0=ALU.mult, op1=ALU.add))
        top(lambda h: h.tensor_scalar(out=Tf(T_Q), in0=Tf(T_TMP), scalar1=math.pi, scalar2=2 * math.pi, op0=ALU.is_gt, op1=ALU.mult))
        top(lambda h: h.tensor_tensor(out=Tf(T_TMP), in0=Tf(T_TMP), in1=Tf(T_Q), op=ALU.subtract))
        top(lambda h: h.tensor_scalar(out=Tf(T_Q), in0=Tf(T_TMP), scalar1=-math.pi, scalar2=2 * math.pi, op0=ALU.is_lt, op1=ALU.mult))
        top(lambda h: h.tensor_tensor(out=Tf(T_TMP), in0=Tf(T_TMP), in1=Tf(T_Q), op=ALU.add))
        S.op("act", lambda h: h.activation(out=Tf(dst_t), in_=Tf(T_TMP), func=AF.Sin), reads=[b_tab], writes=[b_tab])

    sin_of(T_SIN, 0.0)
    sin_of(T_COS, math.pi / 2)
    tt = lambda o, a, b, op: S.op("dve", lambda h: h.tensor_tensor(out=Tf(o), in0=Tf(a), in1=Tf(b), op=op), reads=[b_tab], writes=[b_tab])
    tt(T_WRE, T_MAG, T_COS, ALU.mult)
    tt(T_WIM, T_MAG, T_SIN, ALU.mult)
    tt(T_VRE, T_MAGN, T_COS, ALU.mult)
    tt(T_VIM, T_MAGN, T_SIN, ALU.mult)
    S.op("dve", lambda h: h.tensor_scalar(out=Tf(T_VIM), in0=Tf(T_VIM), scalar1=-1.0, scalar2=None, op0=ALU.mult), reads=[b_tab], writes=[b_tab])
    are = sm[:, 1, :]; aim = sm[:, 2, :]
    abre = tb_[:, T_WRE, :, 1]; abim = tb_[:, T_WIM, :, 1]
    vr = (b_vec, b_sm, b_tab)
    vop(lambda h: h.tensor_tensor(out=vec[:, V_DEN, :], in0=are, in1=are, op=ALU.mult), r=vr)
    vop(lambda h: h.tensor_tensor(out=vec[:, V_T1, :], in0=aim, in1=aim, op=ALU.mult), r=vr)
    vop(lambda h: h.tensor_tensor(out=vec[:, V_DEN, :], in0=vec[:, V_DEN, :], in1=vec[:, V_T1, :], op=ALU.add), r=vr)
    vop(lambda h: h.reciprocal(out=vec[:, V_RDEN, :], in_=vec[:, V_DEN, :]), r=vr)
    vop(lambda h: h.tensor_scalar(out=vec[:, V_XRE, :], in0=abre, scalar1=-1.0, scalar2=None, op0=ALU.add), r=vr)
    vop(lambda h: h.tensor_tensor(out=vec[:, V_T1, :], in0=vec[:, V_XRE, :], in1=are, op=ALU.mult), r=vr)
    vop(lambda h: h.tensor_tensor(out=vec[:, V_T2, :], in0=abim, in1=aim, op=ALU.mult), r=vr)
    vop(lambda h: h.tensor_tensor(out=vec[:, V_T1, :], in0=vec[:, V_T1, :], in1=vec[:, V_T2, :], op=ALU.add), r=vr)
    vop(lambda h: h.tensor_tensor(out=vec[:, V_FRE, :], in0=vec[:, V_T1, :], in1=vec[:, V_RDEN, :], op=ALU.mult), r=vr)
    vop(lambda h: h.tensor_tensor(out=vec[:, V_T1, :], in0=abim, in1=are, op=ALU.mult), r=vr)
    vop(lambda h: h.tensor_tensor(out=vec[:, V_T2, :], in0=vec[:, V_XRE, :], in1=aim, op=ALU.mult), r=vr)
    vop(lambda h: h.tensor_tensor(out=vec[:, V_T1, :], in0=vec[:, V_T1, :], in1=vec[:, V_T2, :], op=ALU.subtract), r=vr)
    vop(lambda h: h.tensor_tensor(out=vec[:, V_FIM, :], in0=vec[:, V_T1, :], in1=vec[:, V_RDEN, :], op=ALU.mult), r=vr)
    bbar = blockB[:, 5632:6656].rearrange("p (r g c) -> p r g c", r=2, g=NG); b_bbar = Buf("bbar")
    tmpb = blockB[:, 6656:7168].rearrange("p (g c) -> p g c", g=NG); b_tmpb = Buf("tmpb")
    fre_b = mkap(vec[:, V_FRE, :], 0, [[1, NG], [0, 16]]); fim_b = mkap(vec[:, V_FIM, :], 0, [[1, NG], [0, 16]])
    bo = lambda fn, r, w: S.op("dve", fn, reads=r, writes=w)
    bo(lambda h: h.tensor_tensor(out=bbar[:, 0], in0=sbv[:, 0], in1=fre_b, op=ALU.mult), [b_bc, b_vec], [b_bbar])
    bo(lambda h: h.tensor_tensor(out=tmpb[:], in0=sbv[:, 1], in1=fim_b, op=ALU.mult), [b_bc, b_vec], [b_tmpb])
    bo(lambda h: h.tensor_tensor(out=bbar[:, 0], in0=bbar[:, 0], in1=tmpb[:], op=ALU.subtract), [b_bbar, b_tmpb], [b_bbar])
    bo(lambda h: h.tensor_tensor(out=bbar[:, 1], in0=sbv[:, 1], in1=fre_b, op=ALU.mult), [b_bc, b_vec], [b_bbar])
    bo(lambda h: h.tensor_tensor(out=tmpb[:], in0=sbv[:, 0], in1=fim_b, op=ALU.mult), [b_bc, b_vec], [b_tmpb])
    bo(lambda h: h.tensor_tensor(out=bbar[:, 1], in0=bbar[:, 1], in1=tmpb[:], op=ALU.add), [b_bbar, b_tmpb], [b_bbar])

    WB, WBp, WC = [blockB[:, 7168 + i * 512:7168 + (i + 1) * 512].rearrange("p (r g k) -> p r g k", r=2, g=NG) for i in range(3)]; b_W = Buf("W")

    def fwd_slice(t, lanes, k0):
        return tb_[lanes, t, :, k0:k0 + 8]

    def rev_slice(t, lanes, k_hi):
        base = tb_[lanes, t, :, k_hi:k_hi + 1]
        return mkap(base, 0, [[9, NG], [-1, 8]])
    LF = slice(0, 64); LB = slice(64, 128)
    for ri, (tw, tv) in enumerate(((T_WRE, T_VRE), (T_WIM, T_VIM))):
        cp = lambda o, i_: S.op("dve", lambda h: h.tensor_copy(out=o, in_=i_), reads=[b_tab], writes=[b_W])
        cp(WB[LF, ri], rev_slice(tw, LF, 7))
        cp(WB[LB, ri], fwd_slice(tw, LB, 0))
        cp(WBp[LF, ri], fwd_slice(tv, LF, 1))
        cp(WBp[LB, ri], rev_slice(tv, LB, 8))
        cp(WC[LF, ri], fwd_slice(tw, LF, 1))
        cp(WC[LB, ri], rev_slice(tw, LB, 8))

    D0m = sb("D0m", [128, NG, 128], BF16); b_D0 = Buf("D0")
    PTre = sb("PTre", [128, NG, 128], BF16); PTim = sb("PTim", [128, NG, 128], BF16); b_PT = Buf("PT")
    Qre = sb("Qre", [128, NG, 128], BF16); Qimn = sb("Qimn", [128, NG, 128], BF16); b_Q = Buf("Q")
    A1 = sb("A1", [128, NG, 2]); A2 = sb("A2", [128, NG, 2]); b_A = Buf("A12")
    S.op("dve", lambda h: h.tensor_copy(out=A1[:], in_=mkap(tb_[:, T_WRE, :, 8:9], 0, [[9, NG], [0, 2]])), reads=[b_tab], writes=[b_A])
    S.op("dve", lambda h: h.tensor_copy(out=A2[:, :, 1], in_=tb_[:, T_WIM, :, 8]), reads=[b_tab], writes=[b_A])
    S.op("dve", lambda h: h.tensor_scalar(out=A2[:, :, 0], in0=tb_[:, T_WIM, :, 8], scalar1=-1.0, scalar2=None, op0=ALU.mult), reads=[b_tab], writes=[b_A])

    GC = 8
    gen = arena[:, 0:8192]

    def gslot(i):
        return gen[:, i * 1024:(i + 1) * 1024].rearrange("p (g i c) -> p g i c", g=GC, i=8)

    def cprod(Wt, X, g0, o_re, o_im, breads):
        def wv(ri):
            return mkap(Wt[:, ri, g0:g0 + GC, :], 0, [[8, GC], [1, 8], [0, 16]])

        def xv(ri):
            return mkap(X[:, ri, g0:g0 + GC, :], 0, [[16, GC], [0, 8], [1, 16]])
        t1 = gslot(6); t2 = gslot(7)
        o = lambda fn: S.op("dve", fn, reads=[b_W, b_arena] + breads, writes=[b_arena])
        o(lambda h: h.tensor_tensor(out=t1, in0=wv(0), in1=xv(0), op=ALU.mult))
        o(lambda h: h.tensor_tensor(out=t2, in0=wv(1), in1=xv(1), op=ALU.mult))
        o(lambda h: h.tensor_tensor(out=o_re, in0=t1, in1=t2, op=ALU.subtract))
        o(lambda h: h.tensor_tensor(out=t1, in0=wv(0), in1=xv(1), op=ALU.mult))
        o(lambda h: h.tensor_tensor(out=t2, in0=wv(1), in1=xv(0), op=ALU.mult))
        o(lambda h: h.tensor_tensor(out=o_im, in0=t1, in1=t2, op=ALU.add))

    for gch in range(NG // GC):
        g0 = gch * GC
        Bt_re, Bt_im, Bp_re, Bp_im, Ct_re, Ct_im = [gslot(i) for i in range(6)]
        cprod(WB, bbar, g0, Bt_re, Bt_im, [b_bbar])
        cprod(WBp, bbar, g0, Bp_re, Bp_im, [b_bbar])
        cprod(WC, scv, g0, Ct_re, Ct_im, [b_bc])
        fl = lambda a: a.rearrange("p g i c -> p g (i c)")
        S.op("act", lambda h, g0=g0, Ct_re=Ct_re: h.activation(out=Qre[:, g0:g0 + GC, :], in_=Ct_re.rearrange("p g i c -> p g (i c)"), func=AF.Identity), reads=[b_arena], writes=[b_Q])
        S.op("act", lambda h, g0=g0, Ct_im=Ct_im: h.activation(out=Qimn[:, g0:g0 + GC, :], in_=Ct_im.rearrange("p g i c -> p g (i c)"), func=AF.Identity, scale=-1.0), reads=[b_arena], writes=[b_Q])
        S.op("dve", lambda h, Ct_im=Ct_im: h.tensor_scalar(out=Ct_im.rearrange("p g i c -> p g (i c)"), in0=Ct_im.rearrange("p g i c -> p g (i c)"), scalar1=-1.0, scalar2=None, op0=ALU.mult), reads=[b_arena, b_Q], writes=[b_arena])
        for ri, (Bt, PT) in enumerate(((Bt_re, PTre), (Bt_im, PTim))):
            for gg in range(GC):
                S.op("pe", lambda h, gg=gg, Bt=Bt: h.transpose(out=psT[:, gg * 128:(gg + 1) * 128], in_=Bt.rearrange("p g i c -> p g (i c)")[:, gg, :], identity=ident[:]),
                     reads=[b_arena, b_ident], writes=[b_psT])
            S.op("act", lambda h, PT=PT, g0=g0: h.activation(out=PT[:, g0:g0 + GC, :], in_=psT[:, :].rearrange("p (g m) -> p g m", g=GC), func=AF.Identity),
                 reads=[b_psT], writes=[b_PT])
        for g4 in range(GC // 4):
            pF, bpF = ps_next(); pB, bpB = ps_next()
            for gg in range(4):
                g = g4 * 4 + gg
                for lanes, pp, bpp in ((LF, pF, bpF), (LB, pB, bpB)):
                    S.op("pe", lambda h, g=g, gg=gg, lanes=lanes, pp=pp, Bp_re=Bp_re, Ct_re=Ct_re: h.matmul(pp[:, gg * 128:(gg + 1) * 128], lhsT=Bp_re.rearrange("p g i c -> p g (i c)")[lanes, g, :],
                                                                                  rhs=Ct_re.rearrange("p g i c -> p g (i c)")[lanes, g, :], start=True, stop=False),
                         reads=[b_arena], writes=[bpp])
                    S.op("pe", lambda h, g=g, gg=gg, lanes=lanes, pp=pp, Bp_im=Bp_im, Ct_im=Ct_im: h.matmul(pp[:, gg * 128:(gg + 1) * 128], lhsT=Bp_im.rearrange("p g i c -> p g (i c)")[lanes, g, :],
                                                                                  rhs=Ct_im.rearrange("p g i c -> p g (i c)")[lanes, g, :], start=False, stop=True),
                         reads=[b_arena], writes=[bpp])
            t1 = gslot(6).rearrange("p g i c -> p (g i c)")[:, 0:512]
            t2 = gslot(7).rearrange("p g i c -> p (g i c)")[:, 0:512]
            S.op("dve", lambda h, pF=pF, t1=t1: h.tensor_tensor(out=t1, in0=pF[:, :], in1=masks[:, 0, :], op=ALU.mult), reads=[bpF, b_masks, b_arena], writes=[b_arena])
            S.op("dve", lambda h, pB=pB, t2=t2: h.tensor_tensor(out=t2, in0=pB[:, :], in1=masks[:, 1, :], op=ALU.mult), reads=[bpB, b_masks, b_arena], writes=[b_arena])
            S.op("dve", lambda h, t1=t1, t2=t2: h.tensor_tensor(out=t1, in0=t1, in1=t2, op=ALU.add), reads=[b_arena], writes=[b_arena])
            for gg in range(4):
                g = g0 + g4 * 4 + gg
                S.op("dve", lambda h, g=g, gg=gg, t1=t1: h.scalar_tensor_tensor(out=D0m[:, g, :], in0=ident[:], scalar=drep[:, g:g + 1],
                                                                               in1=t1[:, gg * 128:(gg + 1) * 128], op0=ALU.mult, op1=ALU.add),
                     reads=[b_arena, b_ident, b_drep], writes=[b_D0])

    winu = sb("winu", [128, 8, 512], BF16); b_winu = Buf("winu")
    S.dma("pool", lambda h: h.dma_start(out=winu[:], in_=w_in[:, 0:512].rearrange("(k p) n -> p k n", p=128)), b_winu, writes=[b_winu])
    hTb = blockA[:, 0:4096].rearrange("p (k t) -> p k t", k=8)
    U_sb = blockA[:, 4096:8192]; b_Usb = Buf("U_sb")
    UT = blockB[:, 0:8192].bitcast(BF16).rearrange("p (g j) -> p g j", g=NG); b_UT = Buf("UT")
    UTo = blockA[:, 8192:10240].rearrange("p (g j) -> p g j", g=NG); b_UTo = Buf("UTo")
    Zoth = blockA[:, 10240:14336].rearrange("p (j e) -> p j e", e=64); b_Zoth = Buf("Zoth")
    Zctx = blockA[:, 14336:16384].rearrange("p (j e) -> p j e", e=64); b_Zctx = Buf("Zctx")
    Zown = arena[:, :].bitcast(BF16).rearrange("p (j e) -> p j e", e=64)
    b_Zown = b_arena
    cur = [sb(f"cur{i}", [128, NG, 2]) for i in range(2)]; b_cur = [Buf(f"cur{i}") for i in range(2)]
    st1 = sb("st1", [128, NG, 2]); st2 = sb("st2", [128, NG, 2]); b_st = Buf("st12")
    S.op("dve", lambda h: h.memset(cur[0][:], 0.0), writes=[b_cur[0]])
    scan_k = [0]

    def scan_step(zap, bz, lanes, store):
        k = scan_k[0]
        scan_k[0] += 1
        c0, bc0 = cur[k % 2], b_cur[k % 2]
        c1, bc1 = cur[(k + 1) % 2], b_cur[(k + 1) % 2]
        L = lanes
        sw = mkap(c0[L, :, 1:2], 0, [[2, NG], [-1, 2]])
        S.op("dve", lambda h: h.tensor_tensor(out=st1[L], in0=c0[L], in1=A1[L], op=ALU.mult), reads=[bc0, b_A], writes=[b_st])
        S.op("dve", lambda h: h.tensor_tensor(out=st2[L], in0=sw, in1=A2[L], op=ALU.mult), reads=[bc0, b_A, b_st], writes=[b_st])
        S.op("dve", lambda h: h.tensor_tensor(out=st1[L], in0=st1[L], in1=st2[L], op=ALU.add), reads=[b_st], writes=[b_st])
        S.op("dve", lambda h: h.tensor_tensor(out=c1[L], in0=st1[L], in1=zap, op=ALU.add), reads=[b_st, bz], writes=[bc1])
        if store:
            S.op("dve", lambda h: h.tensor_copy(out=zap, in_=c0[L]), reads=[bc0, bz], writes=[bz])
        if L != slice(0, 128):
            other = slice(64, 128) if L == slice(0, 64) else slice(0, 64)
            S.op("dve", lambda h: h.tensor_copy(out=c1[other], in_=c0[other]), reads=[bc0], writes=[bc1])

    def phaseA_block(xsrc, possrc, t0, ntok, sh_fn, sc_fn, UTdst, bUT, J0):
        ntile = ntok // 128
        nJ = ntok // 8
        for tl in range(ntile):
            xt, bx = load_tile(xsrc, possrc, t0 + tl * 128)
            ln_stats(xt, bx, xn_t, b_xn)
            transpose_mod(xn_t, b_xn, lambda kc, tl=tl: hTb[:, kc, tl * 128:(tl + 1) * 128], b_hTb, sc_fn, sh_fn, [b_modfm])
        for i in range(8):
            ps, bps = ps_next()
            for kc in range(8):
                S.op("pe", lambda h, ps=ps, kc=kc, i=i: h.matmul(ps[0:nJ, :], lhsT=mkap(hTb[:, kc, i:i + 1], 0, [[8, nJ]]), rhs=winu[:, kc, :],
                                                                start=(kc == 0), stop=(kc == 7)),
                     reads=[b_hTb, b_winu], writes=[bps], mode=f"up{nJ}")
            S.op("act", lambda h, ps=ps, i=i: h.activation(out=mkap(U_sb[0:nJ, i * 16:i * 16 + 1], 0, [[128, NG], [1, 16]]), in_=ps[0:nJ, :].rearrange("p (g c) -> p g c", g=NG), func=AF.Identity), reads=[bps], writes=[b_Usb])
        for g8 in range(4):
            for gg in range(8):
                g = g8 * 8 + gg
                S.op("pe", lambda h, g=g, gg=gg: h.transpose(out=psB[:, gg * 128:gg * 128 + nJ], in_=U_sb[0:nJ, g * 128:(g + 1) * 128],
                                                             identity=identb[0:nJ, 0:nJ]),
                     reads=[b_Usb, b_identb], writes=[b_psB], mode=f"trb{nJ}")
            S.op("dve", lambda h, g8=g8: h.tensor_copy(out=UTdst[:, g8 * 8:(g8 + 1) * 8, J0:J0 + nJ],
                                                       in_=psB[:, :].rearrange("p (g j) -> p g j", g=8)[:, :, 0:nJ]),
                 reads=[b_psB], writes=[bUT])

    def z_block(UTsrc, bUT, J0, nJ, Zdst_fn, bZ, lanes_list):
        for g in range(NG):
            ps, bps = ps_next()
            S.op("pe", lambda h, ps=ps, g=g: h.matmul(ps[:, 0:nJ], lhsT=PTre[:, g, :], rhs=UTsrc[:, g, J0:J0 + nJ], start=True, stop=True),
                 reads=[b_PT, bUT], writes=[bps], mode="bulk")
            S.op("pe", lambda h, ps=ps, g=g: h.matmul(ps[:, 256:256 + nJ], lhsT=PTim[:, g, :], rhs=UTsrc[:, g, J0:J0 + nJ], start=True, stop=True),
                 reads=[b_PT, bUT, bps], writes=[bps], mode="bulk")
            for lanes in lanes_list:
                if lanes == LF:
                    S.op("act", lambda h, ps=ps, g=g, lanes=lanes: h.activation(
                        out=Zdst_fn(lanes, g), in_=ps[lanes, :].rearrange("p (r j) -> p r j", r=2)[:, :, 0:nJ], func=AF.Identity),
                        reads=[bps], writes=[bZ])
                else:
                    S.op("dve", lambda h, ps=ps, g=g, lanes=lanes: h.tensor_copy(
                        out=Zdst_fn(lanes, g), in_=ps[lanes, :].rearrange("p (r j) -> p r j", r=2)[:, :, 0:nJ]),
                        reads=[bps], writes=[bZ])

    def zdst(Zt, nJtot, Jbase, nJ):
        def fn(lanes, g):
            if lanes == LF:
                base = Zt[LF, Jbase:Jbase + 1, 2 * g:2 * g + 1]
                return mkap(base, 0, [[1, 2], [64, nJ]])
            base = Zt[LB, nJtot - 1 - Jbase:nJtot - Jbase, 2 * g:2 * g + 1]
            return mkap(base, 0, [[1, 2], [-64, nJ]])
        return fn

    ALL = slice(0, 128)
    sh1_fn = lambda kc: modfm[:, 0 + kc, 0:1]; sc1_fn = lambda kc: sc1p[:, kc, 0:1]
    csh1_fn = lambda kc: modfm[:, 0 + kc, 1:2]; csc1_fn = lambda kc: sc1p[:, kc, 1:2]
    phaseA_block(ctx_in, None, 0, 256, csh1_fn, csc1_fn, UTo, b_UTo, 0)
    z_block(UTo, b_UTo, 0, 32, zdst(Zctx, 32, 0, 32), b_Zctx, [LF, LB])
    for k in range(32):
        scan_step(Zctx[:, k, :].rearrange("p (g r) -> p g r", r=2), b_Zctx, ALL, False)
    for blk in range(8):
        phaseA_block(x_oth, pos_oth, blk * 512, 512, sh1_fn, sc1_fn, UTo, b_UTo, 0)
        z_block(UTo, b_UTo, 0, 64, zdst(Zoth, 64, 0, 64), b_Zoth, [LF])
        for k in range(64):
            scan_step(Zoth[LF, k, :].rearrange("p (g r) -> p g r", r=2), b_Zoth, LF, False)
    for blk in range(8):
        phaseA_block(x_own, pos_own, blk * 512, 512, sh1_fn, sc1_fn, UT, b_UT, blk * 64)
    for jb in range(4):
        z_block(UT, b_UT, jb * 128, 128, zdst(Zown, 512, jb * 128, 128), b_Zown, [LF, LB])
    for k in range(512):
        scan_step(Zown[:, k, :].rearrange("p (g r) -> p g r", r=2), b_Zown, ALL, True)


    Ysb = blockA[:, :].rearrange("p (g j) -> p g j", g=NG); b_Ysb = Buf("Ysb")
    TM = arena[:, 8192:10240].bitcast(BF16).rearrange("p (j c) -> p j c", j=8); b_TM = b_arena
    ya = arena[:, 0:8192].bitcast(BF16).rearrange("p (a t) -> p a t", a=4); b_ya = b_arena
    for g in range(NG):
        ps, bps = ps_next()
        S.op("pe", lambda h, ps=ps, g=g: h.matmul(ps[:, :], lhsT=D0m[:, g, :], rhs=UT[:, g, :], start=True, stop=False), reads=[b_D0, b_UT], writes=[bps])
        for lanes in (LF, LB):
            for ri, Qm in enumerate((Qre, Qimn)):
                if lanes == LF:
                    rhs = mkap(Zown[LF, 0:1, 2 * g + ri:2 * g + ri + 1], 0, [[64, 512]])
                else:
                    rhs = mkap(Zown[LB, 511:512, 2 * g + ri:2 * g + ri + 1], 0, [[-64, 512]])
                last = (lanes == LB and ri == 1)
                S.op("pe", lambda h, ps=ps, g=g, lanes=lanes, Qm=Qm, rhs=rhs, last=last: h.matmul(ps[:, :], lhsT=Qm[lanes, g, :], rhs=rhs, start=False, stop=last),
                     reads=[b_Q, b_Zown, bps], writes=[bps])
        S.op("act", lambda h, ps=ps, g=g: h.activation(out=Ysb[:, g, :], in_=ps[:, :], func=AF.Identity), reads=[bps], writes=[b_Ysb, b_hTb, b_Usb, b_UTo, b_Zoth, b_Zctx])
    gt = [arena[:, 10240 + i * 1024:10240 + (i + 1) * 1024] for i in range(2)]; b_gt = b_arena
    for jb in range(4):
        for g8 in range(4):
            for gg in range(8):
                g = g8 * 8 + gg
                S.op("pe", lambda h, g=g, gg=gg, jb=jb: h.transpose(out=psB[:, gg * 128:(gg + 1) * 128], in_=Ysb[:, g, jb * 128:(jb + 1) * 128], identity=identb[:]),
                     reads=[b_Ysb, b_identb], writes=[b_psB], mode="trb128")
            S.op("dve", lambda h, g8=g8: h.tensor_copy(
                out=mkap(TM[:, 0:1, g8 * 128:g8 * 128 + 1], 0, [[16, 8], [512, 8], [1, 16]]),
                in_=psB[:, :].rearrange("p (g j c) -> p g j c", g=8, j=8)), reads=[b_psB], writes=[b_TM])
        for ct in range(4):
            for j in range(8):
                S.op("pe", lambda h, ct=ct, j=j: h.transpose(out=psB[:, j * 128:(j + 1) * 128], in_=TM[:, j, ct * 128:(ct + 1) * 128], identity=identb[:]),
                     reads=[b_TM, b_identb], writes=[b_psB], mode="trb128")
            xin = psB[:, :]
            g0t, g1t = gt[0], gt[1]
            S.op("act", lambda h: h.activation(out=g0t, in_=xin, func=AF.Square), reads=[b_psB], writes=[b_gt])
            S.op("dve", lambda h: h.tensor_scalar(out=g0t, in0=g0t, scalar1=0.044715, scalar2=1.0, op0=ALU.mult, op1=ALU.add), reads=[b_gt], writes=[b_gt])
            S.op("dve", lambda h: h.tensor_tensor(out=g0t, in0=g0t, in1=xin, op=ALU.mult), reads=[b_gt, b_psB], writes=[b_gt])
            S.op("act", lambda h: h.activation(out=g1t, in_=g0t, func=AF.Sigmoid, scale=GELU_C), reads=[b_gt], writes=[b_gt])
            S.op("dve", lambda h, ct=ct, jb=jb: h.tensor_tensor(
                out=mkap(ya[:, ct, jb * 1024:jb * 1024 + 1], 0, [[1, 8], [8, 128]]),
                in0=g1t.rearrange("p (j J) -> p j J", j=8), in1=psB[:, :].rearrange("p (j J) -> p j J", j=8), op=ALU.mult),
                reads=[b_gt, b_psB], writes=[b_ya])

    if debug == "ya":
        b_dd = b_arena
        S.dma("sp", lambda h: h.dma_start(out=dbg_d, in_=ya.rearrange("p a b -> p (a b)")), b_dd, reads=[b_dd])
        S.final_wait("sp", [b_dd])
        S.emit()
        return nc

    def alias(new, olds):
        for o in olds:
            if o.writer is not None:
                new.readers.append(o.writer)
            new.readers.extend(o.readers)
        return new

    dead_A = [b_Ysb, b_hTb, b_Usb, b_UTo, b_Zoth, b_Zctx]
    dead_B = [b_UT, b_bc, b_tab, b_bbar, b_tmpb, b_W]
    b_blkA = alias(Buf("blkA"), dead_A)
    b_up0 = alias(Buf("up0"), [b_arena])
    gsec = blockA[:, :].bitcast(F32).rearrange("p (k n) -> p k n", k=8)
    crep = arena[:, 8192:9216].rearrange("p (k m) -> p k m", k=8)
    badrow = arena[:, 9216:10240]
    g1bc = blockB[:, 6144:7168]; g2bc = blockB[:, 7168:8192]; b_gbc = alias(Buf("gbc"), dead_B)
    lnp = blockB[:, 8192:10240]; b_lnp = alias(Buf("lnp"), dead_B)
    S.op("dve", lambda h: h.tensor_copy(out=crep, in_=mkap(csil[:], 0, [[1, 8], [0, 128]])), reads=[b_csil], writes=[b_up0])
    for sec, gdst in ((2, g1bc), (5, g2bc)):
        S.dma("sp", lambda h, sec=sec: h.dma_start(out=gsec, in_=w_ada[:, sec * D:(sec + 1) * D].rearrange("(k p) n -> p k n", p=128)),
              b_blkA, writes=[b_blkA])
        S.dma("sp", lambda h, sec=sec: h.dma_start(out=badrow, in_=b_ada[:, sec * D:(sec + 1) * D].partition_broadcast(128)),
              b_up0, writes=[b_up0])
        for nb in range(2):
            ps, bps = ps_next()
            for kc in range(8):
                S.op("pe", lambda h, ps=ps, kc=kc, nb=nb: h.matmul(ps[:, :], lhsT=crep[:, kc, :], rhs=gsec[:, kc, nb * 512:(nb + 1) * 512],
                                                                  start=(kc == 0), stop=(kc == 7)),
                     reads=[b_up0, b_blkA], writes=[bps])
            S.op("dve", lambda h, ps=ps, nb=nb, gdst=gdst: h.tensor_tensor(out=gdst[:, nb * 512:(nb + 1) * 512], in0=ps[:, :],
                                                                          in1=badrow[:, nb * 512:(nb + 1) * 512], op=ALU.add),
                 reads=[bps, b_up0], writes=[b_gbc])
    S.dma("sp", lambda h: h.dma_start(out=lnp, in_=lnp_in[:, 0:2048].partition_broadcast(128)), b_lnp, writes=[b_lnp])

    winA = blockA[:, :].rearrange("p (k n) -> p k n", k=8)
    winB = blockB[:, 0:6144].bitcast(BF16).rearrange("p (k n) -> p k n", k=8)
    b_winB = alias(Buf("winB"), dead_B)
    for c0, c1, dst, bd in ((512, 1536, winA[:, :, 0:1024], b_blkA), (1536, 2560, winA[:, :, 1024:2048], b_blkA),
                            (2560, 3584, winB[:, :, 0:1024], b_winB), (3584, 4096, winB[:, :, 1024:1536], b_winB)):
        S.dma("pool", lambda h, c0=c0, c1=c1, dst=dst: h.dma_start(out=dst, in_=w_in[:, c0:c1].rearrange("(k p) n -> p k n", p=128)),
              bd, writes=[bd])
    flat = lambda t: t[:].rearrange("p g m -> p (g m)")
    wval = flat(PTre).rearrange("p (k n) -> p k n", k=4); wgate = flat(PTim).rearrange("p (k n) -> p k n", k=4)
    convo = flat(Qre).rearrange("p (k n) -> p k n", k=4)
    wo_lo = flat(Qimn).rearrange("p (k n) -> p k n", k=4); wo_hi = flat(D0m).rearrange("p (k n) -> p k n", k=4)
    b_wglu = alias(Buf("wglu"), [b_PT]); b_convo = alias(Buf("convo"), [b_Q]); b_wo = alias(Buf("wo"), [b_Q, b_D0])
    S.dma("pool", lambda h: h.dma_start(out=wval, in_=w_val.rearrange("(k p) n -> p k n", p=128)), b_wglu, writes=[b_wglu])
    S.dma("pool", lambda h: h.dma_start(out=wgate, in_=w_gate.rearrange("(k p) n -> p k n", p=128)), b_wglu, writes=[b_wglu])
    S.dma("pool", lambda h: h.dma_start(out=convo, in_=conv_out.rearrange("(k p) n -> p k n", p=128)), b_convo, writes=[b_convo])
    S.dma("pool", lambda h: h.dma_start(out=wo_lo, in_=w_o[0:512, :].rearrange("(k p) n -> p k n", p=128)), b_wo, writes=[b_wo])
    S.dma("pool", lambda h: h.dma_start(out=wo_hi, in_=w_o[512:1024, :].rearrange("(k p) n -> p k n", p=128)), b_wo, writes=[b_wo])
    rw32 = masks[:, 1, 0:288].rearrange("p (k n) -> p k n", k=8); b_rw = alias(Buf("rw32"), [b_masks])
    S.dma("sp", lambda h: h.dma_start(out=rw32, in_=rw_in.rearrange("(k p) n -> p k n", p=128)), b_rw, writes=[b_rw])
    rt = masks[:, 0, 0:160]; b_rt = alias(Buf("rt"), [b_masks])

    cst = masks[:, 1, 288:480]; b_cst = alias(Buf("cst"), [b_masks])
    S.dma("sp", lambda h: h.dma_start(out=cst, in_=cst_in), b_cst, writes=[b_cst])
    RT = masks[:, 0, 256:384].rearrange("p (t f) -> p t f", f=4); b_RT = alias(Buf("RT"), [b_masks])
    DEST = masks[:, 0, 384:448].bitcast(I32).rearrange("p (t f) -> p t f", f=2); b_DEST = alias(Buf("DEST"), [b_masks])
    hT = arena[:, 8192:10240].bitcast(BF16).rearrange("p (k t) -> p k t", k=8); b_hT = b_up0
    merged = arena[:, 10240:12288].bitcast(BF16).rearrange("p (k t) -> p k t", k=8); b_merged = alias(Buf("merged"), [b_arena])
    gv = arena[:, 12288:13312].bitcast(BF16).rearrange("p (k t) -> p k t", k=4); b_gv = alias(Buf("gv"), [b_arena])
    tmp = [arena[:, 13312 + i * 512:13312 + (i + 1) * 512] for i in range(5)]
    b_tmp = [alias(Buf(f"tmp{i}"), [b_arena]) for i in range(5)]
    h2b = arena[:, 15872:16384].bitcast(BF16).rearrange("p (k t) -> p k t", k=8); b_h2b = alias(Buf("h2b"), [b_arena])
    wr_f = winu[:].rearrange("p k n -> p (k n)").bitcast(F32)
    A_all = wr_f[:, 0:1024].rearrange("p (t e) -> p t e", e=32); b_Aall = alias(Buf("A_all"), [b_winu])
    h2f = wr_f[:, 1024:2048].rearrange("p (k t) -> p k t", k=8); b_h2f = alias(Buf("h2f"), [b_winu])
    b_x1s = [Buf(f"x1s{i}") for i in range(32)]
    b_h2s = [Buf(f"h2s{i}") for i in range(32)]
    BIG = 1.0e30

    def proj(ft, rhs_ap, brhs):
        piece, off, bp = (winA, (ft - 4) * 128, b_blkA) if ft < 20 else (winB, (ft - 20) * 128, b_winB)
        ps, bps = ps_next()
        for kc in range(8):
            S.op("pe", lambda h, ps=ps, kc=kc, piece=piece, off=off: h.matmul(ps[:, :], lhsT=piece[:, kc, off:off + 128], rhs=rhs_ap(kc),
                                                                             start=(kc == 0), stop=(kc == 7)),
                 reads=[bp, brhs], writes=[bps], mode="bulk")
        return ps, bps

    def mm4(wt, bw, dt, rhs_fn, brhs):
        ps, bps = ps_next()
        for ct in range(4):
            S.op("pe", lambda h, ps=ps, ct=ct: h.matmul(ps[:, :], lhsT=wt[:, ct, dt * 128:(dt + 1) * 128], rhs=rhs_fn(ct), start=(ct == 0), stop=(ct == 3)),
                 reads=[bw, brhs], writes=[bps], mode="bulk")
        return ps, bps

    def dv(fn, r, w):
        S.op("dve", fn, reads=r, writes=w)

    for tb in range(8):
        t0 = tb * 512
        for tl in range(4):
            xt, bx = load_tile(x_own, pos_own, t0 + tl * 128)
            ln_stats(xt, bx, xn_t, b_xn)
            transpose_mod(xn_t, b_xn, lambda kc, tl=tl: hT[:, kc, tl * 128:(tl + 1) * 128], b_hT, sc1_fn, sh1_fn, [b_modfm])
        hrhs = lambda kc: hT[:, kc, :]
        for ct in range(4):
            pz, bpz = proj(4 + ct, hrhs, b_hT)
            pgc, bpgc = proj(12 + ct, hrhs, b_hT)
            S.op("act", lambda h, pgc=pgc: h.activation(out=tmp[0], in_=pgc[:, :], func=AF.Identity), reads=[bpgc], writes=[b_tmp[0]])
            dv(lambda h, pz=pz: h.tensor_tensor(out=tmp[1], in0=pz[:, :], in1=tmp[0], op=ALU.mult), [bpz, b_tmp[0]], [b_tmp[1]])
            dv(lambda h, ct=ct: h.tensor_scalar(out=tmp[2], in0=tmp[1], scalar1=convw[:, ct, 1:2], scalar2=None, op0=ALU.mult), [b_tmp[1], b_convw], [b_tmp[2]])
            zv = tmp[1].rearrange("p (r c) -> p r c", c=64); vv = tmp[2].rearrange("p (r c) -> p r c", c=64)
            dv(lambda h, ct=ct, zv=zv, vv=vv: h.scalar_tensor_tensor(out=vv[:, :, 1:64], in0=zv[:, :, 0:63], scalar=convw[:, ct, 0:1], in1=vv[:, :, 1:64],
                                                                     op0=ALU.mult, op1=ALU.add), [b_tmp[1], b_tmp[2], b_convw], [b_tmp[2]])
            dv(lambda h, ct=ct, zv=zv, vv=vv: h.scalar_tensor_tensor(out=vv[:, :, 0:63], in0=zv[:, :, 1:64], scalar=convw[:, ct, 2:3], in1=vv[:, :, 0:63],
                                                                     op0=ALU.mult, op1=ALU.add), [b_tmp[1], b_tmp[2], b_convw], [b_tmp[2]])
            pgb, bpgb = proj(8 + ct, hrhs, b_hT)
            dv(lambda h, ct=ct, pgb=pgb: h.tensor_tensor(out=gv[:, ct, :], in0=pgb[:, :], in1=tmp[2], op=ALU.mult), [bpgb, b_tmp[2]], [b_gv])
        for dt in range(8):
            pob, bpob = mm4(convo, b_convo, dt, lambda ct: gv[:, ct, :], b_gv)
            pval, bpval = mm4(wval, b_wglu, dt, lambda ct, t0=t0: ya[:, ct, t0:t0 + 512], b_arena)
            pgt, bpgt = mm4(wgate, b_wglu, dt, lambda ct, t0=t0: ya[:, ct, t0:t0 + 512], b_arena)
            pma, bpma = proj(16 + dt, hrhs, b_hT)
            pmb, bpmb = proj(24 + dt, hrhs, b_hT)
            S.op("act", lambda h, pgt=pgt: h.activation(out=tmp[0], in_=pgt[:, :], func=AF.Sigmoid), reads=[bpgt], writes=[b_tmp[0]])
            S.op("act", lambda h, pma=pma: h.activation(out=tmp[1], in_=pma[:, :], func=AF.Sigmoid), reads=[bpma], writes=[b_tmp[1]])
            S.op("act", lambda h, pmb=pmb: h.activation(out=tmp[2], in_=pmb[:, :], func=AF.Sigmoid), reads=[bpmb], writes=[b_tmp[2]])
            dv(lambda h, pval=pval: h.tensor_tensor(out=tmp[3], in0=pval[:, :], in1=tmp[0], op=ALU.mult), [bpval, b_tmp[0]], [b_tmp[3]])
            dv(lambda h: h.tensor_tensor(out=tmp[3], in0=tmp[3], in1=tmp[1], op=ALU.mult), [b_tmp[3], b_tmp[1]], [b_tmp[3]])
            dv(lambda h, pob=pob: h.tensor_tensor(out=tmp[4], in0=pob[:, :], in1=tmp[2], op=ALU.mult), [bpob, b_tmp[2]], [b_tmp[4]])
            dv(lambda h, dt=dt: h.tensor_tensor(out=merged[:, dt, :], in0=tmp[3], in1=tmp[4], op=ALU.add), [b_tmp[3], b_tmp[4]], [b_merged])
        def w_o_pe(tl):
            outs = []
            for nb in range(2):
                ps, bps = ps_next()
                for dt in range(8):
                    wsl = wo_lo[:, dt, nb * 512:(nb + 1) * 512] if dt < 4 else wo_hi[:, dt - 4, nb * 512:(nb + 1) * 512]
                    S.op("pe", lambda h, ps=ps, dt=dt, tl=tl, wsl=wsl: h.matmul(ps[:, :], lhsT=merged[:, dt, tl * 128:(tl + 1) * 128], rhs=wsl,
                                                                               start=(dt == 0), stop=(dt == 7)),
                         reads=[b_merged, b_wo], writes=[bps], mode="bulk")
                outs.append((ps, bps))
            return outs
        tiles_ = [load_tile(x_own, pos_own, (tb * 4) * 128)]
        pend_ = w_o_pe(0)
        for tl in range(4):
            gti = tb * 4 + tl
            r0 = gti * 128
            xt, bx = tiles_[tl]
            cur_ = pend_
            if tl < 3:
                tiles_.append(load_tile(x_own, pos_own, r0 + 128))
                pend_ = w_o_pe(tl + 1)
            for nb in range(2):
                ps, bps = cur_[nb]
                dv(lambda h, ps=ps, nb=nb: h.tensor_tensor(out=xn_t[:, nb * 512:(nb + 1) * 512], in0=ps[:, :], in1=g1bc[:, nb * 512:(nb + 1) * 512], op=ALU.mult),
                   [bps, b_gbc], [b_xn])
            dv(lambda h, xt=xt: h.scalar_tensor_tensor(out=xt[:, :], in0=xt[:, :], scalar=ALPHA, in1=xn_t[:, :], op0=ALU.mult, op1=ALU.add), [bx, b_xn], [bx])
            ln_stats(xt, bx, xn_t, b_xn)
            dv(lambda h: h.tensor_tensor(out=xn_t[:, :], in0=xn_t[:, :], in1=lnp[:, 0:1024], op=ALU.mult), [b_xn, b_lnp], [b_xn])
            dv(lambda h, xt=xt: h.tensor_tensor(out=xt[:, :], in0=xn_t[:, :], in1=lnp[:, 1024:2048], op=ALU.add), [b_xn, b_lnp], [bx])
            S.dma("sp", lambda h, xt=xt, r0=r0: h.dma_start(out=x1s[r0:r0 + 128, :], in_=xt[:, :]), bx, reads=[bx], writes=[b_x1s[gti]])
            ln_stats(xt, bx, xn_t, b_xn)
            for kc in range(8):
                S.op("pe", lambda h, kc=kc: h.transpose(out=psT[:, kc * 128:(kc + 1) * 128], in_=xn_t[:, kc * 128:(kc + 1) * 128], identity=ident[:]),
                     reads=[b_xn, b_ident], writes=[b_psT], mode="trf")
            for kc in range(8):
                S.op("act", lambda h, kc=kc: h.activation(out=h2f[:, kc, :], in_=psT[:, kc * 128:(kc + 1) * 128], func=AF.Identity,
                                                         bias=modfm[:, 24 + kc, 0:1], scale=sc2p[:, kc:kc + 1]),
                     reads=[b_psT, b_modfm], writes=[b_h2f])
            ps, bps = ps_next()
            for kc in range(8):
                S.op("pe", lambda h, ps=ps, kc=kc: h.matmul(ps[:, 0:36], lhsT=h2f[:, kc, :], rhs=rw32[:, kc, :], start=(kc == 0), stop=(kc == 7)),
                     reads=[b_h2f, b_rw], writes=[bps])
            R = lambda a, b_: rt[:, a:b_]
            rr = [b_rt]
            dv(lambda h, ps=ps: h.tensor_tensor(out=R(0, 36), in0=ps[:, 0:36], in1=rb_bc[:, :], op=ALU.add), [bps, b_rb, b_rt], rr)
            dv(lambda h: h.tensor_reduce(out=R(36, 37), in_=R(0, 4), axis=AX.X, op=ALU.max), rr, rr)
            dv(lambda h: h.tensor_scalar(out=R(38, 42), in0=R(0, 4), scalar1=R(36, 37), scalar2=None, op0=ALU.is_equal), rr, rr)
            dv(lambda h: h.tensor_scalar(out=R(37, 38), in0=R(36, 37), scalar1=-1.0, scalar2=None, op0=ALU.mult), rr, rr)
            S.op("act", lambda h: h.activation(out=R(42, 46), in_=R(0, 4), func=AF.Exp, bias=R(37, 38), scale=1.0), reads=rr, writes=rr)
            dv(lambda h: h.tensor_reduce(out=R(46, 47), in_=R(42, 46), axis=AX.X, op=ALU.add), rr, rr)
            dv(lambda h: h.reciprocal(out=R(47, 48), in_=R(46, 47)), rr, rr)
            dv(lambda h: h.tensor_scalar(out=R(48, 52), in0=R(38, 42), scalar1=BIG, scalar2=-BIG, op0=ALU.mult, op1=ALU.add), rr, rr)
            dv(lambda h: h.tensor_tensor(out=R(52, 84).rearrange("p (g e) -> p g e", e=8), in0=R(4, 36).rearrange("p (g e) -> p g e", e=8),
                                         in1=mkap(R(48, 52), 0, [[1, 4], [0, 8]]), op=ALU.add), rr, rr)
            dv(lambda h: h.tensor_reduce(out=R(84, 85), in_=R(52, 84), axis=AX.X, op=ALU.max), rr, rr)
            dv(lambda h: h.tensor_scalar(out=R(85, 117), in0=R(52, 84), scalar1=R(84, 85), scalar2=None, op0=ALU.is_equal), rr, rr)
            dv(lambda h: h.scalar_tensor_tensor(out=R(117, 149), in0=R(85, 117), scalar=-BIG, in1=R(52, 84), op0=ALU.mult, op1=ALU.add), rr, rr)
            dv(lambda h: h.tensor_reduce(out=R(149, 150), in_=R(117, 149), axis=AX.X, op=ALU.max), rr, rr)
            dv(lambda h: h.tensor_scalar(out=R(52, 84), in0=R(117, 149), scalar1=R(149, 150), scalar2=None, op0=ALU.is_equal), rr, rr)
            dv(lambda h: h.tensor_tensor(out=R(150, 151), in0=R(149, 150), in1=R(84, 85), op=ALU.subtract), rr, rr)
            S.op("act", lambda h: h.activation(out=R(151, 152), in_=R(150, 151), func=AF.Exp), reads=rr, writes=rr)
            dv(lambda h: h.tensor_scalar(out=R(152, 153), in0=R(151, 152), scalar1=1.0, scalar2=None, op0=ALU.add), rr, rr)
            dv(lambda h: h.reciprocal(out=R(153, 154), in_=R(152, 153)), rr, rr)
            dv(lambda h: h.tensor_tensor(out=R(154, 155), in0=R(151, 152), in1=R(153, 154), op=ALU.mult), rr, rr)
            dv(lambda h: h.tensor_tensor(out=R(155, 156), in0=R(153, 154), in1=R(47, 48), op=ALU.mult), rr, rr)
            dv(lambda h: h.tensor_tensor(out=R(156, 157), in0=R(154, 155), in1=R(47, 48), op=ALU.mult), rr, rr)
            dv(lambda h, gti=gti: h.tensor_tensor(out=A_all[:, gti, :], in0=R(85, 117), in1=R(52, 84), op=ALU.add), rr + [b_Aall], [b_Aall])
            dv(lambda h: h.tensor_tensor(out=R(117, 149), in0=R(85, 117), in1=cst[:, 0:32], op=ALU.mult), rr + [b_cst], rr)
            dv(lambda h, gti=gti: h.tensor_reduce(out=RT[:, gti, 0:1], in_=R(117, 149), axis=AX.X, op=ALU.add), rr + [b_RT], [b_RT])
            dv(lambda h: h.tensor_tensor(out=R(117, 149), in0=R(52, 84), in1=cst[:, 0:32], op=ALU.mult), rr + [b_cst, b_RT], rr)
            dv(lambda h, gti=gti: h.tensor_reduce(out=RT[:, gti, 1:2], in_=R(117, 149), axis=AX.X, op=ALU.add), rr + [b_RT], [b_RT])
            dv(lambda h, gti=gti: h.tensor_copy(out=RT[:, gti, 2:4], in_=R(155, 157)), rr + [b_RT], [b_RT])

    IOA = bass.IndirectOffsetOnAxis
    NBLK = 48
    rA = blockA[:, :].bitcast(F32)
    b_R = alias(Buf("phaseR"), [b_blkA])
    Dt = rA[:, 4096:5120].rearrange("p (t e) -> p t e", e=32)
    OH = rA[:, 5120:6144].rearrange("p (t e) -> p t e", e=32)
    CMP = rA[:, 6144:7680]
    tri = rA[:, 7680:7936]
    sm_ = rA[:, 7936:8192]
    run = sm_[:, 0:32]; nblk = sm_[:, 32:64]; pe_ = sm_[:, 64:96]; psr = sm_[:, 96:128]; ones32 = sm_[:, 128:160]
    blkE = sm_[:, 160:208]; tE = sm_[:, 208:256]
    S.dma("sp", lambda h: h.dma_start(out=tri, in_=tri_in), b_R, writes=[b_R])
    rdv = lambda fn, extra=(): S.op("dve", fn, reads=[b_R] + list(extra), writes=[b_R])
    pT1, pT2 = ps_next(), ps_next()
    for hf_ in range(2):
        asl = A_all[:, hf_ * 16:(hf_ + 1) * 16, :].rearrange("p t e -> p (t e)")
        S.op("pe", lambda h, hf_=hf_, asl=asl: h.matmul(psT[:, hf_ * 512:(hf_ + 1) * 512], lhsT=tri[:, 0:128], rhs=asl, start=True, stop=True),
             reads=[b_R, b_Aall], writes=[b_psT])
        pt, bpt = (pT1, pT2)[hf_]
        S.op("pe", lambda h, pt=pt, asl=asl: h.matmul(pt[:, :], lhsT=tri[:, 128:256], rhs=asl, start=True, stop=True),
             reads=[b_R, b_Aall], writes=[bpt])
    rdv(lambda h: h.memset(run, 0.0))
    rdv(lambda h: h.memset(ones32, 1.0))
    for t in range(32):
        pt, bpt = (pT1, pT2)[t // 16]
        tt_ = t % 16
        rdv(lambda h, t=t: h.tensor_tensor(out=Dt[:, t, :], in0=psT[:, t * 32:(t + 1) * 32], in1=run, op=ALU.add), [b_psT])
        rdv(lambda h, pt=pt, tt_=tt_: h.tensor_tensor(out=run, in0=pt[:, tt_ * 32:(tt_ + 1) * 32], in1=run, op=ALU.add), [bpt])
    cmp8 = CMP[:, 0:256].rearrange("p (e j) -> p e j", j=8)
    rdv(lambda h: h.tensor_tensor(out=cmp8, in0=mkap(run, 0, [[1, 32], [0, 8]]), in1=mkap(cst[:, 72:80], 0, [[0, 32], [1, 8]]), op=ALU.is_gt), [b_cst])
    rdv(lambda h: h.tensor_reduce(out=nblk, in_=cmp8, axis=AX.X, op=ALU.add))
    rdv(lambda h: h.tensor_tensor_scan(out=pe_, data0=ones32, data1=nblk, initial=0.0, op0=ALU.mult, op1=ALU.add))
    rdv(lambda h: h.tensor_tensor(out=psr, in0=pe_, in1=nblk, op=ALU.subtract))
    rdv(lambda h: h.tensor_scalar(out=psr, in0=psr, scalar1=512.0, scalar2=None, op0=ALU.mult))
    rdv(lambda h: h.tensor_tensor(out=Dt, in0=Dt, in1=mkap(psr, 0, [[0, 32], [1, 32]]), op=ALU.add))
    destF = CMP[:, 256:320].rearrange("p (t f) -> p t f", f=2)
    for k in range(2):
        rdv(lambda h, k=k: h.tensor_tensor(out=OH, in0=mkap(cst[:, 0:32], 0, [[0, 32], [1, 32]]), in1=mkap(RT[:, 0, k:k + 1], 0, [[4, 32], [0, 32]]), op=ALU.is_equal),
            [b_cst, b_RT])
        rdv(lambda h: h.tensor_tensor(out=OH, in0=OH, in1=Dt, op=ALU.mult))
        rdv(lambda h, k=k: h.tensor_reduce(out=destF[:, :, k], in_=OH, axis=AX.X, op=ALU.add))
    S.op("dve", lambda h: h.tensor_copy(out=DEST, in_=destF), reads=[b_R], writes=[b_DEST])
    cmpb = CMP[:, 0:1536].rearrange("p (b e) -> p b e", e=32)
    rdv(lambda h: h.tensor_tensor(out=cmpb, in0=mkap(pe_, 0, [[0, NBLK], [1, 32]]), in1=mkap(cst[:, 0:NBLK], 0, [[1, NBLK], [0, 32]]), op=ALU.is_le), [b_cst, b_DEST])
    rdv(lambda h: h.tensor_reduce(out=blkE, in_=cmpb, axis=AX.X, op=ALU.add))
    rdv(lambda h: h.tensor_scalar(out=blkE, in0=blkE, scalar1=31.0, scalar2=None, op0=ALU.min))
    gif = CMP[:, 0:576]
    gif_g = gif[:, 0:384].rearrange("p (b k) -> p b k", k=8); gif_d = gif[:, 384:576].rearrange("p (b k) -> p b k", k=4)
    rdv(lambda h: h.tensor_scalar(out=tE, in0=blkE, scalar1=1024.0, scalar2=None, op0=ALU.mult))
    rdv(lambda h: h.tensor_tensor(out=gif_g, in0=mkap(tE, 0, [[1, NBLK], [0, 8]]), in1=mkap(cst[:, 64:72], 0, [[0, NBLK], [1, 8]]), op=ALU.add), [b_cst])
    rdv(lambda h: h.tensor_scalar(out=tE, in0=blkE, scalar1=512.0, scalar2=None, op0=ALU.mult))
    rdv(lambda h: h.tensor_tensor(out=gif_d, in0=mkap(tE, 0, [[1, NBLK], [0, 4]]), in1=mkap(cst[:, 64:68], 0, [[0, NBLK], [1, 4]]), op=ALU.add), [b_cst])
    GI = wr_f[:, 1024:1600].bitcast(I32); b_GI = alias(Buf("GI"), [b_h2f])
    GI_g = GI[:, 0:384].rearrange("p (b k) -> p b k", k=8); GI_d = GI[:, 384:576].rearrange("p (b k) -> p b k", k=4)
    S.op("dve", lambda h: h.tensor_copy(out=GI, in_=gif), reads=[b_R], writes=[b_GI])

    if debug == "d":
        dbgr = nc.dram_tensor("dbgr", [128, 32 * 4 + 64 + 576], F32, kind="ExternalOutput").ap()
        dd = rA[:, 4096:4096 + 768]
        S.op("dve", lambda h: h.tensor_copy(out=dd[:, 0:128], in_=RT.rearrange("p t f -> p (t f)")), reads=[b_R, b_RT], writes=[b_R])
        S.op("dve", lambda h: h.tensor_copy(out=dd[:, 128:192], in_=DEST.rearrange("p t f -> p (t f)")), reads=[b_R, b_DEST], writes=[b_R])
        S.op("dve", lambda h: h.tensor_copy(out=dd[:, 192:768], in_=GI), reads=[b_R, b_GI], writes=[b_R])
        S.dma("sp", lambda h: h.dma_start(out=dbgr, in_=dd), b_R, reads=[b_R])
        S.final_wait("sp", b_x1s + [b_R])
        S.emit()
        return nc

    xs = nc.dram_tensor("xs", [NBLK * 512, D], F32, kind="Internal").ap()
    ybd = nc.dram_tensor("ybd", [NBLK * 512, D], F32, kind="Internal").ap()
    b_xs = Buf("xs")
    for t in range(32):
        r0 = t * 128
        i = xctr[0] % 2
        xctr[0] += 1
        xt, bx = xt_t[i], b_xt[i]
        S.dma("sp", lambda h, xt=xt, r0=r0: h.dma_start(out=xt[:], in_=x1s[r0:r0 + 128, :]), bx, reads=[b_x1s[t]], writes=[bx])
        ln_stats(xt, bx, xn_t, b_xn)
        for k in range(2):
            S.dma("pool", lambda h, t=t, k=k: h.indirect_dma_start(out=xs[:, :], out_offset=IOA(ap=DEST[:, t, k:k + 1], axis=0), in_=xn_t[:, :], in_offset=None),
                  b_xs, reads=[b_xn, b_DEST], writes=[b_xs])

    lnp_e = lnp
    S.dma("sp", lambda h: h.dma_start(out=lnp_e, in_=lnp_in[:, 2048:4096].partition_broadcast(128)), b_lnp, writes=[b_lnp])
    xinb = [arena[:, i * 4096:(i + 1) * 4096].rearrange("p (t d) -> p t d", d=1024) for i in range(2)]
    b_xin = [alias(Buf(f"xin{i}"), [b_arena, b_up0, b_merged, b_gv, b_h2b] + b_tmp) for i in range(2)]
    ybuf = [arena[:, 8192 + i * 4096:8192 + (i + 1) * 4096].rearrange("p (t d) -> p t d", d=1024) for i in range(2)]
    b_ybuf = [alias(Buf(f"ybuf{i}"), [b_arena, b_up0, b_merged, b_gv, b_h2b] + b_tmp) for i in range(2)]
    b_ybd = [Buf("ybd0"), Buf("ybd1")]
    hblk = [blockA[:, i * 4096:(i + 1) * 4096].rearrange("p (k t) -> p k t", k=8) for i in range(2)]
    b_hblk = [alias(Buf(f"hblk{i}"), [b_blkA]) for i in range(2)]
    wbuf = [
        (flat(PTre).rearrange("p (k n) -> p k n", k=8), flat(PTim).rearrange("p (k n) -> p k n", k=8), flat(Qre).rearrange("p (k n) -> p k n", k=4)),
        (flat(Qimn).rearrange("p (k n) -> p k n", k=8), flat(D0m).rearrange("p (k n) -> p k n", k=8),
         blockB[:, 0:2048].bitcast(BF16).rearrange("p (k n) -> p k n", k=4)),
    ]
    b_wb = [alias(Buf("wb0"), [b_wglu, b_convo]), alias(Buf("wb1"), [b_wo, b_winB])]
    hid = [blockB[:, 2048 + i * 1024:2048 + (i + 1) * 1024].bitcast(BF16).rearrange("p (k t) -> p k t", k=4) for i in range(2)]
    b_hid = [alias(Buf(f"hid{i}"), [b_winB]) for i in range(2)]
    stmp = [blockB[:, 4096 + i * 512:4096 + (i + 1) * 512] for i in range(2)]
    b_stmp = [alias(Buf(f"stmp{i}"), [b_winB]) for i in range(2)]
    bregs = {}

    def breg(h, v):
        if v not in bregs:
            bregs[v] = h.to_reg(v)
        return bregs[v]
    wg_flat = wg_in.rearrange("e k n -> (e k) n"); wu_flat = wu_in.rearrange("e k n -> (e k) n"); wd_flat = wd_in.rearrange("e k n -> (e k) n")
    for b in range(NBLK):
        par = b % 2
        xi, bxi = xinb[par], b_xin[par]
        S.dma("sp", lambda h, b=b, xi=xi: h.dma_start(out=xi, in_=xs[b * 512:(b + 1) * 512, :].rearrange("(t p) d -> p t d", p=128)), bxi,
              reads=[b_xs], writes=[bxi])
        wg_t, wu_t, wd_t = wbuf[par]
        bw = b_wb[par]
        for kc in range(8):
            S.dma("pool", lambda h, b=b, kc=kc, wg_t=wg_t: h.indirect_dma_start(out=wg_t[:, kc, :], out_offset=None, in_=wg_flat[:, :],
                                                                             in_offset=IOA(ap=GI_g[:, b, kc:kc + 1], axis=0)), bw, reads=[b_GI], writes=[bw])
            S.dma("pool", lambda h, b=b, kc=kc, wu_t=wu_t: h.indirect_dma_start(out=wu_t[:, kc, :], out_offset=None, in_=wu_flat[:, :],
                                                                             in_offset=IOA(ap=GI_g[:, b, kc:kc + 1], axis=0)), bw, reads=[b_GI], writes=[bw])
        for fc in range(4):
            S.dma("pool", lambda h, b=b, fc=fc, wd_t=wd_t: h.indirect_dma_start(out=wd_t[:, fc, :], out_offset=None, in_=wd_flat[:, :],
                                                                             in_offset=IOA(ap=GI_d[:, b, fc:fc + 1], axis=0)), bw, reads=[b_GI], writes=[bw])
        hbk, bhbk = hblk[par], b_hblk[par]
        for tl in range(4):
            for kc in range(8):
                S.op("pe", lambda h, xi=xi, tl=tl, kc=kc: h.transpose(out=psT[:, kc * 128:(kc + 1) * 128], in_=xi[:, tl, kc * 128:(kc + 1) * 128], identity=ident[:]),
                     reads=[bxi, b_ident], writes=[b_psT], mode="trf")
            for kc in range(8):
                S.op("act", lambda h, hbk=hbk, tl=tl, kc=kc: h.activation(out=hbk[:, kc, tl * 128:(tl + 1) * 128], in_=psT[:, kc * 128:(kc + 1) * 128], func=AF.Identity,
                                                                         bias=modfm[:, 24 + kc, 0:1], scale=sc2p[:, kc:kc + 1]),
                     reads=[b_psT, b_modfm], writes=[bhbk])
        hb, bhb = hid[par], b_hid[par]
        for fc in range(4):
            pg, bpg = ps_next()
            for kc in range(8):
                S.op("pe", lambda h, pg=pg, kc=kc, fc=fc, wg_t=wg_t, hbk=hbk: h.matmul(pg[:, :], lhsT=wg_t[:, kc, fc * 128:(fc + 1) * 128], rhs=hbk[:, kc, :],
                                                                                      start=(kc == 0), stop=(kc == 7)), reads=[bw, bhbk], writes=[bpg], mode="bulk")
            pu, bpu = ps_next()
            for kc in range(8):
                S.op("pe", lambda h, pu=pu, kc=kc, fc=fc, wu_t=wu_t, hbk=hbk: h.matmul(pu[:, :], lhsT=wu_t[:, kc, fc * 128:(fc + 1) * 128], rhs=hbk[:, kc, :],
                                                                                      start=(kc == 0), stop=(kc == 7)), reads=[bw, bhbk], writes=[bpu], mode="bulk")
            st_, bst_ = stmp[fc % 2], b_stmp[fc % 2]
            S.op("act", lambda h, pg=pg, st_=st_: h.activation(out=st_, in_=pg[:, :], func=AF.Silu), reads=[bpg], writes=[bst_])
            dv(lambda h, pu=pu, st_=st_, hb=hb, fc=fc: h.tensor_tensor(out=hb[:, fc, :], in0=pu[:, :], in1=st_, op=ALU.mult), [bpu, bst_], [bhb])
        yb_, byb_ = ybuf[par], b_ybuf[par]
        for tl in range(4):
            for nb in range(2):
                pd, bpd = ps_next()
                for fc in range(4):
                    S.op("pe", lambda h, pd=pd, fc=fc, tl=tl, nb=nb, hb=hb, wd_t=wd_t: h.matmul(pd[:, :], lhsT=hb[:, fc, tl * 128:(tl + 1) * 128],
                                                                                             rhs=wd_t[:, fc, nb * 512:(nb + 1) * 512], start=(fc == 0), stop=(fc == 3)),
                         reads=[bhb, bw], writes=[bpd], mode="bulk")
                eng = "act" if (tl * 2 + nb) % 2 == 0 else "dve"
                if eng == "act":
                    S.op("act", lambda h, pd=pd, yb_=yb_, tl=tl, nb=nb: h.activation(out=yb_[:, tl, nb * 512:(nb + 1) * 512], in_=pd[:, :], func=AF.Identity),
                         reads=[bpd], writes=[byb_])
                else:
                    dv(lambda h, pd=pd, yb_=yb_, tl=tl, nb=nb: h.tensor_copy(out=yb_[:, tl, nb * 512:(nb + 1) * 512], in_=pd[:, :]), [bpd], [byb_])
        S.dma("sp", lambda h, b=b, yb_=yb_: h.dma_start(out=ybd[b * 512:(b + 1) * 512, :].rearrange("(t p) d -> p t d", p=128), in_=yb_), byb_,
              reads=[byb_], writes=[b_ybd[par]])

    y12 = [rA[:, 4096 + i * 1024:4096 + (i + 1) * 1024] for i in range(2)]
    b_y12 = [alias(Buf(f"y12_{i}"), [b_R]) for i in range(2)]
    b_out = [Buf(f"out{i}") for i in range(32)]
    for t in range(32):
        r0 = t * 128
        i = xctr[0] % 2
        xctr[0] += 1
        xt, bx = xt_t[i], b_xt[i]
        S.dma("sp", lambda h, xt=xt, r0=r0: h.dma_start(out=xt[:], in_=x1s[r0:r0 + 128, :]), bx, reads=[b_x1s[t]], writes=[bx])
        for k in range(2):
            S.dma("pool", lambda h, t=t, k=k: h.indirect_dma_start(out=y12[k], out_offset=None, in_=ybd[:, :], in_offset=IOA(ap=DEST[:, t, k:k + 1], axis=0)),
                  b_y12[k], reads=[b_ybd[0], b_ybd[1], b_DEST], writes=[b_y12[k]])
        dv(lambda h, t=t: h.tensor_scalar(out=y12[0], in0=y12[0], scalar1=RT[:, t, 2:3], scalar2=None, op0=ALU.mult), [b_y12[0], b_RT], [b_y12[0]])
        dv(lambda h, t=t: h.scalar_tensor_tensor(out=y12[0], in0=y12[1], scalar=RT[:, t, 3:4], in1=y12[0], op0=ALU.mult, op1=ALU.add), [b_y12[0], b_y12[1], b_RT], [b_y12[0]])
        dv(lambda h: h.tensor_tensor(out=y12[0], in0=y12[0], in1=g2bc, op=ALU.mult), [b_y12[0], b_gbc], [b_y12[0]])
        dv(lambda h, xt=xt: h.scalar_tensor_tensor(out=xt[:, :], in0=xt[:, :], scalar=ALPHA, in1=y12[0], op0=ALU.mult, op1=ALU.add), [bx, b_y12[0]], [bx])
        ln_stats(xt, bx, xn_t, b_xn)
        dv(lambda h: h.tensor_tensor(out=xn_t[:, :], in0=xn_t[:, :], in1=lnp_e[:, 0:1024], op=ALU.mult), [b_xn, b_lnp], [b_xn])
        dv(lambda h, xt=xt: h.tensor_tensor(out=xt[:, :], in0=xn_t[:, :], in1=lnp_e[:, 1024:2048], op=ALU.add), [b_xn, b_lnp], [bx])
        S.dma("sp", lambda h, xt=xt, r0=r0: h.dma_start(out=out_d[r0:r0 + 128, :], in_=xt[:, :]), bx, reads=[bx], writes=[b_out[t]])
    S.final_wait("sp", b_out)
    S.emit()
    return nc


def host_inputs(inputs):
    f32 = np.float32
    g = {k: np.asarray(v) for k, v in inputs.items()}
    D_ = 1024
    q = D_ // 4
    omega = (1.0 / (10000.0 ** (np.arange(q, dtype=f32) / f32(q)))).astype(f32)
    r = (np.arange(128, dtype=f32)[:, None] * omega).astype(f32)
    cl = (np.arange(64, dtype=f32)[:, None] * omega).astype(f32)
    r_emb = np.concatenate([np.sin(r), np.cos(r)], -1).astype(f32)
    c_emb = np.concatenate([np.sin(cl), np.cos(cl)], -1).astype(f32)
    pos = np.concatenate([np.broadcast_to(r_emb[:, None, :], (128, 64, 2 * q)),
                          np.broadcast_to(c_emb[None, :, :], (128, 64, 2 * q))], -1).reshape(8192, D_).astype(f32)
    ident = np.eye(128, dtype=f32)
    ii = np.arange(128) // 16
    mF = (ii[None, :] >= ii[:, None]).astype(f32)
    mB = (ii[:, None] >= ii[None, :]).astype(f32)
    masks = np.stack([np.tile(mF, (1, 4)), np.tile(mB, (1, 4))], 1).astype(f32)
    cst = np.zeros((128, 192), f32)
    cst[:, 0:64] = np.arange(64, dtype=f32)[None, :]
    cst[:, 64:72] = np.arange(8, dtype=f32)[None, :] * 128.0 + np.arange(128, dtype=f32)[:, None]
    cst[:, 72:80] = np.arange(8, dtype=f32)[None, :] * 512.0
    tri = np.zeros((128, 256), f32)
    tri[:, 0:128] = (np.arange(128)[:, None] < np.arange(128)[None, :]).astype(f32)
    tri[:, 128:256] = 1.0

    def tr(a):
        return np.ascontiguousarray(a.T)
    maps = []
    for core in range(8):
        b, hf = core // 2, core % 2
        xb = g["x"][b]
        if hf == 1:
            x_oth, x_own = xb[0:4096], xb[4096:8192]
            p_oth, p_own = pos[0:4096], pos[4096:8192]
            ctxl = g["ctx"][b]
            F, B = "f", "b"
            convw = g["conv_w"][0]
        else:
            x_oth, x_own = xb[4096:8192][::-1], xb[0:4096][::-1]
            p_oth, p_own = pos[4096:8192][::-1], pos[0:4096][::-1]
            ctxl = g["ctx"][b][::-1]
            F, B = "b", "f"
            convw = g["conv_w"][0][::-1]
        cT = np.concatenate([g["c"][b].reshape(8, 128).T, g["c_ctx"].reshape(8, 128).T], 1)
        small = np.zeros((128, 3, 32), f32)
        sbb = np.zeros((128, 2, 32, 16), f32)
        scc = np.zeros((128, 2, 32, 16), f32)
        for li, dname in ((0, F), (1, B)):
            L = slice(li * 64, li * 64 + 64)
            small[L, 0, :] = np.broadcast_to(g["s5_log_dt_" + dname][0][None, :], (64, 32))
            small[L, 1, :] = tr(g["s5_a_re_" + dname][0])
            small[L, 2, :] = tr(g["s5_a_im_" + dname][0])
            sbb[L, 0] = g["s5_b_re_" + dname][0].transpose(1, 0, 2)
            sbb[L, 1] = g["s5_b_im_" + dname][0].transpose(1, 0, 2)
            scc[L, 0] = g["s5_c_re_" + dname][0].transpose(2, 0, 1)
            scc[L, 1] = g["s5_c_im_" + dname][0].transpose(2, 0, 1)
        drep = np.tile(g["s5_d"][0].T, (8, 1))
        m = {
            "x_own": x_own, "x_oth": x_oth, "ctx": ctxl, "pos_own": p_own, "pos_oth": p_oth,
            "cT": cT, "w_ada": g["w_ada"][0], "b_adaT": g["b_ada"][0].reshape(48, 128).T, "b_ada": g["b_ada"][0][None, :],
            "w_in": g["w_in"][0], "s5_small": small, "s5_b": sbb, "s5_c": scc, "s5_drep": drep,
            "masks": masks, "ident": ident, "cst": cst, "tri": tri,
            "w_val": g["s5_w_glu_val"][0], "w_gate": g["s5_w_glu_gate"][0],
            "convwT": convw.reshape(3, 4, 128).transpose(2, 1, 0), "conv_out": g["conv_w_out"][0], "w_o": g["w_o"][0],
            "lnp": np.concatenate([g["ln1_g"][0], g["ln1_b"][0], g["ln2_g"][0], g["ln2_b"][0]])[None, :],
            "rw": np.concatenate([g["router_w_group"][0], g["router_w_expert"][0]], 1),
            "rb": np.concatenate([g["router_b_group"][0], g["router_b_expert"][0]])[None, :],
            "wg": g["exp_w_gate"][0], "wu": g["exp_w_up"][0], "wd": g["exp_w_down"][0],
        }
        maps.append({k: np.ascontiguousarray(v, dtype=f32) for k, v in m.items()})
    return maps


def kernel(**inputs):
    maps = host_inputs(inputs)
    nc = build()
    res = run_bass_kernel_spmd(nc, maps, core_ids=list(range(8)))
    out = np.zeros((4, 8192, 1024), np.float32)
    for core in range(8):
        b, hf = core // 2, core % 2
        o = res.results[core]["out"]
        if hf == 1:
            out[b, 4096:8192] = o
        else:
            out[b, 0:4096] = o[::-1]
    return out
```

```python
import math
import numpy as np
import concourse.bass as bass
import concourse.mybir as mybir
from concourse.bass_utils import run_bass_kernel_spmd

F32 = mybir.dt.float32
BF16 = mybir.dt.bfloat16
I32 = mybir.dt.int32
AF = mybir.ActivationFunctionType
ALU = mybir.AluOpType
AX = mybir.AxisListType

ALPHA = 2.0 ** 0.25
LN_EPS = 1e-6
NT = 4096
D = 1024
NG = 32
GELU_C = 2.0 * math.sqrt(2.0 / math.pi)


class Buf:
    __slots__ = ("name", "writer", "readers", "sem", "semcount")

    def __init__(self, name):
        self.name = name
        self.writer = None
        self.readers = []
        self.sem = None
        self.semcount = 0


class Sched:
    SEM_CAP = 30000

    def __init__(self, nc):
        self.nc = nc
        self.eng = {n: dict(prog=[], sem=None, count=0, waited={}, nsem=0) for n in ("pe", "act", "dve", "pool", "sp")}
        self.nbufsem = 0

    def _engsem(self, E, name):
        if E["sem"] is None or E["count"] >= self.SEM_CAP:
            E["sem"] = self.nc.alloc_semaphore(f"s_{name}_{E['nsem']}")
            E.setdefault("own", set()).add(id(E["sem"]))
            E["nsem"] += 1
            E["count"] = 0
        return E["sem"]

    def _waits(self, E, reads, writes):
        need = {}

        def add(tok):
            if tok is None:
                return
            s, v = tok
            k = id(s)
            if k not in need or need[k][1] < v:
                need[k] = (s, v)
        for b in reads:
            add(b.writer)
        for b in writes:
            add(b.writer)
            for r in b.readers:
                add(r)
        out = []
        for k, (s, v) in need.items():
            if E["waited"].get(k, 0) < v:
                E["waited"][k] = v
                out.append((s, v))
        return out

    def _commit(self, tok, reads, writes):
        for b in writes:
            b.writer = tok
            b.readers = []
        for b in reads:
            if b not in writes:
                b.readers.append(tok)
                if len(b.readers) > 48:
                    d = {}
                    for s, v in b.readers:
                        if id(s) not in d or d[id(s)][1] < v:
                            d[id(s)] = (s, v)
                    b.readers = list(d.values())

    def op(self, name, fn, reads=(), writes=(), mode=None):
        E = self.eng[name]
        waits = self._waits(E, reads, writes)
        if name == "pe":
            if mode is not None and E.get("last_mode") == mode:
                own = E.get("own", set())
                waits = [(s_, v_) for (s_, v_) in waits if id(s_) not in own]
            E["last_mode"] = mode
        sem = self._engsem(E, name)
        E["count"] += 1
        val = E["count"]

        def run(h, waits=waits, fn=fn, sem=sem):
            for s, v in waits:
                h.wait_ge(s, v)
            fn(h).then_inc(sem, 1)
        E["prog"].append(run)
        self._commit((sem, val), reads, writes)

    def dma(self, qname, fn, owner, reads=(), writes=()):
        E = self.eng[qname]
        waits = self._waits(E, reads, writes)
        if owner.sem is None:
            owner.sem = self.nc.alloc_semaphore(f"d_{self.nbufsem}")
            self.nbufsem += 1
        owner.semcount += 16
        sem, val = owner.sem, owner.semcount

        def run(h, waits=waits, fn=fn, sem=sem):
            for s, v in waits:
                h.wait_ge(s, v)
            fn(h).then_inc(sem, 16)
        E["prog"].append(run)
        self._commit((sem, val), reads, writes)

    def final_wait(self, qname, bufs):
        E = self.eng[qname]
        waits = self._waits(E, bufs, ())

        def run(h, waits=waits):
            for s, v in waits:
                h.wait_ge(s, v)
        E["prog"].append(run)

    def emit(self):
        with self.nc.Block() as block:
            @block.tensor
            def _(h):
                for f in self.eng["pe"]["prog"]:
                    f(h)

            @block.scalar
            def _(h):
                for f in self.eng["act"]["prog"]:
                    f(h)

            @block.vector
            def _(h):
                for f in self.eng["dve"]["prog"]:
                    f(h)

            @block.gpsimd
            def _(h):
                for f in self.eng["pool"]["prog"]:
                    f(h)

            @block.sync
            def _(h):
                for f in self.eng["sp"]["prog"]:
                    f(h)


def mkap(base, off_elems, dims):
    return bass.AP(base.tensor, base.offset + off_elems, [list(base.ap[0])] + [list(d) for d in dims])


def build(debug=None):
    nc = bass.Bass("TRN2", target_bir_lowering=False)
    S = Sched(nc)

    def din(name, shape, dt=F32):
        return nc.dram_tensor(name, list(shape), dt, kind="ExternalInput").ap()

    x_own = din("x_own", [NT, D]); x_oth = din("x_oth", [NT, D]); ctx_in = din("ctx", [256, D])
    pos_own = din("pos_own", [NT, D]); pos_oth = din("pos_oth", [NT, D])
    cT_in = din("cT", [128, 16])
    w_ada = din("w_ada", [D, 6 * D]); b_adaT = din("b_adaT", [128, 48]); b_ada = din("b_ada", [1, 6 * D])
    w_in = din("w_in", [D, 4096])
    s5_small = din("s5_small", [128, 3, NG])
    s5_b = din("s5_b", [128, 2, NG, 16]); s5_c = din("s5_c", [128, 2, NG, 16]); s5_drep = din("s5_drep", [128, NG])
    masks_in = din("masks", [128, 2, 512]); ident_in = din("ident", [128, 128]); cst_in = din("cst", [128, 192]); tri_in = din("tri", [128, 256])
    w_val = din("w_val", [512, D]); w_gate = din("w_gate", [512, D])
    convw_in = din("convwT", [128, 4, 3]); conv_out = din("conv_out", [512, D]); w_o = din("w_o", [D, D])
    lnp_in = din("lnp", [1, 4 * D])
    rw_in = din("rw", [D, 36]); rb_in = din("rb", [1, 36])
    wg_in = din("wg", [32, D, 512]); wu_in = din("wu", [32, D, 512]); wd_in = din("wd", [32, 512, D])
    out_d = nc.dram_tensor("out", [NT, D], F32, kind="ExternalOutput").ap()
    x1s = nc.dram_tensor("x1s", [NT, D], F32, kind=("ExternalOutput" if debug else "Internal")).ap()
    h2s = nc.dram_tensor("h2s", [128, 8, NT], BF16, kind=("ExternalOutput" if debug else "Internal")).ap()
    wrs = nc.dram_tensor("wrs", [128, 32, 32], F32, kind="ExternalOutput").ap() if debug else None
    dbg_d = None
    if debug == "ya":
        dbg_d = nc.dram_tensor("dbg", [128, 4 * NT], BF16, kind="ExternalOutput").ap()

    def sb(name, shape, dt=F32):
        return nc.alloc_sbuf_tensor("sb_" + name, list(shape), dt)

    ident = sb("ident", [128, 128]); b_ident = Buf("ident")
    identb = sb("identb", [128, 128], BF16); b_identb = Buf("identb")
    masks = sb("masks", [128, 2, 512]); b_masks = Buf("masks")
    modfm = sb("modfm", [128, 48, 2]); b_modfm = Buf("modfm")
    sc1p = sb("sc1p", [128, 8, 2]); sc2p = sb("sc2p", [128, 8])
    convw = sb("convw", [128, 4, 3]); b_convw = Buf("convw")
    rb_bc = sb("rb_bc", [128, 36]); b_rb = Buf("rb")
    epst = sb("epst", [128, 1]); b_eps = Buf("eps")

    S.dma("sp", lambda h: h.dma_start(out=ident[:], in_=ident_in), b_ident, writes=[b_ident])
    S.dma("sp", lambda h: h.dma_start(out=masks[:], in_=masks_in), b_masks, writes=[b_masks])
    S.dma("sp", lambda h: h.dma_start(out=convw[:], in_=convw_in), b_convw, writes=[b_convw])
    S.dma("sp", lambda h: h.dma_start(out=rb_bc[:], in_=rb_in.partition_broadcast(128)), b_rb, writes=[b_rb])
    S.op("dve", lambda h: h.tensor_copy(out=identb[:], in_=ident[:]), reads=[b_ident], writes=[b_identb])
    S.op("dve", lambda h: h.memset(epst[:], LN_EPS), writes=[b_eps])

    psg = [nc.alloc_psum_tensor(f"psg{i}", [128, 512], F32) for i in range(5)]
    b_psg = [Buf(f"psg{i}") for i in range(5)]
    psT = nc.alloc_psum_tensor("psT", [128, 1024], F32); b_psT = Buf("psT")
    psB = nc.alloc_psum_tensor("psB", [128, 1024], BF16); b_psB = Buf("psB")
    pctr = [0]

    def ps_next():
        i = pctr[0] % 5
        pctr[0] += 1
        return psg[i], b_psg[i]

    cT = sb("cT", [128, 16]); b_cT = Buf("cT")
    csil = sb("csil", [128, 16]); b_csil = Buf("csil")
    csil2 = sb("csil2", [128, 8, 2]); b_csil2 = Buf("csil2")
    blockA = sb("blockA", [128, 16384], BF16)
    b_hTb = Buf("hTb")
    badT = sb("badT", [128, 48]); b_badT = Buf("badT")
    S.dma("sp", lambda h: h.dma_start(out=cT[:], in_=cT_in), b_cT, writes=[b_cT])
    S.dma("sp", lambda h: h.dma_start(out=badT[:], in_=b_adaT), b_badT, writes=[b_badT])
    S.op("act", lambda h: h.activation(out=csil[:], in_=cT[:], func=AF.Silu), reads=[b_cT], writes=[b_csil])
    S.op("dve", lambda h: h.tensor_copy(out=csil2[:, :, 0], in_=csil[:, 0:8]), reads=[b_csil], writes=[b_csil2])
    S.op("dve", lambda h: h.tensor_copy(out=csil2[:, :, 1], in_=csil[:, 8:16]), reads=[b_csil], writes=[b_csil2])

    arena = sb("arena", [128, 16384])
    b_arena = Buf("arena")
    wsec = arena[:, 0:8192].rearrange("p (k n) -> p k n", k=8)
    for sec in (0, 1, 3, 4):
        S.dma("sp", lambda h, sec=sec: h.dma_start(out=wsec, in_=w_ada[:, sec * D:(sec + 1) * D].rearrange("(k p) n -> p k n", p=128)),
              b_arena, writes=[b_arena])
        ps, bps = ps_next()
        for jj in range(8):
            for kc in range(8):
                S.op("pe", lambda h, ps=ps, kc=kc, jj=jj: h.matmul(ps[:, jj * 2:jj * 2 + 2], lhsT=wsec[:, kc, jj * 128:(jj + 1) * 128],
                                                                  rhs=csil2[:, kc, :], start=(kc == 0), stop=(kc == 7)),
                     reads=[b_csil2, b_arena], writes=[bps], mode="f32")
        S.op("dve", lambda h, ps=ps, sec=sec: h.tensor_tensor(
            out=modfm[:, sec * 8:(sec + 1) * 8, :], in0=ps[:, 0:16].rearrange("p (j t) -> p j t", t=2),
            in1=mkap(badT[:, sec * 8:(sec + 1) * 8], 0, [[1, 8], [0, 2]]), op=ALU.add),
            reads=[bps, b_badT], writes=[b_modfm])
    S.op("dve", lambda h: h.tensor_scalar(out=sc1p[:], in0=modfm[:, 8:16, :], scalar1=1.0, scalar2=None, op0=ALU.add), reads=[b_modfm], writes=[b_modfm])
    S.op("dve", lambda h: h.tensor_scalar(out=sc2p[:], in0=modfm[:, 32:40, 0], scalar1=1.0, scalar2=None, op0=ALU.add), reads=[b_modfm], writes=[b_modfm])

    xt_t = [sb(f"xt{i}", [128, D]) for i in range(2)]; b_xt = [Buf(f"xt{i}") for i in range(2)]
    xn_t = sb("xn", [128, D]); b_xn = Buf("xn")
    stt = sb("stt", [128, 16]); b_stt = Buf("stt")
    xctr = [0]

    def ln_stats(src, bsrc, dst, bdst):
        S.op("dve", lambda h: h.bn_stats(out=stt[:, 0:6], in_=src[:, 0:512]), reads=[bsrc], writes=[b_stt])
        S.op("dve", lambda h: h.bn_stats(out=stt[:, 6:12], in_=src[:, 512:1024]), reads=[bsrc], writes=[b_stt])
        S.op("dve", lambda h: h.bn_aggr(out=stt[:, 12:14], in_=stt[:, 0:12]), reads=[b_stt], writes=[b_stt])
        S.op("act", lambda h: h.activation(out=stt[:, 14:15], in_=stt[:, 13:14], func=AF.Sqrt, bias=epst[:, 0:1], scale=1.0),
             reads=[b_stt, b_eps], writes=[b_stt])
        S.op("dve", lambda h: h.reciprocal(out=stt[:, 15:16], in_=stt[:, 14:15]), reads=[b_stt], writes=[b_stt])
        S.op("dve", lambda h: h.tensor_scalar(out=dst[:, :], in0=src[:, :], scalar1=stt[:, 12:13], scalar2=stt[:, 15:16],
                                              op0=ALU.subtract, op1=ALU.mult), reads=[bsrc, b_stt], writes=[bdst])

    def transpose_mod(src, bsrc, dst_fn, bdst, scale_fn, shift_fn, bmods):
        for kc in range(8):
            S.op("pe", lambda h, kc=kc: h.transpose(out=psT[:, kc * 128:(kc + 1) * 128], in_=src[:, kc * 128:(kc + 1) * 128], identity=ident[:]),
                 reads=[bsrc, b_ident], writes=[b_psT], mode="trf")
        for kc in range(8):
            S.op("act", lambda h, kc=kc: h.activation(out=dst_fn(kc), in_=psT[:, kc * 128:(kc + 1) * 128], func=AF.Identity,
                                                     bias=shift_fn(kc), scale=scale_fn(kc)),
                 reads=[b_psT] + bmods, writes=[bdst])

    def load_tile(xsrc, possrc, r0):
        i = xctr[0] % 2
        xctr[0] += 1
        xt, bx = xt_t[i], b_xt[i]
        S.dma("sp", lambda h: h.dma_start(out=xt[:], in_=xsrc[r0:r0 + 128, :]), bx, writes=[bx])
        if possrc is not None:
            S.dma("pool", lambda h: h.dma_start(out=xt[:], in_=possrc[r0:r0 + 128, :], accum_op=ALU.add), bx, writes=[bx])
        return xt, bx

    blockB = sb("blockB", [128, 10240])
    sm = sb("s5sm", [128, 3, NG]); b_sm = Buf("s5sm")
    sbv = blockB[:, 0:1024].rearrange("p (r g c) -> p r g c", r=2, g=NG); scv = blockB[:, 1024:2048].rearrange("p (r g c) -> p r g c", r=2, g=NG); b_bc = Buf("s5bc")
    drep = sb("drep", [128, NG]); b_drep = Buf("drep")
    S.dma("sp", lambda h: h.dma_start(out=sm[:], in_=s5_small), b_sm, writes=[b_sm])
    S.dma("sp", lambda h: h.dma_start(out=sbv[:], in_=s5_b), b_bc, writes=[b_bc])
    S.dma("sp", lambda h: h.dma_start(out=scv[:], in_=s5_c), b_bc, writes=[b_bc])
    S.dma("sp", lambda h: h.dma_start(out=drep[:], in_=s5_drep), b_drep, writes=[b_drep])

    tb_ = blockB[:, 2048:2048 + 11 * NG * 9].rearrange("p (t g k) -> p t g k", t=11, g=NG); b_tab = Buf("s5tab")
    T_ANG, T_Q, T_SIN, T_COS, T_MAG, T_MAGN, T_WRE, T_WIM, T_VRE, T_VIM, T_TMP = range(11)
    qi = sb("s5qi", [128, NG * 9], I32)
    vec = sb("s5vec", [128, 12, NG]); b_vec = Buf("s5vec")
    V_DT, V_TH, V_LR, V_DEN, V_XRE, V_FRE, V_FIM, V_T1, V_T2, V_RDEN = range(10)

    def vop(fn, r=(b_vec,), w=(b_vec,)):
        S.op("dve", fn, reads=list(r), writes=list(w))

    S.op("act", lambda h: h.activation(out=vec[:, V_DT, :], in_=sm[:, 0, :], func=AF.Exp), reads=[b_sm], writes=[b_vec])
    vop(lambda h: h.tensor_tensor(out=vec[:, V_TH, :], in0=vec[:, V_DT, :], in1=sm[:, 2, :], op=ALU.mult), r=(b_vec, b_sm))
    vop(lambda h: h.tensor_tensor(out=vec[:, V_LR, :], in0=vec[:, V_DT, :], in1=sm[:, 1, :], op=ALU.mult), r=(b_vec, b_sm))
    T = lambda t: tb_[:, t, :, :]
    Tf = lambda t: tb_[:, t, :, :].rearrange("p g k -> p (g k)")
    for k in range(9):
        S.op("dve", lambda h, k=k: h.tensor_scalar(out=tb_[:, T_ANG, :, k], in0=vec[:, V_TH, :], scalar1=float(k), scalar2=None, op0=ALU.mult),
             reads=[b_vec], writes=[b_tab])
        S.op("act", lambda h, k=k: h.activation(out=tb_[:, T_MAG, :, k], in_=vec[:, V_LR, :], func=AF.Exp, scale=float(k)), reads=[b_vec], writes=[b_tab])
        S.op("act", lambda h, k=k: h.activation(out=tb_[:, T_MAGN, :, k], in_=vec[:, V_LR, :], func=AF.Exp, scale=-float(k)), reads=[b_vec], writes=[b_tab])

    def sin_of(dst_t, shift):
        top = lambda fn: S.op("dve", fn, reads=[b_tab], writes=[b_tab])
        top(lambda h: h.tensor_scalar(out=Tf(T_TMP), in0=Tf(T_ANG), scalar1=shift, scalar2=None, op0=ALU.add))
        top(lambda h: h.tensor_scalar(out=qi[:], in0=Tf(T_TMP), scalar1=1.0 / (2 * math.pi), scalar2=None, op0=ALU.mult))
        top(lambda h: h.tensor_copy(out=Tf(T_Q), in_=qi[:]))
        top(lambda h: h.scalar_tensor_tensor(out=Tf(T_TMP), in0=Tf(T_Q), scalar=-2 * math.pi, in1=Tf(T_TMP), op0=ALU.mult, op1=ALU.add))
        top(lambda h: h.tensor_scalar(out=Tf(T_Q), in0=Tf(T_TMP), scalar1=math.pi, scalar2=2 * math.pi, op0=ALU.is_gt, op1=ALU.mult))
        top(lambda h: h.tensor_tensor(out=Tf(T_TMP), in0=Tf(T_TMP), in1=Tf(T_Q), op=ALU.subtract))
        top(lambda h: h.tensor_scalar(out=Tf(T_Q), in0=Tf(T_TMP), scalar1=-math.pi, scalar2=2 * math.pi, op0=ALU.is_lt, op1=ALU.mult))
        top(lambda h: h.tensor_tensor(out=Tf(T_TMP), in0=Tf(T_TMP), in1=Tf(T_Q), op=ALU.add))
        S.op("act", lambda h: h.activation(out=Tf(dst_t), in_=Tf(T_TMP), func=AF.Sin), reads=[b_tab], writes=[b_tab])

    sin_of(T_SIN, 0.0)
    sin_of(T_COS, math.pi / 2)
    tt = lambda o, a, b, op: S.op("dve", lambda h: h.tensor_tensor(out=Tf(o), in0=Tf(a), in1=Tf(b), op=op), reads=[b_tab], writes=[b_tab])
    tt(T_WRE, T_MAG, T_COS, ALU.mult)
    tt(T_WIM, T_MAG, T_SIN, ALU.mult)
    tt(T_VRE, T_MAGN, T_COS, ALU.mult)
    tt(T_VIM, T_MAGN, T_SIN, ALU.mult)
    S.op("dve", lambda h: h.tensor_scalar(out=Tf(T_VIM), in0=Tf(T_VIM), scalar1=-1.0, scalar2=None, op0=ALU.mult), reads=[b_tab], writes=[b_tab])
    are = sm[:, 1, :]; aim = sm[:, 2, :]
    abre = tb_[:, T_WRE, :, 1]; abim = tb_[:, T_WIM, :, 1]
    vr = (b_vec, b_sm, b_tab)
    vop(lambda h: h.tensor_tensor(out=vec[:, V_DEN, :], in0=are, in1=are, op=ALU.mult), r=vr)
    vop(lambda h: h.tensor_tensor(out=vec[:, V_T1, :], in0=aim, in1=aim, op=ALU.mult), r=vr)
    vop(lambda h: h.tensor_tensor(out=vec[:, V_DEN, :], in0=vec[:, V_DEN, :], in1=vec[:, V_T1, :], op=ALU.add), r=vr)
    vop(lambda h: h.reciprocal(out=vec[:, V_RDEN, :], in_=vec[:, V_DEN, :]), r=vr)
    vop(lambda h: h.tensor_scalar(out=vec[:, V_XRE, :], in0=abre, scalar1=-1.0, scalar2=None, op0=ALU.add), r=vr)
    vop(lambda h: h.tensor_tensor(out=vec[:, V_T1, :], in0=vec[:, V_XRE, :], in1=are, op=ALU.mult), r=vr)
    vop(lambda h: h.tensor_tensor(out=vec[:, V_T2, :], in0=abim, in1=aim, op=ALU.mult), r=vr)
    vop(lambda h: h.tensor_tensor(out=vec[:, V_T1, :], in0=vec[:, V_T1, :], in1=vec[:, V_T2, :], op=ALU.add), r=vr)
    vop(lambda h: h.tensor_tensor(out=vec[:, V_FRE, :], in0=vec[:, V_T1, :], in1=vec[:, V_RDEN, :], op=ALU.mult), r=vr)
    vop(lambda h: h.tensor_tensor(out=vec[:, V_T1, :], in0=abim, in1=are, op=ALU.mult), r=vr)
    vop(lambda h: h.tensor_tensor(out=vec[:, V_T2, :], in0=vec[:, V_XRE, :], in1=aim, op=ALU.mult), r=vr)
    vop(lambda h: h.tensor_tensor(out=vec[:, V_T1, :], in0=vec[:, V_T1, :], in1=vec[:, V_T2, :], op=ALU.subtract), r=vr)
    vop(lambda h: h.tensor_tensor(out=vec[:, V_FIM, :], in0=vec[:, V_T1, :], in1=vec[:, V_RDEN, :], op=ALU.mult), r=vr)
    bbar = blockB[:, 5632:6656].rearrange("p (r g c) -> p r g c", r=2, g=NG); b_bbar = Buf("bbar")
    tmpb = blockB[:, 6656:7168].rearrange("p (g c) -> p g c", g=NG); b_tmpb = Buf("tmpb")
    fre_b = mkap(vec[:, V_FRE, :], 0, [[1, NG], [0, 16]]); fim_b = mkap(vec[:, V_FIM, :], 0, [[1, NG], [0, 16]])
    bo = lambda fn, r, w: S.op("dve", fn, reads=r, writes=w)
    bo(lambda h: h.tensor_tensor(out=bbar[:, 0], in0=sbv[:, 0], in1=fre_b, op=ALU.mult), [b_bc, b_vec], [b_bbar])
    bo(lambda h: h.tensor_tensor(out=tmpb[:], in0=sbv[:, 1], in1=fim_b, op=ALU.mult), [b_bc, b_vec], [b_tmpb])
    bo(lambda h: h.tensor_tensor(out=bbar[:, 0], in0=bbar[:, 0], in1=tmpb[:], op=ALU.subtract), [b_bbar, b_tmpb], [b_bbar])
    bo(lambda h: h.tensor_tensor(out=bbar[:, 1], in0=sbv[:, 1], in1=fre_b, op=ALU.mult), [b_bc, b_vec], [b_bbar])
    bo(lambda h: h.tensor_tensor(out=tmpb[:], in0=sbv[:, 0], in1=fim_b, op=ALU.mult), [b_bc, b_vec], [b_tmpb])
    bo(lambda h: h.tensor_tensor(out=bbar[:, 1], in0=bbar[:, 1], in1=tmpb[:], op=ALU.add), [b_bbar, b_tmpb], [b_bbar])

    WB, WBp, WC = [blockB[:, 7168 + i * 512:7168 + (i + 1) * 512].rearrange("p (r g k) -> p r g k", r=2, g=NG) for i in range(3)]; b_W = Buf("W")

    def fwd_slice(t, lanes, k0):
        return tb_[lanes, t, :, k0:k0 + 8]

    def rev_slice(t, lanes, k_hi):
        base = tb_[lanes, t, :, k_hi:k_hi + 1]
        return mkap(base, 0, [[9, NG], [-1, 8]])
    LF = slice(0, 64); LB = slice(64, 128)
    for ri, (tw, tv) in enumerate(((T_WRE, T_VRE), (T_WIM, T_VIM))):
        cp = lambda o, i_: S.op("dve", lambda h: h.tensor_copy(out=o, in_=i_), reads=[b_tab], writes=[b_W])
        cp(WB[LF, ri], rev_slice(tw, LF, 7))
        cp(WB[LB, ri], fwd_slice(tw, LB, 0))
        cp(WBp[LF, ri], fwd_slice(tv, LF, 1))
        cp(WBp[LB, ri], rev_slice(tv, LB, 8))
        cp(WC[LF, ri], fwd_slice(tw, LF, 1))
        cp(WC[LB, ri], rev_slice(tw, LB, 8))

    D0m = sb("D0m", [128, NG, 128], BF16); b_D0 = Buf("D0")
    PTre = sb("PTre", [128, NG, 128], BF16); PTim = sb("PTim", [128, NG, 128], BF16); b_PT = Buf("PT")
    Qre = sb("Qre", [128, NG, 128], BF16); Qimn = sb("Qimn", [128, NG, 128], BF16); b_Q = Buf("Q")
    A1 = sb("A1", [128, NG, 2]); A2 = sb("A2", [128, NG, 2]); b_A = Buf("A12")
    S.op("dve", lambda h: h.tensor_copy(out=A1[:], in_=mkap(tb_[:, T_WRE, :, 8:9], 0, [[9, NG], [0, 2]])), reads=[b_tab], writes=[b_A])
    S.op("dve", lambda h: h.tensor_copy(out=A2[:, :, 1], in_=tb_[:, T_WIM, :, 8]), reads=[b_tab], writes=[b_A])
    S.op("dve", lambda h: h.tensor_scalar(out=A2[:, :, 0], in0=tb_[:, T_WIM, :, 8], scalar1=-1.0, scalar2=None, op0=ALU.mult), reads=[b_tab], writes=[b_A])

    GC = 8
    gen = arena[:, 0:8192]

    def gslot(i):
        return gen[:, i * 1024:(i + 1) * 1024].rearrange("p (g i c) -> p g i c", g=GC, i=8)

    def cprod(Wt, X, g0, o_re, o_im, breads):
        def wv(ri):
            return mkap(Wt[:, ri, g0:g0 + GC, :], 0, [[8, GC], [1, 8], [0, 16]])

        def xv(ri):
            return mkap(X[:, ri, g0:g0 + GC, :], 0, [[16, GC], [0, 8], [1, 16]])
        t1 = gslot(6); t2 = gslot(7)
        o = lambda fn: S.op("dve", fn, reads=[b_W, b_arena] + breads, writes=[b_arena])
        o(lambda h: h.tensor_tensor(out=t1, in0=wv(0), in1=xv(0), op=ALU.mult))
        o(lambda h: h.tensor_tensor(out=t2, in0=wv(1), in1=xv(1), op=ALU.mult))
        o(lambda h: h.tensor_tensor(out=o_re, in0=t1, in1=t2, op=ALU.subtract))
        o(lambda h: h.tensor_tensor(out=t1, in0=wv(0), in1=xv(1), op=ALU.mult))
        o(lambda h: h.tensor_tensor(out=t2, in0=wv(1), in1=xv(0), op=ALU.mult))
        o(lambda h: h.tensor_tensor(out=o_im, in0=t1, in1=t2, op=ALU.add))

    for gch in range(NG // GC):
        g0 = gch * GC
        Bt_re, Bt_im, Bp_re, Bp_im, Ct_re, Ct_im = [gslot(i) for i in range(6)]
        cprod(WB, bbar, g0, Bt_re, Bt_im, [b_bbar])
        cprod(WBp, bbar, g0, Bp_re, Bp_im, [b_bbar])
        cprod(WC, scv, g0, Ct_re, Ct_im, [b_bc])
        fl = lambda a: a.rearrange("p g i c -> p g (i c)")
        S.op("act", lambda h, g0=g0, Ct_re=Ct_re: h.activation(out=Qre[:, g0:g0 + GC, :], in_=Ct_re.rearrange("p g i c -> p g (i c)"), func=AF.Identity), reads=[b_arena], writes=[b_Q])
        S.op("act", lambda h, g0=g0, Ct_im=Ct_im: h.activation(out=Qimn[:, g0:g0 + GC, :], in_=Ct_im.rearrange("p g i c -> p g (i c)"), func=AF.Identity, scale=-1.0), reads=[b_arena], writes=[b_Q])
        S.op("dve", lambda h, Ct_im=Ct_im: h.tensor_scalar(out=Ct_im.rearrange("p g i c -> p g (i c)"), in0=Ct_im.rearrange("p g i c -> p g (i c)"), scalar1=-1.0, scalar2=None, op0=ALU.mult), reads=[b_arena, b_Q], writes=[b_arena])
        for ri, (Bt, PT) in enumerate(((Bt_re, PTre), (Bt_im, PTim))):
            for gg in range(GC):
                S.op("pe", lambda h, gg=gg, Bt=Bt: h.transpose(out=psT[:, gg * 128:(gg + 1) * 128], in_=Bt.rearrange("p g i c -> p g (i c)")[:, gg, :], identity=ident[:]),
                     reads=[b_arena, b_ident], writes=[b_psT])
            S.op("act", lambda h, PT=PT, g0=g0: h.activation(out=PT[:, g0:g0 + GC, :], in_=psT[:, :].rearrange("p (g m) -> p g m", g=GC), func=AF.Identity),
                 reads=[b_psT], writes=[b_PT])
        for g4 in range(GC // 4):
            pF, bpF = ps_next(); pB, bpB = ps_next()
            for gg in range(4):
                g = g4 * 4 + gg
                for lanes, pp, bpp in ((LF, pF, bpF), (LB, pB, bpB)):
                    S.op("pe", lambda h, g=g, gg=gg, lanes=lanes, pp=pp, Bp_re=Bp_re, Ct_re=Ct_re: h.matmul(pp[:, gg * 128:(gg + 1) * 128], lhsT=Bp_re.rearrange("p g i c -> p g (i c)")[lanes, g, :],
                                                                                  rhs=Ct_re.rearrange("p g i c -> p g (i c)")[lanes, g, :], start=True, stop=False),
                         reads=[b_arena], writes=[bpp])
                    S.op("pe", lambda h, g=g, gg=gg, lanes=lanes, pp=pp, Bp_im=Bp_im, Ct_im=Ct_im: h.matmul(pp[:, gg * 128:(gg + 1) * 128], lhsT=Bp_im.rearrange("p g i c -> p g (i c)")[lanes, g, :],
                                                                                  rhs=Ct_im.rearrange("p g i c -> p g (i c)")[lanes, g, :], start=False, stop=True),
                         reads=[b_arena], writes=[bpp])
            t1 = gslot(6).rearrange("p g i c -> p (g i c)")[:, 0:512]
            t2 = gslot(7).rearrange("p g i c -> p (g i c)")[:, 0:512]
            S.op("dve", lambda h, pF=pF, t1=t1: h.tensor_tensor(out=t1, in0=pF[:, :], in1=masks[:, 0, :], op=ALU.mult), reads=[bpF, b_masks, b_arena], writes=[b_arena])
            S.op("dve", lambda h, pB=pB, t2=t2: h.tensor_tensor(out=t2, in0=pB[:, :], in1=masks[:, 1, :], op=ALU.mult), reads=[bpB, b_masks, b_arena], writes=[b_arena])
            S.op("dve", lambda h, t1=t1, t2=t2: h.tensor_tensor(out=t1, in0=t1, in1=t2, op=ALU.add), reads=[b_arena], writes=[b_arena])
            for gg in range(4):
                g = g0 + g4 * 4 + gg
                S.op("dve", lambda h, g=g, gg=gg, t1=t1: h.scalar_tensor_tensor(out=D0m[:, g, :], in0=ident[:], scalar=drep[:, g:g + 1],
                                                                               in1=t1[:, gg * 128:(gg + 1) * 128], op0=ALU.mult, op1=ALU.add),
                     reads=[b_arena, b_ident, b_drep], writes=[b_D0])

    winu = sb("winu", [128, 8, 512], BF16); b_winu = Buf("winu")
    S.dma("pool", lambda h: h.dma_start(out=winu[:], in_=w_in[:, 0:512].rearrange("(k p) n -> p k n", p=128)), b_winu, writes=[b_winu])
    hTb = blockA[:, 0:4096].rearrange("p (k t) -> p k t", k=8)
    U_sb = blockA[:, 4096:8192]; b_Usb = Buf("U_sb")
    UT = blockB[:, 0:8192].bitcast(BF16).rearrange("p (g j) -> p g j", g=NG); b_UT = Buf("UT")
    UTo = blockA[:, 8192:10240].rearrange("p (g j) -> p g j", g=NG); b_UTo = Buf("UTo")
    Zoth = blockA[:, 10240:14336].rearrange("p (j e) -> p j e", e=64); b_Zoth = Buf("Zoth")
    Zctx = blockA[:, 14336:16384].rearrange("p (j e) -> p j e", e=64); b_Zctx = Buf("Zctx")
    Zown = arena[:, :].bitcast(BF16).rearrange("p (j e) -> p j e", e=64)
    b_Zown = b_arena
    cur = [sb(f"cur{i}", [128, NG, 2]) for i in range(2)]; b_cur = [Buf(f"cur{i}") for i in range(2)]
    st1 = sb("st1", [128, NG, 2]); st2 = sb("st2", [128, NG, 2]); b_st = Buf("st12")
    S.op("dve", lambda h: h.memset(cur[0][:], 0.0), writes=[b_cur[0]])
    scan_k = [0]

    def scan_step(zap, bz, lanes, store):
        k = scan_k[0]
        scan_k[0] += 1
        c0, bc0 = cur[k % 2], b_cur[k % 2]
        c1, bc1 = cur[(k + 1) % 2], b_cur[(k + 1) % 2]
        L = lanes
        sw = mkap(c0[L, :, 1:2], 0, [[2, NG], [-1, 2]])
        S.op("dve", lambda h: h.tensor_tensor(out=st1[L], in0=c0[L], in1=A1[L], op=ALU.mult), reads=[bc0, b_A], writes=[b_st])
        S.op("dve", lambda h: h.tensor_tensor(out=st2[L], in0=sw, in1=A2[L], op=ALU.mult), reads=[bc0, b_A, b_st], writes=[b_st])
        S.op("dve", lambda h: h.tensor_tensor(out=st1[L], in0=st1[L], in1=st2[L], op=ALU.add), reads=[b_st], writes=[b_st])
        S.op("dve", lambda h: h.tensor_tensor(out=c1[L], in0=st1[L], in1=zap, op=ALU.add), reads=[b_st, bz], writes=[bc1])
        if store:
            S.op("dve", lambda h: h.tensor_copy(out=zap, in_=c0[L]), reads=[bc0, bz], writes=[bz])
        if L != slice(0, 128):
            other = slice(64, 128) if L == slice(0, 64) else slice(0, 64)
            S.op("dve", lambda h: h.tensor_copy(out=c1[other], in_=c0[other]), reads=[bc0], writes=[bc1])

    def phaseA_block(xsrc, possrc, t0, ntok, sh_fn, sc_fn, UTdst, bUT, J0):
        ntile = ntok // 128
        nJ = ntok // 8
        for tl in range(ntile):
            xt, bx = load_tile(xsrc, possrc, t0 + tl * 128)
            ln_stats(xt, bx, xn_t, b_xn)
            transpose_mod(xn_t, b_xn, lambda kc, tl=tl: hTb[:, kc, tl * 128:(tl + 1) * 128], b_hTb, sc_fn, sh_fn, [b_modfm])
        for i in range(8):
            ps, bps = ps_next()
            for kc in range(8):
                S.op("pe", lambda h, ps=ps, kc=kc, i=i: h.matmul(ps[0:nJ, :], lhsT=mkap(hTb[:, kc, i:i + 1], 0, [[8, nJ]]), rhs=winu[:, kc, :],
                                                                start=(kc == 0), stop=(kc == 7)),
                     reads=[b_hTb, b_winu], writes=[bps], mode=f"up{nJ}")
            S.op("act", lambda h, ps=ps, i=i: h.activation(out=mkap(U_sb[0:nJ, i * 16:i * 16 + 1], 0, [[128, NG], [1, 16]]), in_=ps[0:nJ, :].rearrange("p (g c) -> p g c", g=NG), func=AF.Identity), reads=[bps], writes=[b_Usb])
        for g8 in range(4):
            for gg in range(8):
                g = g8 * 8 + gg
                S.op("pe", lambda h, g=g, gg=gg: h.transpose(out=psB[:, gg * 128:gg * 128 + nJ], in_=U_sb[0:nJ, g * 128:(g + 1) * 128],
                                                             identity=identb[0:nJ, 0:nJ]),
                     reads=[b_Usb, b_identb], writes=[b_psB], mode=f"trb{nJ}")
            S.op("dve", lambda h, g8=g8: h.tensor_copy(out=UTdst[:, g8 * 8:(g8 + 1) * 8, J0:J0 + nJ],
                                                       in_=psB[:, :].rearrange("p (g j) -> p g j", g=8)[:, :, 0:nJ]),
                 reads=[b_psB], writes=[bUT])

    def z_block(UTsrc, bUT, J0, nJ, Zdst_fn, bZ, lanes_list):
        for g in range(NG):
            ps, bps = ps_next()
            S.op("pe", lambda h, ps=ps, g=g: h.matmul(ps[:, 0:nJ], lhsT=PTre[:, g, :], rhs=UTsrc[:, g, J0:J0 + nJ], start=True, stop=True),
                 reads=[b_PT, bUT], writes=[bps], mode="bulk")
            S.op("pe", lambda h, ps=ps, g=g: h.matmul(ps[:, 256:256 + nJ], lhsT=PTim[:, g, :], rhs=UTsrc[:, g, J0:J0 + nJ], start=True, stop=True),
                 reads=[b_PT, bUT, bps], writes=[bps], mode="bulk")
            for lanes in lanes_list:
                if lanes == LF:
                    S.op("act", lambda h, ps=ps, g=g, lanes=lanes: h.activation(
                        out=Zdst_fn(lanes, g), in_=ps[lanes, :].rearrange("p (r j) -> p r j", r=2)[:, :, 0:nJ], func=AF.Identity),
                        reads=[bps], writes=[bZ])
                else:
                    S.op("dve", lambda h, ps=ps, g=g, lanes=lanes: h.tensor_copy(
                        out=Zdst_fn(lanes, g), in_=ps[lanes, :].rearrange("p (r j) -> p r j", r=2)[:, :, 0:nJ]),
                        reads=[bps], writes=[bZ])

    def zdst(Zt, nJtot, Jbase, nJ):
        def fn(lanes, g):
            if lanes == LF:
                base = Zt[LF, Jbase:Jbase + 1, 2 * g:2 * g + 1]
                return mkap(base, 0, [[1, 2], [64, nJ]])
            base = Zt[LB, nJtot - 1 - Jbase:nJtot - Jbase, 2 * g:2 * g + 1]
            return mkap(base, 0, [[1, 2], [-64, nJ]])
        return fn

    ALL = slice(0, 128)
    sh1_fn = lambda kc: modfm[:, 0 + kc, 0:1]; sc1_fn = lambda kc: sc1p[:, kc, 0:1]
    csh1_fn = lambda kc: modfm[:, 0 + kc, 1:2]; csc1_fn = lambda kc: sc1p[:, kc, 1:2]
    phaseA_block(ctx_in, None, 0, 256, csh1_fn, csc1_fn, UTo, b_UTo, 0)
    z_block(UTo, b_UTo, 0, 32, zdst(Zctx, 32, 0, 32), b_Zctx, [LF, LB])
    for k in range(32):
        scan_step(Zctx[:, k, :].rearrange("p (g r) -> p g r", r=2), b_Zctx, ALL, False)
    for blk in range(8):
        phaseA_block(x_oth, pos_oth, blk * 512, 512, sh1_fn, sc1_fn, UTo, b_UTo, 0)
        z_block(UTo, b_UTo, 0, 64, zdst(Zoth, 64, 0, 64), b_Zoth, [LF])
        for k in range(64):
            scan_step(Zoth[LF, k, :].rearrange("p (g r) -> p g r", r=2), b_Zoth, LF, False)
    for blk in range(8):
        phaseA_block(x_own, pos_own, blk * 512, 512, sh1_fn, sc1_fn, UT, b_UT, blk * 64)
    for jb in range(4):
        z_block(UT, b_UT, jb * 128, 128, zdst(Zown, 512, jb * 128, 128), b_Zown, [LF, LB])
    for k in range(512):
        scan_step(Zown[:, k, :].rearrange("p (g r) -> p g r", r=2), b_Zown, ALL, True)


    Ysb = blockA[:, :].rearrange("p (g j) -> p g j", g=NG); b_Ysb = Buf("Ysb")
    TM = arena[:, 8192:10240].bitcast(BF16).rearrange("p (j c) -> p j c", j=8); b_TM = b_arena
    ya = arena[:, 0:8192].bitcast(BF16).rearrange("p (a t) -> p a t", a=4); b_ya = b_arena
    for g in range(NG):
        ps, bps = ps_next()
        S.op("pe", lambda h, ps=ps, g=g: h.matmul(ps[:, :], lhsT=D0m[:, g, :], rhs=UT[:, g, :], start=True, stop=False), reads=[b_D0, b_UT], writes=[bps], mode="bulk")
        for lanes in (LF, LB):
            for ri, Qm in enumerate((Qre, Qimn)):
                if lanes == LF:
                    rhs = mkap(Zown[LF, 0:1, 2 * g + ri:2 * g + ri + 1], 0, [[64, 512]])
                else:
                    rhs = mkap(Zown[LB, 511:512, 2 * g + ri:2 * g + ri + 1], 0, [[-64, 512]])
                last = (lanes == LB and ri == 1)
                S.op("pe", lambda h, ps=ps, g=g, lanes=lanes, Qm=Qm, rhs=rhs, last=last: h.matmul(ps[:, :], lhsT=Qm[lanes, g, :], rhs=rhs, start=False, stop=last),
                     reads=[b_Q, b_Zown, bps], writes=[bps], mode=("q0" if lanes == LF else "q64"))
        S.op("act", lambda h, ps=ps, g=g: h.activation(out=Ysb[:, g, :], in_=ps[:, :], func=AF.Identity), reads=[bps], writes=[b_Ysb, b_hTb, b_Usb, b_UTo, b_Zoth, b_Zctx])
    gt = [arena[:, 10240 + i * 1024:10240 + (i + 1) * 1024] for i in range(2)]; b_gt = b_arena
    for jb in range(4):
        for g8 in range(4):
            for gg in range(8):
                g = g8 * 8 + gg
                S.op("pe", lambda h, g=g, gg=gg, jb=jb: h.transpose(out=psB[:, gg * 128:(gg + 1) * 128], in_=Ysb[:, g, jb * 128:(jb + 1) * 128], identity=identb[:]),
                     reads=[b_Ysb, b_identb], writes=[b_psB], mode="trb128")
            S.op("dve", lambda h, g8=g8: h.tensor_copy(
                out=mkap(TM[:, 0:1, g8 * 128:g8 * 128 + 1], 0, [[16, 8], [512, 8], [1, 16]]),
                in_=psB[:, :].rearrange("p (g j c) -> p g j c", g=8, j=8)), reads=[b_psB], writes=[b_TM])
        for ct in range(4):
            for j in range(8):
                S.op("pe", lambda h, ct=ct, j=j: h.transpose(out=psB[:, j * 128:(j + 1) * 128], in_=TM[:, j, ct * 128:(ct + 1) * 128], identity=identb[:]),
                     reads=[b_TM, b_identb], writes=[b_psB], mode="trb128")
            xin = psB[:, :]
            g0t, g1t = gt[0], gt[1]
            S.op("act", lambda h: h.activation(out=g0t, in_=xin, func=AF.Square), reads=[b_psB], writes=[b_gt])
            S.op("dve", lambda h: h.tensor_scalar(out=g0t, in0=g0t, scalar1=0.044715, scalar2=1.0, op0=ALU.mult, op1=ALU.add), reads=[b_gt], writes=[b_gt])
            S.op("dve", lambda h: h.tensor_tensor(out=g0t, in0=g0t, in1=xin, op=ALU.mult), reads=[b_gt, b_psB], writes=[b_gt])
            S.op("act", lambda h: h.activation(out=g1t, in_=g0t, func=AF.Sigmoid, scale=GELU_C), reads=[b_gt], writes=[b_gt])
            S.op("dve", lambda h, ct=ct, jb=jb: h.tensor_tensor(
                out=mkap(ya[:, ct, jb * 1024:jb * 1024 + 1], 0, [[1, 8], [8, 128]]),
                in0=g1t.rearrange("p (j J) -> p j J", j=8), in1=psB[:, :].rearrange("p (j J) -> p j J", j=8), op=ALU.mult),
                reads=[b_gt, b_psB], writes=[b_ya])

    if debug == "ya":
        b_dd = b_arena
        S.dma("sp", lambda h: h.dma_start(out=dbg_d, in_=ya.rearrange("p a b -> p (a b)")), b_dd, reads=[b_dd])
        S.final_wait("sp", [b_dd])
        S.emit()
        return nc

    def alias(new, olds):
        for o in olds:
            if o.writer is not None:
                new.readers.append(o.writer)
            new.readers.extend(o.readers)
        return new

    dead_A = [b_Ysb, b_hTb, b_Usb, b_UTo, b_Zoth, b_Zctx]
    dead_B = [b_UT, b_bc, b_tab, b_bbar, b_tmpb, b_W]
    b_blkA = alias(Buf("blkA"), dead_A)
    b_up0 = alias(Buf("up0"), [b_arena])
    gsec = blockA[:, :].bitcast(F32).rearrange("p (k n) -> p k n", k=8)
    crep = arena[:, 8192:9216].rearrange("p (k m) -> p k m", k=8)
    badrow = arena[:, 9216:10240]
    g1bc = blockB[:, 6144:7168]; g2bc = blockB[:, 7168:8192]; b_gbc = alias(Buf("gbc"), dead_B)
    lnp = blockB[:, 8192:10240]; b_lnp = alias(Buf("lnp"), dead_B)
    S.op("dve", lambda h: h.tensor_copy(out=crep, in_=mkap(csil[:], 0, [[1, 8], [0, 128]])), reads=[b_csil], writes=[b_up0])
    for sec, gdst in ((2, g1bc), (5, g2bc)):
        S.dma("sp", lambda h, sec=sec: h.dma_start(out=gsec, in_=w_ada[:, sec * D:(sec + 1) * D].rearrange("(k p) n -> p k n", p=128)),
              b_blkA, writes=[b_blkA])
        S.dma("sp", lambda h, sec=sec: h.dma_start(out=badrow, in_=b_ada[:, sec * D:(sec + 1) * D].partition_broadcast(128)),
              b_up0, writes=[b_up0])
        for nb in range(2):
            ps, bps = ps_next()
            for kc in range(8):
                S.op("pe", lambda h, ps=ps, kc=kc, nb=nb: h.matmul(ps[:, :], lhsT=crep[:, kc, :], rhs=gsec[:, kc, nb * 512:(nb + 1) * 512],
                                                                  start=(kc == 0), stop=(kc == 7)),
                     reads=[b_up0, b_blkA], writes=[bps], mode="f32")
            S.op("dve", lambda h, ps=ps, nb=nb, gdst=gdst: h.tensor_tensor(out=gdst[:, nb * 512:(nb + 1) * 512], in0=ps[:, :],
                                                                          in1=badrow[:, nb * 512:(nb + 1) * 512], op=ALU.add),
                 reads=[bps, b_up0], writes=[b_gbc])
    S.dma("sp", lambda h: h.dma_start(out=lnp, in_=lnp_in[:, 0:2048].partition_broadcast(128)), b_lnp, writes=[b_lnp])

    winA = blockA[:, :].rearrange("p (k n) -> p k n", k=8)
    winB = blockB[:, 0:6144].bitcast(BF16).rearrange("p (k n) -> p k n", k=8)
    b_winB = alias(Buf("winB"), dead_B)
    for c0, c1, dst, bd in ((512, 1536, winA[:, :, 0:1024], b_blkA), (1536, 2560, winA[:, :, 1024:2048], b_blkA),
                            (2560, 3584, winB[:, :, 0:1024], b_winB), (3584, 4096, winB[:, :, 1024:1536], b_winB)):
        S.dma("pool", lambda h, c0=c0, c1=c1, dst=dst: h.dma_start(out=dst, in_=w_in[:, c0:c1].rearrange("(k p) n -> p k n", p=128)),
              bd, writes=[bd])
    flat = lambda t: t[:].rearrange("p g m -> p (g m)")
    wval = flat(PTre).rearrange("p (k n) -> p k n", k=4); wgate = flat(PTim).rearrange("p (k n) -> p k n", k=4)
    convo = flat(Qre).rearrange("p (k n) -> p k n", k=4)
    wo_lo = flat(Qimn).rearrange("p (k n) -> p k n", k=4); wo_hi = flat(D0m).rearrange("p (k n) -> p k n", k=4)
    b_wglu = alias(Buf("wglu"), [b_PT]); b_convo = alias(Buf("convo"), [b_Q]); b_wo = alias(Buf("wo"), [b_Q, b_D0])
    S.dma("pool", lambda h: h.dma_start(out=wval, in_=w_val.rearrange("(k p) n -> p k n", p=128)), b_wglu, writes=[b_wglu])
    S.dma("pool", lambda h: h.dma_start(out=wgate, in_=w_gate.rearrange("(k p) n -> p k n", p=128)), b_wglu, writes=[b_wglu])
    S.dma("pool", lambda h: h.dma_start(out=convo, in_=conv_out.rearrange("(k p) n -> p k n", p=128)), b_convo, writes=[b_convo])
    S.dma("pool", lambda h: h.dma_start(out=wo_lo, in_=w_o[0:512, :].rearrange("(k p) n -> p k n", p=128)), b_wo, writes=[b_wo])
    S.dma("pool", lambda h: h.dma_start(out=wo_hi, in_=w_o[512:1024, :].rearrange("(k p) n -> p k n", p=128)), b_wo, writes=[b_wo])
    rw32 = masks[:, 1, 0:288].rearrange("p (k n) -> p k n", k=8); b_rw = alias(Buf("rw32"), [b_masks])
    S.dma("sp", lambda h: h.dma_start(out=rw32, in_=rw_in.rearrange("(k p) n -> p k n", p=128)), b_rw, writes=[b_rw])
    rt = masks[:, 0, 0:160]; b_rt = alias(Buf("rt"), [b_masks])

    cst = masks[:, 1, 288:480]; b_cst = alias(Buf("cst"), [b_masks])
    S.dma("sp", lambda h: h.dma_start(out=cst, in_=cst_in), b_cst, writes=[b_cst])
    RT = masks[:, 0, 256:384].rearrange("p (t f) -> p t f", f=4); b_RT = alias(Buf("RT"), [b_masks])
    DEST = masks[:, 0, 384:448].bitcast(I32).rearrange("p (t f) -> p t f", f=2); b_DEST = alias(Buf("DEST"), [b_masks])
    hT = arena[:, 8192:10240].bitcast(BF16).rearrange("p (k t) -> p k t", k=8); b_hT = b_up0
    merged = arena[:, 10240:12288].bitcast(BF16).rearrange("p (k t) -> p k t", k=8); b_merged = alias(Buf("merged"), [b_arena])
    gv = arena[:, 12288:13312].bitcast(BF16).rearrange("p (k t) -> p k t", k=4); b_gv = alias(Buf("gv"), [b_arena])
    tmp = [arena[:, 13312 + i * 512:13312 + (i + 1) * 512] for i in range(5)]
    b_tmp = [alias(Buf(f"tmp{i}"), [b_arena]) for i in range(5)]
    h2b = arena[:, 15872:16384].bitcast(BF16).rearrange("p (k t) -> p k t", k=8); b_h2b = alias(Buf("h2b"), [b_arena])
    wr_f = winu[:].rearrange("p k n -> p (k n)").bitcast(F32)
    A_all = wr_f[:, 0:1024].rearrange("p (t e) -> p t e", e=32); b_Aall = alias(Buf("A_all"), [b_winu])
    h2f = wr_f[:, 1024:2048].rearrange("p (k t) -> p k t", k=8); b_h2f = alias(Buf("h2f"), [b_winu])
    b_x1s = [Buf(f"x1s{i}") for i in range(32)]
    b_h2s = [Buf(f"h2s{i}") for i in range(32)]
    BIG = 1.0e30

    def proj(ft, rhs_ap, brhs):
        piece, off, bp = (winA, (ft - 4) * 128, b_blkA) if ft < 20 else (winB, (ft - 20) * 128, b_winB)
        ps, bps = ps_next()
        for kc in range(8):
            S.op("pe", lambda h, ps=ps, kc=kc, piece=piece, off=off: h.matmul(ps[:, :], lhsT=piece[:, kc, off:off + 128], rhs=rhs_ap(kc),
                                                                             start=(kc == 0), stop=(kc == 7)),
                 reads=[bp, brhs], writes=[bps], mode="bulk")
        return ps, bps

    def mm4(wt, bw, dt, rhs_fn, brhs):
        ps, bps = ps_next()
        for ct in range(4):
            S.op("pe", lambda h, ps=ps, ct=ct: h.matmul(ps[:, :], lhsT=wt[:, ct, dt * 128:(dt + 1) * 128], rhs=rhs_fn(ct), start=(ct == 0), stop=(ct == 3)),
                 reads=[bw, brhs], writes=[bps], mode="bulk")
        return ps, bps

    def dv(fn, r, w):
        S.op("dve", fn, reads=r, writes=w)

    for tb in range(8):
        t0 = tb * 512
        for tl in range(4):
            xt, bx = load_tile(x_own, pos_own, t0 + tl * 128)
            ln_stats(xt, bx, xn_t, b_xn)
            transpose_mod(xn_t, b_xn, lambda kc, tl=tl: hT[:, kc, tl * 128:(tl + 1) * 128], b_hT, sc1_fn, sh1_fn, [b_modfm])
        hrhs = lambda kc: hT[:, kc, :]
        for ct in range(4):
            pz, bpz = proj(4 + ct, hrhs, b_hT)
            pgc, bpgc = proj(12 + ct, hrhs, b_hT)
            S.op("act", lambda h, pgc=pgc: h.activation(out=tmp[0], in_=pgc[:, :], func=AF.Identity), reads=[bpgc], writes=[b_tmp[0]])
            dv(lambda h, pz=pz: h.tensor_tensor(out=tmp[1], in0=pz[:, :], in1=tmp[0], op=ALU.mult), [bpz, b_tmp[0]], [b_tmp[1]])
            dv(lambda h, ct=ct: h.tensor_scalar(out=tmp[2], in0=tmp[1], scalar1=convw[:, ct, 1:2], scalar2=None, op0=ALU.mult), [b_tmp[1], b_convw], [b_tmp[2]])
            zv = tmp[1].rearrange("p (r c) -> p r c", c=64); vv = tmp[2].rearrange("p (r c) -> p r c", c=64)
            dv(lambda h, ct=ct, zv=zv, vv=vv: h.scalar_tensor_tensor(out=vv[:, :, 1:64], in0=zv[:, :, 0:63], scalar=convw[:, ct, 0:1], in1=vv[:, :, 1:64],
                                                                     op0=ALU.mult, op1=ALU.add), [b_tmp[1], b_tmp[2], b_convw], [b_tmp[2]])
            dv(lambda h, ct=ct, zv=zv, vv=vv: h.scalar_tensor_tensor(out=vv[:, :, 0:63], in0=zv[:, :, 1:64], scalar=convw[:, ct, 2:3], in1=vv[:, :, 0:63],
                                                                     op0=ALU.mult, op1=ALU.add), [b_tmp[1], b_tmp[2], b_convw], [b_tmp[2]])
            pgb, bpgb = proj(8 + ct, hrhs, b_hT)
            dv(lambda h, ct=ct, pgb=pgb: h.tensor_tensor(out=gv[:, ct, :], in0=pgb[:, :], in1=tmp[2], op=ALU.mult), [bpgb, b_tmp[2]], [b_gv])
        for dt in range(8):
            pob, bpob = mm4(convo, b_convo, dt, lambda ct: gv[:, ct, :], b_gv)
            pval, bpval = mm4(wval, b_wglu, dt, lambda ct, t0=t0: ya[:, ct, t0:t0 + 512], b_arena)
            pgt, bpgt = mm4(wgate, b_wglu, dt, lambda ct, t0=t0: ya[:, ct, t0:t0 + 512], b_arena)
            pma, bpma = proj(16 + dt, hrhs, b_hT)
            pmb, bpmb = proj(24 + dt, hrhs, b_hT)
            S.op("act", lambda h, pgt=pgt: h.activation(out=tmp[0], in_=pgt[:, :], func=AF.Sigmoid), reads=[bpgt], writes=[b_tmp[0]])
            S.op("act", lambda h, pma=pma: h.activation(out=tmp[1], in_=pma[:, :], func=AF.Sigmoid), reads=[bpma], writes=[b_tmp[1]])
            S.op("act", lambda h, pmb=pmb: h.activation(out=tmp[2], in_=pmb[:, :], func=AF.Sigmoid), reads=[bpmb], writes=[b_tmp[2]])
            dv(lambda h, pval=pval: h.tensor_tensor(out=tmp[3], in0=pval[:, :], in1=tmp[0], op=ALU.mult), [bpval, b_tmp[0]], [b_tmp[3]])
            dv(lambda h: h.tensor_tensor(out=tmp[3], in0=tmp[3], in1=tmp[1], op=ALU.mult), [b_tmp[3], b_tmp[1]], [b_tmp[3]])
            dv(lambda h, pob=pob: h.tensor_tensor(out=tmp[4], in0=pob[:, :], in1=tmp[2], op=ALU.mult), [bpob, b_tmp[2]], [b_tmp[4]])
            dv(lambda h, dt=dt: h.tensor_tensor(out=merged[:, dt, :], in0=tmp[3], in1=tmp[4], op=ALU.add), [b_tmp[3], b_tmp[4]], [b_merged])
        def w_o_pe(tl):
            outs = []
            for nb in range(2):
                ps, bps = ps_next()
                for dt in range(8):
                    wsl = wo_lo[:, dt, nb * 512:(nb + 1) * 512] if dt < 4 else wo_hi[:, dt - 4, nb * 512:(nb + 1) * 512]
                    S.op("pe", lambda h, ps=ps, dt=dt, tl=tl, wsl=wsl: h.matmul(ps[:, :], lhsT=merged[:, dt, tl * 128:(tl + 1) * 128], rhs=wsl,
                                                                               start=(dt == 0), stop=(dt == 7)),
                         reads=[b_merged, b_wo], writes=[bps], mode="bulk")
                outs.append((ps, bps))
            return outs
        tiles_ = [load_tile(x_own, pos_own, (tb * 4) * 128)]
        pend_ = w_o_pe(0)
        for tl in range(4):
            gti = tb * 4 + tl
            r0 = gti * 128
            xt, bx = tiles_[tl]
            cur_ = pend_
            if tl < 3:
                tiles_.append(load_tile(x_own, pos_own, r0 + 128))
                pend_ = w_o_pe(tl + 1)
            for nb in range(2):
                ps, bps = cur_[nb]
                dv(lambda h, ps=ps, nb=nb: h.tensor_tensor(out=xn_t[:, nb * 512:(nb + 1) * 512], in0=ps[:, :], in1=g1bc[:, nb * 512:(nb + 1) * 512], op=ALU.mult),
                   [bps, b_gbc], [b_xn])
            dv(lambda h, xt=xt: h.scalar_tensor_tensor(out=xt[:, :], in0=xt[:, :], scalar=ALPHA, in1=xn_t[:, :], op0=ALU.mult, op1=ALU.add), [bx, b_xn], [bx])
            ln_stats(xt, bx, xn_t, b_xn)
            dv(lambda h: h.tensor_tensor(out=xn_t[:, :], in0=xn_t[:, :], in1=lnp[:, 0:1024], op=ALU.mult), [b_xn, b_lnp], [b_xn])
            dv(lambda h, xt=xt: h.tensor_tensor(out=xt[:, :], in0=xn_t[:, :], in1=lnp[:, 1024:2048], op=ALU.add), [b_xn, b_lnp], [bx])
            S.dma("sp", lambda h, xt=xt, r0=r0: h.dma_start(out=x1s[r0:r0 + 128, :], in_=xt[:, :]), bx, reads=[bx], writes=[b_x1s[gti]])
            ln_stats(xt, bx, xn_t, b_xn)
            for kc in range(8):
                S.op("pe", lambda h, kc=kc: h.transpose(out=psT[:, kc * 128:(kc + 1) * 128], in_=xn_t[:, kc * 128:(kc + 1) * 128], identity=ident[:]),
                     reads=[b_xn, b_ident], writes=[b_psT], mode="trf")
            for kc in range(8):
                S.op("act", lambda h, kc=kc: h.activation(out=h2f[:, kc, :], in_=psT[:, kc * 128:(kc + 1) * 128], func=AF.Identity,
                                                         bias=modfm[:, 24 + kc, 0:1], scale=sc2p[:, kc:kc + 1]),
                     reads=[b_psT, b_modfm], writes=[b_h2f])
            ps, bps = ps_next()
            for kc in range(8):
                S.op("pe", lambda h, ps=ps, kc=kc: h.matmul(ps[:, 0:36], lhsT=h2f[:, kc, :], rhs=rw32[:, kc, :], start=(kc == 0), stop=(kc == 7)),
                     reads=[b_h2f, b_rw], writes=[bps], mode="f32")
            R = lambda a, b_: rt[:, a:b_]
            rr = [b_rt]
            dv(lambda h, ps=ps: h.tensor_tensor(out=R(0, 36), in0=ps[:, 0:36], in1=rb_bc[:, :], op=ALU.add), [bps, b_rb, b_rt], rr)
            dv(lambda h: h.tensor_reduce(out=R(36, 37), in_=R(0, 4), axis=AX.X, op=ALU.max), rr, rr)
            dv(lambda h: h.tensor_scalar(out=R(38, 42), in0=R(0, 4), scalar1=R(36, 37), scalar2=None, op0=ALU.is_equal), rr, rr)
            dv(lambda h: h.tensor_scalar(out=R(37, 38), in0=R(36, 37), scalar1=-1.0, scalar2=None, op0=ALU.mult), rr, rr)
            S.op("act", lambda h: h.activation(out=R(42, 46), in_=R(0, 4), func=AF.Exp, bias=R(37, 38), scale=1.0), reads=rr, writes=rr)
            dv(lambda h: h.tensor_reduce(out=R(46, 47), in_=R(42, 46), axis=AX.X, op=ALU.add), rr, rr)
            dv(lambda h: h.reciprocal(out=R(47, 48), in_=R(46, 47)), rr, rr)
            dv(lambda h: h.tensor_scalar(out=R(48, 52), in0=R(38, 42), scalar1=BIG, scalar2=-BIG, op0=ALU.mult, op1=ALU.add), rr, rr)
            dv(lambda h: h.tensor_tensor(out=R(52, 84).rearrange("p (g e) -> p g e", e=8), in0=R(4, 36).rearrange("p (g e) -> p g e", e=8),
                                         in1=mkap(R(48, 52), 0, [[1, 4], [0, 8]]), op=ALU.add), rr, rr)
            dv(lambda h: h.tensor_reduce(out=R(84, 85), in_=R(52, 84), axis=AX.X, op=ALU.max), rr, rr)
            dv(lambda h: h.tensor_scalar(out=R(85, 117), in0=R(52, 84), scalar1=R(84, 85), scalar2=None, op0=ALU.is_equal), rr, rr)
            dv(lambda h: h.scalar_tensor_tensor(out=R(117, 149), in0=R(85, 117), scalar=-BIG, in1=R(52, 84), op0=ALU.mult, op1=ALU.add), rr, rr)
            dv(lambda h: h.tensor_reduce(out=R(149, 150), in_=R(117, 149), axis=AX.X, op=ALU.max), rr, rr)
            dv(lambda h: h.tensor_scalar(out=R(52, 84), in0=R(117, 149), scalar1=R(149, 150), scalar2=None, op0=ALU.is_equal), rr, rr)
            dv(lambda h: h.tensor_tensor(out=R(150, 151), in0=R(149, 150), in1=R(84, 85), op=ALU.subtract), rr, rr)
            S.op("act", lambda h: h.activation(out=R(151, 152), in_=R(150, 151), func=AF.Exp), reads=rr, writes=rr)
            dv(lambda h: h.tensor_scalar(out=R(152, 153), in0=R(151, 152), scalar1=1.0, scalar2=None, op0=ALU.add), rr, rr)
            dv(lambda h: h.reciprocal(out=R(153, 154), in_=R(152, 153)), rr, rr)
            dv(lambda h: h.tensor_tensor(out=R(154, 155), in0=R(151, 152), in1=R(153, 154), op=ALU.mult), rr, rr)
            dv(lambda h: h.tensor_tensor(out=R(155, 156), in0=R(153, 154), in1=R(47, 48), op=ALU.mult), rr, rr)
            dv(lambda h: h.tensor_tensor(out=R(156, 157), in0=R(154, 155), in1=R(47, 48), op=ALU.mult), rr, rr)
            dv(lambda h, gti=gti: h.tensor_tensor(out=A_all[:, gti, :], in0=R(85, 117), in1=R(52, 84), op=ALU.add), rr + [b_Aall], [b_Aall])
            dv(lambda h: h.tensor_tensor(out=R(117, 149), in0=R(85, 117), in1=cst[:, 0:32], op=ALU.mult), rr + [b_cst], rr)
            dv(lambda h, gti=gti: h.tensor_reduce(out=RT[:, gti, 0:1], in_=R(117, 149), axis=AX.X, op=ALU.add), rr + [b_RT], [b_RT])
            dv(lambda h: h.tensor_tensor(out=R(117, 149), in0=R(52, 84), in1=cst[:, 0:32], op=ALU.mult), rr + [b_cst, b_RT], rr)
            dv(lambda h, gti=gti: h.tensor_reduce(out=RT[:, gti, 1:2], in_=R(117, 149), axis=AX.X, op=ALU.add), rr + [b_RT], [b_RT])
            dv(lambda h, gti=gti: h.tensor_copy(out=RT[:, gti, 2:4], in_=R(155, 157)), rr + [b_RT], [b_RT])

    IOA = bass.IndirectOffsetOnAxis
    NBLK = 48
    rA = blockA[:, :].bitcast(F32)
    b_R = alias(Buf("phaseR"), [b_blkA])
    Dt = rA[:, 4096:5120].rearrange("p (t e) -> p t e", e=32)
    OH = rA[:, 5120:6144].rearrange("p (t e) -> p t e", e=32)
    CMP = rA[:, 6144:7680]
    tri = rA[:, 7680:7936]
    sm_ = rA[:, 7936:8192]
    run = sm_[:, 0:32]; nblk = sm_[:, 32:64]; pe_ = sm_[:, 64:96]; psr = sm_[:, 96:128]; ones32 = sm_[:, 128:160]
    blkE = sm_[:, 160:208]; tE = sm_[:, 208:256]
    S.dma("sp", lambda h: h.dma_start(out=tri, in_=tri_in), b_R, writes=[b_R])
    rdv = lambda fn, extra=(): S.op("dve", fn, reads=[b_R] + list(extra), writes=[b_R])
    pT1, pT2 = ps_next(), ps_next()
    for hf_ in range(2):
        asl = A_all[:, hf_ * 16:(hf_ + 1) * 16, :].rearrange("p t e -> p (t e)")
        S.op("pe", lambda h, hf_=hf_, asl=asl: h.matmul(psT[:, hf_ * 512:(hf_ + 1) * 512], lhsT=tri[:, 0:128], rhs=asl, start=True, stop=True),
             reads=[b_R, b_Aall], writes=[b_psT])
        pt, bpt = (pT1, pT2)[hf_]
        S.op("pe", lambda h, pt=pt, asl=asl: h.matmul(pt[:, :], lhsT=tri[:, 128:256], rhs=asl, start=True, stop=True),
             reads=[b_R, b_Aall], writes=[bpt])
    rdv(lambda h: h.memset(run, 0.0))
    rdv(lambda h: h.memset(ones32, 1.0))
    for t in range(32):
        pt, bpt = (pT1, pT2)[t // 16]
        tt_ = t % 16
        rdv(lambda h, t=t: h.tensor_tensor(out=Dt[:, t, :], in0=psT[:, t * 32:(t + 1) * 32], in1=run, op=ALU.add), [b_psT])
        rdv(lambda h, pt=pt, tt_=tt_: h.tensor_tensor(out=run, in0=pt[:, tt_ * 32:(tt_ + 1) * 32], in1=run, op=ALU.add), [bpt])
    cmp8 = CMP[:, 0:256].rearrange("p (e j) -> p e j", j=8)
    rdv(lambda h: h.tensor_tensor(out=cmp8, in0=mkap(run, 0, [[1, 32], [0, 8]]), in1=mkap(cst[:, 72:80], 0, [[0, 32], [1, 8]]), op=ALU.is_gt), [b_cst])
    rdv(lambda h: h.tensor_reduce(out=nblk, in_=cmp8, axis=AX.X, op=ALU.add))
    rdv(lambda h: h.tensor_tensor_scan(out=pe_, data0=ones32, data1=nblk, initial=0.0, op0=ALU.mult, op1=ALU.add))
    rdv(lambda h: h.tensor_tensor(out=psr, in0=pe_, in1=nblk, op=ALU.subtract))
    rdv(lambda h: h.tensor_scalar(out=psr, in0=psr, scalar1=512.0, scalar2=None, op0=ALU.mult))
    rdv(lambda h: h.tensor_tensor(out=Dt, in0=Dt, in1=mkap(psr, 0, [[0, 32], [1, 32]]), op=ALU.add))
    destF = CMP[:, 256:320].rearrange("p (t f) -> p t f", f=2)
    for k in range(2):
        rdv(lambda h, k=k: h.tensor_tensor(out=OH, in0=mkap(cst[:, 0:32], 0, [[0, 32], [1, 32]]), in1=mkap(RT[:, 0, k:k + 1], 0, [[4, 32], [0, 32]]), op=ALU.is_equal),
            [b_cst, b_RT])
        rdv(lambda h: h.tensor_tensor(out=OH, in0=OH, in1=Dt, op=ALU.mult))
        rdv(lambda h, k=k: h.tensor_reduce(out=destF[:, :, k], in_=OH, axis=AX.X, op=ALU.add))
    S.op("dve", lambda h: h.tensor_copy(out=DEST, in_=destF), reads=[b_R], writes=[b_DEST])
    cmpb = CMP[:, 0:1536].rearrange("p (b e) -> p b e", e=32)
    rdv(lambda h: h.tensor_tensor(out=cmpb, in0=mkap(pe_, 0, [[0, NBLK], [1, 32]]), in1=mkap(cst[:, 0:NBLK], 0, [[1, NBLK], [0, 32]]), op=ALU.is_le), [b_cst, b_DEST])
    rdv(lambda h: h.tensor_reduce(out=blkE, in_=cmpb, axis=AX.X, op=ALU.add))
    rdv(lambda h: h.tensor_scalar(out=blkE, in0=blkE, scalar1=31.0, scalar2=None, op0=ALU.min))
    gif = CMP[:, 0:576]
    gif_g = gif[:, 0:384].rearrange("p (b k) -> p b k", k=8); gif_d = gif[:, 384:576].rearrange("p (b k) -> p b k", k=4)
    rdv(lambda h: h.tensor_scalar(out=tE, in0=blkE, scalar1=1024.0, scalar2=None, op0=ALU.mult))
    rdv(lambda h: h.tensor_tensor(out=gif_g, in0=mkap(tE, 0, [[1, NBLK], [0, 8]]), in1=mkap(cst[:, 64:72], 0, [[0, NBLK], [1, 8]]), op=ALU.add), [b_cst])
    rdv(lambda h: h.tensor_scalar(out=tE, in0=blkE, scalar1=512.0, scalar2=None, op0=ALU.mult))
    rdv(lambda h: h.tensor_tensor(out=gif_d, in0=mkap(tE, 0, [[1, NBLK], [0, 4]]), in1=mkap(cst[:, 64:68], 0, [[0, NBLK], [1, 4]]), op=ALU.add), [b_cst])
    GI = wr_f[:, 1024:1600].bitcast(I32); b_GI = alias(Buf("GI"), [b_h2f])
    GI_g = GI[:, 0:384].rearrange("p (b k) -> p b k", k=8); GI_d = GI[:, 384:576].rearrange("p (b k) -> p b k", k=4)
    S.op("dve", lambda h: h.tensor_copy(out=GI, in_=gif), reads=[b_R], writes=[b_GI])

    if debug == "d":
        dbgr = nc.dram_tensor("dbgr", [128, 32 * 4 + 64 + 576], F32, kind="ExternalOutput").ap()
        dd = rA[:, 4096:4096 + 768]
        S.op("dve", lambda h: h.tensor_copy(out=dd[:, 0:128], in_=RT.rearrange("p t f -> p (t f)")), reads=[b_R, b_RT], writes=[b_R])
        S.op("dve", lambda h: h.tensor_copy(out=dd[:, 128:192], in_=DEST.rearrange("p t f -> p (t f)")), reads=[b_R, b_DEST], writes=[b_R])
        S.op("dve", lambda h: h.tensor_copy(out=dd[:, 192:768], in_=GI), reads=[b_R, b_GI], writes=[b_R])
        S.dma("sp", lambda h: h.dma_start(out=dbgr, in_=dd), b_R, reads=[b_R])
        S.final_wait("sp", b_x1s + [b_R])
        S.emit()
        return nc

    xs = nc.dram_tensor("xs", [NBLK * 512, D], F32, kind="Internal").ap()
    ybd = nc.dram_tensor("ybd", [NBLK * 512, D], F32, kind="Internal").ap()
    b_xs = Buf("xs")
    for t in range(32):
        r0 = t * 128
        i = xctr[0] % 2
        xctr[0] += 1
        xt, bx = xt_t[i], b_xt[i]
        S.dma("sp", lambda h, xt=xt, r0=r0: h.dma_start(out=xt[:], in_=x1s[r0:r0 + 128, :]), bx, reads=[b_x1s[t]], writes=[bx])
        ln_stats(xt, bx, xn_t, b_xn)
        for k in range(2):
            S.dma("pool", lambda h, t=t, k=k: h.indirect_dma_start(out=xs[:, :], out_offset=IOA(ap=DEST[:, t, k:k + 1], axis=0), in_=xn_t[:, :], in_offset=None),
                  b_xs, reads=[b_xn, b_DEST], writes=[b_xs])

    lnp_e = lnp
    S.dma("sp", lambda h: h.dma_start(out=lnp_e, in_=lnp_in[:, 2048:4096].partition_broadcast(128)), b_lnp, writes=[b_lnp])
    xinb = [arena[:, i * 4096:(i + 1) * 4096].rearrange("p (t d) -> p t d", d=1024) for i in range(2)]
    b_xin = [alias(Buf(f"xin{i}"), [b_arena, b_up0, b_merged, b_gv, b_h2b] + b_tmp) for i in range(2)]
    ybuf = [arena[:, 8192 + i * 4096:8192 + (i + 1) * 4096].rearrange("p (t d) -> p t d", d=1024) for i in range(2)]
    b_ybuf = [alias(Buf(f"ybuf{i}"), [b_arena, b_up0, b_merged, b_gv, b_h2b] + b_tmp) for i in range(2)]
    b_ybd = [Buf("ybd0"), Buf("ybd1")]
    hblk = [blockA[:, i * 4096:(i + 1) * 4096].rearrange("p (k t) -> p k t", k=8) for i in range(2)]
    b_hblk = [alias(Buf(f"hblk{i}"), [b_blkA]) for i in range(2)]
    wbuf = [
        (flat(PTre).rearrange("p (k n) -> p k n", k=8), flat(PTim).rearrange("p (k n) -> p k n", k=8), flat(Qre).rearrange("p (k n) -> p k n", k=4)),
        (flat(Qimn).rearrange("p (k n) -> p k n", k=8), flat(D0m).rearrange("p (k n) -> p k n", k=8),
         blockB[:, 0:2048].bitcast(BF16).rearrange("p (k n) -> p k n", k=4)),
    ]
    b_wb = [alias(Buf("wb0"), [b_wglu, b_convo]), alias(Buf("wb1"), [b_wo, b_winB])]
    hid = [blockB[:, 2048 + i * 1024:2048 + (i + 1) * 1024].bitcast(BF16).rearrange("p (k t) -> p k t", k=4) for i in range(2)]
    b_hid = [alias(Buf(f"hid{i}"), [b_winB]) for i in range(2)]
    stmp = [blockB[:, 4096 + i * 512:4096 + (i + 1) * 512] for i in range(2)]
    b_stmp = [alias(Buf(f"stmp{i}"), [b_winB]) for i in range(2)]
    bregs = {}

    def breg(h, v):
        if v not in bregs:
            bregs[v] = h.to_reg(v)
        return bregs[v]
    wg_flat = wg_in.rearrange("e k n -> (e k) n"); wu_flat = wu_in.rearrange("e k n -> (e k) n"); wd_flat = wd_in.rearrange("e k n -> (e k) n")
    for b in range(NBLK):
        par = b % 2
        xi, bxi = xinb[par], b_xin[par]
        S.dma("sp", lambda h, b=b, xi=xi: h.dma_start(out=xi, in_=xs[b * 512:(b + 1) * 512, :].rearrange("(t p) d -> p t d", p=128)), bxi,
              reads=[b_xs], writes=[bxi])
        wg_t, wu_t, wd_t = wbuf[par]
        bw = b_wb[par]
        for kc in range(8):
            S.dma("pool", lambda h, b=b, kc=kc, wg_t=wg_t: h.indirect_dma_start(out=wg_t[:, kc, :], out_offset=None, in_=wg_flat[:, :],
                                                                             in_offset=IOA(ap=GI_g[:, b, kc:kc + 1], axis=0)), bw, reads=[b_GI], writes=[bw])
            S.dma("pool", lambda h, b=b, kc=kc, wu_t=wu_t: h.indirect_dma_start(out=wu_t[:, kc, :], out_offset=None, in_=wu_flat[:, :],
                                                                             in_offset=IOA(ap=GI_g[:, b, kc:kc + 1], axis=0)), bw, reads=[b_GI], writes=[bw])
        for fc in range(4):
            S.dma("pool", lambda h, b=b, fc=fc, wd_t=wd_t: h.indirect_dma_start(out=wd_t[:, fc, :], out_offset=None, in_=wd_flat[:, :],
                                                                             in_offset=IOA(ap=GI_d[:, b, fc:fc + 1], axis=0)), bw, reads=[b_GI], writes=[bw])
        hbk, bhbk = hblk[par], b_hblk[par]
        for tl in range(4):
            for kc in range(8):
                S.op("pe", lambda h, xi=xi, tl=tl, kc=kc: h.transpose(out=psT[:, kc * 128:(kc + 1) * 128], in_=xi[:, tl, kc * 128:(kc + 1) * 128], identity=ident[:]),
                     reads=[bxi, b_ident], writes=[b_psT], mode="trf")
            for kc in range(8):
                S.op("act", lambda h, hbk=hbk, tl=tl, kc=kc: h.activation(out=hbk[:, kc, tl * 128:(tl + 1) * 128], in_=psT[:, kc * 128:(kc + 1) * 128], func=AF.Identity,
                                                                         bias=modfm[:, 24 + kc, 0:1], scale=sc2p[:, kc:kc + 1]),
                     reads=[b_psT, b_modfm], writes=[bhbk])
        hb, bhb = hid[par], b_hid[par]
        for fc in range(4):
            pg, bpg = ps_next()
            for kc in range(8):
                S.op("pe", lambda h, pg=pg, kc=kc, fc=fc, wg_t=wg_t, hbk=hbk: h.matmul(pg[:, :], lhsT=wg_t[:, kc, fc * 128:(fc + 1) * 128], rhs=hbk[:, kc, :],
                                                                                      start=(kc == 0), stop=(kc == 7)), reads=[bw, bhbk], writes=[bpg], mode="bulk")
            pu, bpu = ps_next()
            for kc in range(8):
                S.op("pe", lambda h, pu=pu, kc=kc, fc=fc, wu_t=wu_t, hbk=hbk: h.matmul(pu[:, :], lhsT=wu_t[:, kc, fc * 128:(fc + 1) * 128], rhs=hbk[:, kc, :],
                                                                                      start=(kc == 0), stop=(kc == 7)), reads=[bw, bhbk], writes=[bpu], mode="bulk")
            st_, bst_ = stmp[fc % 2], b_stmp[fc % 2]
            S.op("act", lambda h, pg=pg, st_=st_: h.activation(out=st_, in_=pg[:, :], func=AF.Silu), reads=[bpg], writes=[bst_])
            dv(lambda h, pu=pu, st_=st_, hb=hb, fc=fc: h.tensor_tensor(out=hb[:, fc, :], in0=pu[:, :], in1=st_, op=ALU.mult), [bpu, bst_], [bhb])
        yb_, byb_ = ybuf[par], b_ybuf[par]
        for tl in range(4):
            for nb in range(2):
                pd, bpd = ps_next()
                for fc in range(4):
                    S.op("pe", lambda h, pd=pd, fc=fc, tl=tl, nb=nb, hb=hb, wd_t=wd_t: h.matmul(pd[:, :], lhsT=hb[:, fc, tl * 128:(tl + 1) * 128],
                                                                                             rhs=wd_t[:, fc, nb * 512:(nb + 1) * 512], start=(fc == 0), stop=(fc == 3)),
                         reads=[bhb, bw], writes=[bpd], mode="bulk")
                eng = "act" if (tl * 2 + nb) % 2 == 0 else "dve"
                if eng == "act":
                    S.op("act", lambda h, pd=pd, yb_=yb_, tl=tl, nb=nb: h.activation(out=yb_[:, tl, nb * 512:(nb + 1) * 512], in_=pd[:, :], func=AF.Identity),
                         reads=[bpd], writes=[byb_])
                else:
                    dv(lambda h, pd=pd, yb_=yb_, tl=tl, nb=nb: h.tensor_copy(out=yb_[:, tl, nb * 512:(nb + 1) * 512], in_=pd[:, :]), [bpd], [byb_])
        S.dma("sp", lambda h, b=b, yb_=yb_: h.dma_start(out=ybd[b * 512:(b + 1) * 512, :].rearrange("(t p) d -> p t d", p=128), in_=yb_), byb_,
              reads=[byb_], writes=[b_ybd[par]])

    y12 = [rA[:, 4096 + i * 1024:4096 + (i + 1) * 1024] for i in range(2)]
    b_y12 = [alias(Buf(f"y12_{i}"), [b_R]) for i in range(2)]
    b_out = [Buf(f"out{i}") for i in range(32)]
    for t in range(32):
        r0 = t * 128
        i = xctr[0] % 2
        xctr[0] += 1
        xt, bx = xt_t[i], b_xt[i]
        S.dma("sp", lambda h, xt=xt, r0=r0: h.dma_start(out=xt[:], in_=x1s[r0:r0 + 128, :]), bx, reads=[b_x1s[t]], writes=[bx])
        for k in range(2):
            S.dma("pool", lambda h, t=t, k=k: h.indirect_dma_start(out=y12[k], out_offset=None, in_=ybd[:, :], in_offset=IOA(ap=DEST[:, t, k:k + 1], axis=0)),
                  b_y12[k], reads=[b_ybd[0], b_ybd[1], b_DEST], writes=[b_y12[k]])
        dv(lambda h, t=t: h.tensor_scalar(out=y12[0], in0=y12[0], scalar1=RT[:, t, 2:3], scalar2=None, op0=ALU.mult), [b_y12[0], b_RT], [b_y12[0]])
        dv(lambda h, t=t: h.scalar_tensor_tensor(out=y12[0], in0=y12[1], scalar=RT[:, t, 3:4], in1=y12[0], op0=ALU.mult, op1=ALU.add), [b_y12[0], b_y12[1], b_RT], [b_y12[0]])
        dv(lambda h: h.tensor_tensor(out=y12[0], in0=y12[0], in1=g2bc, op=ALU.mult), [b_y12[0], b_gbc], [b_y12[0]])
        dv(lambda h, xt=xt: h.scalar_tensor_tensor(out=xt[:, :], in0=xt[:, :], scalar=ALPHA, in1=y12[0], op0=ALU.mult, op1=ALU.add), [bx, b_y12[0]], [bx])
        ln_stats(xt, bx, xn_t, b_xn)
        dv(lambda h: h.tensor_tensor(out=xn_t[:, :], in0=xn_t[:, :], in1=lnp_e[:, 0:1024], op=ALU.mult), [b_xn, b_lnp], [b_xn])
        dv(lambda h, xt=xt: h.tensor_tensor(out=xt[:, :], in0=xn_t[:, :], in1=lnp_e[:, 1024:2048], op=ALU.add), [b_xn, b_lnp], [bx])
        S.dma("sp", lambda h, xt=xt, r0=r0: h.dma_start(out=out_d[r0:r0 + 128, :], in_=xt[:, :]), bx, reads=[bx], writes=[b_out[t]])
    S.final_wait("sp", b_out)
    S.emit()
    return nc


def host_inputs(inputs):
    f32 = np.float32
    g = {k: np.asarray(v) for k, v in inputs.items()}
    D_ = 1024
    q = D_ // 4
    omega = (1.0 / (10000.0 ** (np.arange(q, dtype=f32) / f32(q)))).astype(f32)
    r = (np.arange(128, dtype=f32)[:, None] * omega).astype(f32)
    cl = (np.arange(64, dtype=f32)[:, None] * omega).astype(f32)
    r_emb = np.concatenate([np.sin(r), np.cos(r)], -1).astype(f32)
    c_emb = np.concatenate([np.sin(cl), np.cos(cl)], -1).astype(f32)
    pos = np.concatenate([np.broadcast_to(r_emb[:, None, :], (128, 64, 2 * q)),
                          np.broadcast_to(c_emb[None, :, :], (128, 64, 2 * q))], -1).reshape(8192, D_).astype(f32)
    ident = np.eye(128, dtype=f32)
    ii = np.arange(128) // 16
    mF = (ii[None, :] >= ii[:, None]).astype(f32)
    mB = (ii[:, None] >= ii[None, :]).astype(f32)
    masks = np.stack([np.tile(mF, (1, 4)), np.tile(mB, (1, 4))], 1).astype(f32)
    cst = np.zeros((128, 192), f32)
    cst[:, 0:64] = np.arange(64, dtype=f32)[None, :]
    cst[:, 64:72] = np.arange(8, dtype=f32)[None, :] * 128.0 + np.arange(128, dtype=f32)[:, None]
    cst[:, 72:80] = np.arange(8, dtype=f32)[None, :] * 512.0
    tri = np.zeros((128, 256), f32)
    tri[:, 0:128] = (np.arange(128)[:, None] < np.arange(128)[None, :]).astype(f32)
    tri[:, 128:256] = 1.0

    def tr(a):
        return np.ascontiguousarray(a.T)
    maps = []
    for core in range(8):
        b, hf = core // 2, core % 2
        xb = g["x"][b]
        if hf == 1:
            x_oth, x_own = xb[0:4096], xb[4096:8192]
            p_oth, p_own = pos[0:4096], pos[4096:8192]
            ctxl = g["ctx"][b]
            F, B = "f", "b"
            convw = g["conv_w"][0]
        else:
            x_oth, x_own = xb[4096:8192][::-1], xb[0:4096][::-1]
            p_oth, p_own = pos[4096:8192][::-1], pos[0:4096][::-1]
            ctxl = g["ctx"][b][::-1]
            F, B = "b", "f"
            convw = g["conv_w"][0][::-1]
        cT = np.concatenate([g["c"][b].reshape(8, 128).T, g["c_ctx"].reshape(8, 128).T], 1)
        small = np.zeros((128, 3, 32), f32)
        sbb = np.zeros((128, 2, 32, 16), f32)
        scc = np.zeros((128, 2, 32, 16), f32)
        for li, dname in ((0, F), (1, B)):
            L = slice(li * 64, li * 64 + 64)
            small[L, 0, :] = np.broadcast_to(g["s5_log_dt_" + dname][0][None, :], (64, 32))
            small[L, 1, :] = tr(g["s5_a_re_" + dname][0])
            small[L, 2, :] = tr(g["s5_a_im_" + dname][0])
            sbb[L, 0] = g["s5_b_re_" + dname][0].transpose(1, 0, 2)
            sbb[L, 1] = g["s5_b_im_" + dname][0].transpose(1, 0, 2)
            scc[L, 0] = g["s5_c_re_" + dname][0].transpose(2, 0, 1)
            scc[L, 1] = g["s5_c_im_" + dname][0].transpose(2, 0, 1)
        drep = np.tile(g["s5_d"][0].T, (8, 1))
        m = {
            "x_own": x_own, "x_oth": x_oth, "ctx": ctxl, "pos_own": p_own, "pos_oth": p_oth,
            "cT": cT, "w_ada": g["w_ada"][0], "b_adaT": g["b_ada"][0].reshape(48, 128).T, "b_ada": g["b_ada"][0][None, :],
            "w_in": g["w_in"][0], "s5_small": small, "s5_b": sbb, "s5_c": scc, "s5_drep": drep,
            "masks": masks, "ident": ident, "cst": cst, "tri": tri,
            "w_val": g["s5_w_glu_val"][0], "w_gate": g["s5_w_glu_gate"][0],
            "convwT": convw.reshape(3, 4, 128).transpose(2, 1, 0), "conv_out": g["conv_w_out"][0], "w_o": g["w_o"][0],
            "lnp": np.concatenate([g["ln1_g"][0], g["ln1_b"][0], g["ln2_g"][0], g["ln2_b"][0]])[None, :],
            "rw": np.concatenate([g["router_w_group"][0], g["router_w_expert"][0]], 1),
            "rb": np.concatenate([g["router_b_group"][0], g["router_b_expert"][0]])[None, :],
            "wg": g["exp_w_gate"][0], "wu": g["exp_w_up"][0], "wd": g["exp_w_down"][0],
        }
        maps.append({k: np.ascontiguousarray(v, dtype=f32) for k, v in m.items()})
    return maps


def kernel(**inputs):
    maps = host_inputs(inputs)
    nc = build()
    res = run_bass_kernel_spmd(nc, maps, core_ids=list(range(8)))
    out = np.zeros((4, 8192, 1024), np.float32)
    for core in range(8):
        b, hf = core // 2, core % 2
        o = res.results[core]["out"]
        if hf == 1:
            out[b, 4096:8192] = o
        else:
            out[b, 0:4096] = o[::-1]
    return out
```
